# Optimizing a Trainium2 kernel written in Bass

```python
import jax, jax.numpy as jnp
from jax import lax
import numpy as np

D_MODEL = 1024
BATCH = 2
SEQ = 16384
DEPTH = 2

GRID_W = 64
CTX_LEN = 256
D_RNN = D_MODEL
RNN_HEADS = 16
RNN_HEAD_DIM = D_RNN // RNN_HEADS
RNN_CONV_W = 4
RNN_PAD_L = 2
RNN_PAD_R = 1
LRU_C = 8.0
D_CONV = D_MODEL
CONV_W = 31
CONV_PAD = CONV_W // 2
D_FF = 2816
N_EXPERTS = 8
TOP_K = 2
N_BRANCH = 2
N_DENSE = (DEPTH + 1) // 2
N_MOE = DEPTH // 2
EPS = 1e-6
IN_COLS = 2 * D_RNN + 2 * D_CONV + N_BRANCH * D_MODEL
SPLITS = (D_RNN, 2 * D_RNN, 2 * D_RNN + D_CONV, 2 * D_RNN + 2 * D_CONV)

kernel_name = 'hybrid_rglru_conformer_moe_dit'


def _rmsnorm(x, g):
    xf = x.astype(jnp.float32)
    y = xf * lax.rsqrt(jnp.mean(xf * xf, axis=-1, keepdims=True) + EPS)
    return (y * g.astype(jnp.float32)).astype(x.dtype)


def _layernorm(x, g, b):
    xf = x.astype(jnp.float32)
    mu = jnp.mean(xf, axis=-1, keepdims=True)
    var = jnp.mean(jnp.square(xf - mu), axis=-1, keepdims=True)
    y = (xf - mu) * lax.rsqrt(var + EPS) * g.astype(jnp.float32) + b.astype(jnp.float32)
    return y.astype(x.dtype)


def _modulate(h, shift, scale):
    return h * (1 + scale) + shift


def _dwconv_seq(u, w, b, pad_l, pad_r):
    y = lax.conv_general_dilated(u, w[:, None, :].astype(u.dtype), window_strides=(1,),
                                 padding=((pad_l, pad_r),), dimension_numbers=('NWC', 'WIO', 'NWC'),
                                 feature_group_count=u.shape[-1])
    return y + b


def _dwconv_rows(u, w, b):
    bsz, L, C = u.shape
    rows = L // GRID_W
    u4 = u.reshape(bsz, rows, GRID_W, C)
    y = lax.conv_general_dilated(u4, w[:, None, None, :].astype(u.dtype), window_strides=(1, 1),
                                 padding=((CONV_PAD, CONV_PAD), (0, 0)),
                                 dimension_numbers=('NHWC', 'HWIO', 'NHWC'), feature_group_count=C)
    return y.reshape(bsz, L, C) + b


def _lin_scan(a, b, reverse, h0=None):
    def combine(first, second):
        a1, b1 = first
        a2, b2 = second
        return a1 * a2, a2 * b1 + b2
    a_cum, h = lax.associative_scan(combine, (a, b), reverse=reverse, axis=1)
    if h0 is None:
        return h
    return h + a_cum * h0[:, None, :]


def _rglru_dir(uc, wr, br, wi, bi, lam, reverse, h0=None):
    bsz, L, _ = uc.shape
    uh = uc.reshape(bsz, L, RNN_HEADS, RNN_HEAD_DIM)
    r = jax.nn.sigmoid(jnp.einsum('blhi,hij->blhj', uh, wr).reshape(bsz, L, D_RNN) + br)
    gi = jax.nn.sigmoid(jnp.einsum('blhi,hij->blhj', uh, wi).reshape(bsz, L, D_RNN) + bi)
    log_a = -LRU_C * r.astype(jnp.float32) * jax.nn.softplus(-lam.astype(jnp.float32))
    a = jnp.exp(log_a)
    b = jnp.sqrt(-jnp.expm1(2.0 * log_a)) * (gi * uc).astype(jnp.float32)
    return _lin_scan(a, b, reverse, h0)


def _rglru_bidir(u, conv_w, conv_b, wr, br, wi, bi, lam, h0_f=None, h0_b=None):
    uc = _dwconv_seq(u, conv_w, conv_b, RNN_PAD_L, RNN_PAD_R)
    hf = _rglru_dir(uc, wr[0], br[0], wi[0], bi[0], lam[0], False, h0_f)
    hb = _rglru_dir(uc, wr[1], br[1], wi[1], bi[1], lam[1], True, h0_b)
    return hf, hb


def _mixer(h, w_in, rnn_conv_w, rnn_conv_b, wr, br, wi, bi, lam, conv_w, conv_b, ln_g, ln_b,
           w_a, w_b, w_out, on_grid, h0_f=None, h0_b=None):
    z = h @ w_in
    u_rnn, g_rnn, u_glu, g_glu, g_mrg = jnp.split(z, SPLITS, axis=-1)
    hf, hb = _rglru_bidir(u_rnn, rnn_conv_w, rnn_conv_b, wr, br, wi, bi, lam, h0_f, h0_b)
    y_a = ((hf + hb).astype(h.dtype) * jax.nn.gelu(g_rnn)) @ w_a
    v = u_glu * jax.nn.sigmoid(g_glu)
    v = _dwconv_rows(v, conv_w, conv_b) if on_grid else _dwconv_seq(v, conv_w, conv_b, CONV_PAD, CONV_PAD)
    y_b = jax.nn.silu(_layernorm(v, ln_g, ln_b)) @ w_b
    g_a, g_b = jnp.split(jax.nn.sigmoid(g_mrg), N_BRANCH, axis=-1)
    out = (g_a * y_a + g_b * y_b) @ w_out
    return out, hf[:, -1], hb[:, 0]


def _swiglu(h, w1, w3, w2):
    return (jax.nn.silu(h @ w1) * (h @ w3)) @ w2


def _moe(h, router, w1, w3, w2):
    logits = (h @ router).astype(jnp.float32)
    top_v, top_i = lax.top_k(logits, TOP_K)
    weights = jax.nn.softmax(top_v, axis=-1)
    combine = jnp.sum(jax.nn.one_hot(top_i, N_EXPERTS, dtype=jnp.float32) * weights[..., None], axis=-2)
    combine = combine.astype(h.dtype)
    out = jnp.zeros_like(h)
    for e in range(N_EXPERTS):
        out = out + combine[..., e:e + 1] * _swiglu(h, w1[e], w3[e], w2[e])
    return out


def _ffn(h, i, ffn_w1, ffn_w3, ffn_w2, moe_router, moe_w1, moe_w3, moe_w2):
    j = i // 2
    if i % 2 == 0:
        return _swiglu(h, ffn_w1[j], ffn_w3[j], ffn_w2[j])
    return _moe(h, moe_router[j], moe_w1[j], moe_w3[j], moe_w2[j])


def setup_inputs(seed: int = 0) -> dict:
    key = jax.random.key(seed)
    ks = iter(jax.random.split(key, 48))
    D = D_MODEL

    def nrm(shape, scale):
        return scale * jax.random.normal(next(ks), shape, jnp.float32)

    a_init = jax.random.uniform(next(ks), (DEPTH, 2, D_RNN), jnp.float32, 0.9, 0.999)
    return {
        'x': nrm((BATCH, SEQ, D), 1.0),
        'c': nrm((BATCH, D), 1.0),
        'ctx': nrm((BATCH, CTX_LEN, D), 1.0),
        'c_ctx': nrm((D,), 1.0),
        'mod_w': nrm((DEPTH, D, 6 * D), 0.5 * D ** -0.5),
        'mod_b': nrm((DEPTH, 6 * D), 0.02),
        'norm1_g': 1.0 + nrm((DEPTH, D), 0.1),
        'norm2_g': 1.0 + nrm((DEPTH, D), 0.1),
        'w_in': nrm((DEPTH, D, IN_COLS), D ** -0.5),
        'rnn_conv_w': nrm((DEPTH, RNN_CONV_W, D_RNN), RNN_CONV_W ** -0.5),
        'rnn_conv_b': nrm((DEPTH, D_RNN), 0.02),
        'lru_wr': nrm((DEPTH, 2, RNN_HEADS, RNN_HEAD_DIM, RNN_HEAD_DIM), RNN_HEAD_DIM ** -0.5),
        'lru_br': nrm((DEPTH, 2, D_RNN), 0.02),
        'lru_wi': nrm((DEPTH, 2, RNN_HEADS, RNN_HEAD_DIM, RNN_HEAD_DIM), RNN_HEAD_DIM ** -0.5),
        'lru_bi': nrm((DEPTH, 2, D_RNN), 0.02),
        'lru_lam': jnp.log(a_init) - jnp.log1p(-a_init),
        'conv_w': nrm((DEPTH, CONV_W, D_CONV), CONV_W ** -0.5),
        'conv_b': nrm((DEPTH, D_CONV), 0.02),
        'conv_ln_g': 1.0 + nrm((DEPTH, D_CONV), 0.1),
        'conv_ln_b': nrm((DEPTH, D_CONV), 0.02),
        'w_branch_a': nrm((DEPTH, D_RNN, D), D_RNN ** -0.5),
        'w_branch_b': nrm((DEPTH, D_CONV, D), D_CONV ** -0.5),
        'w_out': nrm((DEPTH, D, D), D ** -0.5),
        'ffn_w1': nrm((N_DENSE, D, D_FF), D ** -0.5),
        'ffn_w3': nrm((N_DENSE, D, D_FF), D ** -0.5),
        'ffn_w2': nrm((N_DENSE, D_FF, D), D_FF ** -0.5),
        'moe_router': nrm((N_MOE, D, N_EXPERTS), D ** -0.5),
        'moe_w1': nrm((N_MOE, N_EXPERTS, D, D_FF), D ** -0.5),
        'moe_w3': nrm((N_MOE, N_EXPERTS, D, D_FF), D ** -0.5),
        'moe_w2': nrm((N_MOE, N_EXPERTS, D_FF, D), D_FF ** -0.5),
        'final_g': 1.0 + nrm((D,), 0.1),
    }


def reference(x, c, ctx, c_ctx, mod_w, mod_b, norm1_g, norm2_g, w_in, rnn_conv_w, rnn_conv_b,
              lru_wr, lru_br, lru_wi, lru_bi, lru_lam, conv_w, conv_b, conv_ln_g, conv_ln_b,
              w_branch_a, w_branch_b, w_out, ffn_w1, ffn_w3, ffn_w2, moe_router, moe_w1, moe_w3,
              moe_w2, final_g):
    sc = jax.nn.silu(c)
    scc = jax.nn.silu(c_ctx)
    for i in range(DEPTH):
        last = i == DEPTH - 1
        mod_x = jnp.split((sc @ mod_w[i] + mod_b[i])[:, None, :], 6, axis=-1)
        mod_c = jnp.split((scc @ mod_w[i] + mod_b[i])[None, None, :], 6, axis=-1)
        lru = (lru_wr[i], lru_br[i], lru_wi[i], lru_bi[i], lru_lam[i])
        mix_p = (w_in[i], rnn_conv_w[i], rnn_conv_b[i], *lru, conv_w[i], conv_b[i], conv_ln_g[i],
                 conv_ln_b[i], w_branch_a[i], w_branch_b[i], w_out[i])
        hc = _modulate(_rmsnorm(ctx, norm1_g[i]), mod_c[0], mod_c[1])
        if last:
            hf_c, hb_c = _rglru_bidir(hc @ w_in[i][:, :D_RNN], rnn_conv_w[i], rnn_conv_b[i], *lru)
            s_f, s_b = hf_c[:, -1], hb_c[:, 0]
        else:
            out_c, s_f, s_b = _mixer(hc, *mix_p, False)
            ctx = ctx + mod_c[2] * out_c
            hc2 = _modulate(_rmsnorm(ctx, norm2_g[i]), mod_c[3], mod_c[4])
            ctx = ctx + mod_c[5] * _ffn(hc2, i, ffn_w1, ffn_w3, ffn_w2, moe_router, moe_w1, moe_w3, moe_w2)
        hx = _modulate(_rmsnorm(x, norm1_g[i]), mod_x[0], mod_x[1])
        out_x, _, _ = _mixer(hx, *mix_p, True, s_f, s_b)
        x = x + mod_x[2] * out_x
        hx2 = _modulate(_rmsnorm(x, norm2_g[i]), mod_x[3], mod_x[4])
        x = x + mod_x[5] * _ffn(hx2, i, ffn_w1, ffn_w3, ffn_w2, moe_router, moe_w1, moe_w3, moe_w2)
    return _rmsnorm(x, final_g)
```

```python
import numpy as np
from contextlib import ExitStack
import concourse.bass as bass
import concourse.mybir as mybir
from concourse.bass_utils import run_bass_kernel_spmd

F32 = mybir.dt.float32
BF16 = mybir.dt.bfloat16
AF = mybir.ActivationFunctionType
ALU = mybir.AluOpType

D = 1024
KT = 8
NBLK = 16
NB = 512
TL = NBLK * NB
CTX = 256
CPAD = 16
TC = CTX + 2 * CPAD
DFF = 2816
NE = 8
EPS = 1e-6
DEPTH = 2
SEM_ROT = 30000
N_DMA_SEM = 16

DEBUG_OUT = []
STOP_AFTER = None
NO_MOE = False


class _Op:
    __slots__ = ("eng", "fn", "waits", "tok", "clock", "is_dma", "sig")


class Prog:
    ENG = ("pe", "dve", "act", "pool", "sp")

    def __init__(self, nc):
        self.nc = nc
        self.ops = {e: [] for e in self.ENG}
        self.known = {e: {} for e in self.ENG}
        self.cnt = {e: 0 for e in self.ENG}
        self.cur_sem = {}
        self.sems = {}
        self.nsem = 0
        self.own_done = {e: {} for e in self.ENG}
        for e in self.ENG:
            self.cur_sem[e] = self._new_sem(e)
        self.dma_sems = {}
        self.dma_uses = {}
        self.dma_last = {}
        self.dma_rr = {}
        for q in ("sp", "pool"):
            self.dma_sems[q] = [self._new_sem("d" + q) for _ in range(N_DMA_SEM)]
            self.dma_rr[q] = 0
            for s in self.dma_sems[q]:
                self.dma_uses[s] = 0
                self.dma_last[s] = None
        self.last_w = {}
        self.readers = {}
        self.nops = 0
        self.last_op = {e: None for e in self.ENG}
        self.pending_nosig = {e: 0 for e in self.ENG}
        self.uid = 0

    def _new_sem(self, tag):
        sid = self.nsem
        self.nsem += 1
        self.sems[sid] = self.nc.alloc_semaphore(name="s%d_%s" % (sid, tag))
        return sid

    def name(self, base):
        self.uid += 1
        return "%s_%d" % (base, self.uid)

    def _deps(self, eng, reads, writes, is_pe):
        deps = []
        for k in reads:
            w = self.last_w.get(k)
            if w is not None:
                deps.append(w)
        for k in writes:
            w = self.last_w.get(k)
            if w is not None:
                deps.append(w)
            for r in self.readers.get(k, ()):
                deps.append(r)
        waits = {}
        kn = self.known[eng]
        for d in deps:
            if is_pe and d.eng == "pe" and not d.is_dma:
                continue
            sid, val = d.tok
            if kn.get(sid, 0) >= val:
                continue
            if waits.get(sid, 0) < val:
                waits[sid] = val
        for d in deps:
            for sid, val in d.clock.items():
                if kn.get(sid, 0) < val:
                    kn[sid] = val
        return waits

    def _register(self, op, reads, writes):
        for k in reads:
            lst = self.readers.setdefault(k, [])
            if not op.is_dma:
                lst[:] = [r for r in lst if r.is_dma or r.eng != op.eng]
            lst.append(op)
        for k in writes:
            self.last_w[k] = op
            self.readers[k] = []

    def op(self, eng, fn, reads=(), writes=(), sig=True):
        o = _Op()
        o.eng = eng
        o.fn = fn
        o.is_dma = False
        o.sig = sig
        o.waits = self._deps(eng, reads, writes, eng == "pe")
        if self.cnt[eng] >= SEM_ROT and self.pending_nosig[eng] == 0:
            self.own_done[eng][self.cur_sem[eng]] = self.cnt[eng]
            self.cur_sem[eng] = self._new_sem(eng)
            self.cnt[eng] = 0
        if sig:
            self.cnt[eng] += 1
            self.pending_nosig[eng] = 0
            o.tok = (self.cur_sem[eng], self.cnt[eng])
        else:
            self.pending_nosig[eng] += 1
            o.tok = (self.cur_sem[eng], self.cnt[eng] + 1)
        o.clock = dict(self.known[eng])
        o.clock.update(self.own_done[eng])
        o.clock[o.tok[0]] = o.tok[1]
        self._register(o, reads, writes)
        self.ops[eng].append(o)
        self.last_op[eng] = o
        self.nops += 1
        return o

    def dma(self, q, out, in_, reads=(), writes=(), fn=None):
        o = _Op()
        o.eng = q
        o.is_dma = True
        waits = self._deps(q, reads, writes, False)
        sems = self.dma_sems[q]
        sid = sems[self.dma_rr[q] % len(sems)]
        self.dma_rr[q] += 1
        prev = self.dma_last[sid]
        kn = self.known[q]
        if prev is not None and kn.get(sid, 0) < prev.tok[1]:
            waits[sid] = max(waits.get(sid, 0), prev.tok[1])
            kn[sid] = prev.tok[1]
        self.dma_uses[sid] += 1
        o.tok = (sid, 16 * self.dma_uses[sid])
        self.dma_last[sid] = o
        o.waits = waits
        o.fn = ("dynfn", fn) if fn is not None else ("dma", out, in_)
        o.clock = dict(kn)
        o.clock[sid] = o.tok[1]
        self._register(o, reads, writes)
        self.ops[q].append(o)
        self.nops += 1
        return o

    def collective(self, kind, ins, outs, groups, reads=(), writes=()):
        o = _Op()
        o.eng = "pool"
        o.is_dma = True
        o.waits = self._deps("pool", reads, writes, False)
        sid = self._new_sem("cc")
        o.tok = (sid, 1)
        o.fn = ("cc", kind, ins, outs, groups)
        o.clock = dict(self.known["pool"])
        o.clock[sid] = 1
        self._register(o, reads, writes)
        self.ops["pool"].append(o)
        self.nops += 1
        return o

    def barrier(self):
        toks = {}
        for e in self.ENG:
            lo = self.last_op[e]
            if lo is not None:
                toks[lo.tok[0]] = max(toks.get(lo.tok[0], 0), lo.tok[1])
        for sid, lo in self.dma_last.items():
            if lo is not None:
                toks[sid] = max(toks.get(sid, 0), lo.tok[1])
        for k, w in self.last_w.items():
            if w is not None and w.is_dma:
                toks[w.tok[0]] = max(toks.get(w.tok[0], 0), w.tok[1])
        for e in self.ENG:
            kn = self.known[e]
            waits = {}
            for sid, val in toks.items():
                if kn.get(sid, 0) < val:
                    if e == "pe" and sid == self.cur_sem["pe"]:
                        continue
                    waits[sid] = val
                    kn[sid] = val
            if waits:
                o = _Op()
                o.eng = e
                o.is_dma = False
                o.waits = waits
                o.fn = None
                o.tok = None
                o.clock = {}
                self.ops[e].append(o)
        self.last_w = {}
        self.readers = {}

    def _replay(self, ename, e):
        sems = self.sems
        for o in self.ops[ename]:
            for sid, val in o.waits.items():
                e.wait_ge(sems[sid], val)
            if o.fn is None:
                continue
            if o.is_dma:
                if o.fn[0] == "dynfn":
                    o.fn[1](e).then_inc(sems[o.tok[0]], 16)
                elif o.fn[0] == "dma":
                    e.dma_start(out=o.fn[1], in_=o.fn[2]).then_inc(sems[o.tok[0]], 16)
                else:
                    _, kind, ins, outs, groups = o.fn
                    e.collective_compute(kind, ALU.bypass, replica_groups=groups,
                                         ins=ins, outs=outs).then_inc(sems[o.tok[0]])
            elif o.sig:
                o.fn(e).then_inc(sems[o.tok[0]], 1)
            else:
                o.fn(e)

    def emit(self):
        self.barrier()
        with self.nc.Block() as block:
            @block.sync
            def _(e):
                self._replay("sp", e)

            @block.tensor
            def _(e):
                self._replay("pe", e)

            @block.vector
            def _(e):
                self._replay("dve", e)

            @block.scalar
            def _(e):
                self._replay("act", e)

            @block.gpsimd
            def _(e):
                self._replay("pool", e)


def _small_layout():
    off = {}
    n = 0

    def add(name, w):
        nonlocal n
        off[name] = (n, w)
        n += w
    for l in range(DEPTH):
        add("n1g%d" % l, 8)
        add("n2g%d" % l, 8)
        add("modb%d" % l, 48)
        add("cw4_%d" % l, 32)
        add("cb4_%d" % l, 8)
        for d in range(2):
            add("br%d%d" % (l, d), 8)
            add("bi%d%d" % (l, d), 8)
            add("lam%d%d" % (l, d), 8)
        add("cw31_%d" % l, 31 * 8)
        add("cb31_%d" % l, 8)
        add("lng%d" % l, 8)
        add("lnb%d" % l, 8)
    add("fing", 8)
    add("blkmask", NBLK)
    add("one", 1)
    add("boh", 2)
    add("cvec", 32)
    return off, n


SOFF, NS = _small_layout()


def _pc(v):
    return np.ascontiguousarray(np.asarray(v, np.float32).reshape(8, 128).T)


def _build_small(inp, core):
    b, q = core // 4, core % 4
    s = np.zeros((128, NS), np.float32)

    def put(name, arr):
        o, w = SOFF[name]
        s[:, o:o + w] = np.asarray(arr, np.float32).reshape(128, w)
    for l in range(DEPTH):
        put("n1g%d" % l, _pc(inp["norm1_g"][l]))
        put("n2g%d" % l, _pc(inp["norm2_g"][l]))
        put("modb%d" % l, inp["mod_b"][l].reshape(48, 128).T)
        put("cw4_%d" % l, np.concatenate([_pc(inp["rnn_conv_w"][l][j]) for j in range(4)], axis=1))
        put("cb4_%d" % l, _pc(inp["rnn_conv_b"][l]))
        for d in range(2):
            put("br%d%d" % (l, d), _pc(inp["lru_br"][l][d]))
            put("bi%d%d" % (l, d), _pc(inp["lru_bi"][l][d]))
            put("lam%d%d" % (l, d), _pc(inp["lru_lam"][l][d]))
        put("cw31_%d" % l, np.concatenate([_pc(inp["conv_w"][l][j]) for j in range(31)], axis=1))
        put("cb31_%d" % l, _pc(inp["conv_b"][l]))
        put("lng%d" % l, _pc(inp["conv_ln_g"][l]))
        put("lnb%d" % l, _pc(inp["conv_ln_b"][l]))
    put("fing", _pc(inp["final_g"]))
    bm = np.zeros((128, NBLK), np.float32)
    for lb in range(NBLK):
        g0 = q * 4096 + (lb - 4) * NB
        bm[:, lb] = 1.0 if (0 <= g0 < 16384) else 0.0
    put("blkmask", bm)
    put("one", np.ones((128, 1), np.float32))
    boh = np.zeros((128, 2), np.float32)
    boh[:, b] = 1.0
    put("boh", boh)
    cv = np.zeros((128, 8, 4), np.float32)
    cv[:, :, 0] = _pc(inp["c"][0])
    cv[:, :, 1] = _pc(inp["c"][1])
    cv[:, :, 2] = _pc(inp["c_ctx"])
    put("cvec", cv.reshape(128, 32))
    return s


class G:
    pass


def build_program():
    nc = bass.Bass("TRN2", target_bir_lowering=False)
    P = Prog(nc)
    g = G()
    g.nc, g.P = nc, P

    def din(name, shape, dt=F32):
        return nc.dram_tensor(name, list(shape), dt, kind="ExternalInput").ap()

    def dscr(name, shape, dt=F32):
        kind = "ExternalOutput" if name in DEBUG_OUT else "Internal"
        return nc.dram_tensor(name, list(shape), dt, kind=kind).ap()

    g.x_loc = din("x_loc", [TL, D])
    g.ctx_in = din("ctx_in", [CTX, D])
    g.small_in = din("small", [128, NS])
    g.modw_in = din("modw", [DEPTH, D, 1536])
    g.win_in = din("w_in", [DEPTH * D, 6144])
    g.wa_in = din("w_a", [DEPTH * D, D])
    g.wb_in = din("w_b", [DEPTH * D, D])
    g.wo_in = din("w_o", [DEPTH * D, D])
    g.f1_in = din("ffn_w1", [D, DFF])
    g.f3_in = din("ffn_w3", [D, DFF])
    g.f2_in = din("ffn_w2", [DFF, D])
    if not NO_MOE:
        g.m1_in = din("moe_w1", [NE * D, DFF])
        g.m3_in = din("moe_w3", [NE * D, DFF])
        g.m2_in = din("moe_w2", [NE * DFF, D])
    g.gw_in = din("gatew", [DEPTH * 2 * 2 * 8 * 128, 128])
    g.rt_in = din("router", [D, NE])
    g.out = nc.dram_tensor("out", [8 * NB, D], F32, kind="ExternalOutput").ap()

    g.win = dscr("win_bf", [DEPTH * D, 6144], BF16)
    g.wa = dscr("wa_bf", [DEPTH * D, D], BF16)
    g.wb = dscr("wb_bf", [DEPTH * D, D], BF16)
    g.wo = dscr("wo_bf", [DEPTH * D, D], BF16)
    g.f1 = dscr("f1_bf", [D, DFF], BF16)
    g.f3 = dscr("f3_bf", [D, DFF], BF16)
    g.f2 = dscr("f2_bf", [DFF, D], BF16)
    g.m1 = dscr("m1_bf", [NE * D, DFF], BF16)
    g.m3 = dscr("m3_bf", [NE * D, DFF], BF16)
    g.m2 = dscr("m2_bf", [NE * DFF, D], BF16)
    g.gw = dscr("gw_bf", [DEPTH * 2 * 2 * 8 * 128, 128], BF16)

    def seg_arrays(pfx, T):
        a = {}
        for nm, dt in (("XT", F32), ("HX", BF16), ("U", F32), ("G", BF16), ("V", BF16),
                       ("SGA", BF16), ("SGB", BF16), ("S", F32), ("AF", BF16), ("AB", BF16),
                       ("CV", F32)):
            a[nm] = dscr(pfx + nm, [KT, 128, T], dt)
        return a
    g.lat = seg_arrays("L_", TL)
    g.ctxa = seg_arrays("C_", TC)
    g.comb = dscr("COMB", [NE, 128, TL], F32)
    g.sum_in = dscr("sum_in", [128, 256], F32)
    g.sum_all = dscr("sum_all", [4 * 128, 256], F32)
    g.mod_in = dscr("mod_in", [128, 96], F32)
    g.mod_all = dscr("mod_all", [4 * 128, 96], F32)
    g.htab = dscr("htab", [2, 128, KT, 48], F32)
    g.ex_in = dscr("ex_in", [8, 128, 2048], F32)
    g.ex_all = [dscr("ex_all%d" % u, [4, 128, 2048], F32) for u in range(8)]

    def sb(name, shape, dt=F32):
        return nc.alloc_sbuf_tensor("sb_" + name, list(shape), dt)
    g.small = sb("small", [128, NS])
    g.ident = sb("ident", [128, 128])
    g.ones_b = sb("ones_b", [128, 128], BF16)
    g.zeros = sb("zeros", [128, NB])
    g.modx = sb("modx", [128, DEPTH, 48])
    g.modc = sb("modc", [128, DEPTH, 48])
    g.nrm = sb("nrm", [128, DEPTH, 2, 4, 8])
    g.cl = sb("cl", [128, DEPTH, 2, 2, 8])
    g.sumt = sb("sumt", [128, KT, 8, 4])
    g.sctx = sb("sctx", [128, 2, KT])
    g.hin = sb("hin", [128, 2, KT, 12])
    g.hzero = sb("hzero", [128, 2, KT, 12])
    g.ps = [nc.alloc_psum_tensor("psb%d" % i, [128, NB], F32) for i in range(8)]
    return g


def S(g, name, w=None):
    o, ww = SOFF[name]
    return g.small[:, o:o + (ww if w is None else w)]


def Scol(g, name, i):
    o, _ = SOFF[name]
    return g.small[:, o + i:o + i + 1]


def dynval(g, e, name):
    if not hasattr(g, "_dyn"):
        pid = e.partition_id()
        g._dyn = {
            "rl": e.snap((pid + 3) % 4, min_val=0, max_val=3),
            "rr": e.snap((pid + 1) % 4, min_val=0, max_val=3),
            "b0": e.snap((pid % 4) * 8 + 6, min_val=6, max_val=30),
            "b1": e.snap((pid % 4) * 8 + 7, min_val=7, max_val=31),
        }
    return g._dyn[name]


class Stage:
    def __init__(self, g):
        self.g = g
        self.es = ExitStack()

    def __enter__(self):
        self.g.P.barrier()
        return self

    def __exit__(self, *a):
        self.g.P.barrier()
        self.es.close()
        return False

    def sb(self, base, shape, dt=F32):
        nm = self.g.P.name(base)
        t = self.es.enter_context(self.g.nc.sbuf_tensor(nm, list(shape), dt))
        return t, nm

    def ring(self, base, shape, dt, n):
        return [self.sb(base, shape, dt) for _ in range(n)]


def blk_view(arr, s, n):
    return arr[:, :, s:s + n].rearrange("c p t -> p c t")


def st_setup(g):
    P, nc = g.P, g.nc
    with Stage(g) as st:
        P.dma("sp", g.small[:], g.small_in, reads=[], writes=["small"])
        P.op("pool", lambda e: e.memset(g.ident[:], 0.0), writes=["ident"])
        P.op("pool", lambda e: e.affine_select(out=g.ident[:], in_=g.ident[:], pattern=[[-1, 128]],
                                               compare_op=ALU.not_equal, fill=1.0, base=0,
                                               channel_multiplier=1),
             reads=["ident"], writes=["ident"])
        P.op("pool", lambda e: e.memset(g.ones_b[:], 1.0), writes=["ones_b"])
        P.op("pool", lambda e: e.memset(g.zeros[:], 0.0), writes=["zeros"])
        P.op("pool", lambda e: e.memset(g.hzero[:], 0.0), writes=["hzero"])
        zt, zk = st.sb("zt", [128, KT, TC], F32)
        zb, zbk = st.sb("zb", [128, KT, TC], BF16)
        P.op("dve", lambda e: e.memset(zt[:], 0.0), writes=[zk])
        P.op("dve", lambda e: e.memset(zb[:], 0.0), writes=[zbk])
        P.dma("sp", blk_view(g.ctxa["U"], 0, TC), zt[:], reads=[zk], writes=["C_U"])
        P.dma("sp", blk_view(g.ctxa["V"], 0, TC), zb[:], reads=[zbk], writes=["C_V"])
        sc, sck = st.sb("sc", [128, 8, 4], F32)
        P.op("act", lambda e: e.activation(out=sc[:].rearrange("p a b -> p (a b)"), in_=S(g, "cvec"),
                                           func=AF.Silu), reads=["small"], writes=[sck])
        mw = st.ring("mw", [128, 8, 1536], F32, 1)
        mo, mok = st.sb("mo", [128, 96], F32)
        psm = g.ps[0]
        for l in range(DEPTH):
            t, tk = mw[0]
            P.dma("sp", t[:], g.modw_in[l].rearrange("(k p) n -> p k n", p=128), reads=[], writes=[tk])
            for c in range(12):
                for k in range(8):
                    P.op("pe", lambda e, t=t, c=c, k=k, l=l: e.matmul(
                        psm[:, (l * 12 + c) * 4:(l * 12 + c) * 4 + 4], lhsT=t[:, k, c * 128:(c + 1) * 128],
                        rhs=sc[:, k, :], start=(k == 0), stop=(k == 7)),
                        reads=[tk, sck], writes=["ps0"], sig=(k == 7))
        P.op("dve", lambda e: e.tensor_copy(out=mo[:], in_=psm[:, 0:96]), reads=["ps0"], writes=[mok])
        P.dma("sp", g.mod_in, mo[:], reads=[mok], writes=["mod_in"])
        P.collective("AllGather", ins=[g.mod_in], outs=[g.mod_all], groups=[[0, 1, 2, 3], [4, 5, 6, 7]],
                     reads=["mod_in"], writes=["mod_all"])
        ma, mak = st.sb("ma", [128, DEPTH, 4, 12, 4], F32)
        for l in range(DEPTH):
            P.dma("sp", ma[:, l], g.mod_all.rearrange("(r p) (l c j) -> p l r c j", p=128, l=DEPTH, c=12)[:, l],
                  reads=["mod_all"], writes=[mak])
        for l in range(DEPTH):
            v = ma[:, l].rearrange("p r c j -> p (r c) j")
            mb = S(g, "modb%d" % l)
            P.op("dve", lambda e, v=v, l=l: e.tensor_single_scalar(out=g.modx[:, l, :], in_=v[:, :, 0],
                                                            scalar=Scol(g, "boh", 0), op=ALU.mult),
                 reads=[mak, "small"], writes=["modx"])
            P.op("dve", lambda e, v=v, l=l: e.scalar_tensor_tensor(out=g.modx[:, l, :], in0=v[:, :, 1],
                                                                   scalar=Scol(g, "boh", 1), in1=g.modx[:, l, :],
                                                                   op0=ALU.mult, op1=ALU.add),
                 reads=[mak, "small", "modx"], writes=["modx"])
            P.op("dve", lambda e, l=l, mb=mb: e.tensor_tensor(out=g.modx[:, l, :], in0=g.modx[:, l, :], in1=mb, op=ALU.add),
                 reads=["modx", "small"], writes=["modx"])
            P.op("dve", lambda e, v=v, l=l, mb=mb: e.tensor_tensor(out=g.modc[:, l, :], in0=v[:, :, 2], in1=mb, op=ALU.add),
                 reads=[mak, "small"], writes=["modc"])
            for si, mv in enumerate((g.modx, g.modc)):
                P.op("dve", lambda e, l=l, si=si, mv=mv: e.scalar_tensor_tensor(
                    out=g.nrm[:, l, si, 0, :], in0=mv[:, l, 8:16], scalar=1.0, in1=S(g, "n1g%d" % l),
                    op0=ALU.add, op1=ALU.mult), reads=["modx", "modc", "small"], writes=["nrm"])
                P.op("dve", lambda e, l=l, si=si, mv=mv: e.scalar_tensor_tensor(
                    out=g.nrm[:, l, si, 1, :], in0=mv[:, l, 32:40], scalar=1.0, in1=S(g, "n2g%d" % l),
                    op0=ALU.add, op1=ALU.mult), reads=["modx", "modc", "small", "nrm"], writes=["nrm"])
            for d in range(2):
                tmp, tmk = st.sb("cltmp", [128, 8], F32)
                P.op("act", lambda e, l=l, d=d, tmp=tmp: e.activation(out=tmp[:], in_=S(g, "lam%d%d" % (l, d)),
                                                                      func=AF.Exp, scale=-1.0),
                     reads=["small"], writes=[tmk])
                P.op("act", lambda e, tmp=tmp: e.activation(out=tmp[:], in_=tmp[:], func=AF.Ln, bias=1.0),
                     reads=[tmk], writes=[tmk])
                P.op("dve", lambda e, l=l, d=d, tmp=tmp: e.tensor_single_scalar(out=g.cl[:, l, d, 0, :], in_=tmp[:],
                                                                         scalar=-8.0, op=ALU.mult),
                     reads=[tmk], writes=["cl"])
                P.op("dve", lambda e, l=l, d=d, tmp=tmp: e.tensor_single_scalar(out=g.cl[:, l, d, 1, :], in_=tmp[:],
                                                                         scalar=-16.0, op=ALU.mult),
                     reads=[tmk, "cl"], writes=["cl"])


def modvec(g, l, si, which):
    mv = g.modx if si == 0 else g.modc
    if which == "A1":
        return g.nrm[:, l, si, 0, :]
    if which == "A2":
        return g.nrm[:, l, si, 1, :]
    return {"B1": mv[:, l, 0:8], "G1": mv[:, l, 16:24], "B2": mv[:, l, 24:32], "G2": mv[:, l, 40:48]}[which]


def st_convert(g, which):
    P = g.P
    if which == "mix":
        pairs = [(g.win_in, g.win), (g.wa_in, g.wa), (g.wb_in, g.wb), (g.wo_in, g.wo), (g.gw_in, g.gw),
                 (g.f1_in, g.f1), (g.f3_in, g.f3), (g.f2_in, g.f2)]
    else:
        pairs = [(g.m1_in, g.m1), (g.m3_in, g.m3), (g.m2_in, g.m2)]
    wi_ = 0
    W = 4096
    with Stage(g) as st:
        src = st.ring("cvs", [128, W], F32, 3)
        dst = st.ring("cvd", [128, W], BF16, 3)
        i = 0
        for a, b in pairs:
            av = a.rearrange("(p r) n -> p (r n)", p=128)
            bv = b.rearrange("(p r) n -> p (r n)", p=128)
            Fd = av.shape[1]
            for o in range(0, Fd, W):
                w = min(W, Fd - o)
                s, sk = src[i % 3]
                d, dk = dst[i % 3]
                P.dma("sp", s[:, 0:w], av[:, o:o + w], reads=[], writes=[sk])
                eng = ("act", "dve", "act")[i % 3]
                if eng == "act":
                    P.op("act", lambda e, s=s, d=d, w=w: e.copy(out=d[:, 0:w], in_=s[:, 0:w]), reads=[sk], writes=[dk])
                else:
                    P.op(eng, lambda e, s=s, d=d, w=w: e.tensor_copy(out=d[:, 0:w], in_=s[:, 0:w]), reads=[sk], writes=[dk])
                P.dma("pool", bv[:, o:o + w], d[:, 0:w], reads=[dk], writes=[("wcv", id(b), o)])
                i += 1


def conv_pieces(g, which, W=4096):
    if which == "mix":
        pairs = [(g.win_in, g.win), (g.wa_in, g.wa), (g.wb_in, g.wb), (g.wo_in, g.wo), (g.gw_in, g.gw),
                 (g.f1_in, g.f1), (g.f3_in, g.f3), (g.f2_in, g.f2)]
    else:
        pairs = [(g.m1_in, g.m1), (g.m3_in, g.m3), (g.m2_in, g.m2)]
    out = []
    for a, b in pairs:
        av = a.rearrange("(p r) n -> p (r n)", p=128)
        bv = b.rearrange("(p r) n -> p (r n)", p=128)
        Fd = av.shape[1]
        for o in range(0, Fd, W):
            out.append((av, bv, o, min(W, Fd - o), id(b)))
    return out


def bg_convert(g, st, pieces, eng="dve", W=4096):
    P = g.P
    src = st.ring("bgs", [128, W], F32, 3)
    dst = st.ring("bgd", [128, W], BF16, 3)
    for i, (av, bv, o, w, bid) in enumerate(pieces):
        s_, sk = src[i % 3]
        d_, dk = dst[i % 3]
        P.dma("sp", s_[:, 0:w], av[:, o:o + w], reads=[], writes=[sk])
        if eng == "act":
            P.op("act", lambda e, s_=s_, d_=d_, w=w: e.copy(out=d_[:, 0:w], in_=s_[:, 0:w]), reads=[sk], writes=[dk])
        else:
            P.op(eng, lambda e, s_=s_, d_=d_, w=w: e.tensor_copy(out=d_[:, 0:w], in_=s_[:, 0:w]), reads=[sk], writes=[dk])
        P.dma("pool", bv[:, o:o + w], d_[:, 0:w], reads=[dk], writes=[("wcv", bid, o)])
        yield


def st_transpose_in(g):
    P = g.P
    with Stage(g) as st:
        xin = st.ring("xin", [128, 4, D], F32, 2)
        xtb = st.ring("xtb", [128, KT, NB], F32, 2)
        jobs = [("L", lb) for lb in range(2, 14)] + [("C", 0)]
        for ji, (kind, lb) in enumerate(jobs):
            xi, xik = xin[ji % 2]
            xo, xok = xtb[ji % 2]
            if kind == "L":
                na = 4
                P.dma("sp", xi[:], g.x_loc[lb * NB:(lb + 1) * NB, :].rearrange("(a p) d -> p a d", p=128), writes=[xik])
            else:
                na = 2
                P.dma("sp", xi[:, 0:2], g.ctx_in.rearrange("(a p) d -> p a d", p=128), writes=[xik])
            for c in range(KT):
                ps = g.ps[c]
                for a in range(na):
                    P.op("pe", lambda e, ps=ps, xi=xi, a=a, c=c: e.transpose(
                        out=ps[:, a * 128:(a + 1) * 128], in_=xi[:, a, c * 128:(c + 1) * 128], identity=g.ident[:]),
                        reads=[xik, "ident"], writes=["ps%d" % c])
                n = na * 128
                if c % 2 == 0:
                    P.op("act", lambda e, ps=ps, xo=xo, c=c, n=n: e.copy(out=xo[:, c, 0:n], in_=ps[:, 0:n]),
                         reads=["ps%d" % c], writes=[(xok, c)])
                else:
                    P.op("dve", lambda e, ps=ps, xo=xo, c=c, n=n: e.tensor_copy(out=xo[:, c, 0:n], in_=ps[:, 0:n]),
                         reads=["ps%d" % c], writes=[(xok, c)])
            rk = [(xok, c) for c in range(KT)]
            if kind == "L":
                P.dma("pool", blk_view(g.lat["XT"], lb * NB, NB), xo[:], reads=rk, writes=[("L_XT", lb)])
            else:
                P.dma("pool", blk_view(g.ctxa["XT"], CPAD, CTX), xo[:, :, 0:CTX], reads=rk, writes=[("C_XT", 0)])


def norm_core(g, st, xt, xtk, n, A, B, bufs, out_bf=None, out_f32=None, psi=7, sq_eng="act"):
    P = g.P
    sq, sqk = bufs["sq"]
    rs, rsk = bufs["rs"]
    ps = g.ps[psi]
    psk = "ps%d" % psi
    P.op("act", lambda e: e.activation(out=sq[:, :, 0:n], in_=xt[:, :, 0:n], func=AF.Square),
         reads=[xtk], writes=[sqk])
    for k in range(KT):
        P.op("pe", lambda e, k=k: e.matmul(ps[:, 0:n], lhsT=g.ones_b[:], rhs=sq[:, k, 0:n], start=(k == 0), stop=(k == 7)),
             reads=[sqk, "ones_b"], writes=[psk], sig=(k == 7))
    P.op("act", lambda e: e.activation(out=rs[:, 0:n], in_=ps[:, 0:n], func=AF.Sqrt, scale=1.0 / D, bias=EPS),
         reads=[psk], writes=[rsk])
    P.op("dve", lambda e: e.reciprocal(out=rs[:, 0:n], in_=rs[:, 0:n]), reads=[rsk], writes=[rsk])
    P.op("dve", lambda e: e.tensor_tensor(out=xt[:, :, 0:n], in0=xt[:, :, 0:n],
                                          in1=rs[:, 0:n].unsqueeze(1).to_broadcast([128, KT, n]), op=ALU.mult),
         reads=[xtk, rsk], writes=[xtk])
    for c in range(KT):
        eng = "act" if c % 2 == 0 else "dve"
        for (ot, wants) in ((out_bf, True), (out_f32, True)):
            if ot is None:
                continue
            o, ok = ot
            if B is None:
                if eng == "act":
                    P.op("act", lambda e, o=o, c=c: e.activation(out=o[:, c, 0:n], in_=xt[:, c, 0:n], func=AF.Identity,
                                                                 scale=A[:, c:c + 1]),
                         reads=[xtk, "nrm", "small"], writes=[(ok, c)])
                else:
                    P.op("dve", lambda e, o=o, c=c: e.tensor_single_scalar(out=o[:, c, 0:n], in_=xt[:, c, 0:n],
                                                                    scalar=A[:, c:c + 1], op=ALU.mult),
                         reads=[xtk, "nrm", "small"], writes=[(ok, c)])
            else:
                if eng == "act":
                    P.op("act", lambda e, o=o, c=c: e.activation(out=o[:, c, 0:n], in_=xt[:, c, 0:n], func=AF.Identity,
                                                                 scale=A[:, c:c + 1], bias=B[:, c:c + 1]),
                         reads=[xtk, "nrm", "modx", "modc"], writes=[(ok, c)])
                else:
                    P.op("dve", lambda e, o=o, c=c: e.tensor_scalar(out=o[:, c, 0:n], in0=xt[:, c, 0:n],
                                                                    scalar1=A[:, c:c + 1], scalar2=B[:, c:c + 1],
                                                                    op0=ALU.mult, op1=ALU.add),
                         reads=[xtk, "nrm", "modx", "modc"], writes=[(ok, c)])


def seg_blocks(kind, lbs):
    if kind == "L":
        return [("L", lb * NB, NB, lb) for lb in lbs]
    return [("C", CPAD, CTX, 0)]


def arrs(g, kind):
    return g.lat if kind == "L" else g.ctxa


def st_norm(g, l, which, jobs, router=False):
    P = g.P
    with Stage(g) as st:
        xts = st.ring("nxt", [128, KT, NB], F32, 2)
        hxs = st.ring("nhx", [128, KT, NB], BF16, 2)
        bufs = {"sq": st.sb("nsq", [128, KT, NB], BF16), "rs": st.sb("nrs", [128, NB], F32)}
        if router:
            yfs = st.sb("nyf", [128, KT, NB], F32)
            rt, rtk = st.sb("rt", [128, KT, NE], F32)
            P.dma("sp", rt[:], g.rt_in.rearrange("(k p) e -> p k e", p=128), writes=[rtk])
            lg, lgk = st.sb("lg", [128, 4, NE], F32)
            mx, mxk = st.sb("mx", [128, 4, 8], F32)
            ex, exk = st.sb("ex", [128, 4, NE], F32)
            mk, mkk = st.sb("mk", [128, 4, NE], F32)
            dn, dnk = st.sb("dn", [128, 4], F32)
            nm1, nm1k = st.sb("nm1", [128, 4], F32)
            cbb, cbbk = st.sb("cbb", [128, NE, 128], F32)
            cmo = st.ring("cmo", [128, NE, NB], F32, 2)
        def job(ji, kind, s, n, lb):
            A = arrs(g, kind)
            si = 0 if kind == "L" else 1
            xt, xtk = xts[ji % 2]
            hx, hxk = hxs[ji % 2]
            P.dma("sp", xt[:, :, 0:n], blk_view(A["XT"], s, n), reads=[(kind + "_XT", lb)], writes=[xtk])
            Av = modvec(g, l, si, "A%d" % which)
            Bv = modvec(g, l, si, "B%d" % which)
            if router:
                norm_core(g, st, xt, xtk, n, Av, Bv, bufs, out_f32=yfs)
                yf, yfk = yfs
                P.op("pool", lambda e, hx=hx, yf=yf: e.tensor_copy(out=hx[:, :, 0:n], in_=yf[:, :, 0:n]),
                     reads=[(yfk, c) for c in range(KT)], writes=[(hxk, c) for c in range(KT)])
                psl = g.ps[6]
                for a in range(4):
                    for k in range(KT):
                        P.op("pe", lambda e, a=a, k=k, yf=yf: e.matmul(psl[:, a * 8:(a + 1) * 8],
                                                                      lhsT=yf[:, k, a * 128:(a + 1) * 128], rhs=rt[:, k, :],
                                                                      start=(k == 0), stop=(k == 7)),
                             reads=[(yfk, k), rtk], writes=["ps6"], sig=(k == 7))
                P.op("dve", lambda e: e.tensor_copy(out=lg[:].rearrange("p a e -> p (a e)"), in_=psl[:, 0:32]),
                     reads=["ps6"], writes=[lgk])
                for a in range(4):
                    P.op("dve", lambda e, a=a: e.max(out=mx[:, a, :], in_=lg[:, a, :]), reads=[lgk], writes=[mxk])
                P.op("dve", lambda e: e.tensor_single_scalar(out=nm1[:], in_=mx[:, :, 0], scalar=-1.0, op=ALU.mult),
                     reads=[mxk], writes=[nm1k])
                for a in range(4):
                    P.op("act", lambda e, a=a: e.activation(out=ex[:, a, :], in_=lg[:, a, :], func=AF.Exp,
                                                            bias=nm1[:, a:a + 1], scale=1.0),
                         reads=[lgk, nm1k], writes=[exk])
                    P.op("dve", lambda e, a=a: e.tensor_single_scalar(out=mk[:, a, :], in_=lg[:, a, :], scalar=mx[:, a, 1:2], op=ALU.is_ge),
                         reads=[lgk, mxk], writes=[mkk])
                P.op("dve", lambda e: e.tensor_tensor(out=ex[:], in0=ex[:], in1=mk[:], op=ALU.mult),
                     reads=[exk, mkk], writes=[exk])
                P.op("dve", lambda e: e.tensor_reduce(out=dn[:], in_=ex[:], axis=mybir.AxisListType.X, op=ALU.add),
                     reads=[exk], writes=[dnk])
                P.op("dve", lambda e: e.reciprocal(out=dn[:], in_=dn[:]), reads=[dnk], writes=[dnk])
                P.op("dve", lambda e: e.tensor_tensor(out=ex[:], in0=ex[:], in1=dn[:].unsqueeze(2).to_broadcast([128, 4, NE]),
                                                      op=ALU.mult), reads=[exk, dnk], writes=[exk])
                co, cok = cmo[ji % 2]
                for a in range(4):
                    for ee in range(NE):
                        P.op("dve", lambda e, a=a, ee=ee: e.tensor_scalar(out=cbb[:, ee, :], in0=g.ident[:], scalar1=g.zeros[:, 0:1],
                                                                          scalar2=ex[:, a, ee:ee + 1], op0=ALU.mult, op1=ALU.add),
                             reads=[exk, "ident", (cbbk, ee)], writes=[(cbbk, ee)])
                    for ee in range(NE):
                        pse = g.ps[ee % 4]
                        P.op("pe", lambda e, a=a, ee=ee, pse=pse: e.matmul(pse[:, 0:128], lhsT=cbb[:, ee, :], rhs=g.ident[:],
                                                                          start=True, stop=True),
                             reads=[(cbbk, ee), "ident"], writes=["ps%d" % (ee % 4)])
                        P.op("act", lambda e, a=a, ee=ee, pse=pse, co=co: e.copy(out=co[:, ee, a * 128:(a + 1) * 128],
                                                                                in_=pse[:, 0:128]),
                             reads=["ps%d" % (ee % 4)], writes=[(cok, ee)])
                P.dma("pool", g.comb[:, :, s:s + n].rearrange("e p t -> p e t"), co[:],
                      reads=[(cok, ee) for ee in range(NE)], writes=[("COMB", lb)])
            else:
                norm_core(g, st, xt, xtk, n, Av, Bv, bufs, out_bf=(hx, hxk))
            P.dma("pool", blk_view(A["HX"], s, n), hx[:, :, 0:n], reads=[(hxk, c) for c in range(KT)],
                  writes=[(kind + "_HX", lb)])
        for ji, (kind, s, n, lb) in enumerate(jobs):
            job(ji, kind, s, n, lb)


def load_w(g, st, name, wdram, row0, col0, ncols, kt=KT):
    t, k = st.sb(name, [128, kt, ncols], BF16)
    g.P.dma("sp", t[:], wdram[row0:row0 + kt * 128, col0:col0 + ncols].rearrange("(k p) n -> p k n", p=128),
            reads=[], writes=[k])
    return t, k


def st_zu(g, l, jobs):
    P = g.P
    with Stage(g) as st:
        w, wk = load_w(g, st, "wu", g.win, l * D, 0, 1024)
        hxs = st.ring("zhx", [128, KT, NB], BF16, 2)
        ub = st.ring("zub", [128, KT, NB], F32, 2)
        def job(ji, kind, s, n, lb):
            A = arrs(g, kind)
            hx, hxk = hxs[ji % 2]
            u, uk = ub[ji % 2]
            P.dma("sp", hx[:, :, 0:n], blk_view(A["HX"], s, n), reads=[(kind + "_HX", lb)], writes=[hxk])
            msk = Scol(g, "blkmask", lb) if kind == "L" else Scol(g, "one", 0)
            for m in range(KT):
                ps = g.ps[m % 4]
                for k in range(KT):
                    P.op("pe", lambda e, ps=ps, m=m, k=k, hx=hx: e.matmul(ps[:, 0:n], lhsT=w[:, k, m * 128:(m + 1) * 128],
                                                                         rhs=hx[:, k, 0:n], start=(k == 0), stop=(k == 7)),
                         reads=[wk, hxk], writes=["ps%d" % (m % 4)], sig=(k == 7))
                P.op("dve", lambda e, ps=ps, m=m, u=u, msk=msk: e.tensor_single_scalar(out=u[:, m, 0:n], in_=ps[:, 0:n], scalar=msk, op=ALU.mult),
                     reads=["ps%d" % (m % 4), "small"], writes=[(uk, m)])
            P.dma("pool", blk_view(A["U"], s, n), u[:, :, 0:n], reads=[(uk, m) for m in range(KT)],
                  writes=[(kind + "_U", lb)])
        for ji, (kind, s, n, lb) in enumerate(jobs):
            job(ji, kind, s, n, lb)


def st_zgm(g, l, jobs, bg_pieces=None, bg_every=2):
    P = g.P
    with Stage(g) as st:
        wg, wgk = load_w(g, st, "wg", g.win, l * D, 1024, 1024)
        wm, wmk = load_w(g, st, "wm", g.win, l * D, 4096, 2048)
        hxs = st.ring("zhx", [128, KT, NB], BF16, 2)
        ob = st.ring("zgo", [128, 3 * KT, NB], BF16, 2)
        bg = bg_convert(g, st, bg_pieces) if bg_pieces else None
        bgc = 0
        pi = 0
        def job(ji, kind, s, n, lb):
            nonlocal pi, bgc
            A = arrs(g, kind)
            hx, hxk = hxs[ji % 2]
            o, ok = ob[ji % 2]
            P.dma("sp", hx[:, :, 0:n], blk_view(A["HX"], s, n), reads=[(kind + "_HX", lb)], writes=[hxk])
            for m in range(3 * KT):
                ps = g.ps[pi % 6]
                psk = "ps%d" % (pi % 6)
                pi += 1
                wt, wtk, mm = (wg, wgk, m) if m < KT else (wm, wmk, m - KT)
                for k in range(KT):
                    P.op("pe", lambda e, ps=ps, wt=wt, mm=mm, k=k, hx=hx: e.matmul(
                        ps[:, 0:n], lhsT=wt[:, k, mm * 128:(mm + 1) * 128], rhs=hx[:, k, 0:n], start=(k == 0), stop=(k == 7)),
                        reads=[wtk, hxk], writes=[psk], sig=(k == 7))
                fn = AF.Gelu_apprx_tanh if m < KT else AF.Sigmoid
                P.op("act", lambda e, ps=ps, o=o, m=m, fn=fn: e.activation(out=o[:, m, 0:n], in_=ps[:, 0:n], func=fn),
                     reads=[psk], writes=[(ok, m)])
                if bg is not None:
                    bgc += 1
                    if bgc % bg_every == 0:
                        next(bg, None)
            P.dma("pool", blk_view(A["G"], s, n), o[:, 0:KT, 0:n], reads=[(ok, m) for m in range(KT)],
                  writes=[(kind + "_G", lb)])
            P.dma("pool", blk_view(A["SGA"], s, n), o[:, KT:2 * KT, 0:n], reads=[(ok, m) for m in range(KT, 2 * KT)],
                  writes=[(kind + "_SGA", lb)])
            P.dma("pool", blk_view(A["SGB"], s, n), o[:, 2 * KT:3 * KT, 0:n], reads=[(ok, m) for m in range(2 * KT, 3 * KT)],
                  writes=[(kind + "_SGB", lb)])
        for ji, (kind, s, n, lb) in enumerate(jobs):
            job(ji, kind, s, n, lb)
        if bg is not None:
            for _ in bg:
                pass


def st_zv(g, l, jobs, bg_pieces=None, bg_every=2):
    P = g.P
    with Stage(g) as st:
        w, wk = load_w(g, st, "wv", g.win, l * D, 2048, 2048)
        hxs = st.ring("zhx", [128, KT, NB], BF16, 2)
        vb = st.ring("zvb", [128, KT, NB], BF16, 2)
        sgs = st.ring("zsg", [128, NB], F32, 3)
        bg = bg_convert(g, st, bg_pieces, eng="act") if bg_pieces else None
        bgc = 0
        pi = 0
        def job(ji, kind, s, n, lb):
            nonlocal pi, bgc
            A = arrs(g, kind)
            hx, hxk = hxs[ji % 2]
            v, vk = vb[ji % 2]
            P.dma("sp", hx[:, :, 0:n], blk_view(A["HX"], s, n), reads=[(kind + "_HX", lb)], writes=[hxk])
            msk = Scol(g, "blkmask", lb) if kind == "L" else Scol(g, "one", 0)
            for m in range(KT):
                pa, pb = g.ps[(2 * pi) % 6], g.ps[(2 * pi + 1) % 6]
                pak, pbk = "ps%d" % ((2 * pi) % 6), "ps%d" % ((2 * pi + 1) % 6)
                sg, sgk = sgs[pi % 3]
                pi += 1
                for (ps, psk, mm) in ((pa, pak, m), (pb, pbk, KT + m)):
                    for k in range(KT):
                        P.op("pe", lambda e, ps=ps, mm=mm, k=k, hx=hx: e.matmul(
                            ps[:, 0:n], lhsT=w[:, k, mm * 128:(mm + 1) * 128], rhs=hx[:, k, 0:n], start=(k == 0), stop=(k == 7)),
                            reads=[wk, hxk], writes=[psk], sig=(k == 7))
                P.op("act", lambda e, pb=pb, sg=sg: e.activation(out=sg[:, 0:n], in_=pb[:, 0:n], func=AF.Sigmoid),
                     reads=[pbk], writes=[sgk])
                P.op("dve", lambda e, pa=pa, sg=sg, v=v, m=m, msk=msk: e.scalar_tensor_tensor(
                    out=v[:, m, 0:n], in0=pa[:, 0:n], scalar=msk, in1=sg[:, 0:n], op0=ALU.mult, op1=ALU.mult),
                    reads=[pak, sgk, "small"], writes=[(vk, m)])
                if bg is not None:
                    bgc += 1
                    if bgc % bg_every == 0:
                        next(bg, None)
            P.dma("pool", blk_view(A["V"], s, n), v[:, :, 0:n], reads=[(vk, m) for m in range(KT)],
                  writes=[(kind + "_V", lb)])
        for ji, (kind, s, n, lb) in enumerate(jobs):
            job(ji, kind, s, n, lb)
        if bg is not None:
            for _ in bg:
                pass


def st_scan(g, l, jobs):
    P = g.P
    with Stage(g) as st:
        gw, gwk = st.sb("gw", [128, 2, 2, KT, 128], BF16)
        P.dma("sp", gw[:].rearrange("p d t c m -> p (d t c) m"),
              g.gw[l * 4096:(l + 1) * 4096, :].rearrange("(x p) m -> p x m", p=128), reads=["gw_bf"], writes=[gwk])
        uhs = st.ring("uh", [128, KT, NB + 3], F32, 2)
        ucs = st.ring("uc", [128, KT, NB], F32, 2)
        ucb, ucbk = st.sb("ucb", [128, KT, NB], BF16)
        so = st.ring("so", [128, KT, NB], F32, 2)
        afo = st.ring("afo", [128, KT, NB], BF16, 2)
        abo = st.ring("abo", [128, KT, NB], BF16, 2)
        R = lambda nm, k=4: st.ring(nm, [128, NB], F32, k)
        rr, gi, aa, a2, bb, hh = R("rr"), R("gi"), R("aa"), R("a2"), R("bb"), R("hh", 4)
        rsm = st.ring("rsm", [128, 2], F32, 4)
        cw = S(g, "cw4_%d" % l)
        cb = S(g, "cb4_%d" % l)
        it = 0

        def job(ji, kind, s, n, lb):
            nonlocal it
            A = arrs(g, kind)
            uh, uhk = uhs[ji % 2]
            uc, uck = ucs[ji % 2]
            sot, sok = so[ji % 2]
            aft, afk = afo[ji % 2]
            abt, abk = abo[ji % 2]
            rks = [(kind + "_U", b) for b in ((lb - 1, lb, lb + 1) if kind == "L" else (0,))]
            P.dma("sp", uh[:, :, 0:n + 3], blk_view(A["U"], s - 2, n + 3), reads=rks, writes=[uhk])
            for c in range(KT):
                P.op("dve", lambda e, c=c: e.tensor_scalar(out=uc[:, c, 0:n], in0=uh[:, c, 0:n],
                                                          scalar1=cw[:, c:c + 1], scalar2=cb[:, c:c + 1],
                                                          op0=ALU.mult, op1=ALU.add),
                     reads=[uhk, "small"], writes=[(uck, c)])
                for j in range(1, 4):
                    P.op("dve", lambda e, c=c, j=j: e.scalar_tensor_tensor(
                        out=uc[:, c, 0:n], in0=uh[:, c, j:j + n], scalar=cw[:, j * 8 + c:j * 8 + c + 1], in1=uc[:, c, 0:n],
                        op0=ALU.mult, op1=ALU.add), reads=[uhk, "small", (uck, c)], writes=[(uck, c)])
                P.op("pool", lambda e, c=c: e.tensor_copy(out=ucb[:, c, 0:n], in_=uc[:, c, 0:n]),
                     reads=[(uck, c)], writes=[(ucbk, c)])
            def stA(c):
                nonlocal it
                T = []
                for d in range(2):
                    T.append((rr[it % 4], gi[it % 4], aa[it % 4], a2[it % 4], bb[it % 4], hh[it % 4], rsm[it % 4],
                              g.ps[(2 * it) % 8], "ps%d" % ((2 * it) % 8), g.ps[(2 * it + 1) % 8], "ps%d" % ((2 * it + 1) % 8)))
                    it += 1
                for d in range(2):
                    pr, prk, pi_, pik = T[d][7], T[d][8], T[d][9], T[d][10]
                    P.op("pe", lambda e, pr=pr, d=d, c=c: e.matmul(pr[:, 0:n], lhsT=gw[:, d, 0, c, :], rhs=ucb[:, c, 0:n],
                                                                  start=True, stop=True),
                         reads=[gwk, (ucbk, c)], writes=[prk])
                    P.op("pe", lambda e, pi_=pi_, d=d, c=c: e.matmul(pi_[:, 0:n], lhsT=gw[:, d, 1, c, :], rhs=ucb[:, c, 0:n],
                                                                    start=True, stop=True),
                         reads=[gwk, (ucbk, c)], writes=[pik])
                for d in range(2):
                    (r_, rk_), (gi_, gik_), _, _, _, _, (rs_, rsk_), pr, prk, pi_, pik = T[d]
                    br = Scol(g, "br%d%d" % (l, d), c)
                    bi = Scol(g, "bi%d%d" % (l, d), c)
                    P.op("act", lambda e, pr=pr, r_=r_, br=br, rs_=rs_: e.activation(out=r_[:, 0:n], in_=pr[:, 0:n], func=AF.Sigmoid, bias=br,
                                                                                accum_out=rs_[:, 0:1]),
                         reads=[prk, "small"], writes=[rk_, rsk_])
                    P.op("act", lambda e, pi_=pi_, gi_=gi_, bi=bi: e.activation(out=gi_[:, 0:n], in_=pi_[:, 0:n], func=AF.Sigmoid, bias=bi),
                         reads=[pik, "small"], writes=[gik_])
                for d in range(2):
                    (r_, rk_), _, (a_, ak_), _, _, _, (rs_, rsk_) = T[d][0:7]
                    P.op("act", lambda e, r_=r_, a_=a_, d=d, c=c: e.activation(out=a_[:, 0:n], in_=r_[:, 0:n], func=AF.Exp,
                                                                             scale=g.cl[:, l, d, 0, c:c + 1]),
                         reads=[rk_, "cl"], writes=[ak_])
                    if kind == "L" and 4 <= lb < 12:
                        P.op("act", lambda e, rs_=rs_, c=c, d=d: e.activation(
                            out=g.sumt[:, c, lb - 4, 2 * d:2 * d + 1], in_=rs_[:, 0:1], func=AF.Exp, scale=g.cl[:, l, d, 0, c:c + 1]),
                            reads=[rsk_, "cl"], writes=["sumt"])
                for d in range(2):
                    _, _, (a_, ak_), (a2_, a2k_) = T[d][0:4]
                    P.op("dve", lambda e, a_=a_, a2_=a2_: e.tensor_tensor(out=a2_[:, 0:n], in0=a_[:, 0:n], in1=a_[:, 0:n], op=ALU.mult),
                         reads=[ak_], writes=[a2k_])
                return T

            def stB(c, T):
                for d in range(2):
                    (a2_, a2k_) = T[d][3]
                    P.op("act", lambda e, a2_=a2_: e.activation(out=a2_[:, 0:n], in_=a2_[:, 0:n], func=AF.Sqrt, scale=-1.0, bias=1.0),
                         reads=[a2k_], writes=[a2k_])
                for d in range(2):
                    _, (gi_, gik_), (a_, ak_), (a2_, a2k_), (b_, bk_), (h_, hk_) = T[d][0:6]
                    P.op("pool", lambda e, gi_=gi_, c=c, b_=b_: e.tensor_tensor(out=b_[:, 0:n], in0=gi_[:, 0:n], in1=uc[:, c, 0:n], op=ALU.mult),
                         reads=[gik_, (uck, c)], writes=[bk_])
                    P.op("pool", lambda e, a2_=a2_, b_=b_: e.tensor_tensor(out=b_[:, 0:n], in0=b_[:, 0:n], in1=a2_[:, 0:n], op=ALU.mult),
                         reads=[bk_, a2k_], writes=[bk_])
                    At, Atk = (aft, afk) if d == 0 else (abt, abk)
                    if d == 0:
                        P.op("dve", lambda e, a_=a_, b_=b_, h_=h_: e.tensor_tensor_scan(out=h_[:, 0:n], data0=a_[:, 0:n], data1=b_[:, 0:n],
                                                                                       initial=0.0, op0=ALU.mult, op1=ALU.add),
                             reads=[ak_, bk_], writes=[hk_])
                        P.op("dve", lambda e, a_=a_, At=At, c=c: e.tensor_tensor_scan(out=At[:, c, 0:n], data0=a_[:, 0:n], data1=g.zeros[:, 0:n],
                                                                                     initial=1.0, op0=ALU.mult, op1=ALU.add),
                             reads=[ak_, "zeros"], writes=[(Atk, c)])
                        hfwd = (h_, hk_)
                        e0, e1 = n - 1, n
                    else:
                        P.op("dve", lambda e, a_=a_, b_=b_, h_=h_: e.tensor_tensor_scan(out=h_[:, 0:n][:, ::-1],
                                                                                       data0=a_[:, 0:n][:, ::-1], data1=b_[:, 0:n][:, ::-1],
                                                                                       initial=0.0, op0=ALU.mult, op1=ALU.add),
                             reads=[ak_, bk_], writes=[hk_])
                        P.op("dve", lambda e, a_=a_, At=At, c=c: e.tensor_tensor_scan(out=At[:, c, 0:n][:, ::-1], data0=a_[:, 0:n][:, ::-1],
                                                                                     data1=g.zeros[:, 0:n], initial=1.0, op0=ALU.mult, op1=ALU.add),
                             reads=[ak_, "zeros"], writes=[(Atk, c)])
                        hf_, hfk_ = hfwd
                        P.op("dve", lambda e, h_=h_, hf_=hf_, c=c: e.tensor_tensor(out=sot[:, c, 0:n], in0=hf_[:, 0:n], in1=h_[:, 0:n], op=ALU.add),
                             reads=[hk_, hfk_], writes=[(sok, c)])
                        e0, e1 = 0, 1
                    if kind == "L" and 4 <= lb < 12:
                        P.op("pool", lambda e, h_=h_, c=c, d=d, e0=e0, e1=e1: e.tensor_copy(
                            out=g.sumt[:, c, lb - 4, 2 * d + 1:2 * d + 2], in_=h_[:, e0:e1]), reads=[hk_, "sumt"], writes=["sumt"])
                    if kind == "C":
                        P.op("pool", lambda e, h_=h_, c=c, d=d, e0=e0, e1=e1: e.tensor_copy(
                            out=g.sctx[:, d, c:c + 1], in_=h_[:, e0:e1]), reads=[hk_, "sctx"], writes=["sctx"])

            Tn = stA(0)
            for c in range(KT):
                Tc = Tn
                if c + 1 < KT:
                    Tn = stA(c + 1)
                stB(c, Tc)
            P.dma("pool", blk_view(A["S"], s, n), sot[:, :, 0:n], reads=[(sok, c) for c in range(KT)], writes=[(kind + "_S", lb)])
            P.dma("pool", blk_view(A["AF"], s, n), aft[:, :, 0:n], reads=[(afk, c) for c in range(KT)], writes=[(kind + "_AF", lb)])
            P.dma("pool", blk_view(A["AB"], s, n), abt[:, :, 0:n], reads=[(abk, c) for c in range(KT)], writes=[(kind + "_AB", lb)])
        for ji, (kind, s, n, lb) in enumerate(jobs):
            job(ji, kind, s, n, lb)


def st_carry(g, l):
    P = g.P
    with Stage(g) as st:
        P.dma("sp", g.sum_in, g.sumt[:].rearrange("p c b f -> p (c b f)"), reads=["sumt"], writes=["sum_in"])
        P.collective("AllGather", ins=[g.sum_in], outs=[g.sum_all], groups=[[0, 1, 2, 3], [4, 5, 6, 7]],
                     reads=["sum_in"], writes=["sum_all"])
        sa, sak = st.sb("sa", [128, 4, KT, 8, 4], F32)
        P.dma("sp", sa[:], g.sum_all.rearrange("(r p) (c b f) -> p r c b f", p=128, c=KT, b=8), reads=["sum_all"], writes=[sak])
        ht, htk = st.sb("ht", [128, 2, KT, 48], F32)
        P.op("dve", lambda e: e.memset(ht[:], 0.0), writes=[htk])
        tmp, tmk = st.sb("ctmp", [128, KT], F32)
        P.op("dve", lambda e: e.tensor_copy(out=ht[:, 0, :, 8], in_=g.sctx[:, 0, :]), reads=["sctx", htk], writes=[htk])
        for gb in range(32):
            r, b = gb // 8, gb % 8
            P.op("dve", lambda e, r=r, b=b, gb=gb: e.tensor_tensor(out=tmp[:], in0=sa[:, r, :, b, 0], in1=ht[:, 0, :, 8 + gb], op=ALU.mult),
                 reads=[sak, htk], writes=[tmk])
            P.op("dve", lambda e, r=r, b=b, gb=gb: e.tensor_tensor(out=ht[:, 0, :, 9 + gb], in0=tmp[:], in1=sa[:, r, :, b, 1], op=ALU.add),
                 reads=[sak, tmk, htk], writes=[htk])
        P.op("dve", lambda e: e.tensor_copy(out=ht[:, 1, :, 40], in_=g.sctx[:, 1, :]), reads=["sctx", htk], writes=[htk])
        for gb in range(31, -1, -1):
            r, b = gb // 8, gb % 8
            P.op("dve", lambda e, r=r, b=b, gb=gb: e.tensor_tensor(out=tmp[:], in0=sa[:, r, :, b, 2], in1=ht[:, 1, :, 9 + gb], op=ALU.mult),
                 reads=[sak, htk], writes=[tmk])
            P.op("dve", lambda e, r=r, b=b, gb=gb: e.tensor_tensor(out=ht[:, 1, :, 8 + gb], in0=tmp[:], in1=sa[:, r, :, b, 3], op=ALU.add),
                 reads=[sak, tmk, htk], writes=[htk])
        P.dma("sp", g.htab.rearrange("d p c x -> p d c x"), ht[:], reads=[htk], writes=["htab"])

        def dyn(e, d):
            base = dynval(g, e, "b%d" % d)
            return e.dma_start(out=g.hin[:, d, :, :],
                               in_=g.htab[d:d + 1, :, :, bass.ds(base, 12)].rearrange("o p c x -> p (o c) x"))
        P.dma("sp", None, None, reads=["htab"], writes=["hin"], fn=lambda e: dyn(e, 0))
        P.dma("sp", None, None, reads=["htab"], writes=["hin"], fn=lambda e: dyn(e, 1))


def st_conv(g, l, kinds, bg_pieces=None):
    P = g.P
    with Stage(g) as st:
        dg = st.ring("dg", [128, 31, 128], BF16, 2)
        vrl = st.ring("vrl", [128, TL], BF16, 2)
        cvo = st.ring("cvo", [128, NB], F32, 3)
        bg = bg_convert(g, st, bg_pieces, eng="act") if bg_pieces else None
        oi = 0
        for c in range(KT):
            dgt, dgk = dg[c % 2]
            for j in range(31):
                P.op("dve", lambda e, j=j, c=c, dgt=dgt: e.tensor_single_scalar(out=dgt[:, j, :], in_=g.ident[:],
                                                                        scalar=Scol(g, "cw31_%d" % l, j * 8 + c), op=ALU.mult),
                     reads=["ident", "small", (dgk, j)], writes=[(dgk, j)])
            for (kind, lbs) in kinds:
                A = arrs(g, kind)
                vr, vrk = vrl[oi % 2]
                if kind == "L":
                    lo, hi = (lbs[0] - 2) * NB, (lbs[-1] + 3) * NB
                    P.dma("sp", vr[:, lo:hi], A["V"][c, :, lo:hi], reads=[("L_V", b) for b in range(lbs[0] - 2, lbs[-1] + 3)],
                          writes=[vrk])
                    stride = 64
                    blocks = [(lb * NB, NB, lb) for lb in lbs]
                else:
                    P.dma("sp", vr[:, 0:TC], A["V"][c, :, :], reads=[("C_V", 0), "C_V"], writes=[vrk])
                    stride = 1
                    blocks = [(CPAD, CTX, 0)]
                for (s, n, lb) in blocks:
                    ps = g.ps[oi % 4]
                    psk = "ps%d" % (oi % 4)
                    o, ok = cvo[oi % 3]
                    oi += 1
                    for j in range(31):
                        off = s + (j - 15) * stride
                        P.op("pe", lambda e, ps=ps, j=j, off=off, vr=vr, dgt=dgt, n=n: e.matmul(
                            ps[:, 0:n], lhsT=dgt[:, j, :], rhs=vr[:, off:off + n], start=(j == 0), stop=(j == 30)),
                            reads=[(dgk, j), vrk], writes=[psk], sig=(j == 30))
                    P.op("act", lambda e, ps=ps, o=o, n=n, c=c: e.activation(out=o[:, 0:n], in_=ps[:, 0:n], func=AF.Identity,
                                                                           bias=Scol(g, "cb31_%d" % l, c), scale=1.0),
                         reads=[psk, "small"], writes=[ok])
                    P.dma("pool", A["CV"][c, :, s:s + n], o[:, 0:n], reads=[ok], writes=[(kind + "_CV", lb, c)])
                    if bg is not None:
                        next(bg, None)
        if bg is not None:
            for _ in bg:
                pass


def st_mix(g, l, jobs):
    P = g.P
    H = 256
    with Stage(g) as st:
        wa, wak = load_w(g, st, "wa", g.wa, l * D, 0, D)
        wb, wbk = load_w(g, st, "wb", g.wb, l * D, 0, D)
        wo, wok = load_w(g, st, "wo", g.wo, l * D, 0, D)
        cvs = st.ring("mcv", [128, KT, H], F32, 2)
        ss = st.ring("ms", [128, KT, H], F32, 2)
        afs = st.ring("maf", [128, KT, H], BF16, 2)
        abs_ = st.ring("mab", [128, KT, H], BF16, 2)
        gs = st.ring("mg", [128, KT, H], BF16, 2)
        sgas = st.ring("msga", [128, KT, H], BF16, 2)
        sgbs = st.ring("msgb", [128, KT, H], BF16, 2)
        xts = st.ring("mxt", [128, KT, H], F32, 2)
        cvb, cvbk = st.sb("cvb", [128, KT, H], BF16)
        sqb, sqbk = st.sb("sqb", [128, KT, H], BF16)
        lno, lnok = st.sb("lno", [128, KT, H], BF16)
        mbt, mbk = st.sb("mbt", [128, KT, H], F32)
        hst, hsk = st.sb("hst", [128, KT, H], F32)
        hsb, hsbk = st.sb("hsb", [128, KT, H], BF16)
        mt, mtk = st.sb("mt", [128, KT, H], BF16)
        mu, muk = st.sb("mu", [128, H], F32)
        var, vark = st.sb("var", [128, H], F32)
        tmp1, tmp1k = st.sb("tmp1", [128, H], F32)
        lng, lnb = S(g, "lng%d" % l), S(g, "lnb%d" % l)
        it = 0
        pi = 0

        def nps():
            nonlocal pi
            r = (g.ps[pi % 6], "ps%d" % (pi % 6))
            pi += 1
            return r
        for (kind, s0, n0, lb) in jobs:
            A = arrs(g, kind)
            si = 0 if kind == "L" else 1
            def half_body(kind, lb, s0, n0, s, A, si):
                nonlocal it
                n = min(H, s0 + n0 - s)
                cv, cvk = cvs[it % 2]
                sst, ssk = ss[it % 2]
                af, afk = afs[it % 2]
                ab, abk = abs_[it % 2]
                gt, gk = gs[it % 2]
                sga, sgak = sgas[it % 2]
                sgb, sgbk = sgbs[it % 2]
                xt, xtk = xts[it % 2]
                it += 1
                P.dma("sp", cv[:, :, 0:n], blk_view(A["CV"], s, n), reads=[(kind + "_CV", lb, c) for c in range(KT)], writes=[cvk])
                P.dma("sp", sst[:, :, 0:n], blk_view(A["S"], s, n), reads=[(kind + "_S", lb)], writes=[ssk])
                P.dma("sp", af[:, :, 0:n], blk_view(A["AF"], s, n), reads=[(kind + "_AF", lb)], writes=[afk])
                P.dma("sp", ab[:, :, 0:n], blk_view(A["AB"], s, n), reads=[(kind + "_AB", lb)], writes=[abk])
                P.dma("sp", gt[:, :, 0:n], blk_view(A["G"], s, n), reads=[(kind + "_G", lb)], writes=[gk])
                P.dma("sp", sga[:, :, 0:n], blk_view(A["SGA"], s, n), reads=[(kind + "_SGA", lb)], writes=[sgak])
                P.dma("sp", sgb[:, :, 0:n], blk_view(A["SGB"], s, n), reads=[(kind + "_SGB", lb)], writes=[sgbk])
                P.dma("sp", xt[:, :, 0:n], blk_view(A["XT"], s, n), reads=[(kind + "_XT", lb)], writes=[xtk])
                P.op("act", lambda e, cv=cv: e.copy(out=cvb[:, :, 0:n], in_=cv[:, :, 0:n]), reads=[cvk], writes=[cvbk])
                P.op("act", lambda e, cv=cv: e.activation(out=sqb[:, :, 0:n], in_=cv[:, :, 0:n], func=AF.Square), reads=[cvk], writes=[sqbk])
                p1, p1k = g.ps[6], "ps6"
                p2, p2k = g.ps[7], "ps7"
                for k in range(KT):
                    P.op("pe", lambda e, k=k: e.matmul(p1[:, 0:n], lhsT=g.ones_b[:], rhs=cvb[:, k, 0:n], start=(k == 0), stop=(k == 7)),
                         reads=[cvbk, "ones_b"], writes=[p1k], sig=(k == 7))
                for k in range(KT):
                    P.op("pe", lambda e, k=k: e.matmul(p2[:, 0:n], lhsT=g.ones_b[:], rhs=sqb[:, k, 0:n], start=(k == 0), stop=(k == 7)),
                         reads=[sqbk, "ones_b"], writes=[p2k], sig=(k == 7))
                P.op("dve", lambda e: e.tensor_single_scalar(out=mu[:, 0:n], in_=p1[:, 0:n], scalar=1.0 / D, op=ALU.mult),
                     reads=[p1k], writes=[muk])
                P.op("dve", lambda e: e.tensor_tensor(out=tmp1[:, 0:n], in0=mu[:, 0:n], in1=mu[:, 0:n], op=ALU.mult),
                     reads=[muk], writes=[tmp1k])
                P.op("dve", lambda e: e.scalar_tensor_tensor(out=var[:, 0:n], in0=p2[:, 0:n], scalar=1.0 / D, in1=tmp1[:, 0:n],
                                                             op0=ALU.mult, op1=ALU.subtract),
                     reads=[p2k, tmp1k], writes=[vark])
                P.op("dve", lambda e: e.tensor_single_scalar(out=var[:, 0:n], in_=var[:, 0:n], scalar=0.0, op=ALU.max),
                     reads=[vark], writes=[vark])
                P.op("act", lambda e: e.activation(out=var[:, 0:n], in_=var[:, 0:n], func=AF.Sqrt, bias=EPS, scale=1.0),
                     reads=[vark], writes=[vark])
                P.op("dve", lambda e: e.reciprocal(out=var[:, 0:n], in_=var[:, 0:n]), reads=[vark], writes=[vark])
                P.op("dve", lambda e, cv=cv: e.tensor_tensor(out=cv[:, :, 0:n], in0=cv[:, :, 0:n],
                                                            in1=mu[:, 0:n].unsqueeze(1).to_broadcast([128, KT, n]), op=ALU.subtract),
                     reads=[cvk, muk], writes=[cvk])
                P.op("dve", lambda e, cv=cv: e.tensor_tensor(out=cv[:, :, 0:n], in0=cv[:, :, 0:n],
                                                            in1=var[:, 0:n].unsqueeze(1).to_broadcast([128, KT, n]), op=ALU.mult),
                     reads=[cvk, vark], writes=[cvk])
                for c in range(KT):
                    P.op("act", lambda e, c=c, cv=cv: e.activation(out=lno[:, c, 0:n], in_=cv[:, c, 0:n], func=AF.Silu,
                                                                  scale=lng[:, c:c + 1], bias=lnb[:, c:c + 1]),
                         reads=[cvk, "small"], writes=[(lnok, c)])
                for m in range(KT):
                    ps, psk = nps()
                    for k in range(KT):
                        P.op("pe", lambda e, ps=ps, m=m, k=k: e.matmul(ps[:, 0:n], lhsT=wb[:, k, m * 128:(m + 1) * 128], rhs=lno[:, k, 0:n],
                                                                      start=(k == 0), stop=(k == 7)),
                             reads=[wbk, (lnok, k)], writes=[psk], sig=(k == 7))
                    P.op("dve", lambda e, ps=ps, m=m, sgb=sgb: e.tensor_tensor(out=mbt[:, m, 0:n], in0=ps[:, 0:n], in1=sgb[:, m, 0:n], op=ALU.mult),
                         reads=[psk, sgbk], writes=[(mbk, m)])
                hin = g.hin if kind == "L" else g.hzero
                bidx = (lb - 2) if kind == "L" else 0
                for c in range(KT):
                    P.op("dve", lambda e, c=c, af=af, sst=sst: e.scalar_tensor_tensor(
                        out=hst[:, c, 0:n], in0=af[:, c, 0:n], scalar=hin[:, 0, c, bidx:bidx + 1], in1=sst[:, c, 0:n],
                        op0=ALU.mult, op1=ALU.add), reads=[afk, ssk, "hin", "hzero"], writes=[(hsk, c)])
                    P.op("dve", lambda e, c=c, ab=ab: e.scalar_tensor_tensor(
                        out=hst[:, c, 0:n], in0=ab[:, c, 0:n], scalar=hin[:, 1, c, bidx:bidx + 1], in1=hst[:, c, 0:n],
                        op0=ALU.mult, op1=ALU.add), reads=[abk, (hsk, c), "hin", "hzero"], writes=[(hsk, c)])
                    P.op("pool", lambda e, c=c, gt=gt: e.tensor_tensor(out=hsb[:, c, 0:n], in0=hst[:, c, 0:n], in1=gt[:, c, 0:n], op=ALU.mult),
                         reads=[(hsk, c), gk], writes=[(hsbk, c)])
                for m in range(KT):
                    ps, psk = nps()
                    for k in range(KT):
                        P.op("pe", lambda e, ps=ps, m=m, k=k: e.matmul(ps[:, 0:n], lhsT=wa[:, k, m * 128:(m + 1) * 128], rhs=hsb[:, k, 0:n],
                                                                      start=(k == 0), stop=(k == 7)),
                             reads=[wak, (hsbk, k)], writes=[psk], sig=(k == 7))
                    P.op("dve", lambda e, ps=ps, m=m, sga=sga: e.tensor_tensor(out=hst[:, m, 0:n], in0=ps[:, 0:n], in1=sga[:, m, 0:n], op=ALU.mult),
                         reads=[psk, sgak], writes=[(hsk, m)])
                    P.op("pool", lambda e, m=m: e.tensor_tensor(out=mt[:, m, 0:n], in0=hst[:, m, 0:n], in1=mbt[:, m, 0:n], op=ALU.add),
                         reads=[(hsk, m), (mbk, m)], writes=[(mtk, m)])
                g1 = modvec(g, l, si, "G1")
                for m in range(KT):
                    ps, psk = nps()
                    for k in range(KT):
                        P.op("pe", lambda e, ps=ps, m=m, k=k: e.matmul(ps[:, 0:n], lhsT=wo[:, k, m * 128:(m + 1) * 128], rhs=mt[:, k, 0:n],
                                                                      start=(k == 0), stop=(k == 7)),
                             reads=[wok, (mtk, k)], writes=[psk], sig=(k == 7))
                    P.op("dve", lambda e, ps=ps, m=m, xt=xt: e.scalar_tensor_tensor(
                        out=xt[:, m, 0:n], in0=ps[:, 0:n], scalar=g1[:, m:m + 1], in1=xt[:, m, 0:n], op0=ALU.mult, op1=ALU.add),
                        reads=[psk, xtk, "modx", "modc"], writes=[xtk])
                P.dma("pool", blk_view(A["XT"], s, n), xt[:, :, 0:n], reads=[xtk], writes=[(kind + "_XT", lb)])
            for s in range(s0, s0 + n0, H):
                half_body(kind, lb, s0, n0, s, A, si)


def st_ffn(g, l, sblocks, moe, publish=False):
    P = g.P
    CH = 256
    NCH = DFF // CH
    TB = 1024
    with Stage(g) as st:
        hxs = st.ring("fhx", [128, KT, TB], BF16, 1)
        acc, acck = st.sb("facc", [128, KT, TB], F32)
        cmb = st.ring("fcmb", [128, TB], F32, 2)
        w1s = st.ring("fw1", [128, KT, 2 * CH], BF16, 3)
        w3s = st.ring("fw3", [128, KT, 2 * CH], BF16, 3)
        w2s = st.ring("fw2", [128, 4, D], BF16, 3)
        sil = st.ring("fsil", [128, NB], BF16, 3)
        gts = st.ring("fgt", [128, 4, NB], BF16, 2)
        xts = st.ring("fxt", [128, KT, NB], F32, 2)
        ne = NE if moe else 1
        wi = 0
        hi_ = 0
        gi_ = 0
        pi = 0
        groups = [(c0, min(2, NCH - c0)) for c0 in range(0, NCH, 2)]
        def sb_body(kind, s0, n0, lbs):
            nonlocal wi, hi_, gi_, pi
            A = arrs(g, kind)
            si = 0 if kind == "L" else 1
            hx, hxk = hxs[0]
            P.dma("sp", hx[:, :, 0:n0], blk_view(A["HX"], s0, n0), reads=[(kind + "_HX", lb) for lb in lbs], writes=[hxk])
            halves = [(o, min(NB, n0 - o)) for o in range(0, n0, NB)]
            steps = [(ex, gi2, c0, nc_, ho, hn) for ex in range(ne) for gi2, (c0, nc_) in enumerate(groups) for (ho, hn) in halves]
            loaded = {}
            cms = {}

            def ensure(ex, gi2, c0, nc_):
                nonlocal wi
                if moe and ex not in cms:
                    cm, cmk = cmb[ex % 2]
                    P.dma("sp", cm[:, 0:n0], g.comb[ex, :, s0:s0 + n0], reads=[("COMB", lb) for lb in lbs], writes=[cmk])
                    cms[ex] = (cm, cmk)
                if (ex, gi2) in loaded:
                    return
                if moe:
                    W1, W3, W2 = g.m1, g.m3, g.m2
                    r1, r2 = ex * D, ex * DFF
                else:
                    W1, W3, W2 = g.f1, g.f3, g.f2
                    r1, r2 = 0, 0
                w1, w1k = w1s[wi % 3]
                w3, w3k = w3s[wi % 3]
                w2, w2k = w2s[wi % 3]
                wi += 1
                cw_ = nc_ * CH
                nj = nc_ * 2
                P.dma("sp", w1[:, :, 0:cw_], W1[r1:r1 + D, c0 * CH:c0 * CH + cw_].rearrange("(k p) n -> p k n", p=128),
                      reads=[], writes=[w1k])
                P.dma("sp", w3[:, :, 0:cw_], W3[r1:r1 + D, c0 * CH:c0 * CH + cw_].rearrange("(k p) n -> p k n", p=128),
                      reads=[], writes=[w3k])
                P.dma("sp", w2[:, 0:nj, :], W2[r2 + c0 * CH:r2 + c0 * CH + cw_, :].rearrange("(k p) n -> p k n", p=128),
                      reads=[], writes=[w2k])
                loaded[(ex, gi2)] = (w1, w1k, w3, w3k, w2, w2k, nj)

            def emit_h(step):
                nonlocal hi_, gi_, pi
                ex, gi2, c0, nc_, ho, hn = step
                ensure(ex, gi2, c0, nc_)
                w1, w1k, w3, w3k, w2, w2k, nj = loaded[(ex, gi2)]
                gt, gtk = gts[gi_ % 2]
                gi_ += 1
                for j in range(nj):
                    p1, p1k = g.ps[pi % 4], "ps%d" % (pi % 4)
                    p3, p3k = g.ps[(pi + 1) % 4], "ps%d" % ((pi + 1) % 4)
                    pi += 2
                    for k in range(KT):
                        P.op("pe", lambda e, p1=p1, w1=w1, j=j, k=k, ho=ho, hn=hn: e.matmul(
                            p1[:, 0:hn], lhsT=w1[:, k, j * 128:(j + 1) * 128], rhs=hx[:, k, ho:ho + hn], start=(k == 0), stop=(k == 7)),
                            reads=[w1k, hxk], writes=[p1k], sig=(k == 7))
                    for k in range(KT):
                        P.op("pe", lambda e, p3=p3, w3=w3, j=j, k=k, ho=ho, hn=hn: e.matmul(
                            p3[:, 0:hn], lhsT=w3[:, k, j * 128:(j + 1) * 128], rhs=hx[:, k, ho:ho + hn], start=(k == 0), stop=(k == 7)),
                            reads=[w3k, hxk], writes=[p3k], sig=(k == 7))
                    sl, slk = sil[hi_ % 3]
                    hi_ += 1
                    P.op("act", lambda e, p1=p1, sl=sl, hn=hn: e.activation(out=sl[:, 0:hn], in_=p1[:, 0:hn], func=AF.Silu),
                         reads=[p1k], writes=[slk])
                    if moe:
                        cm, cmk = cms[ex]
                        P.op("pool", lambda e, sl=sl, cm=cm, ho=ho, hn=hn: e.tensor_tensor(out=sl[:, 0:hn], in0=sl[:, 0:hn],
                                                                                          in1=cm[:, ho:ho + hn], op=ALU.mult),
                             reads=[slk, cmk], writes=[slk])
                    P.op("dve", lambda e, p3=p3, sl=sl, gt=gt, j=j, hn=hn: e.tensor_tensor(out=gt[:, j, 0:hn], in0=p3[:, 0:hn],
                                                                                          in1=sl[:, 0:hn], op=ALU.mult),
                         reads=[p3k, slk], writes=[(gtk, j)])
                return (gt, gtk)

            def emit_w2(step, H, first):
                ex, gi2, c0, nc_, ho, hn = step
                w1, w1k, w3, w3k, w2, w2k, nj = loaded[(ex, gi2)]
                gt, gtk = H
                for m in range(KT):
                    po, pok = g.ps[4 + (m % 4)], "ps%d" % (4 + (m % 4))
                    for j in range(nj):
                        P.op("pe", lambda e, po=po, w2=w2, j=j, m=m, gt=gt, hn=hn, nj=nj: e.matmul(
                            po[:, 0:hn], lhsT=w2[:, j, m * 128:(m + 1) * 128], rhs=gt[:, j, 0:hn], start=(j == 0), stop=(j == nj - 1)),
                            reads=[w2k, (gtk, j)], writes=[pok], sig=(j == nj - 1))
                    if first:
                        P.op("act", lambda e, po=po, m=m, ho=ho, hn=hn: e.copy(out=acc[:, m, ho:ho + hn], in_=po[:, 0:hn]),
                             reads=[pok], writes=[(acck, m, ho)])
                    else:
                        P.op("dve", lambda e, po=po, m=m, ho=ho, hn=hn: e.tensor_tensor(out=acc[:, m, ho:ho + hn], in0=po[:, 0:hn],
                                                                                       in1=acc[:, m, ho:ho + hn], op=ALU.add),
                             reads=[pok, (acck, m, ho)], writes=[(acck, m, ho)])

            Hc = emit_h(steps[0])
            for i in range(len(steps)):
                Hn = emit_h(steps[i + 1]) if i + 1 < len(steps) else None
                emit_w2(steps[i], Hc, first=(i < len(halves)))
                Hc = Hn
            g2 = modvec(g, l, si, "G2")
            for hi2, (ho, hn) in enumerate(halves):
                xt, xtk = xts[hi2 % 2]
                lb = lbs[hi2] if kind == "L" else 0
                P.dma("sp", xt[:, :, 0:hn], blk_view(A["XT"], s0 + ho, hn), reads=[(kind + "_XT", lb)], writes=[xtk])
                for m in range(KT):
                    P.op("dve", lambda e, m=m, xt=xt, ho=ho, hn=hn: e.scalar_tensor_tensor(
                        out=xt[:, m, 0:hn], in0=acc[:, m, ho:ho + hn], scalar=g2[:, m:m + 1], in1=xt[:, m, 0:hn], op0=ALU.mult, op1=ALU.add),
                        reads=[(acck, m, ho), xtk, "modx", "modc"], writes=[xtk])
                P.dma("pool", blk_view(A["XT"], s0 + ho, hn), xt[:, :, 0:hn], reads=[xtk], writes=[(kind + "_XT", lb)])
                if publish and kind == "L" and lb in (4, 5, 10, 11):
                    eb = (4, 5, 10, 11).index(lb)
                    for cg in range(2):
                        P.dma("pool", g.ex_in[2 * eb + cg].rearrange("p (c t) -> p c t", c=4), xt[:, 4 * cg:4 * cg + 4, :],
                              reads=[xtk], writes=[("ex_in", 2 * eb + cg)])
                        P.collective("AllGather", ins=[g.ex_in[2 * eb + cg]], outs=[g.ex_all[2 * eb + cg].rearrange("r p f -> (r p) f")],
                                     groups=[[0, 1, 2, 3], [4, 5, 6, 7]], reads=[("ex_in", 2 * eb + cg)],
                                     writes=[("ex_all", 2 * eb + cg)])
        for (kind, s0, n0, lbs) in sblocks:
            sb_body(kind, s0, n0, lbs)


def st_exchange(g):
    P = g.P
    XT = g.lat["XT"]
    with Stage(g) as st:
        hb = st.ring("exh", [128, 4, NB], F32, 3)
        i = 0
        for (lb, eb, off) in ((2, 2, 3), (3, 3, 3), (12, 0, 1), (13, 1, 1)):
            for cg in range(2):
                u = 2 * eb + cg
                t, tk = hb[i % 3]
                i += 1

                for hh_ in range(2):
                    def dyn(e, t=t, u=u, off=off, hh_=hh_):
                        r = dynval(g, e, "rl" if off == 3 else "rr")
                        return e.dma_start(out=t[:, 2 * hh_:2 * hh_ + 2, :].rearrange("p c t -> p (c t)"),
                                           in_=g.ex_all[u][bass.ds(r, 1), :, 1024 * hh_:1024 * hh_ + 1024].rearrange("o p f -> p (o f)"))
                    P.dma("sp", None, None, reads=[("ex_all", u)], writes=[tk], fn=dyn)
                P.dma("sp", XT[4 * cg:4 * cg + 4, :, lb * NB:(lb + 1) * NB].rearrange("c p t -> p c t"), t[:],
                      reads=[tk], writes=[("L_XT", lb, cg)])


def st_final(g):
    P = g.P
    with Stage(g) as st:
        xts = st.ring("oxt", [128, KT, NB], F32, 2)
        ys = st.ring("oy", [128, KT, NB], F32, 2)
        outs = st.ring("oo", [128, 4, D], F32, 2)
        bufs = {"sq": st.sb("osq", [128, KT, NB], BF16), "rs": st.sb("ors", [128, NB], F32)}
        fg = S(g, "fing")
        for ji, lb in enumerate(range(4, 12)):
            xt, xtk = xts[ji % 2]
            y, yk = ys[ji % 2]
            oo, ook = outs[ji % 2]
            P.dma("sp", xt[:], blk_view(g.lat["XT"], lb * NB, NB), reads=[("L_XT", lb)], writes=[xtk])
            norm_core(g, st, xt, xtk, NB, fg, None, bufs, out_f32=(y, yk))
            for a in range(4):
                for half in range(2):
                    ps = g.ps[(a * 2 + half) % 6]
                    psk = "ps%d" % ((a * 2 + half) % 6)
                    for cc in range(4):
                        c = half * 4 + cc
                        P.op("pe", lambda e, ps=ps, y=y, a=a, c=c, cc=cc: e.transpose(
                            out=ps[:, cc * 128:(cc + 1) * 128], in_=y[:, c, a * 128:(a + 1) * 128], identity=g.ident[:]),
                            reads=[(yk, c), "ident"], writes=[psk])
                    if half == 0:
                        P.op("act", lambda e, ps=ps, oo=oo, a=a: e.copy(out=oo[:, a, 0:512], in_=ps[:, 0:512]),
                             reads=[psk], writes=[(ook, a, 0)])
                    else:
                        P.op("dve", lambda e, ps=ps, oo=oo, a=a: e.tensor_copy(out=oo[:, a, 512:1024], in_=ps[:, 0:512]),
                             reads=[psk], writes=[(ook, a, 1)])
            P.dma("pool", g.out[(lb - 4) * NB:(lb - 3) * NB, :].rearrange("(a p) d -> p a d", p=128), oo[:],
                  reads=[(ook, a, h) for a in range(4) for h in range(2)], writes=[("out", lb)])


def build():
    g = build_program()
    L = lambda lbs: seg_blocks("L", lbs)
    C = seg_blocks("C", None)

    def stop(name):
        return STOP_AFTER == name
    st_setup(g)
    if stop("setup"):
        g.P.emit(); return g
    st_convert(g, "mix")
    st_transpose_in(g)
    if stop("tin"):
        g.P.emit(); return g
    stopped = False
    for l in range(DEPTH):
        last = l == DEPTH - 1
        nblk = list(range(2, 14))
        ublk = list(range(3, 13))
        pblk = list(range(4, 12))
        if l == 1:
            st_exchange(g)
        st_norm(g, l, 1, L(nblk) + C)
        st_zu(g, l, L(ublk) + C)
        st_scan(g, l, C + L(pblk))
        if stop("scan%d" % l):
            stopped = True
            break
        st_carry(g, l)
        if stop("carry%d" % l):
            stopped = True
            break
        bgA = bgB = bgC = None
        if l == 0 and not NO_MOE:
            pcs = conv_pieces(g, "moe")
            n1_, n2_ = (len(pcs) * 4) // 10, (len(pcs) * 7) // 10
            bgA, bgB, bgC = pcs[:n1_], pcs[n1_:n2_], pcs[n2_:]
        st_zgm(g, l, L(pblk) + (C if not last else []), bg_pieces=bgA, bg_every=4)
        st_zv(g, l, L(nblk) + (C if not last else []), bg_pieces=bgB, bg_every=3)
        st_conv(g, l, [("L", pblk)] + ([("C", None)] if not last else []), bg_pieces=bgC)
        if stop("conv%d" % l):
            stopped = True
            break
        st_mix(g, l, L(pblk) + (C if not last else []))
        if stop("mix%d" % l):
            stopped = True
            break
        moe = (l % 2 == 1)
        st_norm(g, l, 2, L(pblk) + (C if not last else []), router=moe)
        sbl = [("L", pblk[i] * NB, 2 * NB, [pblk[i], pblk[i + 1]]) for i in range(0, len(pblk), 2)]
        if not last:
            sbl = [sbl[0], sbl[-1]] + sbl[1:-1]
            sbl.append(("C", CPAD, CTX, [0]))
        st_ffn(g, l, sbl, moe, publish=not last)
        if stop("ffn%d" % l):
            stopped = True
            break
    if not stopped:
        st_final(g)
    g.P.emit()
    return g


def make_in_maps(inp):
    x = np.asarray(inp["x"], np.float32)
    maps = []
    gw = np.zeros((DEPTH, 2, 2, 8, 128, 128), np.float32)
    for l in range(DEPTH):
        for d in range(2):
            for ti, nm in enumerate(("lru_wr", "lru_wi")):
                w = np.asarray(inp[nm][l][d], np.float32)
                for c in range(8):
                    gw[l, d, ti, c, 0:64, 0:64] = w[2 * c]
                    gw[l, d, ti, c, 64:128, 64:128] = w[2 * c + 1]
    gw = gw.reshape(-1, 128)
    shared = {} if NO_MOE else {
        "moe_w1": np.ascontiguousarray(np.asarray(inp["moe_w1"], np.float32)[0].reshape(NE * D, DFF)),
        "moe_w3": np.ascontiguousarray(np.asarray(inp["moe_w3"], np.float32)[0].reshape(NE * D, DFF)),
        "moe_w2": np.ascontiguousarray(np.asarray(inp["moe_w2"], np.float32)[0].reshape(NE * DFF, D)),
    }
    shared.update({
        "w_in": np.ascontiguousarray(np.asarray(inp["w_in"], np.float32).reshape(DEPTH * D, 6144)),
        "w_a": np.ascontiguousarray(np.asarray(inp["w_branch_a"], np.float32).reshape(DEPTH * D, D)),
        "w_b": np.ascontiguousarray(np.asarray(inp["w_branch_b"], np.float32).reshape(DEPTH * D, D)),
        "w_o": np.ascontiguousarray(np.asarray(inp["w_out"], np.float32).reshape(DEPTH * D, D)),
        "ffn_w1": np.ascontiguousarray(np.asarray(inp["ffn_w1"], np.float32)[0]),
        "ffn_w3": np.ascontiguousarray(np.asarray(inp["ffn_w3"], np.float32)[0]),
        "ffn_w2": np.ascontiguousarray(np.asarray(inp["ffn_w2"], np.float32)[0]),
        "gatew": gw,
        "router": np.ascontiguousarray(np.asarray(inp["moe_router"], np.float32)[0]),
    })
    modw = np.asarray(inp["mod_w"], np.float32)
    for core in range(8):
        b, q = core // 4, core % 4
        xl = np.zeros((TL, D), np.float32)
        g0 = q * 4096 - 4 * NB
        lo, hi = max(g0, 0), min(g0 + TL, 16384)
        xl[lo - g0:hi - g0] = x[b, lo:hi]
        m = dict(shared)
        m["x_loc"] = xl
        m["ctx_in"] = np.ascontiguousarray(np.asarray(inp["ctx"], np.float32)[b])
        m["small"] = _build_small(inp, core)
        m["modw"] = np.ascontiguousarray(modw[:, :, q * 1536:(q + 1) * 1536])
        maps.append(m)
    return maps


_CACHE = {}


def kernel(**inputs):
    if "g" not in _CACHE:
        _CACHE["g"] = build()
    g = _CACHE["g"]
    maps = make_in_maps(inputs)
    res = run_bass_kernel_spmd(g.nc, maps, core_ids=list(range(8)))
    out = np.zeros((2, 16384, D), np.float32)
    for core in range(8):
        b, q = core // 4, core % 4
        out[b, q * 4096:(q + 1) * 4096] = res.results[core]["out"]
    _CACHE["last"] = res
    return out
```

```python
import numpy as np
from contextlib import ExitStack
import concourse.bass as bass
import concourse.mybir as mybir
from concourse.bass_utils import run_bass_kernel_spmd

F32 = mybir.dt.float32
BF16 = mybir.dt.bfloat16
AF = mybir.ActivationFunctionType
ALU = mybir.AluOpType

D = 1024
KT = 8
NBLK = 16
NB = 512
TL = NBLK * NB
CTX = 256
CPAD = 16
TC = CTX + 2 * CPAD
DFF = 2816
NE = 8
EPS = 1e-6
DEPTH = 2
SEM_ROT = 30000
N_DMA_SEM = 16

DEBUG_OUT = []
STOP_AFTER = None
NO_MOE = False


class _Op:
    __slots__ = ("eng", "fn", "waits", "tok", "clock", "is_dma", "sig")


class Prog:
    ENG = ("pe", "dve", "act", "pool", "sp")

    def __init__(self, nc):
        self.nc = nc
        self.ops = {e: [] for e in self.ENG}
        self.known = {e: {} for e in self.ENG}
        self.cnt = {e: 0 for e in self.ENG}
        self.cur_sem = {}
        self.sems = {}
        self.nsem = 0
        self.own_done = {e: {} for e in self.ENG}
        for e in self.ENG:
            self.cur_sem[e] = self._new_sem(e)
        self.dma_sems = {}
        self.dma_uses = {}
        self.dma_last = {}
        self.dma_rr = {}
        for q in ("sp", "pool"):
            self.dma_sems[q] = [self._new_sem("d" + q) for _ in range(N_DMA_SEM)]
            self.dma_rr[q] = 0
            for s in self.dma_sems[q]:
                self.dma_uses[s] = 0
                self.dma_last[s] = None
        self.last_w = {}
        self.readers = {}
        self.nops = 0
        self.last_op = {e: None for e in self.ENG}
        self.pending_nosig = {e: 0 for e in self.ENG}
        self.uid = 0

    def _new_sem(self, tag):
        sid = self.nsem
        self.nsem += 1
        self.sems[sid] = self.nc.alloc_semaphore(name="s%d_%s" % (sid, tag))
        return sid

    def name(self, base):
        self.uid += 1
        return "%s_%d" % (base, self.uid)

    def _deps(self, eng, reads, writes, is_pe):
        deps = []
        for k in reads:
            w = self.last_w.get(k)
            if w is not None:
                deps.append(w)
        for k in writes:
            w = self.last_w.get(k)
            if w is not None:
                deps.append(w)
            for r in self.readers.get(k, ()):
                deps.append(r)
        waits = {}
        kn = self.known[eng]
        for d in deps:
            if is_pe and d.eng == "pe" and not d.is_dma:
                continue
            sid, val = d.tok
            if kn.get(sid, 0) >= val:
                continue
            if waits.get(sid, 0) < val:
                waits[sid] = val
        for d in deps:
            for sid, val in d.clock.items():
                if kn.get(sid, 0) < val:
                    kn[sid] = val
        return waits

    def _register(self, op, reads, writes):
        for k in reads:
            lst = self.readers.setdefault(k, [])
            if not op.is_dma:
                lst[:] = [r for r in lst if r.is_dma or r.eng != op.eng]
            lst.append(op)
        for k in writes:
            self.last_w[k] = op
            self.readers[k] = []

    def op(self, eng, fn, reads=(), writes=(), sig=True):
        o = _Op()
        o.eng = eng
        o.fn = fn
        o.is_dma = False
        o.sig = sig
        o.waits = self._deps(eng, reads, writes, eng == "pe")
        if self.cnt[eng] >= SEM_ROT and self.pending_nosig[eng] == 0:
            self.own_done[eng][self.cur_sem[eng]] = self.cnt[eng]
            self.cur_sem[eng] = self._new_sem(eng)
            self.cnt[eng] = 0
        if sig:
            self.cnt[eng] += 1
            self.pending_nosig[eng] = 0
            o.tok = (self.cur_sem[eng], self.cnt[eng])
        else:
            self.pending_nosig[eng] += 1
            o.tok = (self.cur_sem[eng], self.cnt[eng] + 1)
        o.clock = dict(self.known[eng])
        o.clock.update(self.own_done[eng])
        o.clock[o.tok[0]] = o.tok[1]
        self._register(o, reads, writes)
        self.ops[eng].append(o)
        self.last_op[eng] = o
        self.nops += 1
        return o

    def dma(self, q, out, in_, reads=(), writes=(), fn=None):
        o = _Op()
        o.eng = q
        o.is_dma = True
        waits = self._deps(q, reads, writes, False)
        sems = self.dma_sems[q]
        sid = sems[self.dma_rr[q] % len(sems)]
        self.dma_rr[q] += 1
        prev = self.dma_last[sid]
        kn = self.known[q]
        if prev is not None and kn.get(sid, 0) < prev.tok[1]:
            waits[sid] = max(waits.get(sid, 0), prev.tok[1])
            kn[sid] = prev.tok[1]
        self.dma_uses[sid] += 1
        o.tok = (sid, 16 * self.dma_uses[sid])
        self.dma_last[sid] = o
        o.waits = waits
        o.fn = ("dynfn", fn) if fn is not None else ("dma", out, in_)
        o.clock = dict(kn)
        o.clock[sid] = o.tok[1]
        self._register(o, reads, writes)
        self.ops[q].append(o)
        self.nops += 1
        return o

    def collective(self, kind, ins, outs, groups, reads=(), writes=()):
        o = _Op()
        o.eng = "pool"
        o.is_dma = True
        o.waits = self._deps("pool", reads, writes, False)
        sid = self._new_sem("cc")
        o.tok = (sid, 1)
        o.fn = ("cc", kind, ins, outs, groups)
        o.clock = dict(self.known["pool"])
        o.clock[sid] = 1
        self._register(o, reads, writes)
        self.ops["pool"].append(o)
        self.nops += 1
        return o

    def barrier(self):
        toks = {}
        for e in self.ENG:
            lo = self.last_op[e]
            if lo is not None:
                toks[lo.tok[0]] = max(toks.get(lo.tok[0], 0), lo.tok[1])
        for sid, lo in self.dma_last.items():
            if lo is not None:
                toks[sid] = max(toks.get(sid, 0), lo.tok[1])
        for k, w in self.last_w.items():
            if w is not None and w.is_dma:
                toks[w.tok[0]] = max(toks.get(w.tok[0], 0), w.tok[1])
        for e in self.ENG:
            kn = self.known[e]
            waits = {}
            for sid, val in toks.items():
                if kn.get(sid, 0) < val:
                    if e == "pe" and sid == self.cur_sem["pe"]:
                        continue
                    waits[sid] = val
                    kn[sid] = val
            if waits:
                o = _Op()
                o.eng = e
                o.is_dma = False
                o.waits = waits
                o.fn = None
                o.tok = None
                o.clock = {}
                self.ops[e].append(o)
        self.last_w = {}
        self.readers = {}

    def _replay(self, ename, e):
        sems = self.sems
        for o in self.ops[ename]:
            for sid, val in o.waits.items():
                e.wait_ge(sems[sid], val)
            if o.fn is None:
                continue
            if o.is_dma:
                if o.fn[0] == "dynfn":
                    o.fn[1](e).then_inc(sems[o.tok[0]], 16)
                elif o.fn[0] == "dma":
                    e.dma_start(out=o.fn[1], in_=o.fn[2]).then_inc(sems[o.tok[0]], 16)
                else:
                    _, kind, ins, outs, groups = o.fn
                    e.collective_compute(kind, ALU.bypass, replica_groups=groups,
                                         ins=ins, outs=outs).then_inc(sems[o.tok[0]])
            elif o.sig:
                o.fn(e).then_inc(sems[o.tok[0]], 1)
            else:
                o.fn(e)

    def emit(self):
        self.barrier()
        with self.nc.Block() as block:
            @block.sync
            def _(e):
                self._replay("sp", e)

            @block.tensor
            def _(e):
                self._replay("pe", e)

            @block.vector
            def _(e):
                self._replay("dve", e)

            @block.scalar
            def _(e):
                self._replay("act", e)

            @block.gpsimd
            def _(e):
                self._replay("pool", e)


def _small_layout():
    off = {}
    n = 0

    def add(name, w):
        nonlocal n
        off[name] = (n, w)
        n += w
    for l in range(DEPTH):
        add("n1g%d" % l, 8)
        add("n2g%d" % l, 8)
        add("modb%d" % l, 48)
        add("cw4_%d" % l, 32)
        add("cb4_%d" % l, 8)
        for d in range(2):
            add("br%d%d" % (l, d), 8)
            add("bi%d%d" % (l, d), 8)
            add("lam%d%d" % (l, d), 8)
        add("cw31_%d" % l, 31 * 8)
        add("cb31_%d" % l, 8)
        add("lng%d" % l, 8)
        add("lnb%d" % l, 8)
    add("fing", 8)
    add("blkmask", NBLK)
    add("one", 1)
    add("boh", 2)
    add("cvec", 32)
    return off, n


SOFF, NS = _small_layout()


def _pc(v):
    return np.ascontiguousarray(np.asarray(v, np.float32).reshape(8, 128).T)


def _build_small(inp, core):
    b, q = core // 4, core % 4
    s = np.zeros((128, NS), np.float32)

    def put(name, arr):
        o, w = SOFF[name]
        s[:, o:o + w] = np.asarray(arr, np.float32).reshape(128, w)
    for l in range(DEPTH):
        put("n1g%d" % l, _pc(inp["norm1_g"][l]))
        put("n2g%d" % l, _pc(inp["norm2_g"][l]))
        put("modb%d" % l, inp["mod_b"][l].reshape(48, 128).T)
        put("cw4_%d" % l, np.concatenate([_pc(inp["rnn_conv_w"][l][j]) for j in range(4)], axis=1))
        put("cb4_%d" % l, _pc(inp["rnn_conv_b"][l]))
        for d in range(2):
            put("br%d%d" % (l, d), _pc(inp["lru_br"][l][d]))
            put("bi%d%d" % (l, d), _pc(inp["lru_bi"][l][d]))
            put("lam%d%d" % (l, d), _pc(inp["lru_lam"][l][d]))
        put("cw31_%d" % l, np.concatenate([_pc(inp["conv_w"][l][j]) for j in range(31)], axis=1))
        put("cb31_%d" % l, _pc(inp["conv_b"][l]))
        put("lng%d" % l, _pc(inp["conv_ln_g"][l]))
        put("lnb%d" % l, _pc(inp["conv_ln_b"][l]))
    put("fing", _pc(inp["final_g"]))
    bm = np.zeros((128, NBLK), np.float32)
    for lb in range(NBLK):
        g0 = q * 4096 + (lb - 4) * NB
        bm[:, lb] = 1.0 if (0 <= g0 < 16384) else 0.0
    put("blkmask", bm)
    put("one", np.ones((128, 1), np.float32))
    boh = np.zeros((128, 2), np.float32)
    boh[:, b] = 1.0
    put("boh", boh)
    cv = np.zeros((128, 8, 4), np.float32)
    cv[:, :, 0] = _pc(inp["c"][0])
    cv[:, :, 1] = _pc(inp["c"][1])
    cv[:, :, 2] = _pc(inp["c_ctx"])
    put("cvec", cv.reshape(128, 32))
    return s


class G:
    pass


def build_program():
    nc = bass.Bass("TRN2", target_bir_lowering=False)
    P = Prog(nc)
    g = G()
    g.nc, g.P = nc, P

    def din(name, shape, dt=F32):
        return nc.dram_tensor(name, list(shape), dt, kind="ExternalInput").ap()

    def dscr(name, shape, dt=F32):
        kind = "ExternalOutput" if name in DEBUG_OUT else "Internal"
        return nc.dram_tensor(name, list(shape), dt, kind=kind).ap()

    g.x_loc = din("x_loc", [TL, D])
    g.ctx_in = din("ctx_in", [CTX, D])
    g.small_in = din("small", [128, NS])
    g.modw_in = din("modw", [DEPTH, D, 1536])
    g.win_in = din("w_in", [DEPTH * D, 6144])
    g.wa_in = din("w_a", [DEPTH * D, D])
    g.wb_in = din("w_b", [DEPTH * D, D])
    g.wo_in = din("w_o", [DEPTH * D, D])
    g.f1_in = din("ffn_w1", [D, DFF])
    g.f3_in = din("ffn_w3", [D, DFF])
    g.f2_in = din("ffn_w2", [DFF, D])
    if not NO_MOE:
        g.m1_in = din("moe_w1", [NE * D, DFF])
        g.m3_in = din("moe_w3", [NE * D, DFF])
        g.m2_in = din("moe_w2", [NE * DFF, D])
    g.gw_in = din("gatew", [DEPTH * 2 * 2 * 8 * 128, 128])
    g.rt_in = din("router", [D, NE])
    g.out = nc.dram_tensor("out", [8 * NB, D], F32, kind="ExternalOutput").ap()

    g.win = dscr("win_bf", [DEPTH * D, 6144], BF16)
    g.wa = dscr("wa_bf", [DEPTH * D, D], BF16)
    g.wb = dscr("wb_bf", [DEPTH * D, D], BF16)
    g.wo = dscr("wo_bf", [DEPTH * D, D], BF16)
    g.f1 = dscr("f1_bf", [D, DFF], BF16)
    g.f3 = dscr("f3_bf", [D, DFF], BF16)
    g.f2 = dscr("f2_bf", [DFF, D], BF16)
    g.m1 = dscr("m1_bf", [NE * D, DFF], BF16)
    g.m3 = dscr("m3_bf", [NE * D, DFF], BF16)
    g.m2 = dscr("m2_bf", [NE * DFF, D], BF16)
    g.gw = dscr("gw_bf", [DEPTH * 2 * 2 * 8 * 128, 128], BF16)

    def seg_arrays(pfx, T):
        a = {}
        for nm, dt in (("XT", F32), ("HX", BF16), ("U", F32), ("G", BF16), ("V", BF16),
                       ("SGA", BF16), ("SGB", BF16), ("S", F32), ("AF", BF16), ("AB", BF16),
                       ("CV", F32)):
            a[nm] = dscr(pfx + nm, [KT, 128, T], dt)
        return a
    g.lat = seg_arrays("L_", TL)
    g.ctxa = seg_arrays("C_", TC)
    g.comb = dscr("COMB", [NE, 128, TL], F32)
    g.sum_in = dscr("sum_in", [128, 256], F32)
    g.sum_all = dscr("sum_all", [4 * 128, 256], F32)
    g.mod_in = dscr("mod_in", [128, 96], F32)
    g.mod_all = dscr("mod_all", [4 * 128, 96], F32)
    g.htab = dscr("htab", [2, 128, KT, 48], F32)
    g.ex_in = dscr("ex_in", [8, 128, 2048], F32)
    g.ex_all = [dscr("ex_all%d" % u, [4, 128, 2048], F32) for u in range(8)]

    def sb(name, shape, dt=F32):
        return nc.alloc_sbuf_tensor("sb_" + name, list(shape), dt)
    g.small = sb("small", [128, NS])
    g.ident = sb("ident", [128, 128])
    g.ones_b = sb("ones_b", [128, 128], BF16)
    g.zeros = sb("zeros", [128, NB])
    g.modx = sb("modx", [128, DEPTH, 48])
    g.modc = sb("modc", [128, DEPTH, 48])
    g.nrm = sb("nrm", [128, DEPTH, 2, 4, 8])
    g.cl = sb("cl", [128, DEPTH, 2, 2, 8])
    g.sumt = sb("sumt", [128, KT, 8, 4])
    g.sctx = sb("sctx", [128, 2, KT])
    g.hin = sb("hin", [128, 2, KT, 12])
    g.hzero = sb("hzero", [128, 2, KT, 12])
    g.ps = [nc.alloc_psum_tensor("psb%d" % i, [128, NB], F32) for i in range(8)]
    return g


def S(g, name, w=None):
    o, ww = SOFF[name]
    return g.small[:, o:o + (ww if w is None else w)]


def Scol(g, name, i):
    o, _ = SOFF[name]
    return g.small[:, o + i:o + i + 1]


def dynval(g, e, name):
    if not hasattr(g, "_dyn"):
        pid = e.partition_id()
        g._dyn = {
            "rl": e.snap((pid + 3) % 4, min_val=0, max_val=3),
            "rr": e.snap((pid + 1) % 4, min_val=0, max_val=3),
            "b0": e.snap((pid % 4) * 8 + 6, min_val=6, max_val=30),
            "b1": e.snap((pid % 4) * 8 + 7, min_val=7, max_val=31),
        }
    return g._dyn[name]


class Stage:
    def __init__(self, g):
        self.g = g
        self.es = ExitStack()

    def __enter__(self):
        self.g.P.barrier()
        return self

    def __exit__(self, *a):
        self.g.P.barrier()
        self.es.close()
        return False

    def sb(self, base, shape, dt=F32):
        nm = self.g.P.name(base)
        t = self.es.enter_context(self.g.nc.sbuf_tensor(nm, list(shape), dt))
        return t, nm

    def ring(self, base, shape, dt, n):
        return [self.sb(base, shape, dt) for _ in range(n)]


def blk_view(arr, s, n):
    return arr[:, :, s:s + n].rearrange("c p t -> p c t")


def st_setup(g):
    P, nc = g.P, g.nc
    with Stage(g) as st:
        P.dma("sp", g.small[:], g.small_in, reads=[], writes=["small"])
        P.op("pool", lambda e: e.memset(g.ident[:], 0.0), writes=["ident"])
        P.op("pool", lambda e: e.affine_select(out=g.ident[:], in_=g.ident[:], pattern=[[-1, 128]],
                                               compare_op=ALU.not_equal, fill=1.0, base=0,
                                               channel_multiplier=1),
             reads=["ident"], writes=["ident"])
        P.op("pool", lambda e: e.memset(g.ones_b[:], 1.0), writes=["ones_b"])
        P.op("pool", lambda e: e.memset(g.zeros[:], 0.0), writes=["zeros"])
        P.op("pool", lambda e: e.memset(g.hzero[:], 0.0), writes=["hzero"])
        zt, zk = st.sb("zt", [128, KT, TC], F32)
        zb, zbk = st.sb("zb", [128, KT, TC], BF16)
        P.op("dve", lambda e: e.memset(zt[:], 0.0), writes=[zk])
        P.op("dve", lambda e: e.memset(zb[:], 0.0), writes=[zbk])
        P.dma("sp", blk_view(g.ctxa["U"], 0, TC), zt[:], reads=[zk], writes=["C_U"])
        P.dma("sp", blk_view(g.ctxa["V"], 0, TC), zb[:], reads=[zbk], writes=["C_V"])
        sc, sck = st.sb("sc", [128, 8, 4], F32)
        P.op("act", lambda e: e.activation(out=sc[:].rearrange("p a b -> p (a b)"), in_=S(g, "cvec"),
                                           func=AF.Silu), reads=["small"], writes=[sck])
        mw = st.ring("mw", [128, 8, 1536], F32, 1)
        mo, mok = st.sb("mo", [128, 96], F32)
        psm = g.ps[0]
        for l in range(DEPTH):
            t, tk = mw[0]
            P.dma("sp", t[:], g.modw_in[l].rearrange("(k p) n -> p k n", p=128), reads=[], writes=[tk])
            for c in range(12):
                for k in range(8):
                    P.op("pe", lambda e, t=t, c=c, k=k, l=l: e.matmul(
                        psm[:, (l * 12 + c) * 4:(l * 12 + c) * 4 + 4], lhsT=t[:, k, c * 128:(c + 1) * 128],
                        rhs=sc[:, k, :], start=(k == 0), stop=(k == 7)),
                        reads=[tk, sck], writes=["ps0"], sig=(k == 7))
        P.op("dve", lambda e: e.tensor_copy(out=mo[:], in_=psm[:, 0:96]), reads=["ps0"], writes=[mok])
        P.dma("sp", g.mod_in, mo[:], reads=[mok], writes=["mod_in"])
        P.collective("AllGather", ins=[g.mod_in], outs=[g.mod_all], groups=[[0, 1, 2, 3], [4, 5, 6, 7]],
                     reads=["mod_in"], writes=["mod_all"])
        ma, mak = st.sb("ma", [128, DEPTH, 4, 12, 4], F32)
        for l in range(DEPTH):
            P.dma("sp", ma[:, l], g.mod_all.rearrange("(r p) (l c j) -> p l r c j", p=128, l=DEPTH, c=12)[:, l],
                  reads=["mod_all"], writes=[mak])
        for l in range(DEPTH):
            v = ma[:, l].rearrange("p r c j -> p (r c) j")
            mb = S(g, "modb%d" % l)
            P.op("dve", lambda e, v=v, l=l: e.tensor_single_scalar(out=g.modx[:, l, :], in_=v[:, :, 0],
                                                            scalar=Scol(g, "boh", 0), op=ALU.mult),
                 reads=[mak, "small"], writes=["modx"])
            P.op("dve", lambda e, v=v, l=l: e.scalar_tensor_tensor(out=g.modx[:, l, :], in0=v[:, :, 1],
                                                                   scalar=Scol(g, "boh", 1), in1=g.modx[:, l, :],
                                                                   op0=ALU.mult, op1=ALU.add),
                 reads=[mak, "small", "modx"], writes=["modx"])
            P.op("dve", lambda e, l=l, mb=mb: e.tensor_tensor(out=g.modx[:, l, :], in0=g.modx[:, l, :], in1=mb, op=ALU.add),
                 reads=["modx", "small"], writes=["modx"])
            P.op("dve", lambda e, v=v, l=l, mb=mb: e.tensor_tensor(out=g.modc[:, l, :], in0=v[:, :, 2], in1=mb, op=ALU.add),
                 reads=[mak, "small"], writes=["modc"])
            for si, mv in enumerate((g.modx, g.modc)):
                P.op("dve", lambda e, l=l, si=si, mv=mv: e.scalar_tensor_tensor(
                    out=g.nrm[:, l, si, 0, :], in0=mv[:, l, 8:16], scalar=1.0, in1=S(g, "n1g%d" % l),
                    op0=ALU.add, op1=ALU.mult), reads=["modx", "modc", "small"], writes=["nrm"])
                P.op("dve", lambda e, l=l, si=si, mv=mv: e.scalar_tensor_tensor(
                    out=g.nrm[:, l, si, 1, :], in0=mv[:, l, 32:40], scalar=1.0, in1=S(g, "n2g%d" % l),
                    op0=ALU.add, op1=ALU.mult), reads=["modx", "modc", "small", "nrm"], writes=["nrm"])
            for d in range(2):
                tmp, tmk = st.sb("cltmp", [128, 8], F32)
                P.op("act", lambda e, l=l, d=d, tmp=tmp: e.activation(out=tmp[:], in_=S(g, "lam%d%d" % (l, d)),
                                                                      func=AF.Exp, scale=-1.0),
                     reads=["small"], writes=[tmk])
                P.op("act", lambda e, tmp=tmp: e.activation(out=tmp[:], in_=tmp[:], func=AF.Ln, bias=1.0),
                     reads=[tmk], writes=[tmk])
                P.op("dve", lambda e, l=l, d=d, tmp=tmp: e.tensor_single_scalar(out=g.cl[:, l, d, 0, :], in_=tmp[:],
                                                                         scalar=-8.0, op=ALU.mult),
                     reads=[tmk], writes=["cl"])
                P.op("dve", lambda e, l=l, d=d, tmp=tmp: e.tensor_single_scalar(out=g.cl[:, l, d, 1, :], in_=tmp[:],
                                                                         scalar=-16.0, op=ALU.mult),
                     reads=[tmk, "cl"], writes=["cl"])


def modvec(g, l, si, which):
    mv = g.modx if si == 0 else g.modc
    if which == "A1":
        return g.nrm[:, l, si, 0, :]
    if which == "A2":
        return g.nrm[:, l, si, 1, :]
    return {"B1": mv[:, l, 0:8], "G1": mv[:, l, 16:24], "B2": mv[:, l, 24:32], "G2": mv[:, l, 40:48]}[which]


def st_convert(g, which):
    P = g.P
    if which == "mix":
        pairs = [(g.win_in, g.win), (g.wa_in, g.wa), (g.wb_in, g.wb), (g.wo_in, g.wo), (g.gw_in, g.gw),
                 (g.f1_in, g.f1), (g.f3_in, g.f3), (g.f2_in, g.f2)]
    else:
        pairs = [(g.m1_in, g.m1), (g.m3_in, g.m3), (g.m2_in, g.m2)]
    wi_ = 0
    W = 4096
    with Stage(g) as st:
        src = st.ring("cvs", [128, W], F32, 3)
        dst = st.ring("cvd", [128, W], BF16, 3)
        i = 0
        for a, b in pairs:
            av = a.rearrange("(p r) n -> p (r n)", p=128)
            bv = b.rearrange("(p r) n -> p (r n)", p=128)
            Fd = av.shape[1]
            for o in range(0, Fd, W):
                w = min(W, Fd - o)
                s, sk = src[i % 3]
                d, dk = dst[i % 3]
                P.dma("sp", s[:, 0:w], av[:, o:o + w], reads=[], writes=[sk])
                eng = ("act", "dve", "act")[i % 3]
                if eng == "act":
                    P.op("act", lambda e, s=s, d=d, w=w: e.copy(out=d[:, 0:w], in_=s[:, 0:w]), reads=[sk], writes=[dk])
                else:
                    P.op(eng, lambda e, s=s, d=d, w=w: e.tensor_copy(out=d[:, 0:w], in_=s[:, 0:w]), reads=[sk], writes=[dk])
                P.dma("pool", bv[:, o:o + w], d[:, 0:w], reads=[dk], writes=[("wcv", id(b), o)])
                i += 1


def conv_pieces(g, which, W=4096):
    if which == "mix":
        pairs = [(g.win_in, g.win), (g.wa_in, g.wa), (g.wb_in, g.wb), (g.wo_in, g.wo), (g.gw_in, g.gw),
                 (g.f1_in, g.f1), (g.f3_in, g.f3), (g.f2_in, g.f2)]
    else:
        pairs = [(g.m1_in, g.m1), (g.m3_in, g.m3), (g.m2_in, g.m2)]
    out = []
    for a, b in pairs:
        av = a.rearrange("(p r) n -> p (r n)", p=128)
        bv = b.rearrange("(p r) n -> p (r n)", p=128)
        Fd = av.shape[1]
        for o in range(0, Fd, W):
            out.append((av, bv, o, min(W, Fd - o), id(b)))
    return out


def bg_convert(g, st, pieces, eng="dve", W=4096):
    P = g.P
    src = st.ring("bgs", [128, W], F32, 3)
    dst = st.ring("bgd", [128, W], BF16, 3)
    for i, (av, bv, o, w, bid) in enumerate(pieces):
        s_, sk = src[i % 3]
        d_, dk = dst[i % 3]
        P.dma("sp", s_[:, 0:w], av[:, o:o + w], reads=[], writes=[sk])
        if eng == "act":
            P.op("act", lambda e, s_=s_, d_=d_, w=w: e.copy(out=d_[:, 0:w], in_=s_[:, 0:w]), reads=[sk], writes=[dk])
        else:
            P.op(eng, lambda e, s_=s_, d_=d_, w=w: e.tensor_copy(out=d_[:, 0:w], in_=s_[:, 0:w]), reads=[sk], writes=[dk])
        P.dma("pool", bv[:, o:o + w], d_[:, 0:w], reads=[dk], writes=[("wcv", bid, o)])
        yield


def st_transpose_in(g):
    P = g.P
    with Stage(g) as st:
        xin = st.ring("xin", [128, 4, D], F32, 2)
        xtb = st.ring("xtb", [128, KT, NB], F32, 2)
        jobs = [("L", lb) for lb in range(2, 14)] + [("C", 0)]
        for ji, (kind, lb) in enumerate(jobs):
            xi, xik = xin[ji % 2]
            xo, xok = xtb[ji % 2]
            if kind == "L":
                na = 4
                P.dma("sp", xi[:], g.x_loc[lb * NB:(lb + 1) * NB, :].rearrange("(a p) d -> p a d", p=128), writes=[xik])
            else:
                na = 2
                P.dma("sp", xi[:, 0:2], g.ctx_in.rearrange("(a p) d -> p a d", p=128), writes=[xik])
            for c in range(KT):
                ps = g.ps[c]
                for a in range(na):
                    P.op("pe", lambda e, ps=ps, xi=xi, a=a, c=c: e.transpose(
                        out=ps[:, a * 128:(a + 1) * 128], in_=xi[:, a, c * 128:(c + 1) * 128], identity=g.ident[:]),
                        reads=[xik, "ident"], writes=["ps%d" % c])
                n = na * 128
                if c % 2 == 0:
                    P.op("act", lambda e, ps=ps, xo=xo, c=c, n=n: e.copy(out=xo[:, c, 0:n], in_=ps[:, 0:n]),
                         reads=["ps%d" % c], writes=[(xok, c)])
                else:
                    P.op("dve", lambda e, ps=ps, xo=xo, c=c, n=n: e.tensor_copy(out=xo[:, c, 0:n], in_=ps[:, 0:n]),
                         reads=["ps%d" % c], writes=[(xok, c)])
            rk = [(xok, c) for c in range(KT)]
            if kind == "L":
                P.dma("pool", blk_view(g.lat["XT"], lb * NB, NB), xo[:], reads=rk, writes=[("L_XT", lb)])
            else:
                P.dma("pool", blk_view(g.ctxa["XT"], CPAD, CTX), xo[:, :, 0:CTX], reads=rk, writes=[("C_XT", 0)])


def norm_core(g, st, xt, xtk, n, A, B, bufs, out_bf=None, out_f32=None, psi=7, sq_eng="act"):
    P = g.P
    sq, sqk = bufs["sq"]
    rs, rsk = bufs["rs"]
    ps = g.ps[psi]
    psk = "ps%d" % psi
    P.op("act", lambda e: e.activation(out=sq[:, :, 0:n], in_=xt[:, :, 0:n], func=AF.Square),
         reads=[xtk], writes=[sqk])
    for k in range(KT):
        P.op("pe", lambda e, k=k: e.matmul(ps[:, 0:n], lhsT=g.ones_b[:], rhs=sq[:, k, 0:n], start=(k == 0), stop=(k == 7)),
             reads=[sqk, "ones_b"], writes=[psk], sig=(k == 7))
    P.op("act", lambda e: e.activation(out=rs[:, 0:n], in_=ps[:, 0:n], func=AF.Sqrt, scale=1.0 / D, bias=EPS),
         reads=[psk], writes=[rsk])
    P.op("dve", lambda e: e.reciprocal(out=rs[:, 0:n], in_=rs[:, 0:n]), reads=[rsk], writes=[rsk])
    P.op("dve", lambda e: e.tensor_tensor(out=xt[:, :, 0:n], in0=xt[:, :, 0:n],
                                          in1=rs[:, 0:n].unsqueeze(1).to_broadcast([128, KT, n]), op=ALU.mult),
         reads=[xtk, rsk], writes=[xtk])
    for c in range(KT):
        eng = "act" if c % 2 == 0 else "dve"
        for (ot, wants) in ((out_bf, True), (out_f32, True)):
            if ot is None:
                continue
            o, ok = ot
            if B is None:
                if eng == "act":
                    P.op("act", lambda e, o=o, c=c: e.activation(out=o[:, c, 0:n], in_=xt[:, c, 0:n], func=AF.Identity,
                                                                 scale=A[:, c:c + 1]),
                         reads=[xtk, "nrm", "small"], writes=[(ok, c)])
                else:
                    P.op("dve", lambda e, o=o, c=c: e.tensor_single_scalar(out=o[:, c, 0:n], in_=xt[:, c, 0:n],
                                                                    scalar=A[:, c:c + 1], op=ALU.mult),
                         reads=[xtk, "nrm", "small"], writes=[(ok, c)])
            else:
                if eng == "act":
                    P.op("act", lambda e, o=o, c=c: e.activation(out=o[:, c, 0:n], in_=xt[:, c, 0:n], func=AF.Identity,
                                                                 scale=A[:, c:c + 1], bias=B[:, c:c + 1]),
                         reads=[xtk, "nrm", "modx", "modc"], writes=[(ok, c)])
                else:
                    P.op("dve", lambda e, o=o, c=c: e.tensor_scalar(out=o[:, c, 0:n], in0=xt[:, c, 0:n],
                                                                    scalar1=A[:, c:c + 1], scalar2=B[:, c:c + 1],
                                                                    op0=ALU.mult, op1=ALU.add),
                         reads=[xtk, "nrm", "modx", "modc"], writes=[(ok, c)])


def seg_blocks(kind, lbs):
    if kind == "L":
        return [("L", lb * NB, NB, lb) for lb in lbs]
    return [("C", CPAD, CTX, 0)]


def arrs(g, kind):
    return g.lat if kind == "L" else g.ctxa


def st_norm(g, l, which, jobs, router=False):
    P = g.P
    with Stage(g) as st:
        xts = st.ring("nxt", [128, KT, NB], F32, 2)
        hxs = st.ring("nhx", [128, KT, NB], BF16, 2)
        bufs = {"sq": st.sb("nsq", [128, KT, NB], BF16), "rs": st.sb("nrs", [128, NB], F32)}
        if router:
            yfs = st.sb("nyf", [128, KT, NB], F32)
            rt, rtk = st.sb("rt", [128, KT, NE], F32)
            P.dma("sp", rt[:], g.rt_in.rearrange("(k p) e -> p k e", p=128), writes=[rtk])
            lg, lgk = st.sb("lg", [128, 4, NE], F32)
            mx, mxk = st.sb("mx", [128, 4, 8], F32)
            ex, exk = st.sb("ex", [128, 4, NE], F32)
            mk, mkk = st.sb("mk", [128, 4, NE], F32)
            dn, dnk = st.sb("dn", [128, 4], F32)
            nm1, nm1k = st.sb("nm1", [128, 4], F32)
            cbb, cbbk = st.sb("cbb", [128, NE, 128], F32)
            cmo = st.ring("cmo", [128, NE, NB], F32, 2)
        def job(ji, kind, s, n, lb):
            A = arrs(g, kind)
            si = 0 if kind == "L" else 1
            xt, xtk = xts[ji % 2]
            hx, hxk = hxs[ji % 2]
            P.dma("sp", xt[:, :, 0:n], blk_view(A["XT"], s, n), reads=[(kind + "_XT", lb)], writes=[xtk])
            Av = modvec(g, l, si, "A%d" % which)
            Bv = modvec(g, l, si, "B%d" % which)
            if router:
                norm_core(g, st, xt, xtk, n, Av, Bv, bufs, out_f32=yfs)
                yf, yfk = yfs
                P.op("pool", lambda e, hx=hx, yf=yf: e.tensor_copy(out=hx[:, :, 0:n], in_=yf[:, :, 0:n]),
                     reads=[(yfk, c) for c in range(KT)], writes=[(hxk, c) for c in range(KT)])
                psl = g.ps[6]
                for a in range(4):
                    for k in range(KT):
                        P.op("pe", lambda e, a=a, k=k, yf=yf: e.matmul(psl[:, a * 8:(a + 1) * 8],
                                                                      lhsT=yf[:, k, a * 128:(a + 1) * 128], rhs=rt[:, k, :],
                                                                      start=(k == 0), stop=(k == 7)),
                             reads=[(yfk, k), rtk], writes=["ps6"], sig=(k == 7))
                P.op("dve", lambda e: e.tensor_copy(out=lg[:].rearrange("p a e -> p (a e)"), in_=psl[:, 0:32]),
                     reads=["ps6"], writes=[lgk])
                for a in range(4):
                    P.op("dve", lambda e, a=a: e.max(out=mx[:, a, :], in_=lg[:, a, :]), reads=[lgk], writes=[mxk])
                P.op("dve", lambda e: e.tensor_single_scalar(out=nm1[:], in_=mx[:, :, 0], scalar=-1.0, op=ALU.mult),
                     reads=[mxk], writes=[nm1k])
                for a in range(4):
                    P.op("act", lambda e, a=a: e.activation(out=ex[:, a, :], in_=lg[:, a, :], func=AF.Exp,
                                                            bias=nm1[:, a:a + 1], scale=1.0),
                         reads=[lgk, nm1k], writes=[exk])
                    P.op("dve", lambda e, a=a: e.tensor_single_scalar(out=mk[:, a, :], in_=lg[:, a, :], scalar=mx[:, a, 1:2], op=ALU.is_ge),
                         reads=[lgk, mxk], writes=[mkk])
                P.op("dve", lambda e: e.tensor_tensor(out=ex[:], in0=ex[:], in1=mk[:], op=ALU.mult),
                     reads=[exk, mkk], writes=[exk])
                P.op("dve", lambda e: e.tensor_reduce(out=dn[:], in_=ex[:], axis=mybir.AxisListType.X, op=ALU.add),
                     reads=[exk], writes=[dnk])
                P.op("dve", lambda e: e.reciprocal(out=dn[:], in_=dn[:]), reads=[dnk], writes=[dnk])
                P.op("dve", lambda e: e.tensor_tensor(out=ex[:], in0=ex[:], in1=dn[:].unsqueeze(2).to_broadcast([128, 4, NE]),
                                                      op=ALU.mult), reads=[exk, dnk], writes=[exk])
                co, cok = cmo[ji % 2]
                for a in range(4):
                    for ee in range(NE):
                        P.op("dve", lambda e, a=a, ee=ee: e.tensor_scalar(out=cbb[:, ee, :], in0=g.ident[:], scalar1=g.zeros[:, 0:1],
                                                                          scalar2=ex[:, a, ee:ee + 1], op0=ALU.mult, op1=ALU.add),
                             reads=[exk, "ident", (cbbk, ee)], writes=[(cbbk, ee)])
                    for ee in range(NE):
                        pse = g.ps[ee % 4]
                        P.op("pe", lambda e, a=a, ee=ee, pse=pse: e.matmul(pse[:, 0:128], lhsT=cbb[:, ee, :], rhs=g.ident[:],
                                                                          start=True, stop=True),
                             reads=[(cbbk, ee), "ident"], writes=["ps%d" % (ee % 4)])
                        P.op("act", lambda e, a=a, ee=ee, pse=pse, co=co: e.copy(out=co[:, ee, a * 128:(a + 1) * 128],
                                                                                in_=pse[:, 0:128]),
                             reads=["ps%d" % (ee % 4)], writes=[(cok, ee)])
                P.dma("pool", g.comb[:, :, s:s + n].rearrange("e p t -> p e t"), co[:],
                      reads=[(cok, ee) for ee in range(NE)], writes=[("COMB", lb)])
            else:
                norm_core(g, st, xt, xtk, n, Av, Bv, bufs, out_bf=(hx, hxk))
            P.dma("pool", blk_view(A["HX"], s, n), hx[:, :, 0:n], reads=[(hxk, c) for c in range(KT)],
                  writes=[(kind + "_HX", lb)])
        for ji, (kind, s, n, lb) in enumerate(jobs):
            job(ji, kind, s, n, lb)


def load_w(g, st, name, wdram, row0, col0, ncols, kt=KT):
    t, k = st.sb(name, [128, kt, ncols], BF16)
    g.P.dma("sp", t[:], wdram[row0:row0 + kt * 128, col0:col0 + ncols].rearrange("(k p) n -> p k n", p=128),
            reads=[], writes=[k])
    return t, k


def st_zu(g, l, jobs):
    P = g.P
    with Stage(g) as st:
        w, wk = load_w(g, st, "wu", g.win, l * D, 0, 1024)
        hxs = st.ring("zhx", [128, KT, NB], BF16, 2)
        ub = st.ring("zub", [128, KT, NB], F32, 2)
        def job(ji, kind, s, n, lb):
            A = arrs(g, kind)
            hx, hxk = hxs[ji % 2]
            u, uk = ub[ji % 2]
            P.dma("sp", hx[:, :, 0:n], blk_view(A["HX"], s, n), reads=[(kind + "_HX", lb)], writes=[hxk])
            msk = Scol(g, "blkmask", lb) if kind == "L" else Scol(g, "one", 0)
            for m in range(KT):
                ps = g.ps[m % 4]
                for k in range(KT):
                    P.op("pe", lambda e, ps=ps, m=m, k=k, hx=hx: e.matmul(ps[:, 0:n], lhsT=w[:, k, m * 128:(m + 1) * 128],
                                                                         rhs=hx[:, k, 0:n], start=(k == 0), stop=(k == 7)),
                         reads=[wk, hxk], writes=["ps%d" % (m % 4)], sig=(k == 7))
                P.op("dve", lambda e, ps=ps, m=m, u=u, msk=msk: e.tensor_single_scalar(out=u[:, m, 0:n], in_=ps[:, 0:n], scalar=msk, op=ALU.mult),
                     reads=["ps%d" % (m % 4), "small"], writes=[(uk, m)])
            P.dma("pool", blk_view(A["U"], s, n), u[:, :, 0:n], reads=[(uk, m) for m in range(KT)],
                  writes=[(kind + "_U", lb)])
        for ji, (kind, s, n, lb) in enumerate(jobs):
            job(ji, kind, s, n, lb)


def st_zgm(g, l, jobs, bg_pieces=None, bg_every=2):
    P = g.P
    with Stage(g) as st:
        wg, wgk = load_w(g, st, "wg", g.win, l * D, 1024, 1024)
        wm, wmk = load_w(g, st, "wm", g.win, l * D, 4096, 2048)
        hxs = st.ring("zhx", [128, KT, NB], BF16, 2)
        ob = st.ring("zgo", [128, 3 * KT, NB], BF16, 2)
        bg = bg_convert(g, st, bg_pieces) if bg_pieces else None
        bgc = 0
        pi = 0
        def job(ji, kind, s, n, lb):
            nonlocal pi, bgc
            A = arrs(g, kind)
            hx, hxk = hxs[ji % 2]
            o, ok = ob[ji % 2]
            P.dma("sp", hx[:, :, 0:n], blk_view(A["HX"], s, n), reads=[(kind + "_HX", lb)], writes=[hxk])
            for m in range(3 * KT):
                ps = g.ps[pi % 6]
                psk = "ps%d" % (pi % 6)
                pi += 1
                wt, wtk, mm = (wg, wgk, m) if m < KT else (wm, wmk, m - KT)
                for k in range(KT):
                    P.op("pe", lambda e, ps=ps, wt=wt, mm=mm, k=k, hx=hx: e.matmul(
                        ps[:, 0:n], lhsT=wt[:, k, mm * 128:(mm + 1) * 128], rhs=hx[:, k, 0:n], start=(k == 0), stop=(k == 7)),
                        reads=[wtk, hxk], writes=[psk], sig=(k == 7))
                fn = AF.Gelu_apprx_tanh if m < KT else AF.Sigmoid
                P.op("act", lambda e, ps=ps, o=o, m=m, fn=fn: e.activation(out=o[:, m, 0:n], in_=ps[:, 0:n], func=fn),
                     reads=[psk], writes=[(ok, m)])
                if bg is not None:
                    bgc += 1
                    if bgc % bg_every == 0:
                        next(bg, None)
            P.dma("pool", blk_view(A["G"], s, n), o[:, 0:KT, 0:n], reads=[(ok, m) for m in range(KT)],
                  writes=[(kind + "_G", lb)])
            P.dma("pool", blk_view(A["SGA"], s, n), o[:, KT:2 * KT, 0:n], reads=[(ok, m) for m in range(KT, 2 * KT)],
                  writes=[(kind + "_SGA", lb)])
            P.dma("pool", blk_view(A["SGB"], s, n), o[:, 2 * KT:3 * KT, 0:n], reads=[(ok, m) for m in range(2 * KT, 3 * KT)],
                  writes=[(kind + "_SGB", lb)])
        for ji, (kind, s, n, lb) in enumerate(jobs):
            job(ji, kind, s, n, lb)
        if bg is not None:
            for _ in bg:
                pass


def st_zv(g, l, jobs, bg_pieces=None, bg_every=2):
    P = g.P
    with Stage(g) as st:
        w, wk = load_w(g, st, "wv", g.win, l * D, 2048, 2048)
        hxs = st.ring("zhx", [128, KT, NB], BF16, 2)
        vb = st.ring("zvb", [128, KT, NB], BF16, 2)
        sgs = st.ring("zsg", [128, NB], F32, 3)
        bg = bg_convert(g, st, bg_pieces, eng="act") if bg_pieces else None
        bgc = 0
        pi = 0
        def job(ji, kind, s, n, lb):
            nonlocal pi, bgc
            A = arrs(g, kind)
            hx, hxk = hxs[ji % 2]
            v, vk = vb[ji % 2]
            P.dma("sp", hx[:, :, 0:n], blk_view(A["HX"], s, n), reads=[(kind + "_HX", lb)], writes=[hxk])
            msk = Scol(g, "blkmask", lb) if kind == "L" else Scol(g, "one", 0)
            for m in range(KT):
                pa, pb = g.ps[(2 * pi) % 6], g.ps[(2 * pi + 1) % 6]
                pak, pbk = "ps%d" % ((2 * pi) % 6), "ps%d" % ((2 * pi + 1) % 6)
                sg, sgk = sgs[pi % 3]
                pi += 1
                for (ps, psk, mm) in ((pa, pak, m), (pb, pbk, KT + m)):
                    for k in range(KT):
                        P.op("pe", lambda e, ps=ps, mm=mm, k=k, hx=hx: e.matmul(
                            ps[:, 0:n], lhsT=w[:, k, mm * 128:(mm + 1) * 128], rhs=hx[:, k, 0:n], start=(k == 0), stop=(k == 7)),
                            reads=[wk, hxk], writes=[psk], sig=(k == 7))
                P.op("act", lambda e, pb=pb, sg=sg: e.activation(out=sg[:, 0:n], in_=pb[:, 0:n], func=AF.Sigmoid),
                     reads=[pbk], writes=[sgk])
                P.op("dve", lambda e, pa=pa, sg=sg, v=v, m=m, msk=msk: e.scalar_tensor_tensor(
                    out=v[:, m, 0:n], in0=pa[:, 0:n], scalar=msk, in1=sg[:, 0:n], op0=ALU.mult, op1=ALU.mult),
                    reads=[pak, sgk, "small"], writes=[(vk, m)])
                if bg is not None:
                    bgc += 1
                    if bgc % bg_every == 0:
                        next(bg, None)
            P.dma("pool", blk_view(A["V"], s, n), v[:, :, 0:n], reads=[(vk, m) for m in range(KT)],
                  writes=[(kind + "_V", lb)])
        for ji, (kind, s, n, lb) in enumerate(jobs):
            job(ji, kind, s, n, lb)
        if bg is not None:
            for _ in bg:
                pass


def st_scan(g, l, jobs):
    P = g.P
    with Stage(g) as st:
        gw, gwk = st.sb("gw", [128, 2, 2, KT, 128], BF16)
        P.dma("sp", gw[:].rearrange("p d t c m -> p (d t c) m"),
              g.gw[l * 4096:(l + 1) * 4096, :].rearrange("(x p) m -> p x m", p=128), reads=["gw_bf"], writes=[gwk])
        uhs = st.ring("uh", [128, KT, NB + 3], F32, 2)
        ucs = st.ring("uc", [128, KT, NB], F32, 2)
        ucb, ucbk = st.sb("ucb", [128, KT, NB], BF16)
        so = st.ring("so", [128, KT, NB], F32, 2)
        afo = st.ring("afo", [128, KT, NB], BF16, 2)
        abo = st.ring("abo", [128, KT, NB], BF16, 2)
        R = lambda nm, k=4: st.ring(nm, [128, NB], F32, k)
        rr, gi, aa, a2, bb, hh = R("rr"), R("gi"), R("aa"), R("a2"), R("bb"), R("hh", 4)
        rsm = st.ring("rsm", [128, 2], F32, 4)
        cw = S(g, "cw4_%d" % l)
        cb = S(g, "cb4_%d" % l)
        it = 0

        def job(ji, kind, s, n, lb):
            nonlocal it
            A = arrs(g, kind)
            uh, uhk = uhs[ji % 2]
            uc, uck = ucs[ji % 2]
            sot, sok = so[ji % 2]
            aft, afk = afo[ji % 2]
            abt, abk = abo[ji % 2]
            rks = [(kind + "_U", b) for b in ((lb - 1, lb, lb + 1) if kind == "L" else (0,))]
            P.dma("sp", uh[:, :, 0:n + 3], blk_view(A["U"], s - 2, n + 3), reads=rks, writes=[uhk])
            for c in range(KT):
                P.op("dve", lambda e, c=c: e.tensor_scalar(out=uc[:, c, 0:n], in0=uh[:, c, 0:n],
                                                          scalar1=cw[:, c:c + 1], scalar2=cb[:, c:c + 1],
                                                          op0=ALU.mult, op1=ALU.add),
                     reads=[uhk, "small"], writes=[(uck, c)])
                for j in range(1, 4):
                    P.op("dve", lambda e, c=c, j=j: e.scalar_tensor_tensor(
                        out=uc[:, c, 0:n], in0=uh[:, c, j:j + n], scalar=cw[:, j * 8 + c:j * 8 + c + 1], in1=uc[:, c, 0:n],
                        op0=ALU.mult, op1=ALU.add), reads=[uhk, "small", (uck, c)], writes=[(uck, c)])
                P.op("pool", lambda e, c=c: e.tensor_copy(out=ucb[:, c, 0:n], in_=uc[:, c, 0:n]),
                     reads=[(uck, c)], writes=[(ucbk, c)])
            def stA(c):
                nonlocal it
                T = []
                for d in range(2):
                    T.append((rr[it % 4], gi[it % 4], aa[it % 4], a2[it % 4], bb[it % 4], hh[it % 4], rsm[it % 4],
                              g.ps[(2 * it) % 8], "ps%d" % ((2 * it) % 8), g.ps[(2 * it + 1) % 8], "ps%d" % ((2 * it + 1) % 8)))
                    it += 1
                for d in range(2):
                    pr, prk, pi_, pik = T[d][7], T[d][8], T[d][9], T[d][10]
                    P.op("pe", lambda e, pr=pr, d=d, c=c: e.matmul(pr[:, 0:n], lhsT=gw[:, d, 0, c, :], rhs=ucb[:, c, 0:n],
                                                                  start=True, stop=True),
                         reads=[gwk, (ucbk, c)], writes=[prk])
                    P.op("pe", lambda e, pi_=pi_, d=d, c=c: e.matmul(pi_[:, 0:n], lhsT=gw[:, d, 1, c, :], rhs=ucb[:, c, 0:n],
                                                                    start=True, stop=True),
                         reads=[gwk, (ucbk, c)], writes=[pik])
                for d in range(2):
                    (r_, rk_), (gi_, gik_), _, _, _, _, (rs_, rsk_), pr, prk, pi_, pik = T[d]
                    br = Scol(g, "br%d%d" % (l, d), c)
                    bi = Scol(g, "bi%d%d" % (l, d), c)
                    P.op("act", lambda e, pr=pr, r_=r_, br=br, rs_=rs_: e.activation(out=r_[:, 0:n], in_=pr[:, 0:n], func=AF.Sigmoid, bias=br,
                                                                                accum_out=rs_[:, 0:1]),
                         reads=[prk, "small"], writes=[rk_, rsk_])
                    P.op("act", lambda e, pi_=pi_, gi_=gi_, bi=bi: e.activation(out=gi_[:, 0:n], in_=pi_[:, 0:n], func=AF.Sigmoid, bias=bi),
                         reads=[pik, "small"], writes=[gik_])
                for d in range(2):
                    (r_, rk_), _, (a_, ak_), _, _, _, (rs_, rsk_) = T[d][0:7]
                    P.op("act", lambda e, r_=r_, a_=a_, d=d, c=c: e.activation(out=a_[:, 0:n], in_=r_[:, 0:n], func=AF.Exp,
                                                                             scale=g.cl[:, l, d, 0, c:c + 1]),
                         reads=[rk_, "cl"], writes=[ak_])
                    if kind == "L" and 4 <= lb < 12:
                        P.op("act", lambda e, rs_=rs_, c=c, d=d: e.activation(
                            out=g.sumt[:, c, lb - 4, 2 * d:2 * d + 1], in_=rs_[:, 0:1], func=AF.Exp, scale=g.cl[:, l, d, 0, c:c + 1]),
                            reads=[rsk_, "cl"], writes=["sumt"])
                for d in range(2):
                    _, _, (a_, ak_), (a2_, a2k_) = T[d][0:4]
                    P.op("dve", lambda e, a_=a_, a2_=a2_: e.tensor_tensor(out=a2_[:, 0:n], in0=a_[:, 0:n], in1=a_[:, 0:n], op=ALU.mult),
                         reads=[ak_], writes=[a2k_])
                return T

            def stB(c, T):
                for d in range(2):
                    (a2_, a2k_) = T[d][3]
                    P.op("act", lambda e, a2_=a2_: e.activation(out=a2_[:, 0:n], in_=a2_[:, 0:n], func=AF.Sqrt, scale=-1.0, bias=1.0),
                         reads=[a2k_], writes=[a2k_])
                for d in range(2):
                    _, (gi_, gik_), (a_, ak_), (a2_, a2k_), (b_, bk_), (h_, hk_) = T[d][0:6]
                    P.op("pool", lambda e, gi_=gi_, c=c, b_=b_: e.tensor_tensor(out=b_[:, 0:n], in0=gi_[:, 0:n], in1=uc[:, c, 0:n], op=ALU.mult),
                         reads=[gik_, (uck, c)], writes=[bk_])
                    P.op("pool", lambda e, a2_=a2_, b_=b_: e.tensor_tensor(out=b_[:, 0:n], in0=b_[:, 0:n], in1=a2_[:, 0:n], op=ALU.mult),
                         reads=[bk_, a2k_], writes=[bk_])
                    At, Atk = (aft, afk) if d == 0 else (abt, abk)
                    if d == 0:
                        P.op("dve", lambda e, a_=a_, b_=b_, h_=h_: e.tensor_tensor_scan(out=h_[:, 0:n], data0=a_[:, 0:n], data1=b_[:, 0:n],
                                                                                       initial=0.0, op0=ALU.mult, op1=ALU.add),
                             reads=[ak_, bk_], writes=[hk_])
                        P.op("dve", lambda e, a_=a_, At=At, c=c: e.tensor_tensor_scan(out=At[:, c, 0:n], data0=a_[:, 0:n], data1=g.zeros[:, 0:n],
                                                                                     initial=1.0, op0=ALU.mult, op1=ALU.add),
                             reads=[ak_, "zeros"], writes=[(Atk, c)])
                        hfwd = (h_, hk_)
                        e0, e1 = n - 1, n
                    else:
                        P.op("dve", lambda e, a_=a_, b_=b_, h_=h_: e.tensor_tensor_scan(out=h_[:, 0:n][:, ::-1],
                                                                                       data0=a_[:, 0:n][:, ::-1], data1=b_[:, 0:n][:, ::-1],
                                                                                       initial=0.0, op0=ALU.mult, op1=ALU.add),
                             reads=[ak_, bk_], writes=[hk_])
                        P.op("dve", lambda e, a_=a_, At=At, c=c: e.tensor_tensor_scan(out=At[:, c, 0:n][:, ::-1], data0=a_[:, 0:n][:, ::-1],
                                                                                     data1=g.zeros[:, 0:n], initial=1.0, op0=ALU.mult, op1=ALU.add),
                             reads=[ak_, "zeros"], writes=[(Atk, c)])
                        hf_, hfk_ = hfwd
                        P.op("dve", lambda e, h_=h_, hf_=hf_, c=c: e.tensor_tensor(out=sot[:, c, 0:n], in0=hf_[:, 0:n], in1=h_[:, 0:n], op=ALU.add),
                             reads=[hk_, hfk_], writes=[(sok, c)])
                        e0, e1 = 0, 1
                    if kind == "L" and 4 <= lb < 12:
                        P.op("pool", lambda e, h_=h_, c=c, d=d, e0=e0, e1=e1: e.tensor_copy(
                            out=g.sumt[:, c, lb - 4, 2 * d + 1:2 * d + 2], in_=h_[:, e0:e1]), reads=[hk_, "sumt"], writes=["sumt"])
                    if kind == "C":
                        P.op("pool", lambda e, h_=h_, c=c, d=d, e0=e0, e1=e1: e.tensor_copy(
                            out=g.sctx[:, d, c:c + 1], in_=h_[:, e0:e1]), reads=[hk_, "sctx"], writes=["sctx"])

            Tn = stA(0)
            for c in range(KT):
                Tc = Tn
                if c + 1 < KT:
                    Tn = stA(c + 1)
                stB(c, Tc)
            P.dma("pool", blk_view(A["S"], s, n), sot[:, :, 0:n], reads=[(sok, c) for c in range(KT)], writes=[(kind + "_S", lb)])
            P.dma("pool", blk_view(A["AF"], s, n), aft[:, :, 0:n], reads=[(afk, c) for c in range(KT)], writes=[(kind + "_AF", lb)])
            P.dma("pool", blk_view(A["AB"], s, n), abt[:, :, 0:n], reads=[(abk, c) for c in range(KT)], writes=[(kind + "_AB", lb)])
        for ji, (kind, s, n, lb) in enumerate(jobs):
            job(ji, kind, s, n, lb)


def st_carry(g, l):
    P = g.P
    with Stage(g) as st:
        P.dma("sp", g.sum_in, g.sumt[:].rearrange("p c b f -> p (c b f)"), reads=["sumt"], writes=["sum_in"])
        P.collective("AllGather", ins=[g.sum_in], outs=[g.sum_all], groups=[[0, 1, 2, 3], [4, 5, 6, 7]],
                     reads=["sum_in"], writes=["sum_all"])
        sa, sak = st.sb("sa", [128, 4, KT, 8, 4], F32)
        P.dma("sp", sa[:], g.sum_all.rearrange("(r p) (c b f) -> p r c b f", p=128, c=KT, b=8), reads=["sum_all"], writes=[sak])
        ht, htk = st.sb("ht", [128, 2, KT, 48], F32)
        P.op("dve", lambda e: e.memset(ht[:], 0.0), writes=[htk])
        tmp, tmk = st.sb("ctmp", [128, KT], F32)
        P.op("dve", lambda e: e.tensor_copy(out=ht[:, 0, :, 8], in_=g.sctx[:, 0, :]), reads=["sctx", htk], writes=[htk])
        for gb in range(32):
            r, b = gb // 8, gb % 8
            P.op("dve", lambda e, r=r, b=b, gb=gb: e.tensor_tensor(out=tmp[:], in0=sa[:, r, :, b, 0], in1=ht[:, 0, :, 8 + gb], op=ALU.mult),
                 reads=[sak, htk], writes=[tmk])
            P.op("dve", lambda e, r=r, b=b, gb=gb: e.tensor_tensor(out=ht[:, 0, :, 9 + gb], in0=tmp[:], in1=sa[:, r, :, b, 1], op=ALU.add),
                 reads=[sak, tmk, htk], writes=[htk])
        P.op("dve", lambda e: e.tensor_copy(out=ht[:, 1, :, 40], in_=g.sctx[:, 1, :]), reads=["sctx", htk], writes=[htk])
        for gb in range(31, -1, -1):
            r, b = gb // 8, gb % 8
            P.op("dve", lambda e, r=r, b=b, gb=gb: e.tensor_tensor(out=tmp[:], in0=sa[:, r, :, b, 2], in1=ht[:, 1, :, 9 + gb], op=ALU.mult),
                 reads=[sak, htk], writes=[tmk])
            P.op("dve", lambda e, r=r, b=b, gb=gb: e.tensor_tensor(out=ht[:, 1, :, 8 + gb], in0=tmp[:], in1=sa[:, r, :, b, 3], op=ALU.add),
                 reads=[sak, tmk, htk], writes=[htk])
        P.dma("sp", g.htab.rearrange("d p c x -> p d c x"), ht[:], reads=[htk], writes=["htab"])

        def dyn(e, d):
            base = dynval(g, e, "b%d" % d)
            return e.dma_start(out=g.hin[:, d, :, :],
                               in_=g.htab[d:d + 1, :, :, bass.ds(base, 12)].rearrange("o p c x -> p (o c) x"))
        P.dma("sp", None, None, reads=["htab"], writes=["hin"], fn=lambda e: dyn(e, 0))
        P.dma("sp", None, None, reads=["htab"], writes=["hin"], fn=lambda e: dyn(e, 1))


def st_conv(g, l, kinds, bg_pieces=None):
    P = g.P
    with Stage(g) as st:
        dg = st.ring("dg", [128, 31, 128], BF16, 2)
        vrl = st.ring("vrl", [128, TL], BF16, 2)
        cvo = st.ring("cvo", [128, NB], F32, 3)
        bg = bg_convert(g, st, bg_pieces, eng="act") if bg_pieces else None
        oi = 0
        for c in range(KT):
            dgt, dgk = dg[c % 2]
            for j in range(31):
                P.op("dve", lambda e, j=j, c=c, dgt=dgt: e.tensor_single_scalar(out=dgt[:, j, :], in_=g.ident[:],
                                                                        scalar=Scol(g, "cw31_%d" % l, j * 8 + c), op=ALU.mult),
                     reads=["ident", "small", (dgk, j)], writes=[(dgk, j)])
            for (kind, lbs) in kinds:
                A = arrs(g, kind)
                vr, vrk = vrl[oi % 2]
                if kind == "L":
                    lo, hi = (lbs[0] - 2) * NB, (lbs[-1] + 3) * NB
                    P.dma("sp", vr[:, lo:hi], A["V"][c, :, lo:hi], reads=[("L_V", b) for b in range(lbs[0] - 2, lbs[-1] + 3)],
                          writes=[vrk])
                    stride = 64
                    blocks = [(lb * NB, NB, lb) for lb in lbs]
                else:
                    P.dma("sp", vr[:, 0:TC], A["V"][c, :, :], reads=[("C_V", 0), "C_V"], writes=[vrk])
                    stride = 1
                    blocks = [(CPAD, CTX, 0)]
                for (s, n, lb) in blocks:
                    ps = g.ps[oi % 4]
                    psk = "ps%d" % (oi % 4)
                    o, ok = cvo[oi % 3]
                    oi += 1
                    for j in range(31):
                        off = s + (j - 15) * stride
                        P.op("pe", lambda e, ps=ps, j=j, off=off, vr=vr, dgt=dgt, n=n: e.matmul(
                            ps[:, 0:n], lhsT=dgt[:, j, :], rhs=vr[:, off:off + n], start=(j == 0), stop=(j == 30)),
                            reads=[(dgk, j), vrk], writes=[psk], sig=(j == 30))
                    P.op("act", lambda e, ps=ps, o=o, n=n, c=c: e.activation(out=o[:, 0:n], in_=ps[:, 0:n], func=AF.Identity,
                                                                           bias=Scol(g, "cb31_%d" % l, c), scale=1.0),
                         reads=[psk, "small"], writes=[ok])
                    P.dma("pool", A["CV"][c, :, s:s + n], o[:, 0:n], reads=[ok], writes=[(kind + "_CV", lb, c)])
                    if bg is not None:
                        next(bg, None)
        if bg is not None:
            for _ in bg:
                pass


def st_mix(g, l, jobs):
    P = g.P
    H = 256
    with Stage(g) as st:
        wa, wak = load_w(g, st, "wa", g.wa, l * D, 0, D)
        wb, wbk = load_w(g, st, "wb", g.wb, l * D, 0, D)
        wo, wok = load_w(g, st, "wo", g.wo, l * D, 0, D)
        cvs = st.ring("mcv", [128, KT, H], F32, 2)
        ss = st.ring("ms", [128, KT, H], F32, 2)
        afs = st.ring("maf", [128, KT, H], BF16, 2)
        abs_ = st.ring("mab", [128, KT, H], BF16, 2)
        gs = st.ring("mg", [128, KT, H], BF16, 2)
        sgas = st.ring("msga", [128, KT, H], BF16, 2)
        sgbs = st.ring("msgb", [128, KT, H], BF16, 2)
        xts = st.ring("mxt", [128, KT, H], F32, 2)
        cvb, cvbk = st.sb("cvb", [128, KT, H], BF16)
        sqb, sqbk = st.sb("sqb", [128, KT, H], BF16)
        lno, lnok = st.sb("lno", [128, KT, H], BF16)
        mbt, mbk = st.sb("mbt", [128, KT, H], F32)
        hst, hsk = st.sb("hst", [128, KT, H], F32)
        hsb, hsbk = st.sb("hsb", [128, KT, H], BF16)
        mt, mtk = st.sb("mt", [128, KT, H], BF16)
        mu, muk = st.sb("mu", [128, H], F32)
        var, vark = st.sb("var", [128, H], F32)
        tmp1, tmp1k = st.sb("tmp1", [128, H], F32)
        lng, lnb = S(g, "lng%d" % l), S(g, "lnb%d" % l)
        it = 0
        pi = 0

        def nps():
            nonlocal pi
            r = (g.ps[pi % 6], "ps%d" % (pi % 6))
            pi += 1
            return r
        halves_ = []
        for (kind, s0, n0, lb) in jobs:
            A = arrs(g, kind)
            si = 0 if kind == "L" else 1
            def half_body(kind, lb, s0, n0, s, A, si):
                nonlocal it
                n = min(H, s0 + n0 - s)
                cv, cvk = cvs[it % 2]
                sst, ssk = ss[it % 2]
                af, afk = afs[it % 2]
                ab, abk = abs_[it % 2]
                gt, gk = gs[it % 2]
                sga, sgak = sgas[it % 2]
                sgb, sgbk = sgbs[it % 2]
                xt, xtk = xts[it % 2]
                it += 1
                def prep():
                    P.dma("sp", cv[:, :, 0:n], blk_view(A["CV"], s, n), reads=[(kind + "_CV", lb, c) for c in range(KT)], writes=[cvk])
                    P.dma("sp", sst[:, :, 0:n], blk_view(A["S"], s, n), reads=[(kind + "_S", lb)], writes=[ssk])
                    P.dma("sp", af[:, :, 0:n], blk_view(A["AF"], s, n), reads=[(kind + "_AF", lb)], writes=[afk])
                    P.dma("sp", ab[:, :, 0:n], blk_view(A["AB"], s, n), reads=[(kind + "_AB", lb)], writes=[abk])
                    P.dma("sp", gt[:, :, 0:n], blk_view(A["G"], s, n), reads=[(kind + "_G", lb)], writes=[gk])
                    P.dma("sp", sga[:, :, 0:n], blk_view(A["SGA"], s, n), reads=[(kind + "_SGA", lb)], writes=[sgak])
                    P.dma("sp", sgb[:, :, 0:n], blk_view(A["SGB"], s, n), reads=[(kind + "_SGB", lb)], writes=[sgbk])
                    P.dma("sp", xt[:, :, 0:n], blk_view(A["XT"], s, n), reads=[(kind + "_XT", lb)], writes=[xtk])
                    P.op("act", lambda e, cv=cv: e.copy(out=cvb[:, :, 0:n], in_=cv[:, :, 0:n]), reads=[cvk], writes=[cvbk])
                    P.op("act", lambda e, cv=cv: e.activation(out=sqb[:, :, 0:n], in_=cv[:, :, 0:n], func=AF.Square), reads=[cvk], writes=[sqbk])
                    p1, p1k = g.ps[6], "ps6"
                    p2, p2k = g.ps[7], "ps7"
                    for k in range(KT):
                        P.op("pe", lambda e, k=k: e.matmul(p1[:, 0:n], lhsT=g.ones_b[:], rhs=cvb[:, k, 0:n], start=(k == 0), stop=(k == 7)),
                             reads=[cvbk, "ones_b"], writes=[p1k], sig=(k == 7))
                    for k in range(KT):
                        P.op("pe", lambda e, k=k: e.matmul(p2[:, 0:n], lhsT=g.ones_b[:], rhs=sqb[:, k, 0:n], start=(k == 0), stop=(k == 7)),
                             reads=[sqbk, "ones_b"], writes=[p2k], sig=(k == 7))
                    P.op("dve", lambda e: e.tensor_single_scalar(out=mu[:, 0:n], in_=p1[:, 0:n], scalar=1.0 / D, op=ALU.mult),
                         reads=[p1k], writes=[muk])
                    P.op("dve", lambda e: e.tensor_tensor(out=tmp1[:, 0:n], in0=mu[:, 0:n], in1=mu[:, 0:n], op=ALU.mult),
                         reads=[muk], writes=[tmp1k])
                    P.op("dve", lambda e: e.scalar_tensor_tensor(out=var[:, 0:n], in0=p2[:, 0:n], scalar=1.0 / D, in1=tmp1[:, 0:n],
                                                                 op0=ALU.mult, op1=ALU.subtract),
                         reads=[p2k, tmp1k], writes=[vark])
                    P.op("dve", lambda e: e.tensor_single_scalar(out=var[:, 0:n], in_=var[:, 0:n], scalar=0.0, op=ALU.max),
                         reads=[vark], writes=[vark])
                    P.op("act", lambda e: e.activation(out=var[:, 0:n], in_=var[:, 0:n], func=AF.Sqrt, bias=EPS, scale=1.0),
                         reads=[vark], writes=[vark])
                    P.op("dve", lambda e: e.reciprocal(out=var[:, 0:n], in_=var[:, 0:n]), reads=[vark], writes=[vark])
                    P.op("dve", lambda e, cv=cv: e.tensor_tensor(out=cv[:, :, 0:n], in0=cv[:, :, 0:n],
                                                                in1=mu[:, 0:n].unsqueeze(1).to_broadcast([128, KT, n]), op=ALU.subtract),
                         reads=[cvk, muk], writes=[cvk])
                    P.op("dve", lambda e, cv=cv: e.tensor_tensor(out=cv[:, :, 0:n], in0=cv[:, :, 0:n],
                                                                in1=var[:, 0:n].unsqueeze(1).to_broadcast([128, KT, n]), op=ALU.mult),
                         reads=[cvk, vark], writes=[cvk])
                    for c in range(KT):
                        P.op("act", lambda e, c=c, cv=cv: e.activation(out=lno[:, c, 0:n], in_=cv[:, c, 0:n], func=AF.Silu,
                                                                      scale=lng[:, c:c + 1], bias=lnb[:, c:c + 1]),
                             reads=[cvk, "small"], writes=[(lnok, c)])
                def main1():
                    for m in range(KT):
                        ps, psk = nps()
                        for k in range(KT):
                            P.op("pe", lambda e, ps=ps, m=m, k=k: e.matmul(ps[:, 0:n], lhsT=wb[:, k, m * 128:(m + 1) * 128], rhs=lno[:, k, 0:n],
                                                                          start=(k == 0), stop=(k == 7)),
                                 reads=[wbk, (lnok, k)], writes=[psk], sig=(k == 7))
                        P.op("dve", lambda e, ps=ps, m=m, sgb=sgb: e.tensor_tensor(out=mbt[:, m, 0:n], in0=ps[:, 0:n], in1=sgb[:, m, 0:n], op=ALU.mult),
                             reads=[psk, sgbk], writes=[(mbk, m)])
                def main2():
                    hin = g.hin if kind == "L" else g.hzero
                    bidx = (lb - 2) if kind == "L" else 0
                    for c in range(KT):
                        P.op("dve", lambda e, c=c, af=af, sst=sst: e.scalar_tensor_tensor(
                            out=hst[:, c, 0:n], in0=af[:, c, 0:n], scalar=hin[:, 0, c, bidx:bidx + 1], in1=sst[:, c, 0:n],
                            op0=ALU.mult, op1=ALU.add), reads=[afk, ssk, "hin", "hzero"], writes=[(hsk, c)])
                        P.op("dve", lambda e, c=c, ab=ab: e.scalar_tensor_tensor(
                            out=hst[:, c, 0:n], in0=ab[:, c, 0:n], scalar=hin[:, 1, c, bidx:bidx + 1], in1=hst[:, c, 0:n],
                            op0=ALU.mult, op1=ALU.add), reads=[abk, (hsk, c), "hin", "hzero"], writes=[(hsk, c)])
                        P.op("pool", lambda e, c=c, gt=gt: e.tensor_tensor(out=hsb[:, c, 0:n], in0=hst[:, c, 0:n], in1=gt[:, c, 0:n], op=ALU.mult),
                             reads=[(hsk, c), gk], writes=[(hsbk, c)])
                    for m in range(KT):
                        ps, psk = nps()
                        for k in range(KT):
                            P.op("pe", lambda e, ps=ps, m=m, k=k: e.matmul(ps[:, 0:n], lhsT=wa[:, k, m * 128:(m + 1) * 128], rhs=hsb[:, k, 0:n],
                                                                          start=(k == 0), stop=(k == 7)),
                                 reads=[wak, (hsbk, k)], writes=[psk], sig=(k == 7))
                        P.op("dve", lambda e, ps=ps, m=m, sga=sga: e.tensor_tensor(out=hst[:, m, 0:n], in0=ps[:, 0:n], in1=sga[:, m, 0:n], op=ALU.mult),
                             reads=[psk, sgak], writes=[(hsk, m)])
                        P.op("pool", lambda e, m=m: e.tensor_tensor(out=mt[:, m, 0:n], in0=hst[:, m, 0:n], in1=mbt[:, m, 0:n], op=ALU.add),
                             reads=[(hsk, m), (mbk, m)], writes=[(mtk, m)])
                    g1 = modvec(g, l, si, "G1")
                    for m in range(KT):
                        ps, psk = nps()
                        for k in range(KT):
                            P.op("pe", lambda e, ps=ps, m=m, k=k: e.matmul(ps[:, 0:n], lhsT=wo[:, k, m * 128:(m + 1) * 128], rhs=mt[:, k, 0:n],
                                                                          start=(k == 0), stop=(k == 7)),
                                 reads=[wok, (mtk, k)], writes=[psk], sig=(k == 7))
                        P.op("dve", lambda e, ps=ps, m=m, xt=xt: e.scalar_tensor_tensor(
                            out=xt[:, m, 0:n], in0=ps[:, 0:n], scalar=g1[:, m:m + 1], in1=xt[:, m, 0:n], op0=ALU.mult, op1=ALU.add),
                            reads=[psk, xtk, "modx", "modc"], writes=[xtk])
                    P.dma("pool", blk_view(A["XT"], s, n), xt[:, :, 0:n], reads=[xtk], writes=[(kind + "_XT", lb)])
                return prep, main1, main2
            for s in range(s0, s0 + n0, H):
                halves_.append(half_body(kind, lb, s0, n0, s, A, si))
        halves_[0][0]()
        for i_ in range(len(halves_)):
            halves_[i_][1]()
            halves_[i_][2]()
            if i_ + 1 < len(halves_):
                halves_[i_ + 1][0]()


def st_ffn(g, l, sblocks, moe, publish=False):
    P = g.P
    CH = 256
    NCH = DFF // CH
    TB = 1024
    with Stage(g) as st:
        hxs = st.ring("fhx", [128, KT, TB], BF16, 1)
        acc, acck = st.sb("facc", [128, KT, TB], F32)
        cmb = st.ring("fcmb", [128, TB], F32, 2)
        w1s = st.ring("fw1", [128, KT, 2 * CH], BF16, 3)
        w3s = st.ring("fw3", [128, KT, 2 * CH], BF16, 3)
        w2s = st.ring("fw2", [128, 4, D], BF16, 3)
        sil = st.ring("fsil", [128, NB], BF16, 3)
        gts = st.ring("fgt", [128, 4, NB], BF16, 2)
        xts = st.ring("fxt", [128, KT, NB], F32, 2)
        ne = NE if moe else 1
        wi = 0
        hi_ = 0
        gi_ = 0
        pi = 0
        groups = [(c0, min(2, NCH - c0)) for c0 in range(0, NCH, 2)]
        def sb_body(kind, s0, n0, lbs):
            nonlocal wi, hi_, gi_, pi
            A = arrs(g, kind)
            si = 0 if kind == "L" else 1
            hx, hxk = hxs[0]
            P.dma("sp", hx[:, :, 0:n0], blk_view(A["HX"], s0, n0), reads=[(kind + "_HX", lb) for lb in lbs], writes=[hxk])
            halves = [(o, min(NB, n0 - o)) for o in range(0, n0, NB)]
            steps = [(ex, gi2, c0, nc_, ho, hn) for ex in range(ne) for gi2, (c0, nc_) in enumerate(groups) for (ho, hn) in halves]
            loaded = {}
            cms = {}

            def ensure(ex, gi2, c0, nc_):
                nonlocal wi
                if moe and ex not in cms:
                    cm, cmk = cmb[ex % 2]
                    P.dma("sp", cm[:, 0:n0], g.comb[ex, :, s0:s0 + n0], reads=[("COMB", lb) for lb in lbs], writes=[cmk])
                    cms[ex] = (cm, cmk)
                if (ex, gi2) in loaded:
                    return
                if moe:
                    W1, W3, W2 = g.m1, g.m3, g.m2
                    r1, r2 = ex * D, ex * DFF
                else:
                    W1, W3, W2 = g.f1, g.f3, g.f2
                    r1, r2 = 0, 0
                w1, w1k = w1s[wi % 3]
                w3, w3k = w3s[wi % 3]
                w2, w2k = w2s[wi % 3]
                wi += 1
                cw_ = nc_ * CH
                nj = nc_ * 2
                P.dma("sp", w1[:, :, 0:cw_], W1[r1:r1 + D, c0 * CH:c0 * CH + cw_].rearrange("(k p) n -> p k n", p=128),
                      reads=[], writes=[w1k])
                P.dma("sp", w3[:, :, 0:cw_], W3[r1:r1 + D, c0 * CH:c0 * CH + cw_].rearrange("(k p) n -> p k n", p=128),
                      reads=[], writes=[w3k])
                P.dma("sp", w2[:, 0:nj, :], W2[r2 + c0 * CH:r2 + c0 * CH + cw_, :].rearrange("(k p) n -> p k n", p=128),
                      reads=[], writes=[w2k])
                loaded[(ex, gi2)] = (w1, w1k, w3, w3k, w2, w2k, nj)

            def emit_h(step):
                nonlocal hi_, gi_, pi
                ex, gi2, c0, nc_, ho, hn = step
                ensure(ex, gi2, c0, nc_)
                w1, w1k, w3, w3k, w2, w2k, nj = loaded[(ex, gi2)]
                gt, gtk = gts[gi_ % 2]
                gi_ += 1
                for j in range(nj):
                    p1, p1k = g.ps[pi % 4], "ps%d" % (pi % 4)
                    p3, p3k = g.ps[(pi + 1) % 4], "ps%d" % ((pi + 1) % 4)
                    pi += 2
                    for k in range(KT):
                        P.op("pe", lambda e, p1=p1, w1=w1, j=j, k=k, ho=ho, hn=hn: e.matmul(
                            p1[:, 0:hn], lhsT=w1[:, k, j * 128:(j + 1) * 128], rhs=hx[:, k, ho:ho + hn], start=(k == 0), stop=(k == 7)),
                            reads=[w1k, hxk], writes=[p1k], sig=(k == 7))
                    for k in range(KT):
                        P.op("pe", lambda e, p3=p3, w3=w3, j=j, k=k, ho=ho, hn=hn: e.matmul(
                            p3[:, 0:hn], lhsT=w3[:, k, j * 128:(j + 1) * 128], rhs=hx[:, k, ho:ho + hn], start=(k == 0), stop=(k == 7)),
                            reads=[w3k, hxk], writes=[p3k], sig=(k == 7))
                    sl, slk = sil[hi_ % 3]
                    hi_ += 1
                    P.op("act", lambda e, p1=p1, sl=sl, hn=hn: e.activation(out=sl[:, 0:hn], in_=p1[:, 0:hn], func=AF.Silu),
                         reads=[p1k], writes=[slk])
                    if moe:
                        cm, cmk = cms[ex]
                        P.op("pool", lambda e, sl=sl, cm=cm, ho=ho, hn=hn: e.tensor_tensor(out=sl[:, 0:hn], in0=sl[:, 0:hn],
                                                                                          in1=cm[:, ho:ho + hn], op=ALU.mult),
                             reads=[slk, cmk], writes=[slk])
                    P.op("dve", lambda e, p3=p3, sl=sl, gt=gt, j=j, hn=hn: e.tensor_tensor(out=gt[:, j, 0:hn], in0=p3[:, 0:hn],
                                                                                          in1=sl[:, 0:hn], op=ALU.mult),
                         reads=[p3k, slk], writes=[(gtk, j)])
                return (gt, gtk)

            def emit_w2(step, H, first):
                ex, gi2, c0, nc_, ho, hn = step
                w1, w1k, w3, w3k, w2, w2k, nj = loaded[(ex, gi2)]
                gt, gtk = H
                for m in range(KT):
                    po, pok = g.ps[4 + (m % 4)], "ps%d" % (4 + (m % 4))
                    for j in range(nj):
                        P.op("pe", lambda e, po=po, w2=w2, j=j, m=m, gt=gt, hn=hn, nj=nj: e.matmul(
                            po[:, 0:hn], lhsT=w2[:, j, m * 128:(m + 1) * 128], rhs=gt[:, j, 0:hn], start=(j == 0), stop=(j == nj - 1)),
                            reads=[w2k, (gtk, j)], writes=[pok], sig=(j == nj - 1))
                    if first:
                        P.op("act", lambda e, po=po, m=m, ho=ho, hn=hn: e.copy(out=acc[:, m, ho:ho + hn], in_=po[:, 0:hn]),
                             reads=[pok], writes=[(acck, m, ho)])
                    else:
                        P.op("dve", lambda e, po=po, m=m, ho=ho, hn=hn: e.tensor_tensor(out=acc[:, m, ho:ho + hn], in0=po[:, 0:hn],
                                                                                       in1=acc[:, m, ho:ho + hn], op=ALU.add),
                             reads=[pok, (acck, m, ho)], writes=[(acck, m, ho)])

            Hc = emit_h(steps[0])
            for i in range(len(steps)):
                Hn = emit_h(steps[i + 1]) if i + 1 < len(steps) else None
                emit_w2(steps[i], Hc, first=(i < len(halves)))
                Hc = Hn
            g2 = modvec(g, l, si, "G2")
            for hi2, (ho, hn) in enumerate(halves):
                xt, xtk = xts[hi2 % 2]
                lb = lbs[hi2] if kind == "L" else 0
                P.dma("sp", xt[:, :, 0:hn], blk_view(A["XT"], s0 + ho, hn), reads=[(kind + "_XT", lb)], writes=[xtk])
                for m in range(KT):
                    P.op("dve", lambda e, m=m, xt=xt, ho=ho, hn=hn: e.scalar_tensor_tensor(
                        out=xt[:, m, 0:hn], in0=acc[:, m, ho:ho + hn], scalar=g2[:, m:m + 1], in1=xt[:, m, 0:hn], op0=ALU.mult, op1=ALU.add),
                        reads=[(acck, m, ho), xtk, "modx", "modc"], writes=[xtk])
                P.dma("pool", blk_view(A["XT"], s0 + ho, hn), xt[:, :, 0:hn], reads=[xtk], writes=[(kind + "_XT", lb)])
                if publish and kind == "L" and lb in (4, 5, 10, 11):
                    eb = (4, 5, 10, 11).index(lb)
                    for cg in range(2):
                        P.dma("pool", g.ex_in[2 * eb + cg].rearrange("p (c t) -> p c t", c=4), xt[:, 4 * cg:4 * cg + 4, :],
                              reads=[xtk], writes=[("ex_in", 2 * eb + cg)])
                        P.collective("AllGather", ins=[g.ex_in[2 * eb + cg]], outs=[g.ex_all[2 * eb + cg].rearrange("r p f -> (r p) f")],
                                     groups=[[0, 1, 2, 3], [4, 5, 6, 7]], reads=[("ex_in", 2 * eb + cg)],
                                     writes=[("ex_all", 2 * eb + cg)])
        for (kind, s0, n0, lbs) in sblocks:
            sb_body(kind, s0, n0, lbs)


def st_exchange(g):
    P = g.P
    XT = g.lat["XT"]
    with Stage(g) as st:
        hb = st.ring("exh", [128, 4, NB], F32, 3)
        i = 0
        for (lb, eb, off) in ((2, 2, 3), (3, 3, 3), (12, 0, 1), (13, 1, 1)):
            for cg in range(2):
                u = 2 * eb + cg
                t, tk = hb[i % 3]
                i += 1

                for hh_ in range(2):
                    def dyn(e, t=t, u=u, off=off, hh_=hh_):
                        r = dynval(g, e, "rl" if off == 3 else "rr")
                        return e.dma_start(out=t[:, 2 * hh_:2 * hh_ + 2, :].rearrange("p c t -> p (c t)"),
                                           in_=g.ex_all[u][bass.ds(r, 1), :, 1024 * hh_:1024 * hh_ + 1024].rearrange("o p f -> p (o f)"))
                    P.dma("sp", None, None, reads=[("ex_all", u)], writes=[tk], fn=dyn)
                P.dma("sp", XT[4 * cg:4 * cg + 4, :, lb * NB:(lb + 1) * NB].rearrange("c p t -> p c t"), t[:],
                      reads=[tk], writes=[("L_XT", lb, cg)])


def st_final(g):
    P = g.P
    with Stage(g) as st:
        xts = st.ring("oxt", [128, KT, NB], F32, 2)
        ys = st.ring("oy", [128, KT, NB], F32, 2)
        outs = st.ring("oo", [128, 4, D], F32, 2)
        bufs = {"sq": st.sb("osq", [128, KT, NB], BF16), "rs": st.sb("ors", [128, NB], F32)}
        fg = S(g, "fing")
        for ji, lb in enumerate(range(4, 12)):
            xt, xtk = xts[ji % 2]
            y, yk = ys[ji % 2]
            oo, ook = outs[ji % 2]
            P.dma("sp", xt[:], blk_view(g.lat["XT"], lb * NB, NB), reads=[("L_XT", lb)], writes=[xtk])
            norm_core(g, st, xt, xtk, NB, fg, None, bufs, out_f32=(y, yk))
            for a in range(4):
                for half in range(2):
                    ps = g.ps[(a * 2 + half) % 6]
                    psk = "ps%d" % ((a * 2 + half) % 6)
                    for cc in range(4):
                        c = half * 4 + cc
                        P.op("pe", lambda e, ps=ps, y=y, a=a, c=c, cc=cc: e.transpose(
                            out=ps[:, cc * 128:(cc + 1) * 128], in_=y[:, c, a * 128:(a + 1) * 128], identity=g.ident[:]),
                            reads=[(yk, c), "ident"], writes=[psk])
                    if half == 0:
                        P.op("act", lambda e, ps=ps, oo=oo, a=a: e.copy(out=oo[:, a, 0:512], in_=ps[:, 0:512]),
                             reads=[psk], writes=[(ook, a, 0)])
                    else:
                        P.op("dve", lambda e, ps=ps, oo=oo, a=a: e.tensor_copy(out=oo[:, a, 512:1024], in_=ps[:, 0:512]),
                             reads=[psk], writes=[(ook, a, 1)])
            P.dma("pool", g.out[(lb - 4) * NB:(lb - 3) * NB, :].rearrange("(a p) d -> p a d", p=128), oo[:],
                  reads=[(ook, a, h) for a in range(4) for h in range(2)], writes=[("out", lb)])


def build():
    g = build_program()
    L = lambda lbs: seg_blocks("L", lbs)
    C = seg_blocks("C", None)

    def stop(name):
        return STOP_AFTER == name
    st_setup(g)
    if stop("setup"):
        g.P.emit(); return g
    st_convert(g, "mix")
    st_transpose_in(g)
    if stop("tin"):
        g.P.emit(); return g
    stopped = False
    for l in range(DEPTH):
        last = l == DEPTH - 1
        nblk = list(range(2, 14))
        ublk = list(range(3, 13))
        pblk = list(range(4, 12))
        if l == 1:
            st_exchange(g)
        st_norm(g, l, 1, L(nblk) + C)
        st_zu(g, l, L(ublk) + C)
        st_scan(g, l, C + L(pblk))
        if stop("scan%d" % l):
            stopped = True
            break
        st_carry(g, l)
        if stop("carry%d" % l):
            stopped = True
            break
        bgA = bgB = bgC = None
        if l == 0 and not NO_MOE:
            pcs = conv_pieces(g, "moe")
            n1_, n2_ = (len(pcs) * 4) // 10, (len(pcs) * 7) // 10
            bgA, bgB, bgC = pcs[:n1_], pcs[n1_:n2_], pcs[n2_:]
        st_zgm(g, l, L(pblk) + (C if not last else []), bg_pieces=bgA, bg_every=4)
        st_zv(g, l, L(nblk) + (C if not last else []), bg_pieces=bgB, bg_every=3)
        st_conv(g, l, [("L", pblk)] + ([("C", None)] if not last else []), bg_pieces=bgC)
        if stop("conv%d" % l):
            stopped = True
            break
        st_mix(g, l, L(pblk) + (C if not last else []))
        if stop("mix%d" % l):
            stopped = True
            break
        moe = (l % 2 == 1)
        st_norm(g, l, 2, L(pblk) + (C if not last else []), router=moe)
        sbl = [("L", pblk[i] * NB, 2 * NB, [pblk[i], pblk[i + 1]]) for i in range(0, len(pblk), 2)]
        if not last:
            sbl = [sbl[0], sbl[-1]] + sbl[1:-1]
            sbl.append(("C", CPAD, CTX, [0]))
        st_ffn(g, l, sbl, moe, publish=not last)
        if stop("ffn%d" % l):
            stopped = True
            break
    if not stopped:
        st_final(g)
    g.P.emit()
    return g


def make_in_maps(inp):
    x = np.asarray(inp["x"], np.float32)
    maps = []
    gw = np.zeros((DEPTH, 2, 2, 8, 128, 128), np.float32)
    for l in range(DEPTH):
        for d in range(2):
            for ti, nm in enumerate(("lru_wr", "lru_wi")):
                w = np.asarray(inp[nm][l][d], np.float32)
                for c in range(8):
                    gw[l, d, ti, c, 0:64, 0:64] = w[2 * c]
                    gw[l, d, ti, c, 64:128, 64:128] = w[2 * c + 1]
    gw = gw.reshape(-1, 128)
    shared = {} if NO_MOE else {
        "moe_w1": np.ascontiguousarray(np.asarray(inp["moe_w1"], np.float32)[0].reshape(NE * D, DFF)),
        "moe_w3": np.ascontiguousarray(np.asarray(inp["moe_w3"], np.float32)[0].reshape(NE * D, DFF)),
        "moe_w2": np.ascontiguousarray(np.asarray(inp["moe_w2"], np.float32)[0].reshape(NE * DFF, D)),
    }
    shared.update({
        "w_in": np.ascontiguousarray(np.asarray(inp["w_in"], np.float32).reshape(DEPTH * D, 6144)),
        "w_a": np.ascontiguousarray(np.asarray(inp["w_branch_a"], np.float32).reshape(DEPTH * D, D)),
        "w_b": np.ascontiguousarray(np.asarray(inp["w_branch_b"], np.float32).reshape(DEPTH * D, D)),
        "w_o": np.ascontiguousarray(np.asarray(inp["w_out"], np.float32).reshape(DEPTH * D, D)),
        "ffn_w1": np.ascontiguousarray(np.asarray(inp["ffn_w1"], np.float32)[0]),
        "ffn_w3": np.ascontiguousarray(np.asarray(inp["ffn_w3"], np.float32)[0]),
        "ffn_w2": np.ascontiguousarray(np.asarray(inp["ffn_w2"], np.float32)[0]),
        "gatew": gw,
        "router": np.ascontiguousarray(np.asarray(inp["moe_router"], np.float32)[0]),
    })
    modw = np.asarray(inp["mod_w"], np.float32)
    for core in range(8):
        b, q = core // 4, core % 4
        xl = np.zeros((TL, D), np.float32)
        g0 = q * 4096 - 4 * NB
        lo, hi = max(g0, 0), min(g0 + TL, 16384)
        xl[lo - g0:hi - g0] = x[b, lo:hi]
        m = dict(shared)
        m["x_loc"] = xl
        m["ctx_in"] = np.ascontiguousarray(np.asarray(inp["ctx"], np.float32)[b])
        m["small"] = _build_small(inp, core)
        m["modw"] = np.ascontiguousarray(modw[:, :, q * 1536:(q + 1) * 1536])
        maps.append(m)
    return maps


_CACHE = {}


def kernel(**inputs):
    if "g" not in _CACHE:
        _CACHE["g"] = build()
    g = _CACHE["g"]
    maps = make_in_maps(inputs)
    res = run_bass_kernel_spmd(g.nc, maps, core_ids=list(range(8)))
    out = np.zeros((2, 16384, D), np.float32)
    for core in range(8):
        b, q = core // 4, core % 4
        out[b, q * 4096:(q + 1) * 4096] = res.results[core]["out"]
    _CACHE["last"] = res
    return out
```

```python
import numpy as np
from contextlib import ExitStack
import concourse.bass as bass
import concourse.mybir as mybir
from concourse.bass_utils import run_bass_kernel_spmd

F32 = mybir.dt.float32
BF16 = mybir.dt.bfloat16
AF = mybir.ActivationFunctionType
ALU = mybir.AluOpType

D = 1024
KT = 8
NBLK = 16
NB = 512
TL = NBLK * NB
CTX = 256
CPAD = 16
TC = CTX + 2 * CPAD
DFF = 2816
NE = 8
EPS = 1e-6
DEPTH = 2
SEM_ROT = 30000
N_DMA_SEM = 16

DEBUG_OUT = []
STOP_AFTER = None
NO_MOE = False


class _Op:
    __slots__ = ("eng", "fn", "waits", "tok", "clock", "is_dma", "sig")


class Prog:
    ENG = ("pe", "dve", "act", "pool", "sp")

    def __init__(self, nc):
        self.nc = nc
        self.ops = {e: [] for e in self.ENG}
        self.known = {e: {} for e in self.ENG}
        self.cnt = {e: 0 for e in self.ENG}
        self.cur_sem = {}
        self.sems = {}
        self.nsem = 0
        self.own_done = {e: {} for e in self.ENG}
        for e in self.ENG:
            self.cur_sem[e] = self._new_sem(e)
        self.dma_sems = {}
        self.dma_uses = {}
        self.dma_last = {}
        self.dma_rr = {}
        for q in ("sp", "pool"):
            self.dma_sems[q] = [self._new_sem("d" + q) for _ in range(N_DMA_SEM)]
            self.dma_rr[q] = 0
            for s in self.dma_sems[q]:
                self.dma_uses[s] = 0
                self.dma_last[s] = None
        self.last_w = {}
        self.readers = {}
        self.nops = 0
        self.last_op = {e: None for e in self.ENG}
        self.pending_nosig = {e: 0 for e in self.ENG}
        self.uid = 0

    def _new_sem(self, tag):
        sid = self.nsem
        self.nsem += 1
        self.sems[sid] = self.nc.alloc_semaphore(name="s%d_%s" % (sid, tag))
        return sid

    def name(self, base):
        self.uid += 1
        return "%s_%d" % (base, self.uid)

    def _deps(self, eng, reads, writes, is_pe):
        deps = []
        for k in reads:
            w = self.last_w.get(k)
            if w is not None:
                deps.append(w)
        for k in writes:
            w = self.last_w.get(k)
            if w is not None:
                deps.append(w)
            for r in self.readers.get(k, ()):
                deps.append(r)
        waits = {}
        kn = self.known[eng]
        for d in deps:
            if is_pe and d.eng == "pe" and not d.is_dma:
                continue
            sid, val = d.tok
            if kn.get(sid, 0) >= val:
                continue
            if waits.get(sid, 0) < val:
                waits[sid] = val
        for d in deps:
            for sid, val in d.clock.items():
                if kn.get(sid, 0) < val:
                    kn[sid] = val
        return waits

    def _register(self, op, reads, writes):
        for k in reads:
            lst = self.readers.setdefault(k, [])
            if not op.is_dma:
                lst[:] = [r for r in lst if r.is_dma or r.eng != op.eng]
            lst.append(op)
        for k in writes:
            self.last_w[k] = op
            self.readers[k] = []

    def op(self, eng, fn, reads=(), writes=(), sig=True):
        o = _Op()
        o.eng = eng
        o.fn = fn
        o.is_dma = False
        o.sig = sig
        o.waits = self._deps(eng, reads, writes, eng == "pe")
        if self.cnt[eng] >= SEM_ROT and self.pending_nosig[eng] == 0:
            self.own_done[eng][self.cur_sem[eng]] = self.cnt[eng]
            self.cur_sem[eng] = self._new_sem(eng)
            self.cnt[eng] = 0
        if sig:
            self.cnt[eng] += 1
            self.pending_nosig[eng] = 0
            o.tok = (self.cur_sem[eng], self.cnt[eng])
        else:
            self.pending_nosig[eng] += 1
            o.tok = (self.cur_sem[eng], self.cnt[eng] + 1)
        o.clock = dict(self.known[eng])
        o.clock.update(self.own_done[eng])
        o.clock[o.tok[0]] = o.tok[1]
        self._register(o, reads, writes)
        self.ops[eng].append(o)
        self.last_op[eng] = o
        self.nops += 1
        return o

    def dma(self, q, out, in_, reads=(), writes=(), fn=None):
        o = _Op()
        o.eng = q
        o.is_dma = True
        waits = self._deps(q, reads, writes, False)
        sems = self.dma_sems[q]
        sid = sems[self.dma_rr[q] % len(sems)]
        self.dma_rr[q] += 1
        prev = self.dma_last[sid]
        kn = self.known[q]
        if prev is not None and kn.get(sid, 0) < prev.tok[1]:
            waits[sid] = max(waits.get(sid, 0), prev.tok[1])
            kn[sid] = prev.tok[1]
        self.dma_uses[sid] += 1
        o.tok = (sid, 16 * self.dma_uses[sid])
        self.dma_last[sid] = o
        o.waits = waits
        o.fn = ("dynfn", fn) if fn is not None else ("dma", out, in_)
        o.clock = dict(kn)
        o.clock[sid] = o.tok[1]
        self._register(o, reads, writes)
        self.ops[q].append(o)
        self.nops += 1
        return o

    def collective(self, kind, ins, outs, groups, reads=(), writes=()):
        o = _Op()
        o.eng = "pool"
        o.is_dma = True
        o.waits = self._deps("pool", reads, writes, False)
        sid = self._new_sem("cc")
        o.tok = (sid, 1)
        o.fn = ("cc", kind, ins, outs, groups)
        o.clock = dict(self.known["pool"])
        o.clock[sid] = 1
        self._register(o, reads, writes)
        self.ops["pool"].append(o)
        self.nops += 1
        return o

    def barrier(self):
        toks = {}
        for e in self.ENG:
            lo = self.last_op[e]
            if lo is not None:
                toks[lo.tok[0]] = max(toks.get(lo.tok[0], 0), lo.tok[1])
        for sid, lo in self.dma_last.items():
            if lo is not None:
                toks[sid] = max(toks.get(sid, 0), lo.tok[1])
        for k, w in self.last_w.items():
            if w is not None and w.is_dma:
                toks[w.tok[0]] = max(toks.get(w.tok[0], 0), w.tok[1])
        for e in self.ENG:
            kn = self.known[e]
            waits = {}
            for sid, val in toks.items():
                if kn.get(sid, 0) < val:
                    if e == "pe" and sid == self.cur_sem["pe"]:
                        continue
                    waits[sid] = val
                    kn[sid] = val
            if waits:
                o = _Op()
                o.eng = e
                o.is_dma = False
                o.waits = waits
                o.fn = None
                o.tok = None
                o.clock = {}
                self.ops[e].append(o)
        self.last_w = {}
        self.readers = {}

    def _replay(self, ename, e):
        sems = self.sems
        for o in self.ops[ename]:
            for sid, val in o.waits.items():
                e.wait_ge(sems[sid], val)
            if o.fn is None:
                continue
            if o.is_dma:
                if o.fn[0] == "dynfn":
                    o.fn[1](e).then_inc(sems[o.tok[0]], 16)
                elif o.fn[0] == "dma":
                    e.dma_start(out=o.fn[1], in_=o.fn[2]).then_inc(sems[o.tok[0]], 16)
                else:
                    _, kind, ins, outs, groups = o.fn
                    e.collective_compute(kind, ALU.bypass, replica_groups=groups,
                                         ins=ins, outs=outs).then_inc(sems[o.tok[0]])
            elif o.sig:
                o.fn(e).then_inc(sems[o.tok[0]], 1)
            else:
                o.fn(e)

    def emit(self):
        self.barrier()
        with self.nc.Block() as block:
            @block.sync
            def _(e):
                self._replay("sp", e)

            @block.tensor
            def _(e):
                self._replay("pe", e)

            @block.vector
            def _(e):
                self._replay("dve", e)

            @block.scalar
            def _(e):
                self._replay("act", e)

            @block.gpsimd
            def _(e):
                self._replay("pool", e)


def _small_layout():
    off = {}
    n = 0

    def add(name, w):
        nonlocal n
        off[name] = (n, w)
        n += w
    for l in range(DEPTH):
        add("n1g%d" % l, 8)
        add("n2g%d" % l, 8)
        add("modb%d" % l, 48)
        add("cw4_%d" % l, 32)
        add("cb4_%d" % l, 8)
        for d in range(2):
            add("br%d%d" % (l, d), 8)
            add("bi%d%d" % (l, d), 8)
            add("lam%d%d" % (l, d), 8)
        add("cw31_%d" % l, 31 * 8)
        add("cb31_%d" % l, 8)
        add("lng%d" % l, 8)
        add("lnb%d" % l, 8)
    add("fing", 8)
    add("blkmask", NBLK)
    add("one", 1)
    add("boh", 2)
    add("cvec", 32)
    return off, n


SOFF, NS = _small_layout()


def _pc(v):
    return np.ascontiguousarray(np.asarray(v, np.float32).reshape(8, 128).T)


def _build_small(inp, core):
    b, q = core // 4, core % 4
    s = np.zeros((128, NS), np.float32)

    def put(name, arr):
        o, w = SOFF[name]
        s[:, o:o + w] = np.asarray(arr, np.float32).reshape(128, w)
    for l in range(DEPTH):
        put("n1g%d" % l, _pc(inp["norm1_g"][l]))
        put("n2g%d" % l, _pc(inp["norm2_g"][l]))
        put("modb%d" % l, inp["mod_b"][l].reshape(48, 128).T)
        put("cw4_%d" % l, np.concatenate([_pc(inp["rnn_conv_w"][l][j]) for j in range(4)], axis=1))
        put("cb4_%d" % l, _pc(inp["rnn_conv_b"][l]))
        for d in range(2):
            put("br%d%d" % (l, d), _pc(inp["lru_br"][l][d]))
            put("bi%d%d" % (l, d), _pc(inp["lru_bi"][l][d]))
            put("lam%d%d" % (l, d), _pc(inp["lru_lam"][l][d]))
        put("cw31_%d" % l, np.concatenate([_pc(inp["conv_w"][l][j]) for j in range(31)], axis=1))
        put("cb31_%d" % l, _pc(inp["conv_b"][l]))
        put("lng%d" % l, _pc(inp["conv_ln_g"][l]))
        put("lnb%d" % l, _pc(inp["conv_ln_b"][l]))
    put("fing", _pc(inp["final_g"]))
    bm = np.zeros((128, NBLK), np.float32)
    for lb in range(NBLK):
        g0 = q * 4096 + (lb - 4) * NB
        bm[:, lb] = 1.0 if (0 <= g0 < 16384) else 0.0
    put("blkmask", bm)
    put("one", np.ones((128, 1), np.float32))
    boh = np.zeros((128, 2), np.float32)
    boh[:, b] = 1.0
    put("boh", boh)
    cv = np.zeros((128, 8, 4), np.float32)
    cv[:, :, 0] = _pc(inp["c"][0])
    cv[:, :, 1] = _pc(inp["c"][1])
    cv[:, :, 2] = _pc(inp["c_ctx"])
    put("cvec", cv.reshape(128, 32))
    return s


class G:
    pass


def build_program():
    nc = bass.Bass("TRN2", target_bir_lowering=False)
    P = Prog(nc)
    g = G()
    g.nc, g.P = nc, P

    def din(name, shape, dt=F32):
        return nc.dram_tensor(name, list(shape), dt, kind="ExternalInput").ap()

    def dscr(name, shape, dt=F32):
        kind = "ExternalOutput" if name in DEBUG_OUT else "Internal"
        return nc.dram_tensor(name, list(shape), dt, kind=kind).ap()

    g.x_loc = din("x_loc", [TL, D])
    g.ctx_in = din("ctx_in", [CTX, D])
    g.small_in = din("small", [128, NS])
    g.modw_in = din("modw", [DEPTH, D, 1536])
    g.win_in = din("w_in", [DEPTH * D, 6144])
    g.wa_in = din("w_a", [DEPTH * D, D])
    g.wb_in = din("w_b", [DEPTH * D, D])
    g.wo_in = din("w_o", [DEPTH * D, D])
    g.f1_in = din("ffn_w1", [D, DFF])
    g.f3_in = din("ffn_w3", [D, DFF])
    g.f2_in = din("ffn_w2", [DFF, D])
    if not NO_MOE:
        g.m1_in = din("moe_w1", [NE * D, DFF])
        g.m3_in = din("moe_w3", [NE * D, DFF])
        g.m2_in = din("moe_w2", [NE * DFF, D])
    g.gw_in = din("gatew", [DEPTH * 2 * 2 * 8 * 128, 128])
    g.rt_in = din("router", [D, NE])
    g.out = nc.dram_tensor("out", [8 * NB, D], F32, kind="ExternalOutput").ap()

    g.win = dscr("win_bf", [DEPTH * D, 6144], BF16)
    g.wa = dscr("wa_bf", [DEPTH * D, D], BF16)
    g.wb = dscr("wb_bf", [DEPTH * D, D], BF16)
    g.wo = dscr("wo_bf", [DEPTH * D, D], BF16)
    g.f1 = dscr("f1_bf", [D, DFF], BF16)
    g.f3 = dscr("f3_bf", [D, DFF], BF16)
    g.f2 = dscr("f2_bf", [DFF, D], BF16)
    g.m1 = dscr("m1_bf", [NE * D, DFF], BF16)
    g.m3 = dscr("m3_bf", [NE * D, DFF], BF16)
    g.m2 = dscr("m2_bf", [NE * DFF, D], BF16)
    g.gw = dscr("gw_bf", [DEPTH * 2 * 2 * 8 * 128, 128], BF16)

    def seg_arrays(pfx, T):
        a = {}
        for nm, dt in (("XT", F32), ("HX", BF16), ("U", F32), ("G", BF16), ("V", BF16),
                       ("SGA", BF16), ("SGB", BF16), ("S", F32), ("AF", BF16), ("AB", BF16),
                       ("CV", F32)):
            a[nm] = dscr(pfx + nm, [KT, 128, T], dt)
        return a
    g.lat = seg_arrays("L_", TL)
    g.ctxa = seg_arrays("C_", TC)
    g.comb = dscr("COMB", [NE, 128, TL], F32)
    g.sum_in = dscr("sum_in", [128, 256], F32)
    g.sum_all = dscr("sum_all", [4 * 128, 256], F32)
    g.mod_in = dscr("mod_in", [128, 96], F32)
    g.mod_all = dscr("mod_all", [4 * 128, 96], F32)
    g.htab = dscr("htab", [2, 128, KT, 48], F32)
    g.ex_in = dscr("ex_in", [8, 128, 2048], F32)
    g.ex_all = [dscr("ex_all%d" % u, [4, 128, 2048], F32) for u in range(8)]

    def sb(name, shape, dt=F32):
        return nc.alloc_sbuf_tensor("sb_" + name, list(shape), dt)
    g.small = sb("small", [128, NS])
    g.ident = sb("ident", [128, 128])
    g.ones_b = sb("ones_b", [128, 128], BF16)
    g.zeros = sb("zeros", [128, NB])
    g.modx = sb("modx", [128, DEPTH, 48])
    g.modc = sb("modc", [128, DEPTH, 48])
    g.nrm = sb("nrm", [128, DEPTH, 2, 4, 8])
    g.cl = sb("cl", [128, DEPTH, 2, 2, 8])
    g.sumt = sb("sumt", [128, KT, 8, 4])
    g.sctx = sb("sctx", [128, 2, KT])
    g.hin = sb("hin", [128, 2, KT, 12])
    g.hzero = sb("hzero", [128, 2, KT, 12])
    g.ps = [nc.alloc_psum_tensor("psb%d" % i, [128, NB], F32) for i in range(8)]
    return g


def S(g, name, w=None):
    o, ww = SOFF[name]
    return g.small[:, o:o + (ww if w is None else w)]


def Scol(g, name, i):
    o, _ = SOFF[name]
    return g.small[:, o + i:o + i + 1]


def dynval(g, e, name):
    if not hasattr(g, "_dyn"):
        pid = e.partition_id()
        g._dyn = {
            "rl": e.snap((pid + 3) % 4, min_val=0, max_val=3),
            "rr": e.snap((pid + 1) % 4, min_val=0, max_val=3),
            "b0": e.snap((pid % 4) * 8 + 6, min_val=6, max_val=30),
            "b1": e.snap((pid % 4) * 8 + 7, min_val=7, max_val=31),
        }
    return g._dyn[name]


class Stage:
    def __init__(self, g):
        self.g = g
        self.es = ExitStack()

    def __enter__(self):
        self.g.P.barrier()
        return self

    def __exit__(self, *a):
        self.g.P.barrier()
        self.es.close()
        return False

    def sb(self, base, shape, dt=F32):
        nm = self.g.P.name(base)
        t = self.es.enter_context(self.g.nc.sbuf_tensor(nm, list(shape), dt))
        return t, nm

    def ring(self, base, shape, dt, n):
        return [self.sb(base, shape, dt) for _ in range(n)]


def blk_view(arr, s, n):
    return arr[:, :, s:s + n].rearrange("c p t -> p c t")


def st_setup(g):
    P, nc = g.P, g.nc
    with Stage(g) as st:
        P.dma("sp", g.small[:], g.small_in, reads=[], writes=["small"])
        P.op("pool", lambda e: e.memset(g.ident[:], 0.0), writes=["ident"])
        P.op("pool", lambda e: e.affine_select(out=g.ident[:], in_=g.ident[:], pattern=[[-1, 128]],
                                               compare_op=ALU.not_equal, fill=1.0, base=0,
                                               channel_multiplier=1),
             reads=["ident"], writes=["ident"])
        P.op("pool", lambda e: e.memset(g.ones_b[:], 1.0), writes=["ones_b"])
        P.op("pool", lambda e: e.memset(g.zeros[:], 0.0), writes=["zeros"])
        P.op("pool", lambda e: e.memset(g.hzero[:], 0.0), writes=["hzero"])
        zt, zk = st.sb("zt", [128, KT, TC], F32)
        zb, zbk = st.sb("zb", [128, KT, TC], BF16)
        P.op("dve", lambda e: e.memset(zt[:], 0.0), writes=[zk])
        P.op("dve", lambda e: e.memset(zb[:], 0.0), writes=[zbk])
        P.dma("sp", blk_view(g.ctxa["U"], 0, TC), zt[:], reads=[zk], writes=["C_U"])
        P.dma("sp", blk_view(g.ctxa["V"], 0, TC), zb[:], reads=[zbk], writes=["C_V"])
        sc, sck = st.sb("sc", [128, 8, 4], F32)
        P.op("act", lambda e: e.activation(out=sc[:].rearrange("p a b -> p (a b)"), in_=S(g, "cvec"),
                                           func=AF.Silu), reads=["small"], writes=[sck])
        mw = st.ring("mw", [128, 8, 1536], F32, 1)
        mo, mok = st.sb("mo", [128, 96], F32)
        psm = g.ps[0]
        for l in range(DEPTH):
            t, tk = mw[0]
            P.dma("sp", t[:], g.modw_in[l].rearrange("(k p) n -> p k n", p=128), reads=[], writes=[tk])
            for c in range(12):
                for k in range(8):
                    P.op("pe", lambda e, t=t, c=c, k=k, l=l: e.matmul(
                        psm[:, (l * 12 + c) * 4:(l * 12 + c) * 4 + 4], lhsT=t[:, k, c * 128:(c + 1) * 128],
                        rhs=sc[:, k, :], start=(k == 0), stop=(k == 7)),
                        reads=[tk, sck], writes=["ps0"], sig=(k == 7))
        P.op("dve", lambda e: e.tensor_copy(out=mo[:], in_=psm[:, 0:96]), reads=["ps0"], writes=[mok])
        P.dma("sp", g.mod_in, mo[:], reads=[mok], writes=["mod_in"])
        P.collective("AllGather", ins=[g.mod_in], outs=[g.mod_all], groups=[[0, 1, 2, 3], [4, 5, 6, 7]],
                     reads=["mod_in"], writes=["mod_all"])
        ma, mak = st.sb("ma", [128, DEPTH, 4, 12, 4], F32)
        for l in range(DEPTH):
            P.dma("sp", ma[:, l], g.mod_all.rearrange("(r p) (l c j) -> p l r c j", p=128, l=DEPTH, c=12)[:, l],
                  reads=["mod_all"], writes=[mak])
        for l in range(DEPTH):
            v = ma[:, l].rearrange("p r c j -> p (r c) j")
            mb = S(g, "modb%d" % l)
            P.op("dve", lambda e, v=v, l=l: e.tensor_single_scalar(out=g.modx[:, l, :], in_=v[:, :, 0],
                                                            scalar=Scol(g, "boh", 0), op=ALU.mult),
                 reads=[mak, "small"], writes=["modx"])
            P.op("dve", lambda e, v=v, l=l: e.scalar_tensor_tensor(out=g.modx[:, l, :], in0=v[:, :, 1],
                                                                   scalar=Scol(g, "boh", 1), in1=g.modx[:, l, :],
                                                                   op0=ALU.mult, op1=ALU.add),
                 reads=[mak, "small", "modx"], writes=["modx"])
            P.op("dve", lambda e, l=l, mb=mb: e.tensor_tensor(out=g.modx[:, l, :], in0=g.modx[:, l, :], in1=mb, op=ALU.add),
                 reads=["modx", "small"], writes=["modx"])
            P.op("dve", lambda e, v=v, l=l, mb=mb: e.tensor_tensor(out=g.modc[:, l, :], in0=v[:, :, 2], in1=mb, op=ALU.add),
                 reads=[mak, "small"], writes=["modc"])
            for si, mv in enumerate((g.modx, g.modc)):
                P.op("dve", lambda e, l=l, si=si, mv=mv: e.scalar_tensor_tensor(
                    out=g.nrm[:, l, si, 0, :], in0=mv[:, l, 8:16], scalar=1.0, in1=S(g, "n1g%d" % l),
                    op0=ALU.add, op1=ALU.mult), reads=["modx", "modc", "small"], writes=["nrm"])
                P.op("dve", lambda e, l=l, si=si, mv=mv: e.scalar_tensor_tensor(
                    out=g.nrm[:, l, si, 1, :], in0=mv[:, l, 32:40], scalar=1.0, in1=S(g, "n2g%d" % l),
                    op0=ALU.add, op1=ALU.mult), reads=["modx", "modc", "small", "nrm"], writes=["nrm"])
            for d in range(2):
                tmp, tmk = st.sb("cltmp", [128, 8], F32)
                P.op("act", lambda e, l=l, d=d, tmp=tmp: e.activation(out=tmp[:], in_=S(g, "lam%d%d" % (l, d)),
                                                                      func=AF.Exp, scale=-1.0),
                     reads=["small"], writes=[tmk])
                P.op("act", lambda e, tmp=tmp: e.activation(out=tmp[:], in_=tmp[:], func=AF.Ln, bias=1.0),
                     reads=[tmk], writes=[tmk])
                P.op("dve", lambda e, l=l, d=d, tmp=tmp: e.tensor_single_scalar(out=g.cl[:, l, d, 0, :], in_=tmp[:],
                                                                         scalar=-8.0, op=ALU.mult),
                     reads=[tmk], writes=["cl"])
                P.op("dve", lambda e, l=l, d=d, tmp=tmp: e.tensor_single_scalar(out=g.cl[:, l, d, 1, :], in_=tmp[:],
                                                                         scalar=-16.0, op=ALU.mult),
                     reads=[tmk, "cl"], writes=["cl"])


def modvec(g, l, si, which):
    mv = g.modx if si == 0 else g.modc
    if which == "A1":
        return g.nrm[:, l, si, 0, :]
    if which == "A2":
        return g.nrm[:, l, si, 1, :]
    return {"B1": mv[:, l, 0:8], "G1": mv[:, l, 16:24], "B2": mv[:, l, 24:32], "G2": mv[:, l, 40:48]}[which]


def st_convert(g, which):
    P = g.P
    if which == "mix":
        pairs = [(g.win_in, g.win), (g.wa_in, g.wa), (g.wb_in, g.wb), (g.wo_in, g.wo), (g.gw_in, g.gw),
                 (g.f1_in, g.f1), (g.f3_in, g.f3), (g.f2_in, g.f2)]
    else:
        pairs = [(g.m1_in, g.m1), (g.m3_in, g.m3), (g.m2_in, g.m2)]
    wi_ = 0
    W = 4096
    with Stage(g) as st:
        src = st.ring("cvs", [128, W], F32, 3)
        dst = st.ring("cvd", [128, W], BF16, 3)
        i = 0
        for a, b in pairs:
            av = a.rearrange("(p r) n -> p (r n)", p=128)
            bv = b.rearrange("(p r) n -> p (r n)", p=128)
            Fd = av.shape[1]
            for o in range(0, Fd, W):
                w = min(W, Fd - o)
                s, sk = src[i % 3]
                d, dk = dst[i % 3]
                P.dma("sp", s[:, 0:w], av[:, o:o + w], reads=[], writes=[sk])
                eng = ("act", "dve", "act")[i % 3]
                if eng == "act":
                    P.op("act", lambda e, s=s, d=d, w=w: e.copy(out=d[:, 0:w], in_=s[:, 0:w]), reads=[sk], writes=[dk])
                else:
                    P.op(eng, lambda e, s=s, d=d, w=w: e.tensor_copy(out=d[:, 0:w], in_=s[:, 0:w]), reads=[sk], writes=[dk])
                P.dma("pool", bv[:, o:o + w], d[:, 0:w], reads=[dk], writes=[("wcv", id(b), o)])
                i += 1


def conv_pieces(g, which, W=4096):
    if which == "mix":
        pairs = [(g.win_in, g.win), (g.wa_in, g.wa), (g.wb_in, g.wb), (g.wo_in, g.wo), (g.gw_in, g.gw),
                 (g.f1_in, g.f1), (g.f3_in, g.f3), (g.f2_in, g.f2)]
    else:
        pairs = [(g.m1_in, g.m1), (g.m3_in, g.m3), (g.m2_in, g.m2)]
    out = []
    for a, b in pairs:
        av = a.rearrange("(p r) n -> p (r n)", p=128)
        bv = b.rearrange("(p r) n -> p (r n)", p=128)
        Fd = av.shape[1]
        for o in range(0, Fd, W):
            out.append((av, bv, o, min(W, Fd - o), id(b)))
    return out


def bg_convert(g, st, pieces, eng="dve", W=4096):
    P = g.P
    src = st.ring("bgs", [128, W], F32, 3)
    dst = st.ring("bgd", [128, W], BF16, 3)
    for i, (av, bv, o, w, bid) in enumerate(pieces):
        s_, sk = src[i % 3]
        d_, dk = dst[i % 3]
        P.dma("sp", s_[:, 0:w], av[:, o:o + w], reads=[], writes=[sk])
        if eng == "act":
            P.op("act", lambda e, s_=s_, d_=d_, w=w: e.copy(out=d_[:, 0:w], in_=s_[:, 0:w]), reads=[sk], writes=[dk])
        else:
            P.op(eng, lambda e, s_=s_, d_=d_, w=w: e.tensor_copy(out=d_[:, 0:w], in_=s_[:, 0:w]), reads=[sk], writes=[dk])
        P.dma("pool", bv[:, o:o + w], d_[:, 0:w], reads=[dk], writes=[("wcv", bid, o)])
        yield


def st_transpose_in(g):
    P = g.P
    with Stage(g) as st:
        xin = st.ring("xin", [128, 4, D], F32, 2)
        xtb = st.ring("xtb", [128, KT, NB], F32, 2)
        jobs = [("L", lb) for lb in range(2, 14)] + [("C", 0)]
        for ji, (kind, lb) in enumerate(jobs):
            xi, xik = xin[ji % 2]
            xo, xok = xtb[ji % 2]
            if kind == "L":
                na = 4
                P.dma("sp", xi[:], g.x_loc[lb * NB:(lb + 1) * NB, :].rearrange("(a p) d -> p a d", p=128), writes=[xik])
            else:
                na = 2
                P.dma("sp", xi[:, 0:2], g.ctx_in.rearrange("(a p) d -> p a d", p=128), writes=[xik])
            for c in range(KT):
                ps = g.ps[c]
                for a in range(na):
                    P.op("pe", lambda e, ps=ps, xi=xi, a=a, c=c: e.transpose(
                        out=ps[:, a * 128:(a + 1) * 128], in_=xi[:, a, c * 128:(c + 1) * 128], identity=g.ident[:]),
                        reads=[xik, "ident"], writes=["ps%d" % c])
                n = na * 128
                if c % 2 == 0:
                    P.op("act", lambda e, ps=ps, xo=xo, c=c, n=n: e.copy(out=xo[:, c, 0:n], in_=ps[:, 0:n]),
                         reads=["ps%d" % c], writes=[(xok, c)])
                else:
                    P.op("dve", lambda e, ps=ps, xo=xo, c=c, n=n: e.tensor_copy(out=xo[:, c, 0:n], in_=ps[:, 0:n]),
                         reads=["ps%d" % c], writes=[(xok, c)])
            rk = [(xok, c) for c in range(KT)]
            if kind == "L":
                P.dma("pool", blk_view(g.lat["XT"], lb * NB, NB), xo[:], reads=rk, writes=[("L_XT", lb)])
            else:
                P.dma("pool", blk_view(g.ctxa["XT"], CPAD, CTX), xo[:, :, 0:CTX], reads=rk, writes=[("C_XT", 0)])


def norm_core(g, st, xt, xtk, n, A, B, bufs, out_bf=None, out_f32=None, psi=7, sq_eng="act"):
    P = g.P
    sq, sqk = bufs["sq"]
    rs, rsk = bufs["rs"]
    ps = g.ps[psi]
    psk = "ps%d" % psi
    P.op("act", lambda e: e.activation(out=sq[:, :, 0:n], in_=xt[:, :, 0:n], func=AF.Square),
         reads=[xtk], writes=[sqk])
    for k in range(KT):
        P.op("pe", lambda e, k=k: e.matmul(ps[:, 0:n], lhsT=g.ones_b[:], rhs=sq[:, k, 0:n], start=(k == 0), stop=(k == 7)),
             reads=[sqk, "ones_b"], writes=[psk], sig=(k == 7))
    P.op("act", lambda e: e.activation(out=rs[:, 0:n], in_=ps[:, 0:n], func=AF.Sqrt, scale=1.0 / D, bias=EPS),
         reads=[psk], writes=[rsk])
    P.op("dve", lambda e: e.reciprocal(out=rs[:, 0:n], in_=rs[:, 0:n]), reads=[rsk], writes=[rsk])
    P.op("dve", lambda e: e.tensor_tensor(out=xt[:, :, 0:n], in0=xt[:, :, 0:n],
                                          in1=rs[:, 0:n].unsqueeze(1).to_broadcast([128, KT, n]), op=ALU.mult),
         reads=[xtk, rsk], writes=[xtk])
    for c in range(KT):
        eng = "act" if c % 2 == 0 else "dve"
        for (ot, wants) in ((out_bf, True), (out_f32, True)):
            if ot is None:
                continue
            o, ok = ot
            if B is None:
                if eng == "act":
                    P.op("act", lambda e, o=o, c=c: e.activation(out=o[:, c, 0:n], in_=xt[:, c, 0:n], func=AF.Identity,
                                                                 scale=A[:, c:c + 1]),
                         reads=[xtk, "nrm", "small"], writes=[(ok, c)])
                else:
                    P.op("dve", lambda e, o=o, c=c: e.tensor_single_scalar(out=o[:, c, 0:n], in_=xt[:, c, 0:n],
                                                                    scalar=A[:, c:c + 1], op=ALU.mult),
                         reads=[xtk, "nrm", "small"], writes=[(ok, c)])
            else:
                if eng == "act":
                    P.op("act", lambda e, o=o, c=c: e.activation(out=o[:, c, 0:n], in_=xt[:, c, 0:n], func=AF.Identity,
                                                                 scale=A[:, c:c + 1], bias=B[:, c:c + 1]),
                         reads=[xtk, "nrm", "modx", "modc"], writes=[(ok, c)])
                else:
                    P.op("dve", lambda e, o=o, c=c: e.tensor_scalar(out=o[:, c, 0:n], in0=xt[:, c, 0:n],
                                                                    scalar1=A[:, c:c + 1], scalar2=B[:, c:c + 1],
                                                                    op0=ALU.mult, op1=ALU.add),
                         reads=[xtk, "nrm", "modx", "modc"], writes=[(ok, c)])


def seg_blocks(kind, lbs):
    if kind == "L":
        return [("L", lb * NB, NB, lb) for lb in lbs]
    return [("C", CPAD, CTX, 0)]


def arrs(g, kind):
    return g.lat if kind == "L" else g.ctxa


def st_norm(g, l, which, jobs, router=False):
    P = g.P
    with Stage(g) as st:
        xts = st.ring("nxt", [128, KT, NB], F32, 2)
        hxs = st.ring("nhx", [128, KT, NB], BF16, 2)
        bufs = {"sq": st.sb("nsq", [128, KT, NB], BF16), "rs": st.sb("nrs", [128, NB], F32)}
        if router:
            yfs = st.sb("nyf", [128, KT, NB], F32)
            rt, rtk = st.sb("rt", [128, KT, NE], F32)
            P.dma("sp", rt[:], g.rt_in.rearrange("(k p) e -> p k e", p=128), writes=[rtk])
            lg, lgk = st.sb("lg", [128, 4, NE], F32)
            mx, mxk = st.sb("mx", [128, 4, 8], F32)
            ex, exk = st.sb("ex", [128, 4, NE], F32)
            mk, mkk = st.sb("mk", [128, 4, NE], F32)
            dn, dnk = st.sb("dn", [128, 4], F32)
            nm1, nm1k = st.sb("nm1", [128, 4], F32)
            cbb, cbbk = st.sb("cbb", [128, NE, 128], F32)
            cmo = st.ring("cmo", [128, NE, NB], F32, 2)
        def job(ji, kind, s, n, lb):
            A = arrs(g, kind)
            si = 0 if kind == "L" else 1
            xt, xtk = xts[ji % 2]
            hx, hxk = hxs[ji % 2]
            P.dma("sp", xt[:, :, 0:n], blk_view(A["XT"], s, n), reads=[(kind + "_XT", lb)], writes=[xtk])
            Av = modvec(g, l, si, "A%d" % which)
            Bv = modvec(g, l, si, "B%d" % which)
            if router:
                norm_core(g, st, xt, xtk, n, Av, Bv, bufs, out_f32=yfs)
                yf, yfk = yfs
                P.op("pool", lambda e, hx=hx, yf=yf: e.tensor_copy(out=hx[:, :, 0:n], in_=yf[:, :, 0:n]),
                     reads=[(yfk, c) for c in range(KT)], writes=[(hxk, c) for c in range(KT)])
                psl = g.ps[6]
                for a in range(4):
                    for k in range(KT):
                        P.op("pe", lambda e, a=a, k=k, yf=yf: e.matmul(psl[:, a * 8:(a + 1) * 8],
                                                                      lhsT=yf[:, k, a * 128:(a + 1) * 128], rhs=rt[:, k, :],
                                                                      start=(k == 0), stop=(k == 7)),
                             reads=[(yfk, k), rtk], writes=["ps6"], sig=(k == 7))
                P.op("dve", lambda e: e.tensor_copy(out=lg[:].rearrange("p a e -> p (a e)"), in_=psl[:, 0:32]),
                     reads=["ps6"], writes=[lgk])
                for a in range(4):
                    P.op("dve", lambda e, a=a: e.max(out=mx[:, a, :], in_=lg[:, a, :]), reads=[lgk], writes=[mxk])
                P.op("dve", lambda e: e.tensor_single_scalar(out=nm1[:], in_=mx[:, :, 0], scalar=-1.0, op=ALU.mult),
                     reads=[mxk], writes=[nm1k])
                for a in range(4):
                    P.op("act", lambda e, a=a: e.activation(out=ex[:, a, :], in_=lg[:, a, :], func=AF.Exp,
                                                            bias=nm1[:, a:a + 1], scale=1.0),
                         reads=[lgk, nm1k], writes=[exk])
                    P.op("dve", lambda e, a=a: e.tensor_single_scalar(out=mk[:, a, :], in_=lg[:, a, :], scalar=mx[:, a, 1:2], op=ALU.is_ge),
                         reads=[lgk, mxk], writes=[mkk])
                P.op("dve", lambda e: e.tensor_tensor(out=ex[:], in0=ex[:], in1=mk[:], op=ALU.mult),
                     reads=[exk, mkk], writes=[exk])
                P.op("dve", lambda e: e.tensor_reduce(out=dn[:], in_=ex[:], axis=mybir.AxisListType.X, op=ALU.add),
                     reads=[exk], writes=[dnk])
                P.op("dve", lambda e: e.reciprocal(out=dn[:], in_=dn[:]), reads=[dnk], writes=[dnk])
                P.op("dve", lambda e: e.tensor_tensor(out=ex[:], in0=ex[:], in1=dn[:].unsqueeze(2).to_broadcast([128, 4, NE]),
                                                      op=ALU.mult), reads=[exk, dnk], writes=[exk])
                co, cok = cmo[ji % 2]
                for a in range(4):
                    for ee in range(NE):
                        P.op("dve", lambda e, a=a, ee=ee: e.tensor_scalar(out=cbb[:, ee, :], in0=g.ident[:], scalar1=g.zeros[:, 0:1],
                                                                          scalar2=ex[:, a, ee:ee + 1], op0=ALU.mult, op1=ALU.add),
                             reads=[exk, "ident", (cbbk, ee)], writes=[(cbbk, ee)])
                    for ee in range(NE):
                        pse = g.ps[ee % 4]
                        P.op("pe", lambda e, a=a, ee=ee, pse=pse: e.matmul(pse[:, 0:128], lhsT=cbb[:, ee, :], rhs=g.ident[:],
                                                                          start=True, stop=True),
                             reads=[(cbbk, ee), "ident"], writes=["ps%d" % (ee % 4)])
                        P.op("act", lambda e, a=a, ee=ee, pse=pse, co=co: e.copy(out=co[:, ee, a * 128:(a + 1) * 128],
                                                                                in_=pse[:, 0:128]),
                             reads=["ps%d" % (ee % 4)], writes=[(cok, ee)])
                P.dma("pool", g.comb[:, :, s:s + n].rearrange("e p t -> p e t"), co[:],
                      reads=[(cok, ee) for ee in range(NE)], writes=[("COMB", lb)])
            else:
                norm_core(g, st, xt, xtk, n, Av, Bv, bufs, out_bf=(hx, hxk))
            P.dma("pool", blk_view(A["HX"], s, n), hx[:, :, 0:n], reads=[(hxk, c) for c in range(KT)],
                  writes=[(kind + "_HX", lb)])
        for ji, (kind, s, n, lb) in enumerate(jobs):
            job(ji, kind, s, n, lb)


def load_w(g, st, name, wdram, row0, col0, ncols, kt=KT):
    t, k = st.sb(name, [128, kt, ncols], BF16)
    g.P.dma("sp", t[:], wdram[row0:row0 + kt * 128, col0:col0 + ncols].rearrange("(k p) n -> p k n", p=128),
            reads=[], writes=[k])
    return t, k


def st_zu(g, l, jobs):
    P = g.P
    with Stage(g) as st:
        w, wk = load_w(g, st, "wu", g.win, l * D, 0, 1024)
        hxs = st.ring("zhx", [128, KT, NB], BF16, 2)
        ub = st.ring("zub", [128, KT, NB], F32, 2)
        def job(ji, kind, s, n, lb):
            A = arrs(g, kind)
            hx, hxk = hxs[ji % 2]
            u, uk = ub[ji % 2]
            P.dma("sp", hx[:, :, 0:n], blk_view(A["HX"], s, n), reads=[(kind + "_HX", lb)], writes=[hxk])
            msk = Scol(g, "blkmask", lb) if kind == "L" else Scol(g, "one", 0)
            for m in range(KT):
                ps = g.ps[m % 4]
                for k in range(KT):
                    P.op("pe", lambda e, ps=ps, m=m, k=k, hx=hx: e.matmul(ps[:, 0:n], lhsT=w[:, k, m * 128:(m + 1) * 128],
                                                                         rhs=hx[:, k, 0:n], start=(k == 0), stop=(k == 7)),
                         reads=[wk, hxk], writes=["ps%d" % (m % 4)], sig=(k == 7))
                P.op("dve", lambda e, ps=ps, m=m, u=u, msk=msk: e.tensor_single_scalar(out=u[:, m, 0:n], in_=ps[:, 0:n], scalar=msk, op=ALU.mult),
                     reads=["ps%d" % (m % 4), "small"], writes=[(uk, m)])
            P.dma("pool", blk_view(A["U"], s, n), u[:, :, 0:n], reads=[(uk, m) for m in range(KT)],
                  writes=[(kind + "_U", lb)])
        for ji, (kind, s, n, lb) in enumerate(jobs):
            job(ji, kind, s, n, lb)


def st_zgm(g, l, jobs, bg_pieces=None, bg_every=2):
    P = g.P
    with Stage(g) as st:
        wg, wgk = load_w(g, st, "wg", g.win, l * D, 1024, 1024)
        wm, wmk = load_w(g, st, "wm", g.win, l * D, 4096, 2048)
        hxs = st.ring("zhx", [128, KT, NB], BF16, 2)
        ob = st.ring("zgo", [128, 3 * KT, NB], BF16, 2)
        bg = bg_convert(g, st, bg_pieces) if bg_pieces else None
        bgc = 0
        pi = 0
        def job(ji, kind, s, n, lb):
            nonlocal pi, bgc
            A = arrs(g, kind)
            hx, hxk = hxs[ji % 2]
            o, ok = ob[ji % 2]
            P.dma("sp", hx[:, :, 0:n], blk_view(A["HX"], s, n), reads=[(kind + "_HX", lb)], writes=[hxk])
            for m in range(3 * KT):
                ps = g.ps[pi % 6]
                psk = "ps%d" % (pi % 6)
                pi += 1
                wt, wtk, mm = (wg, wgk, m) if m < KT else (wm, wmk, m - KT)
                for k in range(KT):
                    P.op("pe", lambda e, ps=ps, wt=wt, mm=mm, k=k, hx=hx: e.matmul(
                        ps[:, 0:n], lhsT=wt[:, k, mm * 128:(mm + 1) * 128], rhs=hx[:, k, 0:n], start=(k == 0), stop=(k == 7)),
                        reads=[wtk, hxk], writes=[psk], sig=(k == 7))
                fn = AF.Gelu_apprx_tanh if m < KT else AF.Sigmoid
                P.op("act", lambda e, ps=ps, o=o, m=m, fn=fn: e.activation(out=o[:, m, 0:n], in_=ps[:, 0:n], func=fn),
                     reads=[psk], writes=[(ok, m)])
                if bg is not None:
                    bgc += 1
                    if bgc % bg_every == 0:
                        next(bg, None)
            P.dma("pool", blk_view(A["G"], s, n), o[:, 0:KT, 0:n], reads=[(ok, m) for m in range(KT)],
                  writes=[(kind + "_G", lb)])
            P.dma("pool", blk_view(A["SGA"], s, n), o[:, KT:2 * KT, 0:n], reads=[(ok, m) for m in range(KT, 2 * KT)],
                  writes=[(kind + "_SGA", lb)])
            P.dma("pool", blk_view(A["SGB"], s, n), o[:, 2 * KT:3 * KT, 0:n], reads=[(ok, m) for m in range(2 * KT, 3 * KT)],
                  writes=[(kind + "_SGB", lb)])
        for ji, (kind, s, n, lb) in enumerate(jobs):
            job(ji, kind, s, n, lb)
        if bg is not None:
            for _ in bg:
                pass


def st_zv(g, l, jobs, bg_pieces=None, bg_every=2):
    P = g.P
    with Stage(g) as st:
        w, wk = load_w(g, st, "wv", g.win, l * D, 2048, 2048)
        hxs = st.ring("zhx", [128, KT, NB], BF16, 2)
        vb = st.ring("zvb", [128, KT, NB], BF16, 2)
        sgs = st.ring("zsg", [128, NB], F32, 3)
        bg = bg_convert(g, st, bg_pieces, eng="act") if bg_pieces else None
        bgc = 0
        pi = 0
        def job(ji, kind, s, n, lb):
            nonlocal pi, bgc
            A = arrs(g, kind)
            hx, hxk = hxs[ji % 2]
            v, vk = vb[ji % 2]
            P.dma("sp", hx[:, :, 0:n], blk_view(A["HX"], s, n), reads=[(kind + "_HX", lb)], writes=[hxk])
            msk = Scol(g, "blkmask", lb) if kind == "L" else Scol(g, "one", 0)
            for m in range(KT):
                pa, pb = g.ps[(2 * pi) % 6], g.ps[(2 * pi + 1) % 6]
                pak, pbk = "ps%d" % ((2 * pi) % 6), "ps%d" % ((2 * pi + 1) % 6)
                sg, sgk = sgs[pi % 3]
                pi += 1
                for (ps, psk, mm) in ((pa, pak, m), (pb, pbk, KT + m)):
                    for k in range(KT):
                        P.op("pe", lambda e, ps=ps, mm=mm, k=k, hx=hx: e.matmul(
                            ps[:, 0:n], lhsT=w[:, k, mm * 128:(mm + 1) * 128], rhs=hx[:, k, 0:n], start=(k == 0), stop=(k == 7)),
                            reads=[wk, hxk], writes=[psk], sig=(k == 7))
                P.op("act", lambda e, pb=pb, sg=sg: e.activation(out=sg[:, 0:n], in_=pb[:, 0:n], func=AF.Sigmoid),
                     reads=[pbk], writes=[sgk])
                P.op("dve", lambda e, pa=pa, sg=sg, v=v, m=m, msk=msk: e.scalar_tensor_tensor(
                    out=v[:, m, 0:n], in0=pa[:, 0:n], scalar=msk, in1=sg[:, 0:n], op0=ALU.mult, op1=ALU.mult),
                    reads=[pak, sgk, "small"], writes=[(vk, m)])
                if bg is not None:
                    bgc += 1
                    if bgc % bg_every == 0:
                        next(bg, None)
            P.dma("pool", blk_view(A["V"], s, n), v[:, :, 0:n], reads=[(vk, m) for m in range(KT)],
                  writes=[(kind + "_V", lb)])
        for ji, (kind, s, n, lb) in enumerate(jobs):
            job(ji, kind, s, n, lb)
        if bg is not None:
            for _ in bg:
                pass


def st_scan(g, l, jobs):
    P = g.P
    with Stage(g) as st:
        gw, gwk = st.sb("gw", [128, 2, 2, KT, 128], BF16)
        P.dma("sp", gw[:].rearrange("p d t c m -> p (d t c) m"),
              g.gw[l * 4096:(l + 1) * 4096, :].rearrange("(x p) m -> p x m", p=128), reads=["gw_bf"], writes=[gwk])
        uhs = st.ring("uh", [128, KT, NB + 3], F32, 2)
        ucs = st.ring("uc", [128, KT, NB], F32, 2)
        ucb, ucbk = st.sb("ucb", [128, KT, NB], BF16)
        so = st.ring("so", [128, KT, NB], F32, 2)
        afo = st.ring("afo", [128, KT, NB], BF16, 2)
        abo = st.ring("abo", [128, KT, NB], BF16, 2)
        R = lambda nm, k=4: st.ring(nm, [128, NB], F32, k)
        rr, gi, aa, a2, bb, hh = R("rr"), R("gi"), R("aa"), R("a2"), R("bb"), R("hh", 4)
        rsm = st.ring("rsm", [128, 2], F32, 4)
        cw = S(g, "cw4_%d" % l)
        cb = S(g, "cb4_%d" % l)
        it = 0

        def job(ji, kind, s, n, lb):
            nonlocal it
            A = arrs(g, kind)
            uh, uhk = uhs[ji % 2]
            uc, uck = ucs[ji % 2]
            sot, sok = so[ji % 2]
            aft, afk = afo[ji % 2]
            abt, abk = abo[ji % 2]
            rks = [(kind + "_U", b) for b in ((lb - 1, lb, lb + 1) if kind == "L" else (0,))]
            P.dma("sp", uh[:, :, 0:n + 3], blk_view(A["U"], s - 2, n + 3), reads=rks, writes=[uhk])
            for c in range(KT):
                P.op("dve", lambda e, c=c: e.tensor_scalar(out=uc[:, c, 0:n], in0=uh[:, c, 0:n],
                                                          scalar1=cw[:, c:c + 1], scalar2=cb[:, c:c + 1],
                                                          op0=ALU.mult, op1=ALU.add),
                     reads=[uhk, "small"], writes=[(uck, c)])
                for j in range(1, 4):
                    P.op("dve", lambda e, c=c, j=j: e.scalar_tensor_tensor(
                        out=uc[:, c, 0:n], in0=uh[:, c, j:j + n], scalar=cw[:, j * 8 + c:j * 8 + c + 1], in1=uc[:, c, 0:n],
                        op0=ALU.mult, op1=ALU.add), reads=[uhk, "small", (uck, c)], writes=[(uck, c)])
                P.op("pool", lambda e, c=c: e.tensor_copy(out=ucb[:, c, 0:n], in_=uc[:, c, 0:n]),
                     reads=[(uck, c)], writes=[(ucbk, c)])
            def stA(c):
                nonlocal it
                T = []
                for d in range(2):
                    T.append((rr[it % 4], gi[it % 4], aa[it % 4], a2[it % 4], bb[it % 4], hh[it % 4], rsm[it % 4],
                              g.ps[(2 * it) % 8], "ps%d" % ((2 * it) % 8), g.ps[(2 * it + 1) % 8], "ps%d" % ((2 * it + 1) % 8)))
                    it += 1
                for d in range(2):
                    pr, prk, pi_, pik = T[d][7], T[d][8], T[d][9], T[d][10]
                    P.op("pe", lambda e, pr=pr, d=d, c=c: e.matmul(pr[:, 0:n], lhsT=gw[:, d, 0, c, :], rhs=ucb[:, c, 0:n],
                                                                  start=True, stop=True),
                         reads=[gwk, (ucbk, c)], writes=[prk])
                    P.op("pe", lambda e, pi_=pi_, d=d, c=c: e.matmul(pi_[:, 0:n], lhsT=gw[:, d, 1, c, :], rhs=ucb[:, c, 0:n],
                                                                    start=True, stop=True),
                         reads=[gwk, (ucbk, c)], writes=[pik])
                for d in range(2):
                    (r_, rk_), (gi_, gik_), _, _, _, _, (rs_, rsk_), pr, prk, pi_, pik = T[d]
                    br = Scol(g, "br%d%d" % (l, d), c)
                    bi = Scol(g, "bi%d%d" % (l, d), c)
                    P.op("act", lambda e, pr=pr, r_=r_, br=br, rs_=rs_: e.activation(out=r_[:, 0:n], in_=pr[:, 0:n], func=AF.Sigmoid, bias=br,
                                                                                accum_out=rs_[:, 0:1]),
                         reads=[prk, "small"], writes=[rk_, rsk_])
                    P.op("act", lambda e, pi_=pi_, gi_=gi_, bi=bi: e.activation(out=gi_[:, 0:n], in_=pi_[:, 0:n], func=AF.Sigmoid, bias=bi),
                         reads=[pik, "small"], writes=[gik_])
                for d in range(2):
                    (r_, rk_), _, (a_, ak_), _, _, _, (rs_, rsk_) = T[d][0:7]
                    P.op("act", lambda e, r_=r_, a_=a_, d=d, c=c: e.activation(out=a_[:, 0:n], in_=r_[:, 0:n], func=AF.Exp,
                                                                             scale=g.cl[:, l, d, 0, c:c + 1]),
                         reads=[rk_, "cl"], writes=[ak_])
                    if kind == "L" and 4 <= lb < 12:
                        P.op("act", lambda e, rs_=rs_, c=c, d=d: e.activation(
                            out=g.sumt[:, c, lb - 4, 2 * d:2 * d + 1], in_=rs_[:, 0:1], func=AF.Exp, scale=g.cl[:, l, d, 0, c:c + 1]),
                            reads=[rsk_, "cl"], writes=["sumt"])
                for d in range(2):
                    _, _, (a_, ak_), (a2_, a2k_) = T[d][0:4]
                    P.op("dve", lambda e, a_=a_, a2_=a2_: e.tensor_tensor(out=a2_[:, 0:n], in0=a_[:, 0:n], in1=a_[:, 0:n], op=ALU.mult),
                         reads=[ak_], writes=[a2k_])
                return T

            def stB(c, T):
                for d in range(2):
                    (a2_, a2k_) = T[d][3]
                    P.op("act", lambda e, a2_=a2_: e.activation(out=a2_[:, 0:n], in_=a2_[:, 0:n], func=AF.Sqrt, scale=-1.0, bias=1.0),
                         reads=[a2k_], writes=[a2k_])
                for d in range(2):
                    _, (gi_, gik_), (a_, ak_), (a2_, a2k_), (b_, bk_), (h_, hk_) = T[d][0:6]
                    P.op("pool", lambda e, gi_=gi_, c=c, b_=b_: e.tensor_tensor(out=b_[:, 0:n], in0=gi_[:, 0:n], in1=uc[:, c, 0:n], op=ALU.mult),
                         reads=[gik_, (uck, c)], writes=[bk_])
                    P.op("pool", lambda e, a2_=a2_, b_=b_: e.tensor_tensor(out=b_[:, 0:n], in0=b_[:, 0:n], in1=a2_[:, 0:n], op=ALU.mult),
                         reads=[bk_, a2k_], writes=[bk_])
                    At, Atk = (aft, afk) if d == 0 else (abt, abk)
                    if d == 0:
                        P.op("dve", lambda e, a_=a_, b_=b_, h_=h_: e.tensor_tensor_scan(out=h_[:, 0:n], data0=a_[:, 0:n], data1=b_[:, 0:n],
                                                                                       initial=0.0, op0=ALU.mult, op1=ALU.add),
                             reads=[ak_, bk_], writes=[hk_])
                        P.op("dve", lambda e, a_=a_, At=At, c=c: e.tensor_tensor_scan(out=At[:, c, 0:n], data0=a_[:, 0:n], data1=g.zeros[:, 0:n],
                                                                                     initial=1.0, op0=ALU.mult, op1=ALU.add),
                             reads=[ak_, "zeros"], writes=[(Atk, c)])
                        hfwd = (h_, hk_)
                        e0, e1 = n - 1, n
                    else:
                        P.op("dve", lambda e, a_=a_, b_=b_, h_=h_: e.tensor_tensor_scan(out=h_[:, 0:n][:, ::-1],
                                                                                       data0=a_[:, 0:n][:, ::-1], data1=b_[:, 0:n][:, ::-1],
                                                                                       initial=0.0, op0=ALU.mult, op1=ALU.add),
                             reads=[ak_, bk_], writes=[hk_])
                        P.op("dve", lambda e, a_=a_, At=At, c=c: e.tensor_tensor_scan(out=At[:, c, 0:n][:, ::-1], data0=a_[:, 0:n][:, ::-1],
                                                                                     data1=g.zeros[:, 0:n], initial=1.0, op0=ALU.mult, op1=ALU.add),
                             reads=[ak_, "zeros"], writes=[(Atk, c)])
                        hf_, hfk_ = hfwd
                        P.op("dve", lambda e, h_=h_, hf_=hf_, c=c: e.tensor_tensor(out=sot[:, c, 0:n], in0=hf_[:, 0:n], in1=h_[:, 0:n], op=ALU.add),
                             reads=[hk_, hfk_], writes=[(sok, c)])
                        e0, e1 = 0, 1
                    if kind == "L" and 4 <= lb < 12:
                        P.op("pool", lambda e, h_=h_, c=c, d=d, e0=e0, e1=e1: e.tensor_copy(
                            out=g.sumt[:, c, lb - 4, 2 * d + 1:2 * d + 2], in_=h_[:, e0:e1]), reads=[hk_, "sumt"], writes=["sumt"])
                    if kind == "C":
                        P.op("pool", lambda e, h_=h_, c=c, d=d, e0=e0, e1=e1: e.tensor_copy(
                            out=g.sctx[:, d, c:c + 1], in_=h_[:, e0:e1]), reads=[hk_, "sctx"], writes=["sctx"])

            Tn = stA(0)
            for c in range(KT):
                Tc = Tn
                if c + 1 < KT:
                    Tn = stA(c + 1)
                stB(c, Tc)
            P.dma("pool", blk_view(A["S"], s, n), sot[:, :, 0:n], reads=[(sok, c) for c in range(KT)], writes=[(kind + "_S", lb)])
            P.dma("pool", blk_view(A["AF"], s, n), aft[:, :, 0:n], reads=[(afk, c) for c in range(KT)], writes=[(kind + "_AF", lb)])
            P.dma("pool", blk_view(A["AB"], s, n), abt[:, :, 0:n], reads=[(abk, c) for c in range(KT)], writes=[(kind + "_AB", lb)])
        for ji, (kind, s, n, lb) in enumerate(jobs):
            job(ji, kind, s, n, lb)


def st_carry(g, l):
    P = g.P
    with Stage(g) as st:
        P.dma("sp", g.sum_in, g.sumt[:].rearrange("p c b f -> p (c b f)"), reads=["sumt"], writes=["sum_in"])
        P.collective("AllGather", ins=[g.sum_in], outs=[g.sum_all], groups=[[0, 1, 2, 3], [4, 5, 6, 7]],
                     reads=["sum_in"], writes=["sum_all"])
        sa, sak = st.sb("sa", [128, 4, KT, 8, 4], F32)
        P.dma("sp", sa[:], g.sum_all.rearrange("(r p) (c b f) -> p r c b f", p=128, c=KT, b=8), reads=["sum_all"], writes=[sak])
        ht, htk = st.sb("ht", [128, 2, KT, 48], F32)
        P.op("dve", lambda e: e.memset(ht[:], 0.0), writes=[htk])
        tmp, tmk = st.sb("ctmp", [128, KT], F32)
        P.op("dve", lambda e: e.tensor_copy(out=ht[:, 0, :, 8], in_=g.sctx[:, 0, :]), reads=["sctx", htk], writes=[htk])
        for gb in range(32):
            r, b = gb // 8, gb % 8
            P.op("dve", lambda e, r=r, b=b, gb=gb: e.tensor_tensor(out=tmp[:], in0=sa[:, r, :, b, 0], in1=ht[:, 0, :, 8 + gb], op=ALU.mult),
                 reads=[sak, htk], writes=[tmk])
            P.op("dve", lambda e, r=r, b=b, gb=gb: e.tensor_tensor(out=ht[:, 0, :, 9 + gb], in0=tmp[:], in1=sa[:, r, :, b, 1], op=ALU.add),
                 reads=[sak, tmk, htk], writes=[htk])
        P.op("dve", lambda e: e.tensor_copy(out=ht[:, 1, :, 40], in_=g.sctx[:, 1, :]), reads=["sctx", htk], writes=[htk])
        for gb in range(31, -1, -1):
            r, b = gb // 8, gb % 8
            P.op("dve", lambda e, r=r, b=b, gb=gb: e.tensor_tensor(out=tmp[:], in0=sa[:, r, :, b, 2], in1=ht[:, 1, :, 9 + gb], op=ALU.mult),
                 reads=[sak, htk], writes=[tmk])
            P.op("dve", lambda e, r=r, b=b, gb=gb: e.tensor_tensor(out=ht[:, 1, :, 8 + gb], in0=tmp[:], in1=sa[:, r, :, b, 3], op=ALU.add),
                 reads=[sak, tmk, htk], writes=[htk])
        P.dma("sp", g.htab.rearrange("d p c x -> p d c x"), ht[:], reads=[htk], writes=["htab"])

        def dyn(e, d):
            base = dynval(g, e, "b%d" % d)
            return e.dma_start(out=g.hin[:, d, :, :],
                               in_=g.htab[d:d + 1, :, :, bass.ds(base, 12)].rearrange("o p c x -> p (o c) x"))
        P.dma("sp", None, None, reads=["htab"], writes=["hin"], fn=lambda e: dyn(e, 0))
        P.dma("sp", None, None, reads=["htab"], writes=["hin"], fn=lambda e: dyn(e, 1))


def st_conv(g, l, kinds, bg_pieces=None):
    P = g.P
    with Stage(g) as st:
        dg = st.ring("dg", [128, 31, 128], BF16, 2)
        vrl = st.ring("vrl", [128, TL], BF16, 2)
        cvo = st.ring("cvo", [128, NB], F32, 3)
        bg = bg_convert(g, st, bg_pieces, eng="act") if bg_pieces else None
        oi = 0
        for c in range(KT):
            dgt, dgk = dg[c % 2]
            for j in range(31):
                P.op("dve", lambda e, j=j, c=c, dgt=dgt: e.tensor_single_scalar(out=dgt[:, j, :], in_=g.ident[:],
                                                                        scalar=Scol(g, "cw31_%d" % l, j * 8 + c), op=ALU.mult),
                     reads=["ident", "small", (dgk, j)], writes=[(dgk, j)])
            for (kind, lbs) in kinds:
                A = arrs(g, kind)
                vr, vrk = vrl[oi % 2]
                if kind == "L":
                    lo, hi = (lbs[0] - 2) * NB, (lbs[-1] + 3) * NB
                    P.dma("sp", vr[:, lo:hi], A["V"][c, :, lo:hi], reads=[("L_V", b) for b in range(lbs[0] - 2, lbs[-1] + 3)],
                          writes=[vrk])
                    stride = 64
                    blocks = [(lb * NB, NB, lb) for lb in lbs]
                else:
                    P.dma("sp", vr[:, 0:TC], A["V"][c, :, :], reads=[("C_V", 0), "C_V"], writes=[vrk])
                    stride = 1
                    blocks = [(CPAD, CTX, 0)]
                for (s, n, lb) in blocks:
                    ps = g.ps[oi % 4]
                    psk = "ps%d" % (oi % 4)
                    o, ok = cvo[oi % 3]
                    oi += 1
                    for j in range(31):
                        off = s + (j - 15) * stride
                        P.op("pe", lambda e, ps=ps, j=j, off=off, vr=vr, dgt=dgt, n=n: e.matmul(
                            ps[:, 0:n], lhsT=dgt[:, j, :], rhs=vr[:, off:off + n], start=(j == 0), stop=(j == 30)),
                            reads=[(dgk, j), vrk], writes=[psk], sig=(j == 30))
                    P.op("act", lambda e, ps=ps, o=o, n=n, c=c: e.activation(out=o[:, 0:n], in_=ps[:, 0:n], func=AF.Identity,
                                                                           bias=Scol(g, "cb31_%d" % l, c), scale=1.0),
                         reads=[psk, "small"], writes=[ok])
                    P.dma("pool", A["CV"][c, :, s:s + n], o[:, 0:n], reads=[ok], writes=[(kind + "_CV", lb, c)])
                    if bg is not None:
                        next(bg, None)
        if bg is not None:
            for _ in bg:
                pass


def st_mix(g, l, jobs):
    P = g.P
    H = 256
    with Stage(g) as st:
        wa, wak = load_w(g, st, "wa", g.wa, l * D, 0, D)
        wb, wbk = load_w(g, st, "wb", g.wb, l * D, 0, D)
        wo, wok = load_w(g, st, "wo", g.wo, l * D, 0, D)
        cvs = st.ring("mcv", [128, KT, H], F32, 2)
        ss = st.ring("ms", [128, KT, H], F32, 2)
        afs = st.ring("maf", [128, KT, H], BF16, 2)
        abs_ = st.ring("mab", [128, KT, H], BF16, 2)
        gs = st.ring("mg", [128, KT, H], BF16, 2)
        sgas = st.ring("msga", [128, KT, H], BF16, 2)
        sgbs = st.ring("msgb", [128, KT, H], BF16, 2)
        xts = st.ring("mxt", [128, KT, H], F32, 2)
        cvb, cvbk = st.sb("cvb", [128, KT, H], BF16)
        sqb, sqbk = st.sb("sqb", [128, KT, H], BF16)
        lno, lnok = st.sb("lno", [128, KT, H], BF16)
        mbt, mbk = st.sb("mbt", [128, KT, H], F32)
        hst, hsk = st.sb("hst", [128, KT, H], F32)
        hsb, hsbk = st.sb("hsb", [128, KT, H], BF16)
        mt, mtk = st.sb("mt", [128, KT, H], BF16)
        mu, muk = st.sb("mu", [128, H], F32)
        var, vark = st.sb("var", [128, H], F32)
        tmp1, tmp1k = st.sb("tmp1", [128, H], F32)
        lng, lnb = S(g, "lng%d" % l), S(g, "lnb%d" % l)
        it = 0
        pi = 0

        def nps():
            nonlocal pi
            r = (g.ps[pi % 6], "ps%d" % (pi % 6))
            pi += 1
            return r
        halves_ = []
        for (kind, s0, n0, lb) in jobs:
            A = arrs(g, kind)
            si = 0 if kind == "L" else 1
            def half_body(kind, lb, s0, n0, s, A, si):
                nonlocal it
                n = min(H, s0 + n0 - s)
                cv, cvk = cvs[it % 2]
                sst, ssk = ss[it % 2]
                af, afk = afs[it % 2]
                ab, abk = abs_[it % 2]
                gt, gk = gs[it % 2]
                sga, sgak = sgas[it % 2]
                sgb, sgbk = sgbs[it % 2]
                xt, xtk = xts[it % 2]
                it += 1
                def prep():
                    P.dma("sp", cv[:, :, 0:n], blk_view(A["CV"], s, n), reads=[(kind + "_CV", lb, c) for c in range(KT)], writes=[cvk])
                    P.dma("sp", sst[:, :, 0:n], blk_view(A["S"], s, n), reads=[(kind + "_S", lb)], writes=[ssk])
                    P.dma("sp", af[:, :, 0:n], blk_view(A["AF"], s, n), reads=[(kind + "_AF", lb)], writes=[afk])
                    P.dma("sp", ab[:, :, 0:n], blk_view(A["AB"], s, n), reads=[(kind + "_AB", lb)], writes=[abk])
                    P.dma("sp", gt[:, :, 0:n], blk_view(A["G"], s, n), reads=[(kind + "_G", lb)], writes=[gk])
                    P.dma("sp", sga[:, :, 0:n], blk_view(A["SGA"], s, n), reads=[(kind + "_SGA", lb)], writes=[sgak])
                    P.dma("sp", sgb[:, :, 0:n], blk_view(A["SGB"], s, n), reads=[(kind + "_SGB", lb)], writes=[sgbk])
                    P.op("act", lambda e, cv=cv: e.copy(out=cvb[:, :, 0:n], in_=cv[:, :, 0:n]), reads=[cvk], writes=[cvbk])
                    P.op("act", lambda e, cv=cv: e.activation(out=sqb[:, :, 0:n], in_=cv[:, :, 0:n], func=AF.Square), reads=[cvk], writes=[sqbk])
                    p1, p1k = g.ps[6], "ps6"
                    p2, p2k = g.ps[7], "ps7"
                    for k in range(KT):
                        P.op("pe", lambda e, k=k: e.matmul(p1[:, 0:n], lhsT=g.ones_b[:], rhs=cvb[:, k, 0:n], start=(k == 0), stop=(k == 7)),
                             reads=[cvbk, "ones_b"], writes=[p1k], sig=(k == 7))
                    for k in range(KT):
                        P.op("pe", lambda e, k=k: e.matmul(p2[:, 0:n], lhsT=g.ones_b[:], rhs=sqb[:, k, 0:n], start=(k == 0), stop=(k == 7)),
                             reads=[sqbk, "ones_b"], writes=[p2k], sig=(k == 7))
                    P.op("dve", lambda e: e.tensor_single_scalar(out=mu[:, 0:n], in_=p1[:, 0:n], scalar=1.0 / D, op=ALU.mult),
                         reads=[p1k], writes=[muk])
                    P.op("dve", lambda e: e.tensor_tensor(out=tmp1[:, 0:n], in0=mu[:, 0:n], in1=mu[:, 0:n], op=ALU.mult),
                         reads=[muk], writes=[tmp1k])
                    P.op("dve", lambda e: e.scalar_tensor_tensor(out=var[:, 0:n], in0=p2[:, 0:n], scalar=1.0 / D, in1=tmp1[:, 0:n],
                                                                 op0=ALU.mult, op1=ALU.subtract),
                         reads=[p2k, tmp1k], writes=[vark])
                    P.op("dve", lambda e: e.tensor_single_scalar(out=var[:, 0:n], in_=var[:, 0:n], scalar=0.0, op=ALU.max),
                         reads=[vark], writes=[vark])
                    P.op("act", lambda e: e.activation(out=var[:, 0:n], in_=var[:, 0:n], func=AF.Sqrt, bias=EPS, scale=1.0),
                         reads=[vark], writes=[vark])
                    P.op("dve", lambda e: e.reciprocal(out=var[:, 0:n], in_=var[:, 0:n]), reads=[vark], writes=[vark])
                    P.op("dve", lambda e, cv=cv: e.tensor_tensor(out=cv[:, :, 0:n], in0=cv[:, :, 0:n],
                                                                in1=mu[:, 0:n].unsqueeze(1).to_broadcast([128, KT, n]), op=ALU.subtract),
                         reads=[cvk, muk], writes=[cvk])
                    P.op("dve", lambda e, cv=cv: e.tensor_tensor(out=cv[:, :, 0:n], in0=cv[:, :, 0:n],
                                                                in1=var[:, 0:n].unsqueeze(1).to_broadcast([128, KT, n]), op=ALU.mult),
                         reads=[cvk, vark], writes=[cvk])
                    for c in range(KT):
                        P.op("act", lambda e, c=c, cv=cv: e.activation(out=lno[:, c, 0:n], in_=cv[:, c, 0:n], func=AF.Silu,
                                                                      scale=lng[:, c:c + 1], bias=lnb[:, c:c + 1]),
                             reads=[cvk, "small"], writes=[(lnok, c)])
                def main1():
                    for m in range(KT):
                        ps, psk = nps()
                        for k in range(KT):
                            P.op("pe", lambda e, ps=ps, m=m, k=k: e.matmul(ps[:, 0:n], lhsT=wb[:, k, m * 128:(m + 1) * 128], rhs=lno[:, k, 0:n],
                                                                          start=(k == 0), stop=(k == 7)),
                                 reads=[wbk, (lnok, k)], writes=[psk], sig=(k == 7))
                        P.op("dve", lambda e, ps=ps, m=m, sgb=sgb: e.tensor_tensor(out=mbt[:, m, 0:n], in0=ps[:, 0:n], in1=sgb[:, m, 0:n], op=ALU.mult),
                             reads=[psk, sgbk], writes=[(mbk, m)])
                def main2():
                    P.dma("sp", xt[:, :, 0:n], blk_view(A["XT"], s, n), reads=[(kind + "_XT", lb)], writes=[xtk])
                    hin = g.hin if kind == "L" else g.hzero
                    bidx = (lb - 2) if kind == "L" else 0
                    for c in range(KT):
                        P.op("dve", lambda e, c=c, af=af, sst=sst: e.scalar_tensor_tensor(
                            out=hst[:, c, 0:n], in0=af[:, c, 0:n], scalar=hin[:, 0, c, bidx:bidx + 1], in1=sst[:, c, 0:n],
                            op0=ALU.mult, op1=ALU.add), reads=[afk, ssk, "hin", "hzero"], writes=[(hsk, c)])
                        P.op("dve", lambda e, c=c, ab=ab: e.scalar_tensor_tensor(
                            out=hst[:, c, 0:n], in0=ab[:, c, 0:n], scalar=hin[:, 1, c, bidx:bidx + 1], in1=hst[:, c, 0:n],
                            op0=ALU.mult, op1=ALU.add), reads=[abk, (hsk, c), "hin", "hzero"], writes=[(hsk, c)])
                        P.op("pool", lambda e, c=c, gt=gt: e.tensor_tensor(out=hsb[:, c, 0:n], in0=hst[:, c, 0:n], in1=gt[:, c, 0:n], op=ALU.mult),
                             reads=[(hsk, c), gk], writes=[(hsbk, c)])
                    for m in range(KT):
                        ps, psk = nps()
                        for k in range(KT):
                            P.op("pe", lambda e, ps=ps, m=m, k=k: e.matmul(ps[:, 0:n], lhsT=wa[:, k, m * 128:(m + 1) * 128], rhs=hsb[:, k, 0:n],
                                                                          start=(k == 0), stop=(k == 7)),
                                 reads=[wak, (hsbk, k)], writes=[psk], sig=(k == 7))
                        P.op("dve", lambda e, ps=ps, m=m, sga=sga: e.tensor_tensor(out=hst[:, m, 0:n], in0=ps[:, 0:n], in1=sga[:, m, 0:n], op=ALU.mult),
                             reads=[psk, sgak], writes=[(hsk, m)])
                        P.op("pool", lambda e, m=m: e.tensor_tensor(out=mt[:, m, 0:n], in0=hst[:, m, 0:n], in1=mbt[:, m, 0:n], op=ALU.add),
                             reads=[(hsk, m), (mbk, m)], writes=[(mtk, m)])
                    g1 = modvec(g, l, si, "G1")
                    for m in range(KT):
                        ps, psk = nps()
                        for k in range(KT):
                            P.op("pe", lambda e, ps=ps, m=m, k=k: e.matmul(ps[:, 0:n], lhsT=wo[:, k, m * 128:(m + 1) * 128], rhs=mt[:, k, 0:n],
                                                                          start=(k == 0), stop=(k == 7)),
                                 reads=[wok, (mtk, k)], writes=[psk], sig=(k == 7))
                        P.op("dve", lambda e, ps=ps, m=m, xt=xt: e.scalar_tensor_tensor(
                            out=xt[:, m, 0:n], in0=ps[:, 0:n], scalar=g1[:, m:m + 1], in1=xt[:, m, 0:n], op0=ALU.mult, op1=ALU.add),
                            reads=[psk, xtk, "modx", "modc"], writes=[xtk])
                    P.dma("pool", blk_view(A["XT"], s, n), xt[:, :, 0:n], reads=[xtk], writes=[(kind + "_XT", lb)])
                return prep, main1, main2
            for s in range(s0, s0 + n0, H):
                halves_.append(half_body(kind, lb, s0, n0, s, A, si))
        halves_[0][0]()
        for i_ in range(len(halves_)):
            halves_[i_][1]()
            if i_ + 1 < len(halves_):
                halves_[i_ + 1][0]()
            halves_[i_][2]()


def st_ffn(g, l, sblocks, moe, publish=False):
    P = g.P
    CH = 256
    NCH = DFF // CH
    TB = 1024
    with Stage(g) as st:
        hxs = st.ring("fhx", [128, KT, TB], BF16, 1)
        acc, acck = st.sb("facc", [128, KT, TB], F32)
        cmb = st.ring("fcmb", [128, TB], F32, 2)
        w1s = st.ring("fw1", [128, KT, 2 * CH], BF16, 3)
        w3s = st.ring("fw3", [128, KT, 2 * CH], BF16, 3)
        w2s = st.ring("fw2", [128, 4, D], BF16, 3)
        sil = st.ring("fsil", [128, NB], BF16, 3)
        gts = st.ring("fgt", [128, 4, NB], BF16, 2)
        xts = st.ring("fxt", [128, KT, NB], F32, 2)
        ne = NE if moe else 1
        wi = 0
        hi_ = 0
        gi_ = 0
        pi = 0
        groups = [(c0, min(2, NCH - c0)) for c0 in range(0, NCH, 2)]
        def sb_body(kind, s0, n0, lbs):
            nonlocal wi, hi_, gi_, pi
            A = arrs(g, kind)
            si = 0 if kind == "L" else 1
            hx, hxk = hxs[0]
            P.dma("sp", hx[:, :, 0:n0], blk_view(A["HX"], s0, n0), reads=[(kind + "_HX", lb) for lb in lbs], writes=[hxk])
            halves = [(o, min(NB, n0 - o)) for o in range(0, n0, NB)]
            steps = [(ex, gi2, c0, nc_, ho, hn) for ex in range(ne) for gi2, (c0, nc_) in enumerate(groups) for (ho, hn) in halves]
            loaded = {}
            cms = {}

            def ensure(ex, gi2, c0, nc_):
                nonlocal wi
                if moe and ex not in cms:
                    cm, cmk = cmb[ex % 2]
                    P.dma("sp", cm[:, 0:n0], g.comb[ex, :, s0:s0 + n0], reads=[("COMB", lb) for lb in lbs], writes=[cmk])
                    cms[ex] = (cm, cmk)
                if (ex, gi2) in loaded:
                    return
                if moe:
                    W1, W3, W2 = g.m1, g.m3, g.m2
                    r1, r2 = ex * D, ex * DFF
                else:
                    W1, W3, W2 = g.f1, g.f3, g.f2
                    r1, r2 = 0, 0
                w1, w1k = w1s[wi % 3]
                w3, w3k = w3s[wi % 3]
                w2, w2k = w2s[wi % 3]
                wi += 1
                cw_ = nc_ * CH
                nj = nc_ * 2
                P.dma("sp", w1[:, :, 0:cw_], W1[r1:r1 + D, c0 * CH:c0 * CH + cw_].rearrange("(k p) n -> p k n", p=128),
                      reads=[], writes=[w1k])
                P.dma("sp", w3[:, :, 0:cw_], W3[r1:r1 + D, c0 * CH:c0 * CH + cw_].rearrange("(k p) n -> p k n", p=128),
                      reads=[], writes=[w3k])
                P.dma("sp", w2[:, 0:nj, :], W2[r2 + c0 * CH:r2 + c0 * CH + cw_, :].rearrange("(k p) n -> p k n", p=128),
                      reads=[], writes=[w2k])
                loaded[(ex, gi2)] = (w1, w1k, w3, w3k, w2, w2k, nj)

            def emit_h(step):
                nonlocal hi_, gi_, pi
                ex, gi2, c0, nc_, ho, hn = step
                ensure(ex, gi2, c0, nc_)
                w1, w1k, w3, w3k, w2, w2k, nj = loaded[(ex, gi2)]
                gt, gtk = gts[gi_ % 2]
                gi_ += 1
                for j in range(nj):
                    p1, p1k = g.ps[pi % 4], "ps%d" % (pi % 4)
                    p3, p3k = g.ps[(pi + 1) % 4], "ps%d" % ((pi + 1) % 4)
                    pi += 2
                    for k in range(KT):
                        P.op("pe", lambda e, p1=p1, w1=w1, j=j, k=k, ho=ho, hn=hn: e.matmul(
                            p1[:, 0:hn], lhsT=w1[:, k, j * 128:(j + 1) * 128], rhs=hx[:, k, ho:ho + hn], start=(k == 0), stop=(k == 7)),
                            reads=[w1k, hxk], writes=[p1k], sig=(k == 7))
                    for k in range(KT):
                        P.op("pe", lambda e, p3=p3, w3=w3, j=j, k=k, ho=ho, hn=hn: e.matmul(
                            p3[:, 0:hn], lhsT=w3[:, k, j * 128:(j + 1) * 128], rhs=hx[:, k, ho:ho + hn], start=(k == 0), stop=(k == 7)),
                            reads=[w3k, hxk], writes=[p3k], sig=(k == 7))
                    sl, slk = sil[hi_ % 3]
                    hi_ += 1
                    P.op("act", lambda e, p1=p1, sl=sl, hn=hn: e.activation(out=sl[:, 0:hn], in_=p1[:, 0:hn], func=AF.Silu),
                         reads=[p1k], writes=[slk])
                    if moe:
                        cm, cmk = cms[ex]
                        P.op("pool", lambda e, sl=sl, cm=cm, ho=ho, hn=hn: e.tensor_tensor(out=sl[:, 0:hn], in0=sl[:, 0:hn],
                                                                                          in1=cm[:, ho:ho + hn], op=ALU.mult),
                             reads=[slk, cmk], writes=[slk])
                    P.op("dve", lambda e, p3=p3, sl=sl, gt=gt, j=j, hn=hn: e.tensor_tensor(out=gt[:, j, 0:hn], in0=p3[:, 0:hn],
                                                                                          in1=sl[:, 0:hn], op=ALU.mult),
                         reads=[p3k, slk], writes=[(gtk, j)])
                return (gt, gtk)

            def emit_w2(step, H, first):
                ex, gi2, c0, nc_, ho, hn = step
                w1, w1k, w3, w3k, w2, w2k, nj = loaded[(ex, gi2)]
                gt, gtk = H
                for m in range(KT):
                    po, pok = g.ps[4 + (m % 4)], "ps%d" % (4 + (m % 4))
                    for j in range(nj):
                        P.op("pe", lambda e, po=po, w2=w2, j=j, m=m, gt=gt, hn=hn, nj=nj: e.matmul(
                            po[:, 0:hn], lhsT=w2[:, j, m * 128:(m + 1) * 128], rhs=gt[:, j, 0:hn], start=(j == 0), stop=(j == nj - 1)),
                            reads=[w2k, (gtk, j)], writes=[pok], sig=(j == nj - 1))
                    if first:
                        P.op("act", lambda e, po=po, m=m, ho=ho, hn=hn: e.copy(out=acc[:, m, ho:ho + hn], in_=po[:, 0:hn]),
                             reads=[pok], writes=[(acck, m, ho)])
                    else:
                        P.op("dve", lambda e, po=po, m=m, ho=ho, hn=hn: e.tensor_tensor(out=acc[:, m, ho:ho + hn], in0=po[:, 0:hn],
                                                                                       in1=acc[:, m, ho:ho + hn], op=ALU.add),
                             reads=[pok, (acck, m, ho)], writes=[(acck, m, ho)])

            Hc = emit_h(steps[0])
            for i in range(len(steps)):
                Hn = emit_h(steps[i + 1]) if i + 1 < len(steps) else None
                emit_w2(steps[i], Hc, first=(i < len(halves)))
                Hc = Hn
            g2 = modvec(g, l, si, "G2")
            for hi2, (ho, hn) in enumerate(halves):
                xt, xtk = xts[hi2 % 2]
                lb = lbs[hi2] if kind == "L" else 0
                P.dma("sp", xt[:, :, 0:hn], blk_view(A["XT"], s0 + ho, hn), reads=[(kind + "_XT", lb)], writes=[xtk])
                for m in range(KT):
                    P.op("dve", lambda e, m=m, xt=xt, ho=ho, hn=hn: e.scalar_tensor_tensor(
                        out=xt[:, m, 0:hn], in0=acc[:, m, ho:ho + hn], scalar=g2[:, m:m + 1], in1=xt[:, m, 0:hn], op0=ALU.mult, op1=ALU.add),
                        reads=[(acck, m, ho), xtk, "modx", "modc"], writes=[xtk])
                P.dma("pool", blk_view(A["XT"], s0 + ho, hn), xt[:, :, 0:hn], reads=[xtk], writes=[(kind + "_XT", lb)])
                if publish and kind == "L" and lb in (4, 5, 10, 11):
                    eb = (4, 5, 10, 11).index(lb)
                    for cg in range(2):
                        P.dma("pool", g.ex_in[2 * eb + cg].rearrange("p (c t) -> p c t", c=4), xt[:, 4 * cg:4 * cg + 4, :],
                              reads=[xtk], writes=[("ex_in", 2 * eb + cg)])
                        P.collective("AllGather", ins=[g.ex_in[2 * eb + cg]], outs=[g.ex_all[2 * eb + cg].rearrange("r p f -> (r p) f")],
                                     groups=[[0, 1, 2, 3], [4, 5, 6, 7]], reads=[("ex_in", 2 * eb + cg)],
                                     writes=[("ex_all", 2 * eb + cg)])
        for (kind, s0, n0, lbs) in sblocks:
            sb_body(kind, s0, n0, lbs)


def st_exchange(g):
    P = g.P
    XT = g.lat["XT"]
    with Stage(g) as st:
        hb = st.ring("exh", [128, 4, NB], F32, 3)
        i = 0
        for (lb, eb, off) in ((2, 2, 3), (3, 3, 3), (12, 0, 1), (13, 1, 1)):
            for cg in range(2):
                u = 2 * eb + cg
                t, tk = hb[i % 3]
                i += 1

                for hh_ in range(2):
                    def dyn(e, t=t, u=u, off=off, hh_=hh_):
                        r = dynval(g, e, "rl" if off == 3 else "rr")
                        return e.dma_start(out=t[:, 2 * hh_:2 * hh_ + 2, :].rearrange("p c t -> p (c t)"),
                                           in_=g.ex_all[u][bass.ds(r, 1), :, 1024 * hh_:1024 * hh_ + 1024].rearrange("o p f -> p (o f)"))
                    P.dma("sp", None, None, reads=[("ex_all", u)], writes=[tk], fn=dyn)
                P.dma("sp", XT[4 * cg:4 * cg + 4, :, lb * NB:(lb + 1) * NB].rearrange("c p t -> p c t"), t[:],
                      reads=[tk], writes=[("L_XT", lb, cg)])


def st_final(g):
    P = g.P
    with Stage(g) as st:
        xts = st.ring("oxt", [128, KT, NB], F32, 2)
        ys = st.ring("oy", [128, KT, NB], F32, 2)
        outs = st.ring("oo", [128, 4, D], F32, 2)
        bufs = {"sq": st.sb("osq", [128, KT, NB], BF16), "rs": st.sb("ors", [128, NB], F32)}
        fg = S(g, "fing")
        for ji, lb in enumerate(range(4, 12)):
            xt, xtk = xts[ji % 2]
            y, yk = ys[ji % 2]
            oo, ook = outs[ji % 2]
            P.dma("sp", xt[:], blk_view(g.lat["XT"], lb * NB, NB), reads=[("L_XT", lb)], writes=[xtk])
            norm_core(g, st, xt, xtk, NB, fg, None, bufs, out_f32=(y, yk))
            for a in range(4):
                for half in range(2):
                    ps = g.ps[(a * 2 + half) % 6]
                    psk = "ps%d" % ((a * 2 + half) % 6)
                    for cc in range(4):
                        c = half * 4 + cc
                        P.op("pe", lambda e, ps=ps, y=y, a=a, c=c, cc=cc: e.transpose(
                            out=ps[:, cc * 128:(cc + 1) * 128], in_=y[:, c, a * 128:(a + 1) * 128], identity=g.ident[:]),
                            reads=[(yk, c), "ident"], writes=[psk])
                    if half == 0:
                        P.op("act", lambda e, ps=ps, oo=oo, a=a: e.copy(out=oo[:, a, 0:512], in_=ps[:, 0:512]),
                             reads=[psk], writes=[(ook, a, 0)])
                    else:
                        P.op("dve", lambda e, ps=ps, oo=oo, a=a: e.tensor_copy(out=oo[:, a, 512:1024], in_=ps[:, 0:512]),
                             reads=[psk], writes=[(ook, a, 1)])
            P.dma("pool", g.out[(lb - 4) * NB:(lb - 3) * NB, :].rearrange("(a p) d -> p a d", p=128), oo[:],
                  reads=[(ook, a, h) for a in range(4) for h in range(2)], writes=[("out", lb)])


def build():
    g = build_program()
    L = lambda lbs: seg_blocks("L", lbs)
    C = seg_blocks("C", None)

    def stop(name):
        return STOP_AFTER == name
    st_setup(g)
    if stop("setup"):
        g.P.emit(); return g
    st_convert(g, "mix")
    st_transpose_in(g)
    if stop("tin"):
        g.P.emit(); return g
    stopped = False
    for l in range(DEPTH):
        last = l == DEPTH - 1
        nblk = list(range(2, 14))
        ublk = list(range(3, 13))
        pblk = list(range(4, 12))
        if l == 1:
            st_exchange(g)
        st_norm(g, l, 1, L(nblk) + C)
        st_zu(g, l, L(ublk) + C)
        st_scan(g, l, C + L(pblk))
        if stop("scan%d" % l):
            stopped = True
            break
        st_carry(g, l)
        if stop("carry%d" % l):
            stopped = True
            break
        bgA = bgB = bgC = None
        if l == 0 and not NO_MOE:
            pcs = conv_pieces(g, "moe")
            n1_, n2_ = (len(pcs) * 4) // 10, (len(pcs) * 7) // 10
            bgA, bgB, bgC = pcs[:n1_], pcs[n1_:n2_], pcs[n2_:]
        st_zgm(g, l, L(pblk) + (C if not last else []), bg_pieces=bgA, bg_every=4)
        st_zv(g, l, L(nblk) + (C if not last else []), bg_pieces=bgB, bg_every=3)
        st_conv(g, l, [("L", pblk)] + ([("C", None)] if not last else []), bg_pieces=bgC)
        if stop("conv%d" % l):
            stopped = True
            break
        st_mix(g, l, L(pblk) + (C if not last else []))
        if stop("mix%d" % l):
            stopped = True
            break
        moe = (l % 2 == 1)
        st_norm(g, l, 2, L(pblk) + (C if not last else []), router=moe)
        sbl = [("L", pblk[i] * NB, 2 * NB, [pblk[i], pblk[i + 1]]) for i in range(0, len(pblk), 2)]
        if not last:
            sbl = [sbl[0], sbl[-1]] + sbl[1:-1]
            sbl.append(("C", CPAD, CTX, [0]))
        st_ffn(g, l, sbl, moe, publish=not last)
        if stop("ffn%d" % l):
            stopped = True
            break
    if not stopped:
        st_final(g)
    g.P.emit()
    return g


def make_in_maps(inp):
    x = np.asarray(inp["x"], np.float32)
    maps = []
    gw = np.zeros((DEPTH, 2, 2, 8, 128, 128), np.float32)
    for l in range(DEPTH):
        for d in range(2):
            for ti, nm in enumerate(("lru_wr", "lru_wi")):
                w = np.asarray(inp[nm][l][d], np.float32)
                for c in range(8):
                    gw[l, d, ti, c, 0:64, 0:64] = w[2 * c]
                    gw[l, d, ti, c, 64:128, 64:128] = w[2 * c + 1]
    gw = gw.reshape(-1, 128)
    shared = {} if NO_MOE else {
        "moe_w1": np.ascontiguousarray(np.asarray(inp["moe_w1"], np.float32)[0].reshape(NE * D, DFF)),
        "moe_w3": np.ascontiguousarray(np.asarray(inp["moe_w3"], np.float32)[0].reshape(NE * D, DFF)),
        "moe_w2": np.ascontiguousarray(np.asarray(inp["moe_w2"], np.float32)[0].reshape(NE * DFF, D)),
    }
    shared.update({
        "w_in": np.ascontiguousarray(np.asarray(inp["w_in"], np.float32).reshape(DEPTH * D, 6144)),
        "w_a": np.ascontiguousarray(np.asarray(inp["w_branch_a"], np.float32).reshape(DEPTH * D, D)),
        "w_b": np.ascontiguousarray(np.asarray(inp["w_branch_b"], np.float32).reshape(DEPTH * D, D)),
        "w_o": np.ascontiguousarray(np.asarray(inp["w_out"], np.float32).reshape(DEPTH * D, D)),
        "ffn_w1": np.ascontiguousarray(np.asarray(inp["ffn_w1"], np.float32)[0]),
        "ffn_w3": np.ascontiguousarray(np.asarray(inp["ffn_w3"], np.float32)[0]),
        "ffn_w2": np.ascontiguousarray(np.asarray(inp["ffn_w2"], np.float32)[0]),
        "gatew": gw,
        "router": np.ascontiguousarray(np.asarray(inp["moe_router"], np.float32)[0]),
    })
    modw = np.asarray(inp["mod_w"], np.float32)
    for core in range(8):
        b, q = core // 4, core % 4
        xl = np.zeros((TL, D), np.float32)
        g0 = q * 4096 - 4 * NB
        lo, hi = max(g0, 0), min(g0 + TL, 16384)
        xl[lo - g0:hi - g0] = x[b, lo:hi]
        m = dict(shared)
        m["x_loc"] = xl
        m["ctx_in"] = np.ascontiguousarray(np.asarray(inp["ctx"], np.float32)[b])
        m["small"] = _build_small(inp, core)
        m["modw"] = np.ascontiguousarray(modw[:, :, q * 1536:(q + 1) * 1536])
        maps.append(m)
    return maps


_CACHE = {}


def kernel(**inputs):
    if "g" not in _CACHE:
        _CACHE["g"] = build()
    g = _CACHE["g"]
    maps = make_in_maps(inputs)
    res = run_bass_kernel_spmd(g.nc, maps, core_ids=list(range(8)))
    out = np.zeros((2, 16384, D), np.float32)
    for core in range(8):
        b, q = core // 4, core % 4
        out[b, q * 4096:(q + 1) * 4096] = res.results[core]["out"]
    _CACHE["last"] = res
    return out
```

```python
import numpy as np
from contextlib import ExitStack
import concourse.bass as bass
import concourse.mybir as mybir
from concourse.bass_utils import run_bass_kernel_spmd

F32 = mybir.dt.float32
BF16 = mybir.dt.bfloat16
AF = mybir.ActivationFunctionType
ALU = mybir.AluOpType

D = 1024
KT = 8
NBLK = 16
NB = 512
TL = NBLK * NB
CTX = 256
CPAD = 16
TC = CTX + 2 * CPAD
DFF = 2816
NE = 8
EPS = 1e-6
DEPTH = 2
SEM_ROT = 30000
N_DMA_SEM = 16

DEBUG_OUT = []
STOP_AFTER = None
NO_MOE = False


class _Op:
    __slots__ = ("eng", "fn", "waits", "tok", "clock", "is_dma", "sig")


class Prog:
    ENG = ("pe", "dve", "act", "pool", "sp")

    def __init__(self, nc):
        self.nc = nc
        self.ops = {e: [] for e in self.ENG}
        self.known = {e: {} for e in self.ENG}
        self.cnt = {e: 0 for e in self.ENG}
        self.cur_sem = {}
        self.sems = {}
        self.nsem = 0
        self.own_done = {e: {} for e in self.ENG}
        for e in self.ENG:
            self.cur_sem[e] = self._new_sem(e)
        self.dma_sems = {}
        self.dma_uses = {}
        self.dma_last = {}
        self.dma_rr = {}
        for q in ("sp", "pool"):
            self.dma_sems[q] = [self._new_sem("d" + q) for _ in range(N_DMA_SEM)]
            self.dma_rr[q] = 0
            for s in self.dma_sems[q]:
                self.dma_uses[s] = 0
                self.dma_last[s] = None
        self.last_w = {}
        self.readers = {}
        self.nops = 0
        self.last_op = {e: None for e in self.ENG}
        self.pending_nosig = {e: 0 for e in self.ENG}
        self.uid = 0

    def _new_sem(self, tag):
        sid = self.nsem
        self.nsem += 1
        self.sems[sid] = self.nc.alloc_semaphore(name="s%d_%s" % (sid, tag))
        return sid

    def name(self, base):
        self.uid += 1
        return "%s_%d" % (base, self.uid)

    def _deps(self, eng, reads, writes, is_pe):
        deps = []
        for k in reads:
            w = self.last_w.get(k)
            if w is not None:
                deps.append(w)
        for k in writes:
            w = self.last_w.get(k)
            if w is not None:
                deps.append(w)
            for r in self.readers.get(k, ()):
                deps.append(r)
        waits = {}
        kn = self.known[eng]
        for d in deps:
            if is_pe and d.eng == "pe" and not d.is_dma:
                continue
            sid, val = d.tok
            if kn.get(sid, 0) >= val:
                continue
            if waits.get(sid, 0) < val:
                waits[sid] = val
        for d in deps:
            for sid, val in d.clock.items():
                if kn.get(sid, 0) < val:
                    kn[sid] = val
        return waits

    def _register(self, op, reads, writes):
        for k in reads:
            lst = self.readers.setdefault(k, [])
            if not op.is_dma:
                lst[:] = [r for r in lst if r.is_dma or r.eng != op.eng]
            lst.append(op)
        for k in writes:
            self.last_w[k] = op
            self.readers[k] = []

    def op(self, eng, fn, reads=(), writes=(), sig=True):
        o = _Op()
        o.eng = eng
        o.fn = fn
        o.is_dma = False
        o.sig = sig
        o.waits = self._deps(eng, reads, writes, eng == "pe")
        if self.cnt[eng] >= SEM_ROT and self.pending_nosig[eng] == 0:
            self.own_done[eng][self.cur_sem[eng]] = self.cnt[eng]
            self.cur_sem[eng] = self._new_sem(eng)
            self.cnt[eng] = 0
        if sig:
            self.cnt[eng] += 1
            self.pending_nosig[eng] = 0
            o.tok = (self.cur_sem[eng], self.cnt[eng])
        else:
            self.pending_nosig[eng] += 1
            o.tok = (self.cur_sem[eng], self.cnt[eng] + 1)
        o.clock = dict(self.known[eng])
        o.clock.update(self.own_done[eng])
        o.clock[o.tok[0]] = o.tok[1]
        self._register(o, reads, writes)
        self.ops[eng].append(o)
        self.last_op[eng] = o
        self.nops += 1
        return o

    def dma(self, q, out, in_, reads=(), writes=(), fn=None):
        o = _Op()
        o.eng = q
        o.is_dma = True
        waits = self._deps(q, reads, writes, False)
        sems = self.dma_sems[q]
        sid = sems[self.dma_rr[q] % len(sems)]
        self.dma_rr[q] += 1
        prev = self.dma_last[sid]
        kn = self.known[q]
        if prev is not None and kn.get(sid, 0) < prev.tok[1]:
            waits[sid] = max(waits.get(sid, 0), prev.tok[1])
            kn[sid] = prev.tok[1]
        self.dma_uses[sid] += 1
        o.tok = (sid, 16 * self.dma_uses[sid])
        self.dma_last[sid] = o
        o.waits = waits
        o.fn = ("dynfn", fn) if fn is not None else ("dma", out, in_)
        o.clock = dict(kn)
        o.clock[sid] = o.tok[1]
        self._register(o, reads, writes)
        self.ops[q].append(o)
        self.nops += 1
        return o

    def collective(self, kind, ins, outs, groups, reads=(), writes=()):
        o = _Op()
        o.eng = "pool"
        o.is_dma = True
        o.waits = self._deps("pool", reads, writes, False)
        sid = self._new_sem("cc")
        o.tok = (sid, 1)
        o.fn = ("cc", kind, ins, outs, groups)
        o.clock = dict(self.known["pool"])
        o.clock[sid] = 1
        self._register(o, reads, writes)
        self.ops["pool"].append(o)
        self.nops += 1
        return o

    def barrier(self):
        toks = {}
        for e in self.ENG:
            lo = self.last_op[e]
            if lo is not None:
                toks[lo.tok[0]] = max(toks.get(lo.tok[0], 0), lo.tok[1])
        for sid, lo in self.dma_last.items():
            if lo is not None:
                toks[sid] = max(toks.get(sid, 0), lo.tok[1])
        for k, w in self.last_w.items():
            if w is not None and w.is_dma:
                toks[w.tok[0]] = max(toks.get(w.tok[0], 0), w.tok[1])
        for e in self.ENG:
            kn = self.known[e]
            waits = {}
            for sid, val in toks.items():
                if kn.get(sid, 0) < val:
                    if e == "pe" and sid == self.cur_sem["pe"]:
                        continue
                    waits[sid] = val
                    kn[sid] = val
            if waits:
                o = _Op()
                o.eng = e
                o.is_dma = False
                o.waits = waits
                o.fn = None
                o.tok = None
                o.clock = {}
                self.ops[e].append(o)
        self.last_w = {}
        self.readers = {}

    def _replay(self, ename, e):
        sems = self.sems
        for o in self.ops[ename]:
            for sid, val in o.waits.items():
                e.wait_ge(sems[sid], val)
            if o.fn is None:
                continue
            if o.is_dma:
                if o.fn[0] == "dynfn":
                    o.fn[1](e).then_inc(sems[o.tok[0]], 16)
                elif o.fn[0] == "dma":
                    e.dma_start(out=o.fn[1], in_=o.fn[2]).then_inc(sems[o.tok[0]], 16)
                else:
                    _, kind, ins, outs, groups = o.fn
                    e.collective_compute(kind, ALU.bypass, replica_groups=groups,
                                         ins=ins, outs=outs).then_inc(sems[o.tok[0]])
            elif o.sig:
                o.fn(e).then_inc(sems[o.tok[0]], 1)
            else:
                o.fn(e)

    def emit(self):
        self.barrier()
        with self.nc.Block() as block:
            @block.sync
            def _(e):
                self._replay("sp", e)

            @block.tensor
            def _(e):
                self._replay("pe", e)

            @block.vector
            def _(e):
                self._replay("dve", e)

            @block.scalar
            def _(e):
                self._replay("act", e)

            @block.gpsimd
            def _(e):
                self._replay("pool", e)


def _small_layout():
    off = {}
    n = 0

    def add(name, w):
        nonlocal n
        off[name] = (n, w)
        n += w
    for l in range(DEPTH):
        add("n1g%d" % l, 8)
        add("n2g%d" % l, 8)
        add("modb%d" % l, 48)
        add("cw4_%d" % l, 32)
        add("cb4_%d" % l, 8)
        for d in range(2):
            add("br%d%d" % (l, d), 8)
            add("bi%d%d" % (l, d), 8)
            add("lam%d%d" % (l, d), 8)
        add("cw31_%d" % l, 31 * 8)
        add("cb31_%d" % l, 8)
        add("lng%d" % l, 8)
        add("lnb%d" % l, 8)
    add("fing", 8)
    add("blkmask", NBLK)
    add("one", 1)
    add("boh", 2)
    add("cvec", 32)
    return off, n


SOFF, NS = _small_layout()


def _pc(v):
    return np.ascontiguousarray(np.asarray(v, np.float32).reshape(8, 128).T)


def _build_small(inp, core):
    b, q = core // 4, core % 4
    s = np.zeros((128, NS), np.float32)

    def put(name, arr):
        o, w = SOFF[name]
        s[:, o:o + w] = np.asarray(arr, np.float32).reshape(128, w)
    for l in range(DEPTH):
        put("n1g%d" % l, _pc(inp["norm1_g"][l]))
        put("n2g%d" % l, _pc(inp["norm2_g"][l]))
        put("modb%d" % l, inp["mod_b"][l].reshape(48, 128).T)
        put("cw4_%d" % l, np.concatenate([_pc(inp["rnn_conv_w"][l][j]) for j in range(4)], axis=1))
        put("cb4_%d" % l, _pc(inp["rnn_conv_b"][l]))
        for d in range(2):
            put("br%d%d" % (l, d), _pc(inp["lru_br"][l][d]))
            put("bi%d%d" % (l, d), _pc(inp["lru_bi"][l][d]))
            put("lam%d%d" % (l, d), _pc(inp["lru_lam"][l][d]))
        put("cw31_%d" % l, np.concatenate([_pc(inp["conv_w"][l][j]) for j in range(31)], axis=1))
        put("cb31_%d" % l, _pc(inp["conv_b"][l]))
        put("lng%d" % l, _pc(inp["conv_ln_g"][l]))
        put("lnb%d" % l, _pc(inp["conv_ln_b"][l]))
    put("fing", _pc(inp["final_g"]))
    bm = np.zeros((128, NBLK), np.float32)
    for lb in range(NBLK):
        g0 = q * 4096 + (lb - 4) * NB
        bm[:, lb] = 1.0 if (0 <= g0 < 16384) else 0.0
    put("blkmask", bm)
    put("one", np.ones((128, 1), np.float32))
    boh = np.zeros((128, 2), np.float32)
    boh[:, b] = 1.0
    put("boh", boh)
    cv = np.zeros((128, 8, 4), np.float32)
    cv[:, :, 0] = _pc(inp["c"][0])
    cv[:, :, 1] = _pc(inp["c"][1])
    cv[:, :, 2] = _pc(inp["c_ctx"])
    put("cvec", cv.reshape(128, 32))
    return s


class G:
    pass


def build_program():
    nc = bass.Bass("TRN2", target_bir_lowering=False)
    P = Prog(nc)
    g = G()
    g.nc, g.P = nc, P

    def din(name, shape, dt=F32):
        return nc.dram_tensor(name, list(shape), dt, kind="ExternalInput").ap()

    def dscr(name, shape, dt=F32):
        kind = "ExternalOutput" if name in DEBUG_OUT else "Internal"
        return nc.dram_tensor(name, list(shape), dt, kind=kind).ap()

    g.x_loc = din("x_loc", [TL, D])
    g.ctx_in = din("ctx_in", [CTX, D])
    g.small_in = din("small", [128, NS])
    g.modw_in = din("modw", [DEPTH, D, 1536])
    g.win_in = din("w_in", [DEPTH * D, 6144])
    g.wa_in = din("w_a", [DEPTH * D, D])
    g.wb_in = din("w_b", [DEPTH * D, D])
    g.wo_in = din("w_o", [DEPTH * D, D])
    g.f1_in = din("ffn_w1", [D, DFF])
    g.f3_in = din("ffn_w3", [D, DFF])
    g.f2_in = din("ffn_w2", [DFF, D])
    if not NO_MOE:
        g.m1_in = din("moe_w1", [NE * D, DFF])
        g.m3_in = din("moe_w3", [NE * D, DFF])
        g.m2_in = din("moe_w2", [NE * DFF, D])
    g.gw_in = din("gatew", [DEPTH * 2 * 2 * 8 * 128, 128])
    g.rt_in = din("router", [D, NE])
    g.out = nc.dram_tensor("out", [8 * NB, D], F32, kind="ExternalOutput").ap()

    g.win = dscr("win_bf", [DEPTH * D, 6144], BF16)
    g.wa = dscr("wa_bf", [DEPTH * D, D], BF16)
    g.wb = dscr("wb_bf", [DEPTH * D, D], BF16)
    g.wo = dscr("wo_bf", [DEPTH * D, D], BF16)
    g.f1 = dscr("f1_bf", [D, DFF], BF16)
    g.f3 = dscr("f3_bf", [D, DFF], BF16)
    g.f2 = dscr("f2_bf", [DFF, D], BF16)
    g.m1 = dscr("m1_bf", [NE * D, DFF], BF16)
    g.m3 = dscr("m3_bf", [NE * D, DFF], BF16)
    g.m2 = dscr("m2_bf", [NE * DFF, D], BF16)
    g.gw = dscr("gw_bf", [DEPTH * 2 * 2 * 8 * 128, 128], BF16)

    def seg_arrays(pfx, T):
        a = {}
        for nm, dt in (("XT", F32), ("HX", BF16), ("U", F32), ("G", BF16), ("V", BF16),
                       ("SGA", BF16), ("SGB", BF16), ("S", F32), ("AF", BF16), ("AB", BF16),
                       ("CV", F32)):
            a[nm] = dscr(pfx + nm, [KT, 128, T], dt)
        return a
    g.lat = seg_arrays("L_", TL)
    g.ctxa = seg_arrays("C_", TC)
    g.comb = dscr("COMB", [NE, 128, TL], F32)
    g.sum_in = dscr("sum_in", [128, 256], F32)
    g.sum_all = dscr("sum_all", [4 * 128, 256], F32)
    g.mod_in = dscr("mod_in", [128, 96], F32)
    g.mod_all = dscr("mod_all", [4 * 128, 96], F32)
    g.htab = dscr("htab", [2, 128, KT, 48], F32)
    g.ex_in = dscr("ex_in", [8, 128, 2048], F32)
    g.ex_all = [dscr("ex_all%d" % u, [4, 128, 2048], F32) for u in range(8)]

    def sb(name, shape, dt=F32):
        return nc.alloc_sbuf_tensor("sb_" + name, list(shape), dt)
    g.small = sb("small", [128, NS])
    g.ident = sb("ident", [128, 128])
    g.ones_b = sb("ones_b", [128, 128], BF16)
    g.zeros = sb("zeros", [128, NB])
    g.modx = sb("modx", [128, DEPTH, 48])
    g.modc = sb("modc", [128, DEPTH, 48])
    g.nrm = sb("nrm", [128, DEPTH, 2, 4, 8])
    g.cl = sb("cl", [128, DEPTH, 2, 2, 8])
    g.sumt = sb("sumt", [128, KT, 8, 4])
    g.sctx = sb("sctx", [128, 2, KT])
    g.hin = sb("hin", [128, 2, KT, 12])
    g.hzero = sb("hzero", [128, 2, KT, 12])
    g.ps = [nc.alloc_psum_tensor("psb%d" % i, [128, NB], F32) for i in range(8)]
    return g


def S(g, name, w=None):
    o, ww = SOFF[name]
    return g.small[:, o:o + (ww if w is None else w)]


def Scol(g, name, i):
    o, _ = SOFF[name]
    return g.small[:, o + i:o + i + 1]


def dynval(g, e, name):
    if not hasattr(g, "_dyn"):
        pid = e.partition_id()
        g._dyn = {
            "rl": e.snap((pid + 3) % 4, min_val=0, max_val=3),
            "rr": e.snap((pid + 1) % 4, min_val=0, max_val=3),
            "b0": e.snap((pid % 4) * 8 + 6, min_val=6, max_val=30),
            "b1": e.snap((pid % 4) * 8 + 7, min_val=7, max_val=31),
        }
    return g._dyn[name]


class Stage:
    def __init__(self, g):
        self.g = g
        self.es = ExitStack()

    def __enter__(self):
        self.g.P.barrier()
        return self

    def __exit__(self, *a):
        self.g.P.barrier()
        self.es.close()
        return False

    def sb(self, base, shape, dt=F32):
        nm = self.g.P.name(base)
        t = self.es.enter_context(self.g.nc.sbuf_tensor(nm, list(shape), dt))
        return t, nm

    def ring(self, base, shape, dt, n):
        return [self.sb(base, shape, dt) for _ in range(n)]


def blk_view(arr, s, n):
    return arr[:, :, s:s + n].rearrange("c p t -> p c t")


def st_setup(g):
    P, nc = g.P, g.nc
    with Stage(g) as st:
        P.dma("sp", g.small[:], g.small_in, reads=[], writes=["small"])
        P.op("pool", lambda e: e.memset(g.ident[:], 0.0), writes=["ident"])
        P.op("pool", lambda e: e.affine_select(out=g.ident[:], in_=g.ident[:], pattern=[[-1, 128]],
                                               compare_op=ALU.not_equal, fill=1.0, base=0,
                                               channel_multiplier=1),
             reads=["ident"], writes=["ident"])
        P.op("pool", lambda e: e.memset(g.ones_b[:], 1.0), writes=["ones_b"])
        P.op("pool", lambda e: e.memset(g.zeros[:], 0.0), writes=["zeros"])
        P.op("pool", lambda e: e.memset(g.hzero[:], 0.0), writes=["hzero"])
        zt, zk = st.sb("zt", [128, KT, TC], F32)
        zb, zbk = st.sb("zb", [128, KT, TC], BF16)
        P.op("dve", lambda e: e.memset(zt[:], 0.0), writes=[zk])
        P.op("dve", lambda e: e.memset(zb[:], 0.0), writes=[zbk])
        P.dma("sp", blk_view(g.ctxa["U"], 0, TC), zt[:], reads=[zk], writes=["C_U"])
        P.dma("sp", blk_view(g.ctxa["V"], 0, TC), zb[:], reads=[zbk], writes=["C_V"])
        sc, sck = st.sb("sc", [128, 8, 4], F32)
        P.op("act", lambda e: e.activation(out=sc[:].rearrange("p a b -> p (a b)"), in_=S(g, "cvec"),
                                           func=AF.Silu), reads=["small"], writes=[sck])
        mw = st.ring("mw", [128, 8, 1536], F32, 1)
        mo, mok = st.sb("mo", [128, 96], F32)
        psm = g.ps[0]
        for l in range(DEPTH):
            t, tk = mw[0]
            P.dma("sp", t[:], g.modw_in[l].rearrange("(k p) n -> p k n", p=128), reads=[], writes=[tk])
            for c in range(12):
                for k in range(8):
                    P.op("pe", lambda e, t=t, c=c, k=k, l=l: e.matmul(
                        psm[:, (l * 12 + c) * 4:(l * 12 + c) * 4 + 4], lhsT=t[:, k, c * 128:(c + 1) * 128],
                        rhs=sc[:, k, :], start=(k == 0), stop=(k == 7)),
                        reads=[tk, sck], writes=["ps0"], sig=(k == 7))
        P.op("dve", lambda e: e.tensor_copy(out=mo[:], in_=psm[:, 0:96]), reads=["ps0"], writes=[mok])
        P.dma("sp", g.mod_in, mo[:], reads=[mok], writes=["mod_in"])
        P.collective("AllGather", ins=[g.mod_in], outs=[g.mod_all], groups=[[0, 1, 2, 3], [4, 5, 6, 7]],
                     reads=["mod_in"], writes=["mod_all"])
        ma, mak = st.sb("ma", [128, DEPTH, 4, 12, 4], F32)
        for l in range(DEPTH):
            P.dma("sp", ma[:, l], g.mod_all.rearrange("(r p) (l c j) -> p l r c j", p=128, l=DEPTH, c=12)[:, l],
                  reads=["mod_all"], writes=[mak])
        for l in range(DEPTH):
            v = ma[:, l].rearrange("p r c j -> p (r c) j")
            mb = S(g, "modb%d" % l)
            P.op("dve", lambda e, v=v, l=l: e.tensor_single_scalar(out=g.modx[:, l, :], in_=v[:, :, 0],
                                                            scalar=Scol(g, "boh", 0), op=ALU.mult),
                 reads=[mak, "small"], writes=["modx"])
            P.op("dve", lambda e, v=v, l=l: e.scalar_tensor_tensor(out=g.modx[:, l, :], in0=v[:, :, 1],
                                                                   scalar=Scol(g, "boh", 1), in1=g.modx[:, l, :],
                                                                   op0=ALU.mult, op1=ALU.add),
                 reads=[mak, "small", "modx"], writes=["modx"])
            P.op("dve", lambda e, l=l, mb=mb: e.tensor_tensor(out=g.modx[:, l, :], in0=g.modx[:, l, :], in1=mb, op=ALU.add),
                 reads=["modx", "small"], writes=["modx"])
            P.op("dve", lambda e, v=v, l=l, mb=mb: e.tensor_tensor(out=g.modc[:, l, :], in0=v[:, :, 2], in1=mb, op=ALU.add),
                 reads=[mak, "small"], writes=["modc"])
            for si, mv in enumerate((g.modx, g.modc)):
                P.op("dve", lambda e, l=l, si=si, mv=mv: e.scalar_tensor_tensor(
                    out=g.nrm[:, l, si, 0, :], in0=mv[:, l, 8:16], scalar=1.0, in1=S(g, "n1g%d" % l),
                    op0=ALU.add, op1=ALU.mult), reads=["modx", "modc", "small"], writes=["nrm"])
                P.op("dve", lambda e, l=l, si=si, mv=mv: e.scalar_tensor_tensor(
                    out=g.nrm[:, l, si, 1, :], in0=mv[:, l, 32:40], scalar=1.0, in1=S(g, "n2g%d" % l),
                    op0=ALU.add, op1=ALU.mult), reads=["modx", "modc", "small", "nrm"], writes=["nrm"])
            for d in range(2):
                tmp, tmk = st.sb("cltmp", [128, 8], F32)
                P.op("act", lambda e, l=l, d=d, tmp=tmp: e.activation(out=tmp[:], in_=S(g, "lam%d%d" % (l, d)),
                                                                      func=AF.Exp, scale=-1.0),
                     reads=["small"], writes=[tmk])
                P.op("act", lambda e, tmp=tmp: e.activation(out=tmp[:], in_=tmp[:], func=AF.Ln, bias=1.0),
                     reads=[tmk], writes=[tmk])
                P.op("dve", lambda e, l=l, d=d, tmp=tmp: e.tensor_single_scalar(out=g.cl[:, l, d, 0, :], in_=tmp[:],
                                                                         scalar=-8.0, op=ALU.mult),
                     reads=[tmk], writes=["cl"])
                P.op("dve", lambda e, l=l, d=d, tmp=tmp: e.tensor_single_scalar(out=g.cl[:, l, d, 1, :], in_=tmp[:],
                                                                         scalar=-16.0, op=ALU.mult),
                     reads=[tmk, "cl"], writes=["cl"])


def modvec(g, l, si, which):
    mv = g.modx if si == 0 else g.modc
    if which == "A1":
        return g.nrm[:, l, si, 0, :]
    if which == "A2":
        return g.nrm[:, l, si, 1, :]
    return {"B1": mv[:, l, 0:8], "G1": mv[:, l, 16:24], "B2": mv[:, l, 24:32], "G2": mv[:, l, 40:48]}[which]


def st_convert(g, which):
    P = g.P
    if which == "mix":
        pairs = [(g.win_in, g.win), (g.wa_in, g.wa), (g.wb_in, g.wb), (g.wo_in, g.wo), (g.gw_in, g.gw),
                 (g.f1_in, g.f1), (g.f3_in, g.f3), (g.f2_in, g.f2)]
    elif which == "mix0":
        pairs = [(g.win_in, g.win), (g.gw_in, g.gw)]
    else:
        pairs = [(g.m1_in, g.m1), (g.m3_in, g.m3), (g.m2_in, g.m2)]
    wi_ = 0
    W = 4096
    with Stage(g) as st:
        src = st.ring("cvs", [128, W], F32, 3)
        dst = st.ring("cvd", [128, W], BF16, 3)
        i = 0
        for a, b in pairs:
            av = a.rearrange("(p r) n -> p (r n)", p=128)
            bv = b.rearrange("(p r) n -> p (r n)", p=128)
            Fd = av.shape[1]
            for o in range(0, Fd, W):
                w = min(W, Fd - o)
                s, sk = src[i % 3]
                d, dk = dst[i % 3]
                P.dma("sp", s[:, 0:w], av[:, o:o + w], reads=[], writes=[sk])
                eng = ("act", "dve", "act")[i % 3]
                if eng == "act":
                    P.op("act", lambda e, s=s, d=d, w=w: e.copy(out=d[:, 0:w], in_=s[:, 0:w]), reads=[sk], writes=[dk])
                else:
                    P.op(eng, lambda e, s=s, d=d, w=w: e.tensor_copy(out=d[:, 0:w], in_=s[:, 0:w]), reads=[sk], writes=[dk])
                P.dma("pool", bv[:, o:o + w], d[:, 0:w], reads=[dk], writes=[("wcv", id(b), o)])
                i += 1


def conv_pieces(g, which, W=4096):
    if which == "mix":
        pairs = [(g.win_in, g.win), (g.wa_in, g.wa), (g.wb_in, g.wb), (g.wo_in, g.wo), (g.gw_in, g.gw),
                 (g.f1_in, g.f1), (g.f3_in, g.f3), (g.f2_in, g.f2)]
    elif which == "mix1":
        pairs = [(g.wa_in, g.wa), (g.wb_in, g.wb), (g.wo_in, g.wo), (g.f1_in, g.f1), (g.f3_in, g.f3), (g.f2_in, g.f2)]
    else:
        pairs = [(g.m1_in, g.m1), (g.m3_in, g.m3), (g.m2_in, g.m2)]
    out = []
    for a, b in pairs:
        av = a.rearrange("(p r) n -> p (r n)", p=128)
        bv = b.rearrange("(p r) n -> p (r n)", p=128)
        Fd = av.shape[1]
        for o in range(0, Fd, W):
            out.append((av, bv, o, min(W, Fd - o), id(b)))
    return out


def bg_convert(g, st, pieces, eng="dve", W=4096):
    P = g.P
    src = st.ring("bgs", [128, W], F32, 3)
    dst = st.ring("bgd", [128, W], BF16, 3)
    for i, (av, bv, o, w, bid) in enumerate(pieces):
        s_, sk = src[i % 3]
        d_, dk = dst[i % 3]
        P.dma("sp", s_[:, 0:w], av[:, o:o + w], reads=[], writes=[sk])
        if eng == "act":
            P.op("act", lambda e, s_=s_, d_=d_, w=w: e.copy(out=d_[:, 0:w], in_=s_[:, 0:w]), reads=[sk], writes=[dk])
        else:
            P.op(eng, lambda e, s_=s_, d_=d_, w=w: e.tensor_copy(out=d_[:, 0:w], in_=s_[:, 0:w]), reads=[sk], writes=[dk])
        P.dma("pool", bv[:, o:o + w], d_[:, 0:w], reads=[dk], writes=[("wcv", bid, o)])
        yield


def st_transpose_in(g):
    P = g.P
    with Stage(g) as st:
        xin = st.ring("xin", [128, 4, D], F32, 2)
        xtb = st.ring("xtb", [128, KT, NB], F32, 2)
        jobs = [("L", lb) for lb in range(2, 14)] + [("C", 0)]
        for ji, (kind, lb) in enumerate(jobs):
            xi, xik = xin[ji % 2]
            xo, xok = xtb[ji % 2]
            if kind == "L":
                na = 4
                P.dma("sp", xi[:], g.x_loc[lb * NB:(lb + 1) * NB, :].rearrange("(a p) d -> p a d", p=128), writes=[xik])
            else:
                na = 2
                P.dma("sp", xi[:, 0:2], g.ctx_in.rearrange("(a p) d -> p a d", p=128), writes=[xik])
            for c in range(KT):
                ps = g.ps[c]
                for a in range(na):
                    P.op("pe", lambda e, ps=ps, xi=xi, a=a, c=c: e.transpose(
                        out=ps[:, a * 128:(a + 1) * 128], in_=xi[:, a, c * 128:(c + 1) * 128], identity=g.ident[:]),
                        reads=[xik, "ident"], writes=["ps%d" % c])
                n = na * 128
                if c % 2 == 0:
                    P.op("act", lambda e, ps=ps, xo=xo, c=c, n=n: e.copy(out=xo[:, c, 0:n], in_=ps[:, 0:n]),
                         reads=["ps%d" % c], writes=[(xok, c)])
                else:
                    P.op("dve", lambda e, ps=ps, xo=xo, c=c, n=n: e.tensor_copy(out=xo[:, c, 0:n], in_=ps[:, 0:n]),
                         reads=["ps%d" % c], writes=[(xok, c)])
            rk = [(xok, c) for c in range(KT)]
            if kind == "L":
                P.dma("pool", blk_view(g.lat["XT"], lb * NB, NB), xo[:], reads=rk, writes=[("L_XT", lb)])
            else:
                P.dma("pool", blk_view(g.ctxa["XT"], CPAD, CTX), xo[:, :, 0:CTX], reads=rk, writes=[("C_XT", 0)])


def norm_core(g, st, xt, xtk, n, A, B, bufs, out_bf=None, out_f32=None, psi=7, sq_eng="act"):
    P = g.P
    sq, sqk = bufs["sq"]
    rs, rsk = bufs["rs"]
    ps = g.ps[psi]
    psk = "ps%d" % psi
    P.op("act", lambda e: e.activation(out=sq[:, :, 0:n], in_=xt[:, :, 0:n], func=AF.Square),
         reads=[xtk], writes=[sqk])
    for k in range(KT):
        P.op("pe", lambda e, k=k: e.matmul(ps[:, 0:n], lhsT=g.ones_b[:], rhs=sq[:, k, 0:n], start=(k == 0), stop=(k == 7)),
             reads=[sqk, "ones_b"], writes=[psk], sig=(k == 7))
    P.op("act", lambda e: e.activation(out=rs[:, 0:n], in_=ps[:, 0:n], func=AF.Sqrt, scale=1.0 / D, bias=EPS),
         reads=[psk], writes=[rsk])
    P.op("dve", lambda e: e.reciprocal(out=rs[:, 0:n], in_=rs[:, 0:n]), reads=[rsk], writes=[rsk])
    P.op("dve", lambda e: e.tensor_tensor(out=xt[:, :, 0:n], in0=xt[:, :, 0:n],
                                          in1=rs[:, 0:n].unsqueeze(1).to_broadcast([128, KT, n]), op=ALU.mult),
         reads=[xtk, rsk], writes=[xtk])
    for c in range(KT):
        eng = "act" if c % 2 == 0 else "dve"
        for (ot, wants) in ((out_bf, True), (out_f32, True)):
            if ot is None:
                continue
            o, ok = ot
            if B is None:
                if eng == "act":
                    P.op("act", lambda e, o=o, c=c: e.activation(out=o[:, c, 0:n], in_=xt[:, c, 0:n], func=AF.Identity,
                                                                 scale=A[:, c:c + 1]),
                         reads=[xtk, "nrm", "small"], writes=[(ok, c)])
                else:
                    P.op("dve", lambda e, o=o, c=c: e.tensor_single_scalar(out=o[:, c, 0:n], in_=xt[:, c, 0:n],
                                                                    scalar=A[:, c:c + 1], op=ALU.mult),
                         reads=[xtk, "nrm", "small"], writes=[(ok, c)])
            else:
                if eng == "act":
                    P.op("act", lambda e, o=o, c=c: e.activation(out=o[:, c, 0:n], in_=xt[:, c, 0:n], func=AF.Identity,
                                                                 scale=A[:, c:c + 1], bias=B[:, c:c + 1]),
                         reads=[xtk, "nrm", "modx", "modc"], writes=[(ok, c)])
                else:
                    P.op("dve", lambda e, o=o, c=c: e.tensor_scalar(out=o[:, c, 0:n], in0=xt[:, c, 0:n],
                                                                    scalar1=A[:, c:c + 1], scalar2=B[:, c:c + 1],
                                                                    op0=ALU.mult, op1=ALU.add),
                         reads=[xtk, "nrm", "modx", "modc"], writes=[(ok, c)])


def seg_blocks(kind, lbs):
    if kind == "L":
        return [("L", lb * NB, NB, lb) for lb in lbs]
    return [("C", CPAD, CTX, 0)]


def arrs(g, kind):
    return g.lat if kind == "L" else g.ctxa


def st_norm(g, l, which, jobs, router=False):
    P = g.P
    with Stage(g) as st:
        xts = st.ring("nxt", [128, KT, NB], F32, 2)
        hxs = st.ring("nhx", [128, KT, NB], BF16, 2)
        bufs = {"sq": st.sb("nsq", [128, KT, NB], BF16), "rs": st.sb("nrs", [128, NB], F32)}
        if router:
            yfs = st.sb("nyf", [128, KT, NB], F32)
            rt, rtk = st.sb("rt", [128, KT, NE], F32)
            P.dma("sp", rt[:], g.rt_in.rearrange("(k p) e -> p k e", p=128), writes=[rtk])
            lg, lgk = st.sb("lg", [128, 4, NE], F32)
            mx, mxk = st.sb("mx", [128, 4, 8], F32)
            ex, exk = st.sb("ex", [128, 4, NE], F32)
            mk, mkk = st.sb("mk", [128, 4, NE], F32)
            dn, dnk = st.sb("dn", [128, 4], F32)
            nm1, nm1k = st.sb("nm1", [128, 4], F32)
            cbb, cbbk = st.sb("cbb", [128, NE, 128], F32)
            cmo = st.ring("cmo", [128, NE, NB], F32, 2)
        def job(ji, kind, s, n, lb):
            A = arrs(g, kind)
            si = 0 if kind == "L" else 1
            xt, xtk = xts[ji % 2]
            hx, hxk = hxs[ji % 2]
            P.dma("sp", xt[:, :, 0:n], blk_view(A["XT"], s, n), reads=[(kind + "_XT", lb)], writes=[xtk])
            Av = modvec(g, l, si, "A%d" % which)
            Bv = modvec(g, l, si, "B%d" % which)
            if router:
                norm_core(g, st, xt, xtk, n, Av, Bv, bufs, out_f32=yfs)
                yf, yfk = yfs
                P.op("pool", lambda e, hx=hx, yf=yf: e.tensor_copy(out=hx[:, :, 0:n], in_=yf[:, :, 0:n]),
                     reads=[(yfk, c) for c in range(KT)], writes=[(hxk, c) for c in range(KT)])
                psl = g.ps[6]
                for a in range(4):
                    for k in range(KT):
                        P.op("pe", lambda e, a=a, k=k, yf=yf: e.matmul(psl[:, a * 8:(a + 1) * 8],
                                                                      lhsT=yf[:, k, a * 128:(a + 1) * 128], rhs=rt[:, k, :],
                                                                      start=(k == 0), stop=(k == 7)),
                             reads=[(yfk, k), rtk], writes=["ps6"], sig=(k == 7))
                P.op("dve", lambda e: e.tensor_copy(out=lg[:].rearrange("p a e -> p (a e)"), in_=psl[:, 0:32]),
                     reads=["ps6"], writes=[lgk])
                for a in range(4):
                    P.op("dve", lambda e, a=a: e.max(out=mx[:, a, :], in_=lg[:, a, :]), reads=[lgk], writes=[mxk])
                P.op("dve", lambda e: e.tensor_single_scalar(out=nm1[:], in_=mx[:, :, 0], scalar=-1.0, op=ALU.mult),
                     reads=[mxk], writes=[nm1k])
                for a in range(4):
                    P.op("act", lambda e, a=a: e.activation(out=ex[:, a, :], in_=lg[:, a, :], func=AF.Exp,
                                                            bias=nm1[:, a:a + 1], scale=1.0),
                         reads=[lgk, nm1k], writes=[exk])
                    P.op("dve", lambda e, a=a: e.tensor_single_scalar(out=mk[:, a, :], in_=lg[:, a, :], scalar=mx[:, a, 1:2], op=ALU.is_ge),
                         reads=[lgk, mxk], writes=[mkk])
                P.op("dve", lambda e: e.tensor_tensor(out=ex[:], in0=ex[:], in1=mk[:], op=ALU.mult),
                     reads=[exk, mkk], writes=[exk])
                P.op("dve", lambda e: e.tensor_reduce(out=dn[:], in_=ex[:], axis=mybir.AxisListType.X, op=ALU.add),
                     reads=[exk], writes=[dnk])
                P.op("dve", lambda e: e.reciprocal(out=dn[:], in_=dn[:]), reads=[dnk], writes=[dnk])
                P.op("dve", lambda e: e.tensor_tensor(out=ex[:], in0=ex[:], in1=dn[:].unsqueeze(2).to_broadcast([128, 4, NE]),
                                                      op=ALU.mult), reads=[exk, dnk], writes=[exk])
                co, cok = cmo[ji % 2]
                for a in range(4):
                    for ee in range(NE):
                        P.op("dve", lambda e, a=a, ee=ee: e.tensor_scalar(out=cbb[:, ee, :], in0=g.ident[:], scalar1=g.zeros[:, 0:1],
                                                                          scalar2=ex[:, a, ee:ee + 1], op0=ALU.mult, op1=ALU.add),
                             reads=[exk, "ident", (cbbk, ee)], writes=[(cbbk, ee)])
                    for ee in range(NE):
                        pse = g.ps[ee % 4]
                        P.op("pe", lambda e, a=a, ee=ee, pse=pse: e.matmul(pse[:, 0:128], lhsT=cbb[:, ee, :], rhs=g.ident[:],
                                                                          start=True, stop=True),
                             reads=[(cbbk, ee), "ident"], writes=["ps%d" % (ee % 4)])
                        P.op("act", lambda e, a=a, ee=ee, pse=pse, co=co: e.copy(out=co[:, ee, a * 128:(a + 1) * 128],
                                                                                in_=pse[:, 0:128]),
                             reads=["ps%d" % (ee % 4)], writes=[(cok, ee)])
                P.dma("pool", g.comb[:, :, s:s + n].rearrange("e p t -> p e t"), co[:],
                      reads=[(cok, ee) for ee in range(NE)], writes=[("COMB", lb)])
            else:
                norm_core(g, st, xt, xtk, n, Av, Bv, bufs, out_bf=(hx, hxk))
            P.dma("pool", blk_view(A["HX"], s, n), hx[:, :, 0:n], reads=[(hxk, c) for c in range(KT)],
                  writes=[(kind + "_HX", lb)])
        for ji, (kind, s, n, lb) in enumerate(jobs):
            job(ji, kind, s, n, lb)


def load_w(g, st, name, wdram, row0, col0, ncols, kt=KT):
    t, k = st.sb(name, [128, kt, ncols], BF16)
    g.P.dma("sp", t[:], wdram[row0:row0 + kt * 128, col0:col0 + ncols].rearrange("(k p) n -> p k n", p=128),
            reads=[], writes=[k])
    return t, k


def st_zu(g, l, jobs, bg_pieces=None):
    P = g.P
    with Stage(g) as st:
        w, wk = load_w(g, st, "wu", g.win, l * D, 0, 1024)
        hxs = st.ring("zhx", [128, KT, NB], BF16, 2)
        ub = st.ring("zub", [128, KT, NB], F32, 2)
        bg = bg_convert(g, st, bg_pieces, eng="act") if bg_pieces else None
        def job(ji, kind, s, n, lb):
            A = arrs(g, kind)
            hx, hxk = hxs[ji % 2]
            u, uk = ub[ji % 2]
            P.dma("sp", hx[:, :, 0:n], blk_view(A["HX"], s, n), reads=[(kind + "_HX", lb)], writes=[hxk])
            msk = Scol(g, "blkmask", lb) if kind == "L" else Scol(g, "one", 0)
            for m in range(KT):
                ps = g.ps[m % 4]
                for k in range(KT):
                    P.op("pe", lambda e, ps=ps, m=m, k=k, hx=hx: e.matmul(ps[:, 0:n], lhsT=w[:, k, m * 128:(m + 1) * 128],
                                                                         rhs=hx[:, k, 0:n], start=(k == 0), stop=(k == 7)),
                         reads=[wk, hxk], writes=["ps%d" % (m % 4)], sig=(k == 7))
                P.op("dve", lambda e, ps=ps, m=m, u=u, msk=msk: e.tensor_single_scalar(out=u[:, m, 0:n], in_=ps[:, 0:n], scalar=msk, op=ALU.mult),
                     reads=["ps%d" % (m % 4), "small"], writes=[(uk, m)])
                if bg is not None and m % 2 == 1:
                    next(bg, None)
            P.dma("pool", blk_view(A["U"], s, n), u[:, :, 0:n], reads=[(uk, m) for m in range(KT)],
                  writes=[(kind + "_U", lb)])
        for ji, (kind, s, n, lb) in enumerate(jobs):
            job(ji, kind, s, n, lb)
        if bg is not None:
            for _ in bg:
                pass


def st_zgm(g, l, jobs, bg_pieces=None, bg_every=2):
    P = g.P
    with Stage(g) as st:
        wg, wgk = load_w(g, st, "wg", g.win, l * D, 1024, 1024)
        wm, wmk = load_w(g, st, "wm", g.win, l * D, 4096, 2048)
        hxs = st.ring("zhx", [128, KT, NB], BF16, 2)
        ob = st.ring("zgo", [128, 3 * KT, NB], BF16, 2)
        bg = bg_convert(g, st, bg_pieces) if bg_pieces else None
        bgc = 0
        pi = 0
        def job(ji, kind, s, n, lb):
            nonlocal pi, bgc
            A = arrs(g, kind)
            hx, hxk = hxs[ji % 2]
            o, ok = ob[ji % 2]
            P.dma("sp", hx[:, :, 0:n], blk_view(A["HX"], s, n), reads=[(kind + "_HX", lb)], writes=[hxk])
            for m in range(3 * KT):
                ps = g.ps[pi % 6]
                psk = "ps%d" % (pi % 6)
                pi += 1
                wt, wtk, mm = (wg, wgk, m) if m < KT else (wm, wmk, m - KT)
                for k in range(KT):
                    P.op("pe", lambda e, ps=ps, wt=wt, mm=mm, k=k, hx=hx: e.matmul(
                        ps[:, 0:n], lhsT=wt[:, k, mm * 128:(mm + 1) * 128], rhs=hx[:, k, 0:n], start=(k == 0), stop=(k == 7)),
                        reads=[wtk, hxk], writes=[psk], sig=(k == 7))
                fn = AF.Gelu_apprx_tanh if m < KT else AF.Sigmoid
                P.op("act", lambda e, ps=ps, o=o, m=m, fn=fn: e.activation(out=o[:, m, 0:n], in_=ps[:, 0:n], func=fn),
                     reads=[psk], writes=[(ok, m)])
                if bg is not None:
                    bgc += 1
                    if bgc % bg_every == 0:
                        next(bg, None)
            P.dma("pool", blk_view(A["G"], s, n), o[:, 0:KT, 0:n], reads=[(ok, m) for m in range(KT)],
                  writes=[(kind + "_G", lb)])
            P.dma("pool", blk_view(A["SGA"], s, n), o[:, KT:2 * KT, 0:n], reads=[(ok, m) for m in range(KT, 2 * KT)],
                  writes=[(kind + "_SGA", lb)])
            P.dma("pool", blk_view(A["SGB"], s, n), o[:, 2 * KT:3 * KT, 0:n], reads=[(ok, m) for m in range(2 * KT, 3 * KT)],
                  writes=[(kind + "_SGB", lb)])
        for ji, (kind, s, n, lb) in enumerate(jobs):
            job(ji, kind, s, n, lb)
        if bg is not None:
            for _ in bg:
                pass


def st_zv(g, l, jobs, bg_pieces=None, bg_every=2):
    P = g.P
    with Stage(g) as st:
        w, wk = load_w(g, st, "wv", g.win, l * D, 2048, 2048)
        hxs = st.ring("zhx", [128, KT, NB], BF16, 2)
        vb = st.ring("zvb", [128, KT, NB], BF16, 2)
        sgs = st.ring("zsg", [128, NB], F32, 3)
        bg = bg_convert(g, st, bg_pieces, eng="act") if bg_pieces else None
        bgc = 0
        pi = 0
        def job(ji, kind, s, n, lb):
            nonlocal pi, bgc
            A = arrs(g, kind)
            hx, hxk = hxs[ji % 2]
            v, vk = vb[ji % 2]
            P.dma("sp", hx[:, :, 0:n], blk_view(A["HX"], s, n), reads=[(kind + "_HX", lb)], writes=[hxk])
            msk = Scol(g, "blkmask", lb) if kind == "L" else Scol(g, "one", 0)
            for m in range(KT):
                pa, pb = g.ps[(2 * pi) % 6], g.ps[(2 * pi + 1) % 6]
                pak, pbk = "ps%d" % ((2 * pi) % 6), "ps%d" % ((2 * pi + 1) % 6)
                sg, sgk = sgs[pi % 3]
                pi += 1
                for (ps, psk, mm) in ((pa, pak, m), (pb, pbk, KT + m)):
                    for k in range(KT):
                        P.op("pe", lambda e, ps=ps, mm=mm, k=k, hx=hx: e.matmul(
                            ps[:, 0:n], lhsT=w[:, k, mm * 128:(mm + 1) * 128], rhs=hx[:, k, 0:n], start=(k == 0), stop=(k == 7)),
                            reads=[wk, hxk], writes=[psk], sig=(k == 7))
                P.op("act", lambda e, pb=pb, sg=sg: e.activation(out=sg[:, 0:n], in_=pb[:, 0:n], func=AF.Sigmoid),
                     reads=[pbk], writes=[sgk])
                P.op("dve", lambda e, pa=pa, sg=sg, v=v, m=m, msk=msk: e.scalar_tensor_tensor(
                    out=v[:, m, 0:n], in0=pa[:, 0:n], scalar=msk, in1=sg[:, 0:n], op0=ALU.mult, op1=ALU.mult),
                    reads=[pak, sgk, "small"], writes=[(vk, m)])
                if bg is not None:
                    bgc += 1
                    if bgc % bg_every == 0:
                        next(bg, None)
            P.dma("pool", blk_view(A["V"], s, n), v[:, :, 0:n], reads=[(vk, m) for m in range(KT)],
                  writes=[(kind + "_V", lb)])
        for ji, (kind, s, n, lb) in enumerate(jobs):
            job(ji, kind, s, n, lb)
        if bg is not None:
            for _ in bg:
                pass


def st_scan(g, l, jobs):
    P = g.P
    with Stage(g) as st:
        gw, gwk = st.sb("gw", [128, 2, 2, KT, 128], BF16)
        P.dma("sp", gw[:].rearrange("p d t c m -> p (d t c) m"),
              g.gw[l * 4096:(l + 1) * 4096, :].rearrange("(x p) m -> p x m", p=128), reads=["gw_bf"], writes=[gwk])
        uhs = st.ring("uh", [128, KT, NB + 3], F32, 2)
        ucs = st.ring("uc", [128, KT, NB], F32, 2)
        ucb, ucbk = st.sb("ucb", [128, KT, NB], BF16)
        so = st.ring("so", [128, KT, NB], F32, 2)
        afo = st.ring("afo", [128, KT, NB], BF16, 2)
        abo = st.ring("abo", [128, KT, NB], BF16, 2)
        R = lambda nm, k=4: st.ring(nm, [128, NB], F32, k)
        rr, gi, aa, a2, bb, hh = R("rr"), R("gi"), R("aa"), R("a2"), R("bb"), R("hh", 4)
        rsm = st.ring("rsm", [128, 2], F32, 4)
        cw = S(g, "cw4_%d" % l)
        cb = S(g, "cb4_%d" % l)
        it = 0

        def job(ji, kind, s, n, lb):
            nonlocal it
            A = arrs(g, kind)
            uh, uhk = uhs[ji % 2]
            uc, uck = ucs[ji % 2]
            sot, sok = so[ji % 2]
            aft, afk = afo[ji % 2]
            abt, abk = abo[ji % 2]
            rks = [(kind + "_U", b) for b in ((lb - 1, lb, lb + 1) if kind == "L" else (0,))]
            P.dma("sp", uh[:, :, 0:n + 3], blk_view(A["U"], s - 2, n + 3), reads=rks, writes=[uhk])
            for c in range(KT):
                P.op("dve", lambda e, c=c: e.tensor_scalar(out=uc[:, c, 0:n], in0=uh[:, c, 0:n],
                                                          scalar1=cw[:, c:c + 1], scalar2=cb[:, c:c + 1],
                                                          op0=ALU.mult, op1=ALU.add),
                     reads=[uhk, "small"], writes=[(uck, c)])
                for j in range(1, 4):
                    P.op("dve", lambda e, c=c, j=j: e.scalar_tensor_tensor(
                        out=uc[:, c, 0:n], in0=uh[:, c, j:j + n], scalar=cw[:, j * 8 + c:j * 8 + c + 1], in1=uc[:, c, 0:n],
                        op0=ALU.mult, op1=ALU.add), reads=[uhk, "small", (uck, c)], writes=[(uck, c)])
                P.op("pool", lambda e, c=c: e.tensor_copy(out=ucb[:, c, 0:n], in_=uc[:, c, 0:n]),
                     reads=[(uck, c)], writes=[(ucbk, c)])
            def stA(c):
                nonlocal it
                T = []
                for d in range(2):
                    T.append((rr[it % 4], gi[it % 4], aa[it % 4], a2[it % 4], bb[it % 4], hh[it % 4], rsm[it % 4],
                              g.ps[(2 * it) % 8], "ps%d" % ((2 * it) % 8), g.ps[(2 * it + 1) % 8], "ps%d" % ((2 * it + 1) % 8)))
                    it += 1
                for d in range(2):
                    pr, prk, pi_, pik = T[d][7], T[d][8], T[d][9], T[d][10]
                    P.op("pe", lambda e, pr=pr, d=d, c=c: e.matmul(pr[:, 0:n], lhsT=gw[:, d, 0, c, :], rhs=ucb[:, c, 0:n],
                                                                  start=True, stop=True),
                         reads=[gwk, (ucbk, c)], writes=[prk])
                    P.op("pe", lambda e, pi_=pi_, d=d, c=c: e.matmul(pi_[:, 0:n], lhsT=gw[:, d, 1, c, :], rhs=ucb[:, c, 0:n],
                                                                    start=True, stop=True),
                         reads=[gwk, (ucbk, c)], writes=[pik])
                for d in range(2):
                    (r_, rk_), (gi_, gik_), _, _, _, _, (rs_, rsk_), pr, prk, pi_, pik = T[d]
                    br = Scol(g, "br%d%d" % (l, d), c)
                    bi = Scol(g, "bi%d%d" % (l, d), c)
                    P.op("act", lambda e, pr=pr, r_=r_, br=br, rs_=rs_: e.activation(out=r_[:, 0:n], in_=pr[:, 0:n], func=AF.Sigmoid, bias=br,
                                                                                accum_out=rs_[:, 0:1]),
                         reads=[prk, "small"], writes=[rk_, rsk_])
                    P.op("act", lambda e, pi_=pi_, gi_=gi_, bi=bi: e.activation(out=gi_[:, 0:n], in_=pi_[:, 0:n], func=AF.Sigmoid, bias=bi),
                         reads=[pik, "small"], writes=[gik_])
                for d in range(2):
                    (r_, rk_), _, (a_, ak_), _, _, _, (rs_, rsk_) = T[d][0:7]
                    P.op("act", lambda e, r_=r_, a_=a_, d=d, c=c: e.activation(out=a_[:, 0:n], in_=r_[:, 0:n], func=AF.Exp,
                                                                             scale=g.cl[:, l, d, 0, c:c + 1]),
                         reads=[rk_, "cl"], writes=[ak_])
                    if kind == "L" and 4 <= lb < 12:
                        P.op("act", lambda e, rs_=rs_, c=c, d=d: e.activation(
                            out=g.sumt[:, c, lb - 4, 2 * d:2 * d + 1], in_=rs_[:, 0:1], func=AF.Exp, scale=g.cl[:, l, d, 0, c:c + 1]),
                            reads=[rsk_, "cl"], writes=["sumt"])
                for d in range(2):
                    _, _, (a_, ak_), (a2_, a2k_) = T[d][0:4]
                    P.op("dve", lambda e, a_=a_, a2_=a2_: e.tensor_tensor(out=a2_[:, 0:n], in0=a_[:, 0:n], in1=a_[:, 0:n], op=ALU.mult),
                         reads=[ak_], writes=[a2k_])
                return T

            def stB(c, T):
                for d in range(2):
                    (a2_, a2k_) = T[d][3]
                    P.op("act", lambda e, a2_=a2_: e.activation(out=a2_[:, 0:n], in_=a2_[:, 0:n], func=AF.Sqrt, scale=-1.0, bias=1.0),
                         reads=[a2k_], writes=[a2k_])
                for d in range(2):
                    _, (gi_, gik_), (a_, ak_), (a2_, a2k_), (b_, bk_), (h_, hk_) = T[d][0:6]
                    P.op("pool", lambda e, gi_=gi_, c=c, b_=b_: e.tensor_tensor(out=b_[:, 0:n], in0=gi_[:, 0:n], in1=uc[:, c, 0:n], op=ALU.mult),
                         reads=[gik_, (uck, c)], writes=[bk_])
                    P.op("pool", lambda e, a2_=a2_, b_=b_: e.tensor_tensor(out=b_[:, 0:n], in0=b_[:, 0:n], in1=a2_[:, 0:n], op=ALU.mult),
                         reads=[bk_, a2k_], writes=[bk_])
                    At, Atk = (aft, afk) if d == 0 else (abt, abk)
                    if d == 0:
                        P.op("dve", lambda e, a_=a_, b_=b_, h_=h_: e.tensor_tensor_scan(out=h_[:, 0:n], data0=a_[:, 0:n], data1=b_[:, 0:n],
                                                                                       initial=0.0, op0=ALU.mult, op1=ALU.add),
                             reads=[ak_, bk_], writes=[hk_])
                        P.op("dve", lambda e, a_=a_, At=At, c=c: e.tensor_tensor_scan(out=At[:, c, 0:n], data0=a_[:, 0:n], data1=g.zeros[:, 0:n],
                                                                                     initial=1.0, op0=ALU.mult, op1=ALU.add),
                             reads=[ak_, "zeros"], writes=[(Atk, c)])
                        hfwd = (h_, hk_)
                        e0, e1 = n - 1, n
                    else:
                        P.op("dve", lambda e, a_=a_, b_=b_, h_=h_: e.tensor_tensor_scan(out=h_[:, 0:n][:, ::-1],
                                                                                       data0=a_[:, 0:n][:, ::-1], data1=b_[:, 0:n][:, ::-1],
                                                                                       initial=0.0, op0=ALU.mult, op1=ALU.add),
                             reads=[ak_, bk_], writes=[hk_])
                        P.op("dve", lambda e, a_=a_, At=At, c=c: e.tensor_tensor_scan(out=At[:, c, 0:n][:, ::-1], data0=a_[:, 0:n][:, ::-1],
                                                                                     data1=g.zeros[:, 0:n], initial=1.0, op0=ALU.mult, op1=ALU.add),
                             reads=[ak_, "zeros"], writes=[(Atk, c)])
                        hf_, hfk_ = hfwd
                        P.op("dve", lambda e, h_=h_, hf_=hf_, c=c: e.tensor_tensor(out=sot[:, c, 0:n], in0=hf_[:, 0:n], in1=h_[:, 0:n], op=ALU.add),
                             reads=[hk_, hfk_], writes=[(sok, c)])
                        e0, e1 = 0, 1
                    if kind == "L" and 4 <= lb < 12:
                        P.op("pool", lambda e, h_=h_, c=c, d=d, e0=e0, e1=e1: e.tensor_copy(
                            out=g.sumt[:, c, lb - 4, 2 * d + 1:2 * d + 2], in_=h_[:, e0:e1]), reads=[hk_, "sumt"], writes=["sumt"])
                    if kind == "C":
                        P.op("pool", lambda e, h_=h_, c=c, d=d, e0=e0, e1=e1: e.tensor_copy(
                            out=g.sctx[:, d, c:c + 1], in_=h_[:, e0:e1]), reads=[hk_, "sctx"], writes=["sctx"])

            Tn = stA(0)
            for c in range(KT):
                Tc = Tn
                if c + 1 < KT:
                    Tn = stA(c + 1)
                stB(c, Tc)
            P.dma("pool", blk_view(A["S"], s, n), sot[:, :, 0:n], reads=[(sok, c) for c in range(KT)], writes=[(kind + "_S", lb)])
            P.dma("pool", blk_view(A["AF"], s, n), aft[:, :, 0:n], reads=[(afk, c) for c in range(KT)], writes=[(kind + "_AF", lb)])
            P.dma("pool", blk_view(A["AB"], s, n), abt[:, :, 0:n], reads=[(abk, c) for c in range(KT)], writes=[(kind + "_AB", lb)])
        for ji, (kind, s, n, lb) in enumerate(jobs):
            job(ji, kind, s, n, lb)


def st_carry(g, l):
    P = g.P
    with Stage(g) as st:
        P.dma("sp", g.sum_in, g.sumt[:].rearrange("p c b f -> p (c b f)"), reads=["sumt"], writes=["sum_in"])
        P.collective("AllGather", ins=[g.sum_in], outs=[g.sum_all], groups=[[0, 1, 2, 3], [4, 5, 6, 7]],
                     reads=["sum_in"], writes=["sum_all"])
        sa, sak = st.sb("sa", [128, 4, KT, 8, 4], F32)
        P.dma("sp", sa[:], g.sum_all.rearrange("(r p) (c b f) -> p r c b f", p=128, c=KT, b=8), reads=["sum_all"], writes=[sak])
        ht, htk = st.sb("ht", [128, 2, KT, 48], F32)
        P.op("dve", lambda e: e.memset(ht[:], 0.0), writes=[htk])
        tmp, tmk = st.sb("ctmp", [128, KT], F32)
        P.op("dve", lambda e: e.tensor_copy(out=ht[:, 0, :, 8], in_=g.sctx[:, 0, :]), reads=["sctx", htk], writes=[htk])
        for gb in range(32):
            r, b = gb // 8, gb % 8
            P.op("dve", lambda e, r=r, b=b, gb=gb: e.tensor_tensor(out=tmp[:], in0=sa[:, r, :, b, 0], in1=ht[:, 0, :, 8 + gb], op=ALU.mult),
                 reads=[sak, htk], writes=[tmk])
            P.op("dve", lambda e, r=r, b=b, gb=gb: e.tensor_tensor(out=ht[:, 0, :, 9 + gb], in0=tmp[:], in1=sa[:, r, :, b, 1], op=ALU.add),
                 reads=[sak, tmk, htk], writes=[htk])
        P.op("dve", lambda e: e.tensor_copy(out=ht[:, 1, :, 40], in_=g.sctx[:, 1, :]), reads=["sctx", htk], writes=[htk])
        for gb in range(31, -1, -1):
            r, b = gb // 8, gb % 8
            P.op("dve", lambda e, r=r, b=b, gb=gb: e.tensor_tensor(out=tmp[:], in0=sa[:, r, :, b, 2], in1=ht[:, 1, :, 9 + gb], op=ALU.mult),
                 reads=[sak, htk], writes=[tmk])
            P.op("dve", lambda e, r=r, b=b, gb=gb: e.tensor_tensor(out=ht[:, 1, :, 8 + gb], in0=tmp[:], in1=sa[:, r, :, b, 3], op=ALU.add),
                 reads=[sak, tmk, htk], writes=[htk])
        P.dma("sp", g.htab.rearrange("d p c x -> p d c x"), ht[:], reads=[htk], writes=["htab"])

        def dyn(e, d):
            base = dynval(g, e, "b%d" % d)
            return e.dma_start(out=g.hin[:, d, :, :],
                               in_=g.htab[d:d + 1, :, :, bass.ds(base, 12)].rearrange("o p c x -> p (o c) x"))
        P.dma("sp", None, None, reads=["htab"], writes=["hin"], fn=lambda e: dyn(e, 0))
        P.dma("sp", None, None, reads=["htab"], writes=["hin"], fn=lambda e: dyn(e, 1))


def st_conv(g, l, kinds, bg_pieces=None):
    P = g.P
    with Stage(g) as st:
        dg = st.ring("dg", [128, 31, 128], BF16, 2)
        vrl = st.ring("vrl", [128, TL], BF16, 2)
        cvo = st.ring("cvo", [128, NB], F32, 3)
        bg = bg_convert(g, st, bg_pieces, eng="act") if bg_pieces else None
        oi = 0
        for c in range(KT):
            dgt, dgk = dg[c % 2]
            for j in range(31):
                P.op("dve", lambda e, j=j, c=c, dgt=dgt: e.tensor_single_scalar(out=dgt[:, j, :], in_=g.ident[:],
                                                                        scalar=Scol(g, "cw31_%d" % l, j * 8 + c), op=ALU.mult),
                     reads=["ident", "small", (dgk, j)], writes=[(dgk, j)])
            for (kind, lbs) in kinds:
                A = arrs(g, kind)
                vr, vrk = vrl[oi % 2]
                if kind == "L":
                    lo, hi = (lbs[0] - 2) * NB, (lbs[-1] + 3) * NB
                    P.dma("sp", vr[:, lo:hi], A["V"][c, :, lo:hi], reads=[("L_V", b) for b in range(lbs[0] - 2, lbs[-1] + 3)],
                          writes=[vrk])
                    stride = 64
                    blocks = [(lb * NB, NB, lb) for lb in lbs]
                else:
                    P.dma("sp", vr[:, 0:TC], A["V"][c, :, :], reads=[("C_V", 0), "C_V"], writes=[vrk])
                    stride = 1
                    blocks = [(CPAD, CTX, 0)]
                for (s, n, lb) in blocks:
                    ps = g.ps[oi % 4]
                    psk = "ps%d" % (oi % 4)
                    o, ok = cvo[oi % 3]
                    oi += 1
                    for j in range(31):
                        off = s + (j - 15) * stride
                        P.op("pe", lambda e, ps=ps, j=j, off=off, vr=vr, dgt=dgt, n=n: e.matmul(
                            ps[:, 0:n], lhsT=dgt[:, j, :], rhs=vr[:, off:off + n], start=(j == 0), stop=(j == 30)),
                            reads=[(dgk, j), vrk], writes=[psk], sig=(j == 30))
                    P.op("act", lambda e, ps=ps, o=o, n=n, c=c: e.activation(out=o[:, 0:n], in_=ps[:, 0:n], func=AF.Identity,
                                                                           bias=Scol(g, "cb31_%d" % l, c), scale=1.0),
                         reads=[psk, "small"], writes=[ok])
                    P.dma("pool", A["CV"][c, :, s:s + n], o[:, 0:n], reads=[ok], writes=[(kind + "_CV", lb, c)])
                    if bg is not None:
                        next(bg, None)
        if bg is not None:
            for _ in bg:
                pass


def st_mix(g, l, jobs):
    P = g.P
    H = 256
    with Stage(g) as st:
        wa, wak = load_w(g, st, "wa", g.wa, l * D, 0, D)
        wb, wbk = load_w(g, st, "wb", g.wb, l * D, 0, D)
        wo, wok = load_w(g, st, "wo", g.wo, l * D, 0, D)
        cvs = st.ring("mcv", [128, KT, H], F32, 2)
        ss = st.ring("ms", [128, KT, H], F32, 2)
        afs = st.ring("maf", [128, KT, H], BF16, 2)
        abs_ = st.ring("mab", [128, KT, H], BF16, 2)
        gs = st.ring("mg", [128, KT, H], BF16, 2)
        sgas = st.ring("msga", [128, KT, H], BF16, 2)
        sgbs = st.ring("msgb", [128, KT, H], BF16, 2)
        xts = st.ring("mxt", [128, KT, H], F32, 2)
        cvb, cvbk = st.sb("cvb", [128, KT, H], BF16)
        sqb, sqbk = st.sb("sqb", [128, KT, H], BF16)
        lno, lnok = st.sb("lno", [128, KT, H], BF16)
        mbt, mbk = st.sb("mbt", [128, KT, H], F32)
        hst, hsk = st.sb("hst", [128, KT, H], F32)
        hsb, hsbk = st.sb("hsb", [128, KT, H], BF16)
        mt, mtk = st.sb("mt", [128, KT, H], BF16)
        mu, muk = st.sb("mu", [128, H], F32)
        var, vark = st.sb("var", [128, H], F32)
        tmp1, tmp1k = st.sb("tmp1", [128, H], F32)
        lng, lnb = S(g, "lng%d" % l), S(g, "lnb%d" % l)
        it = 0
        pi = 0

        def nps():
            nonlocal pi
            r = (g.ps[pi % 6], "ps%d" % (pi % 6))
            pi += 1
            return r
        halves_ = []
        for (kind, s0, n0, lb) in jobs:
            A = arrs(g, kind)
            si = 0 if kind == "L" else 1
            def half_body(kind, lb, s0, n0, s, A, si):
                nonlocal it
                n = min(H, s0 + n0 - s)
                cv, cvk = cvs[it % 2]
                sst, ssk = ss[it % 2]
                af, afk = afs[it % 2]
                ab, abk = abs_[it % 2]
                gt, gk = gs[it % 2]
                sga, sgak = sgas[it % 2]
                sgb, sgbk = sgbs[it % 2]
                xt, xtk = xts[it % 2]
                it += 1
                def prep():
                    P.dma("sp", cv[:, :, 0:n], blk_view(A["CV"], s, n), reads=[(kind + "_CV", lb, c) for c in range(KT)], writes=[cvk])
                    P.dma("sp", sst[:, :, 0:n], blk_view(A["S"], s, n), reads=[(kind + "_S", lb)], writes=[ssk])
                    P.dma("sp", af[:, :, 0:n], blk_view(A["AF"], s, n), reads=[(kind + "_AF", lb)], writes=[afk])
                    P.dma("sp", ab[:, :, 0:n], blk_view(A["AB"], s, n), reads=[(kind + "_AB", lb)], writes=[abk])
                    P.dma("sp", gt[:, :, 0:n], blk_view(A["G"], s, n), reads=[(kind + "_G", lb)], writes=[gk])
                    P.dma("sp", sga[:, :, 0:n], blk_view(A["SGA"], s, n), reads=[(kind + "_SGA", lb)], writes=[sgak])
                    P.dma("sp", sgb[:, :, 0:n], blk_view(A["SGB"], s, n), reads=[(kind + "_SGB", lb)], writes=[sgbk])
                    P.op("act", lambda e, cv=cv: e.copy(out=cvb[:, :, 0:n], in_=cv[:, :, 0:n]), reads=[cvk], writes=[cvbk])
                    P.op("act", lambda e, cv=cv: e.activation(out=sqb[:, :, 0:n], in_=cv[:, :, 0:n], func=AF.Square), reads=[cvk], writes=[sqbk])
                    p1, p1k = g.ps[6], "ps6"
                    p2, p2k = g.ps[7], "ps7"
                    for k in range(KT):
                        P.op("pe", lambda e, k=k: e.matmul(p1[:, 0:n], lhsT=g.ones_b[:], rhs=cvb[:, k, 0:n], start=(k == 0), stop=(k == 7)),
                             reads=[cvbk, "ones_b"], writes=[p1k], sig=(k == 7))
                    for k in range(KT):
                        P.op("pe", lambda e, k=k: e.matmul(p2[:, 0:n], lhsT=g.ones_b[:], rhs=sqb[:, k, 0:n], start=(k == 0), stop=(k == 7)),
                             reads=[sqbk, "ones_b"], writes=[p2k], sig=(k == 7))
                    P.op("dve", lambda e: e.tensor_single_scalar(out=mu[:, 0:n], in_=p1[:, 0:n], scalar=1.0 / D, op=ALU.mult),
                         reads=[p1k], writes=[muk])
                    P.op("dve", lambda e: e.tensor_tensor(out=tmp1[:, 0:n], in0=mu[:, 0:n], in1=mu[:, 0:n], op=ALU.mult),
                         reads=[muk], writes=[tmp1k])
                    P.op("dve", lambda e: e.scalar_tensor_tensor(out=var[:, 0:n], in0=p2[:, 0:n], scalar=1.0 / D, in1=tmp1[:, 0:n],
                                                                 op0=ALU.mult, op1=ALU.subtract),
                         reads=[p2k, tmp1k], writes=[vark])
                    P.op("dve", lambda e: e.tensor_single_scalar(out=var[:, 0:n], in_=var[:, 0:n], scalar=0.0, op=ALU.max),
                         reads=[vark], writes=[vark])
                    P.op("act", lambda e: e.activation(out=var[:, 0:n], in_=var[:, 0:n], func=AF.Sqrt, bias=EPS, scale=1.0),
                         reads=[vark], writes=[vark])
                    P.op("dve", lambda e: e.reciprocal(out=var[:, 0:n], in_=var[:, 0:n]), reads=[vark], writes=[vark])
                    P.op("dve", lambda e, cv=cv: e.tensor_tensor(out=cv[:, :, 0:n], in0=cv[:, :, 0:n],
                                                                in1=mu[:, 0:n].unsqueeze(1).to_broadcast([128, KT, n]), op=ALU.subtract),
                         reads=[cvk, muk], writes=[cvk])
                    P.op("dve", lambda e, cv=cv: e.tensor_tensor(out=cv[:, :, 0:n], in0=cv[:, :, 0:n],
                                                                in1=var[:, 0:n].unsqueeze(1).to_broadcast([128, KT, n]), op=ALU.mult),
                         reads=[cvk, vark], writes=[cvk])
                    for c in range(KT):
                        P.op("act", lambda e, c=c, cv=cv: e.activation(out=lno[:, c, 0:n], in_=cv[:, c, 0:n], func=AF.Silu,
                                                                      scale=lng[:, c:c + 1], bias=lnb[:, c:c + 1]),
                             reads=[cvk, "small"], writes=[(lnok, c)])
                def main1():
                    for m in range(KT):
                        ps, psk = nps()
                        for k in range(KT):
                            P.op("pe", lambda e, ps=ps, m=m, k=k: e.matmul(ps[:, 0:n], lhsT=wb[:, k, m * 128:(m + 1) * 128], rhs=lno[:, k, 0:n],
                                                                          start=(k == 0), stop=(k == 7)),
                                 reads=[wbk, (lnok, k)], writes=[psk], sig=(k == 7))
                        P.op("dve", lambda e, ps=ps, m=m, sgb=sgb: e.tensor_tensor(out=mbt[:, m, 0:n], in0=ps[:, 0:n], in1=sgb[:, m, 0:n], op=ALU.mult),
                             reads=[psk, sgbk], writes=[(mbk, m)])
                def main2():
                    P.dma("sp", xt[:, :, 0:n], blk_view(A["XT"], s, n), reads=[(kind + "_XT", lb)], writes=[xtk])
                    hin = g.hin if kind == "L" else g.hzero
                    bidx = (lb - 2) if kind == "L" else 0
                    for c in range(KT):
                        P.op("dve", lambda e, c=c, af=af, sst=sst: e.scalar_tensor_tensor(
                            out=hst[:, c, 0:n], in0=af[:, c, 0:n], scalar=hin[:, 0, c, bidx:bidx + 1], in1=sst[:, c, 0:n],
                            op0=ALU.mult, op1=ALU.add), reads=[afk, ssk, "hin", "hzero"], writes=[(hsk, c)])
                        P.op("dve", lambda e, c=c, ab=ab: e.scalar_tensor_tensor(
                            out=hst[:, c, 0:n], in0=ab[:, c, 0:n], scalar=hin[:, 1, c, bidx:bidx + 1], in1=hst[:, c, 0:n],
                            op0=ALU.mult, op1=ALU.add), reads=[abk, (hsk, c), "hin", "hzero"], writes=[(hsk, c)])
                        P.op("pool", lambda e, c=c, gt=gt: e.tensor_tensor(out=hsb[:, c, 0:n], in0=hst[:, c, 0:n], in1=gt[:, c, 0:n], op=ALU.mult),
                             reads=[(hsk, c), gk], writes=[(hsbk, c)])
                    for m in range(KT):
                        ps, psk = nps()
                        for k in range(KT):
                            P.op("pe", lambda e, ps=ps, m=m, k=k: e.matmul(ps[:, 0:n], lhsT=wa[:, k, m * 128:(m + 1) * 128], rhs=hsb[:, k, 0:n],
                                                                          start=(k == 0), stop=(k == 7)),
                                 reads=[wak, (hsbk, k)], writes=[psk], sig=(k == 7))
                        P.op("dve", lambda e, ps=ps, m=m, sga=sga: e.tensor_tensor(out=hst[:, m, 0:n], in0=ps[:, 0:n], in1=sga[:, m, 0:n], op=ALU.mult),
                             reads=[psk, sgak], writes=[(hsk, m)])
                        P.op("pool", lambda e, m=m: e.tensor_tensor(out=mt[:, m, 0:n], in0=hst[:, m, 0:n], in1=mbt[:, m, 0:n], op=ALU.add),
                             reads=[(hsk, m), (mbk, m)], writes=[(mtk, m)])
                    g1 = modvec(g, l, si, "G1")
                    for m in range(KT):
                        ps, psk = nps()
                        for k in range(KT):
                            P.op("pe", lambda e, ps=ps, m=m, k=k: e.matmul(ps[:, 0:n], lhsT=wo[:, k, m * 128:(m + 1) * 128], rhs=mt[:, k, 0:n],
                                                                          start=(k == 0), stop=(k == 7)),
                                 reads=[wok, (mtk, k)], writes=[psk], sig=(k == 7))
                        P.op("dve", lambda e, ps=ps, m=m, xt=xt: e.scalar_tensor_tensor(
                            out=xt[:, m, 0:n], in0=ps[:, 0:n], scalar=g1[:, m:m + 1], in1=xt[:, m, 0:n], op0=ALU.mult, op1=ALU.add),
                            reads=[psk, xtk, "modx", "modc"], writes=[xtk])
                    P.dma("pool", blk_view(A["XT"], s, n), xt[:, :, 0:n], reads=[xtk], writes=[(kind + "_XT", lb)])
                return prep, main1, main2
            for s in range(s0, s0 + n0, H):
                halves_.append(half_body(kind, lb, s0, n0, s, A, si))
        halves_[0][0]()
        for i_ in range(len(halves_)):
            halves_[i_][1]()
            if i_ + 1 < len(halves_):
                halves_[i_ + 1][0]()
            halves_[i_][2]()


def st_ffn(g, l, sblocks, moe, publish=False):
    P = g.P
    CH = 256
    NCH = DFF // CH
    TB = 1024
    with Stage(g) as st:
        hxs = st.ring("fhx", [128, KT, TB], BF16, 1)
        acc, acck = st.sb("facc", [128, KT, TB], F32)
        cmb = st.ring("fcmb", [128, TB], F32, 2)
        w1s = st.ring("fw1", [128, KT, 2 * CH], BF16, 3)
        w3s = st.ring("fw3", [128, KT, 2 * CH], BF16, 3)
        w2s = st.ring("fw2", [128, 4, D], BF16, 3)
        sil = st.ring("fsil", [128, NB], BF16, 3)
        gts = st.ring("fgt", [128, 4, NB], BF16, 2)
        xts = st.ring("fxt", [128, KT, NB], F32, 2)
        ne = NE if moe else 1
        wi = 0
        hi_ = 0
        gi_ = 0
        pi = 0
        groups = [(c0, min(2, NCH - c0)) for c0 in range(0, NCH, 2)]
        def sb_body(kind, s0, n0, lbs):
            nonlocal wi, hi_, gi_, pi
            A = arrs(g, kind)
            si = 0 if kind == "L" else 1
            hx, hxk = hxs[0]
            P.dma("sp", hx[:, :, 0:n0], blk_view(A["HX"], s0, n0), reads=[(kind + "_HX", lb) for lb in lbs], writes=[hxk])
            halves = [(o, min(NB, n0 - o)) for o in range(0, n0, NB)]
            steps = [(ex, gi2, c0, nc_, ho, hn) for ex in range(ne) for gi2, (c0, nc_) in enumerate(groups) for (ho, hn) in halves]
            loaded = {}
            cms = {}

            def ensure(ex, gi2, c0, nc_):
                nonlocal wi
                if moe and ex not in cms:
                    cm, cmk = cmb[ex % 2]
                    P.dma("sp", cm[:, 0:n0], g.comb[ex, :, s0:s0 + n0], reads=[("COMB", lb) for lb in lbs], writes=[cmk])
                    cms[ex] = (cm, cmk)
                if (ex, gi2) in loaded:
                    return
                if moe:
                    W1, W3, W2 = g.m1, g.m3, g.m2
                    r1, r2 = ex * D, ex * DFF
                else:
                    W1, W3, W2 = g.f1, g.f3, g.f2
                    r1, r2 = 0, 0
                w1, w1k = w1s[wi % 3]
                w3, w3k = w3s[wi % 3]
                w2, w2k = w2s[wi % 3]
                wi += 1
                cw_ = nc_ * CH
                nj = nc_ * 2
                P.dma("sp", w1[:, :, 0:cw_], W1[r1:r1 + D, c0 * CH:c0 * CH + cw_].rearrange("(k p) n -> p k n", p=128),
                      reads=[], writes=[w1k])
                P.dma("sp", w3[:, :, 0:cw_], W3[r1:r1 + D, c0 * CH:c0 * CH + cw_].rearrange("(k p) n -> p k n", p=128),
                      reads=[], writes=[w3k])
                P.dma("sp", w2[:, 0:nj, :], W2[r2 + c0 * CH:r2 + c0 * CH + cw_, :].rearrange("(k p) n -> p k n", p=128),
                      reads=[], writes=[w2k])
                loaded[(ex, gi2)] = (w1, w1k, w3, w3k, w2, w2k, nj)

            def emit_h(step):
                nonlocal hi_, gi_, pi
                ex, gi2, c0, nc_, ho, hn = step
                ensure(ex, gi2, c0, nc_)
                w1, w1k, w3, w3k, w2, w2k, nj = loaded[(ex, gi2)]
                gt, gtk = gts[gi_ % 2]
                gi_ += 1
                for j in range(nj):
                    p1, p1k = g.ps[pi % 4], "ps%d" % (pi % 4)
                    p3, p3k = g.ps[(pi + 1) % 4], "ps%d" % ((pi + 1) % 4)
                    pi += 2
                    for k in range(KT):
                        P.op("pe", lambda e, p1=p1, w1=w1, j=j, k=k, ho=ho, hn=hn: e.matmul(
                            p1[:, 0:hn], lhsT=w1[:, k, j * 128:(j + 1) * 128], rhs=hx[:, k, ho:ho + hn], start=(k == 0), stop=(k == 7)),
                            reads=[w1k, hxk], writes=[p1k], sig=(k == 7))
                    for k in range(KT):
                        P.op("pe", lambda e, p3=p3, w3=w3, j=j, k=k, ho=ho, hn=hn: e.matmul(
                            p3[:, 0:hn], lhsT=w3[:, k, j * 128:(j + 1) * 128], rhs=hx[:, k, ho:ho + hn], start=(k == 0), stop=(k == 7)),
                            reads=[w3k, hxk], writes=[p3k], sig=(k == 7))
                    sl, slk = sil[hi_ % 3]
                    hi_ += 1
                    P.op("act", lambda e, p1=p1, sl=sl, hn=hn: e.activation(out=sl[:, 0:hn], in_=p1[:, 0:hn], func=AF.Silu),
                         reads=[p1k], writes=[slk])
                    if moe:
                        cm, cmk = cms[ex]
                        P.op("pool", lambda e, sl=sl, cm=cm, ho=ho, hn=hn: e.tensor_tensor(out=sl[:, 0:hn], in0=sl[:, 0:hn],
                                                                                          in1=cm[:, ho:ho + hn], op=ALU.mult),
                             reads=[slk, cmk], writes=[slk])
                    P.op("dve", lambda e, p3=p3, sl=sl, gt=gt, j=j, hn=hn: e.tensor_tensor(out=gt[:, j, 0:hn], in0=p3[:, 0:hn],
                                                                                          in1=sl[:, 0:hn], op=ALU.mult),
                         reads=[p3k, slk], writes=[(gtk, j)])
                return (gt, gtk)

            def emit_w2(step, H, first):
                ex, gi2, c0, nc_, ho, hn = step
                w1, w1k, w3, w3k, w2, w2k, nj = loaded[(ex, gi2)]
                gt, gtk = H
                for m in range(KT):
                    po, pok = g.ps[4 + (m % 4)], "ps%d" % (4 + (m % 4))
                    for j in range(nj):
                        P.op("pe", lambda e, po=po, w2=w2, j=j, m=m, gt=gt, hn=hn, nj=nj: e.matmul(
                            po[:, 0:hn], lhsT=w2[:, j, m * 128:(m + 1) * 128], rhs=gt[:, j, 0:hn], start=(j == 0), stop=(j == nj - 1)),
                            reads=[w2k, (gtk, j)], writes=[pok], sig=(j == nj - 1))
                    if first:
                        P.op("act", lambda e, po=po, m=m, ho=ho, hn=hn: e.copy(out=acc[:, m, ho:ho + hn], in_=po[:, 0:hn]),
                             reads=[pok], writes=[(acck, m, ho)])
                    else:
                        P.op("dve", lambda e, po=po, m=m, ho=ho, hn=hn: e.tensor_tensor(out=acc[:, m, ho:ho + hn], in0=po[:, 0:hn],
                                                                                       in1=acc[:, m, ho:ho + hn], op=ALU.add),
                             reads=[pok, (acck, m, ho)], writes=[(acck, m, ho)])

            Hc = emit_h(steps[0])
            for i in range(len(steps)):
                Hn = emit_h(steps[i + 1]) if i + 1 < len(steps) else None
                emit_w2(steps[i], Hc, first=(i < len(halves)))
                Hc = Hn
            g2 = modvec(g, l, si, "G2")
            for hi2, (ho, hn) in enumerate(halves):
                xt, xtk = xts[hi2 % 2]
                lb = lbs[hi2] if kind == "L" else 0
                P.dma("sp", xt[:, :, 0:hn], blk_view(A["XT"], s0 + ho, hn), reads=[(kind + "_XT", lb)], writes=[xtk])
                for m in range(KT):
                    P.op("dve", lambda e, m=m, xt=xt, ho=ho, hn=hn: e.scalar_tensor_tensor(
                        out=xt[:, m, 0:hn], in0=acc[:, m, ho:ho + hn], scalar=g2[:, m:m + 1], in1=xt[:, m, 0:hn], op0=ALU.mult, op1=ALU.add),
                        reads=[(acck, m, ho), xtk, "modx", "modc"], writes=[xtk])
                P.dma("pool", blk_view(A["XT"], s0 + ho, hn), xt[:, :, 0:hn], reads=[xtk], writes=[(kind + "_XT", lb)])
                if publish and kind == "L" and lb in (4, 5, 10, 11):
                    eb = (4, 5, 10, 11).index(lb)
                    for cg in range(2):
                        P.dma("pool", g.ex_in[2 * eb + cg].rearrange("p (c t) -> p c t", c=4), xt[:, 4 * cg:4 * cg + 4, :],
                              reads=[xtk], writes=[("ex_in", 2 * eb + cg)])
                        P.collective("AllGather", ins=[g.ex_in[2 * eb + cg]], outs=[g.ex_all[2 * eb + cg].rearrange("r p f -> (r p) f")],
                                     groups=[[0, 1, 2, 3], [4, 5, 6, 7]], reads=[("ex_in", 2 * eb + cg)],
                                     writes=[("ex_all", 2 * eb + cg)])
        for (kind, s0, n0, lbs) in sblocks:
            sb_body(kind, s0, n0, lbs)


def st_exchange(g):
    P = g.P
    XT = g.lat["XT"]
    with Stage(g) as st:
        hb = st.ring("exh", [128, 4, NB], F32, 3)
        i = 0
        for (lb, eb, off) in ((2, 2, 3), (3, 3, 3), (12, 0, 1), (13, 1, 1)):
            for cg in range(2):
                u = 2 * eb + cg
                t, tk = hb[i % 3]
                i += 1

                for hh_ in range(2):
                    def dyn(e, t=t, u=u, off=off, hh_=hh_):
                        r = dynval(g, e, "rl" if off == 3 else "rr")
                        return e.dma_start(out=t[:, 2 * hh_:2 * hh_ + 2, :].rearrange("p c t -> p (c t)"),
                                           in_=g.ex_all[u][bass.ds(r, 1), :, 1024 * hh_:1024 * hh_ + 1024].rearrange("o p f -> p (o f)"))
                    P.dma("sp", None, None, reads=[("ex_all", u)], writes=[tk], fn=dyn)
                P.dma("sp", XT[4 * cg:4 * cg + 4, :, lb * NB:(lb + 1) * NB].rearrange("c p t -> p c t"), t[:],
                      reads=[tk], writes=[("L_XT", lb, cg)])


def st_final(g):
    P = g.P
    with Stage(g) as st:
        xts = st.ring("oxt", [128, KT, NB], F32, 2)
        ys = st.ring("oy", [128, KT, NB], F32, 2)
        outs = st.ring("oo", [128, 4, D], F32, 2)
        bufs = {"sq": st.sb("osq", [128, KT, NB], BF16), "rs": st.sb("ors", [128, NB], F32)}
        fg = S(g, "fing")
        for ji, lb in enumerate(range(4, 12)):
            xt, xtk = xts[ji % 2]
            y, yk = ys[ji % 2]
            oo, ook = outs[ji % 2]
            P.dma("sp", xt[:], blk_view(g.lat["XT"], lb * NB, NB), reads=[("L_XT", lb)], writes=[xtk])
            norm_core(g, st, xt, xtk, NB, fg, None, bufs, out_f32=(y, yk))
            for a in range(4):
                for half in range(2):
                    ps = g.ps[(a * 2 + half) % 6]
                    psk = "ps%d" % ((a * 2 + half) % 6)
                    for cc in range(4):
                        c = half * 4 + cc
                        P.op("pe", lambda e, ps=ps, y=y, a=a, c=c, cc=cc: e.transpose(
                            out=ps[:, cc * 128:(cc + 1) * 128], in_=y[:, c, a * 128:(a + 1) * 128], identity=g.ident[:]),
                            reads=[(yk, c), "ident"], writes=[psk])
                    if half == 0:
                        P.op("act", lambda e, ps=ps, oo=oo, a=a: e.copy(out=oo[:, a, 0:512], in_=ps[:, 0:512]),
                             reads=[psk], writes=[(ook, a, 0)])
                    else:
                        P.op("dve", lambda e, ps=ps, oo=oo, a=a: e.tensor_copy(out=oo[:, a, 512:1024], in_=ps[:, 0:512]),
                             reads=[psk], writes=[(ook, a, 1)])
            P.dma("pool", g.out[(lb - 4) * NB:(lb - 3) * NB, :].rearrange("(a p) d -> p a d", p=128), oo[:],
                  reads=[(ook, a, h) for a in range(4) for h in range(2)], writes=[("out", lb)])


def build():
    g = build_program()
    L = lambda lbs: seg_blocks("L", lbs)
    C = seg_blocks("C", None)

    def stop(name):
        return STOP_AFTER == name
    st_setup(g)
    if stop("setup"):
        g.P.emit(); return g
    st_convert(g, "mix0")
    st_transpose_in(g)
    if stop("tin"):
        g.P.emit(); return g
    stopped = False
    for l in range(DEPTH):
        last = l == DEPTH - 1
        nblk = list(range(2, 14))
        ublk = list(range(3, 13))
        pblk = list(range(4, 12))
        if l == 1:
            st_exchange(g)
        st_norm(g, l, 1, L(nblk) + C)
        st_zu(g, l, L(ublk) + C, bg_pieces=(conv_pieces(g, "mix1") if l == 0 else None))
        st_scan(g, l, C + L(pblk))
        if stop("scan%d" % l):
            stopped = True
            break
        st_carry(g, l)
        if stop("carry%d" % l):
            stopped = True
            break
        bgA = bgB = bgC = None
        if l == 0 and not NO_MOE:
            pcs = conv_pieces(g, "moe")
            n1_, n2_ = (len(pcs) * 4) // 10, (len(pcs) * 7) // 10
            bgA, bgB, bgC = pcs[:n1_], pcs[n1_:n2_], pcs[n2_:]
        st_zgm(g, l, L(pblk) + (C if not last else []), bg_pieces=bgA, bg_every=4)
        st_zv(g, l, L(nblk) + (C if not last else []), bg_pieces=bgB, bg_every=3)
        st_conv(g, l, [("L", pblk)] + ([("C", None)] if not last else []), bg_pieces=bgC)
        if stop("conv%d" % l):
            stopped = True
            break
        st_mix(g, l, L(pblk) + (C if not last else []))
        if stop("mix%d" % l):
            stopped = True
            break
        moe = (l % 2 == 1)
        st_norm(g, l, 2, L(pblk) + (C if not last else []), router=moe)
        sbl = [("L", pblk[i] * NB, 2 * NB, [pblk[i], pblk[i + 1]]) for i in range(0, len(pblk), 2)]
        if not last:
            sbl = [sbl[0], sbl[-1]] + sbl[1:-1]
            sbl.append(("C", CPAD, CTX, [0]))
        st_ffn(g, l, sbl, moe, publish=not last)
        if stop("ffn%d" % l):
            stopped = True
            break
    if not stopped:
        st_final(g)
    g.P.emit()
    return g


def make_in_maps(inp):
    x = np.asarray(inp["x"], np.float32)
    maps = []
    gw = np.zeros((DEPTH, 2, 2, 8, 128, 128), np.float32)
    for l in range(DEPTH):
        for d in range(2):
            for ti, nm in enumerate(("lru_wr", "lru_wi")):
                w = np.asarray(inp[nm][l][d], np.float32)
                for c in range(8):
                    gw[l, d, ti, c, 0:64, 0:64] = w[2 * c]
                    gw[l, d, ti, c, 64:128, 64:128] = w[2 * c + 1]
    gw = gw.reshape(-1, 128)
    shared = {} if NO_MOE else {
        "moe_w1": np.ascontiguousarray(np.asarray(inp["moe_w1"], np.float32)[0].reshape(NE * D, DFF)),
        "moe_w3": np.ascontiguousarray(np.asarray(inp["moe_w3"], np.float32)[0].reshape(NE * D, DFF)),
        "moe_w2": np.ascontiguousarray(np.asarray(inp["moe_w2"], np.float32)[0].reshape(NE * DFF, D)),
    }
    shared.update({
        "w_in": np.ascontiguousarray(np.asarray(inp["w_in"], np.float32).reshape(DEPTH * D, 6144)),
        "w_a": np.ascontiguousarray(np.asarray(inp["w_branch_a"], np.float32).reshape(DEPTH * D, D)),
        "w_b": np.ascontiguousarray(np.asarray(inp["w_branch_b"], np.float32).reshape(DEPTH * D, D)),
        "w_o": np.ascontiguousarray(np.asarray(inp["w_out"], np.float32).reshape(DEPTH * D, D)),
        "ffn_w1": np.ascontiguousarray(np.asarray(inp["ffn_w1"], np.float32)[0]),
        "ffn_w3": np.ascontiguousarray(np.asarray(inp["ffn_w3"], np.float32)[0]),
        "ffn_w2": np.ascontiguousarray(np.asarray(inp["ffn_w2"], np.float32)[0]),
        "gatew": gw,
        "router": np.ascontiguousarray(np.asarray(inp["moe_router"], np.float32)[0]),
    })
    modw = np.asarray(inp["mod_w"], np.float32)
    for core in range(8):
        b, q = core // 4, core % 4
        xl = np.zeros((TL, D), np.float32)
        g0 = q * 4096 - 4 * NB
        lo, hi = max(g0, 0), min(g0 + TL, 16384)
        xl[lo - g0:hi - g0] = x[b, lo:hi]
        m = dict(shared)
        m["x_loc"] = xl
        m["ctx_in"] = np.ascontiguousarray(np.asarray(inp["ctx"], np.float32)[b])
        m["small"] = _build_small(inp, core)
        m["modw"] = np.ascontiguousarray(modw[:, :, q * 1536:(q + 1) * 1536])
        maps.append(m)
    return maps


_CACHE = {}


def kernel(**inputs):
    if "g" not in _CACHE:
        _CACHE["g"] = build()
    g = _CACHE["g"]
    maps = make_in_maps(inputs)
    res = run_bass_kernel_spmd(g.nc, maps, core_ids=list(range(8)))
    out = np.zeros((2, 16384, D), np.float32)
    for core in range(8):
        b, q = core // 4, core % 4
        out[b, q * 4096:(q + 1) * 4096] = res.results[core]["out"]
    _CACHE["last"] = res
    return out
```

```python
import numpy as np
from contextlib import ExitStack
import concourse.bass as bass
import concourse.mybir as mybir
from concourse.bass_utils import run_bass_kernel_spmd

F32 = mybir.dt.float32
BF16 = mybir.dt.bfloat16
AF = mybir.ActivationFunctionType
ALU = mybir.AluOpType

D = 1024
KT = 8
NBLK = 16
NB = 512
TL = NBLK * NB
CTX = 256
CPAD = 16
TC = CTX + 2 * CPAD
DFF = 2816
NE = 8
EPS = 1e-6
DEPTH = 2
SEM_ROT = 30000
N_DMA_SEM = 28

DEBUG_OUT = []
STOP_AFTER = None
NO_MOE = False


class _Op:
    __slots__ = ("eng", "fn", "waits", "tok", "clock", "is_dma", "sig")


class Prog:
    ENG = ("pe", "dve", "act", "pool", "sp")

    def __init__(self, nc):
        self.nc = nc
        self.ops = {e: [] for e in self.ENG}
        self.known = {e: {} for e in self.ENG}
        self.cnt = {e: 0 for e in self.ENG}
        self.cur_sem = {}
        self.sems = {}
        self.nsem = 0
        self.own_done = {e: {} for e in self.ENG}
        for e in self.ENG:
            self.cur_sem[e] = self._new_sem(e)
        self.dma_sems = {}
        self.dma_uses = {}
        self.dma_last = {}
        self.dma_rr = {}
        for q in ("sp", "pool"):
            self.dma_sems[q] = [self._new_sem("d" + q) for _ in range(N_DMA_SEM)]
            self.dma_rr[q] = 0
            for s in self.dma_sems[q]:
                self.dma_uses[s] = 0
                self.dma_last[s] = None
        self.last_w = {}
        self.readers = {}
        self.nops = 0
        self.last_op = {e: None for e in self.ENG}
        self.pending_nosig = {e: 0 for e in self.ENG}
        self.uid = 0

    def _new_sem(self, tag):
        sid = self.nsem
        self.nsem += 1
        self.sems[sid] = self.nc.alloc_semaphore(name="s%d_%s" % (sid, tag))
        return sid

    def name(self, base):
        self.uid += 1
        return "%s_%d" % (base, self.uid)

    def _deps(self, eng, reads, writes, is_pe):
        deps = []
        for k in reads:
            w = self.last_w.get(k)
            if w is not None:
                deps.append(w)
        for k in writes:
            w = self.last_w.get(k)
            if w is not None:
                deps.append(w)
            for r in self.readers.get(k, ()):
                deps.append(r)
        waits = {}
        kn = self.known[eng]
        for d in deps:
            if is_pe and d.eng == "pe" and not d.is_dma:
                continue
            sid, val = d.tok
            if kn.get(sid, 0) >= val:
                continue
            if waits.get(sid, 0) < val:
                waits[sid] = val
        for d in deps:
            for sid, val in d.clock.items():
                if kn.get(sid, 0) < val:
                    kn[sid] = val
        return waits

    def _register(self, op, reads, writes):
        for k in reads:
            lst = self.readers.setdefault(k, [])
            if not op.is_dma:
                lst[:] = [r for r in lst if r.is_dma or r.eng != op.eng]
            lst.append(op)
        for k in writes:
            self.last_w[k] = op
            self.readers[k] = []

    def op(self, eng, fn, reads=(), writes=(), sig=True):
        o = _Op()
        o.eng = eng
        o.fn = fn
        o.is_dma = False
        o.sig = sig
        o.waits = self._deps(eng, reads, writes, eng == "pe")
        if self.cnt[eng] >= SEM_ROT and self.pending_nosig[eng] == 0:
            self.own_done[eng][self.cur_sem[eng]] = self.cnt[eng]
            self.cur_sem[eng] = self._new_sem(eng)
            self.cnt[eng] = 0
        if sig:
            self.cnt[eng] += 1
            self.pending_nosig[eng] = 0
            o.tok = (self.cur_sem[eng], self.cnt[eng])
        else:
            self.pending_nosig[eng] += 1
            o.tok = (self.cur_sem[eng], self.cnt[eng] + 1)
        o.clock = dict(self.known[eng])
        o.clock.update(self.own_done[eng])
        o.clock[o.tok[0]] = o.tok[1]
        self._register(o, reads, writes)
        self.ops[eng].append(o)
        self.last_op[eng] = o
        self.nops += 1
        return o

    def dma(self, q, out, in_, reads=(), writes=(), fn=None):
        o = _Op()
        o.eng = q
        o.is_dma = True
        waits = self._deps(q, reads, writes, False)
        sems = self.dma_sems[q]
        sid = sems[self.dma_rr[q] % len(sems)]
        self.dma_rr[q] += 1
        prev = self.dma_last[sid]
        kn = self.known[q]
        if prev is not None and kn.get(sid, 0) < prev.tok[1]:
            waits[sid] = max(waits.get(sid, 0), prev.tok[1])
            kn[sid] = prev.tok[1]
        self.dma_uses[sid] += 1
        o.tok = (sid, 16 * self.dma_uses[sid])
        self.dma_last[sid] = o
        o.waits = waits
        o.fn = ("dynfn", fn) if fn is not None else ("dma", out, in_)
        o.clock = dict(kn)
        o.clock[sid] = o.tok[1]
        self._register(o, reads, writes)
        self.ops[q].append(o)
        self.nops += 1
        return o

    def collective(self, kind, ins, outs, groups, reads=(), writes=()):
        o = _Op()
        o.eng = "pool"
        o.is_dma = True
        o.waits = self._deps("pool", reads, writes, False)
        sid = self._new_sem("cc")
        o.tok = (sid, 1)
        o.fn = ("cc", kind, ins, outs, groups)
        o.clock = dict(self.known["pool"])
        o.clock[sid] = 1
        self._register(o, reads, writes)
        self.ops["pool"].append(o)
        self.nops += 1
        return o

    def barrier(self):
        toks = {}
        for e in self.ENG:
            lo = self.last_op[e]
            if lo is not None:
                toks[lo.tok[0]] = max(toks.get(lo.tok[0], 0), lo.tok[1])
        for sid, lo in self.dma_last.items():
            if lo is not None:
                toks[sid] = max(toks.get(sid, 0), lo.tok[1])
        for k, w in self.last_w.items():
            if w is not None and w.is_dma:
                toks[w.tok[0]] = max(toks.get(w.tok[0], 0), w.tok[1])
        for e in self.ENG:
            kn = self.known[e]
            waits = {}
            for sid, val in toks.items():
                if kn.get(sid, 0) < val:
                    if e == "pe" and sid == self.cur_sem["pe"]:
                        continue
                    waits[sid] = val
                    kn[sid] = val
            if waits:
                o = _Op()
                o.eng = e
                o.is_dma = False
                o.waits = waits
                o.fn = None
                o.tok = None
                o.clock = {}
                self.ops[e].append(o)
        self.last_w = {}
        self.readers = {}

    def _replay(self, ename, e):
        sems = self.sems
        for o in self.ops[ename]:
            for sid, val in o.waits.items():
                e.wait_ge(sems[sid], val)
            if o.fn is None:
                continue
            if o.is_dma:
                if o.fn[0] == "dynfn":
                    o.fn[1](e).then_inc(sems[o.tok[0]], 16)
                elif o.fn[0] == "dma":
                    e.dma_start(out=o.fn[1], in_=o.fn[2]).then_inc(sems[o.tok[0]], 16)
                else:
                    _, kind, ins, outs, groups = o.fn
                    e.collective_compute(kind, ALU.bypass, replica_groups=groups,
                                         ins=ins, outs=outs).then_inc(sems[o.tok[0]])
            elif o.sig:
                o.fn(e).then_inc(sems[o.tok[0]], 1)
            else:
                o.fn(e)

    def emit(self):
        self.barrier()
        with self.nc.Block() as block:
            @block.sync
            def _(e):
                self._replay("sp", e)

            @block.tensor
            def _(e):
                self._replay("pe", e)

            @block.vector
            def _(e):
                self._replay("dve", e)

            @block.scalar
            def _(e):
                self._replay("act", e)

            @block.gpsimd
            def _(e):
                self._replay("pool", e)


def _small_layout():
    off = {}
    n = 0

    def add(name, w):
        nonlocal n
        off[name] = (n, w)
        n += w
    for l in range(DEPTH):
        add("n1g%d" % l, 8)
        add("n2g%d" % l, 8)
        add("modb%d" % l, 48)
        add("cw4_%d" % l, 32)
        add("cb4_%d" % l, 8)
        for d in range(2):
            add("br%d%d" % (l, d), 8)
            add("bi%d%d" % (l, d), 8)
            add("lam%d%d" % (l, d), 8)
        add("cw31_%d" % l, 31 * 8)
        add("cb31_%d" % l, 8)
        add("lng%d" % l, 8)
        add("lnb%d" % l, 8)
    add("fing", 8)
    add("blkmask", NBLK)
    add("one", 1)
    add("boh", 2)
    add("cvec", 32)
    return off, n


SOFF, NS = _small_layout()


def _pc(v):
    return np.ascontiguousarray(np.asarray(v, np.float32).reshape(8, 128).T)


def _build_small(inp, core):
    b, q = core // 4, core % 4
    s = np.zeros((128, NS), np.float32)

    def put(name, arr):
        o, w = SOFF[name]
        s[:, o:o + w] = np.asarray(arr, np.float32).reshape(128, w)
    for l in range(DEPTH):
        put("n1g%d" % l, _pc(inp["norm1_g"][l]))
        put("n2g%d" % l, _pc(inp["norm2_g"][l]))
        put("modb%d" % l, inp["mod_b"][l].reshape(48, 128).T)
        put("cw4_%d" % l, np.concatenate([_pc(inp["rnn_conv_w"][l][j]) for j in range(4)], axis=1))
        put("cb4_%d" % l, _pc(inp["rnn_conv_b"][l]))
        for d in range(2):
            put("br%d%d" % (l, d), _pc(inp["lru_br"][l][d]))
            put("bi%d%d" % (l, d), _pc(inp["lru_bi"][l][d]))
            put("lam%d%d" % (l, d), _pc(inp["lru_lam"][l][d]))
        put("cw31_%d" % l, np.concatenate([_pc(inp["conv_w"][l][j]) for j in range(31)], axis=1))
        put("cb31_%d" % l, _pc(inp["conv_b"][l]))
        put("lng%d" % l, _pc(inp["conv_ln_g"][l]))
        put("lnb%d" % l, _pc(inp["conv_ln_b"][l]))
    put("fing", _pc(inp["final_g"]))
    bm = np.zeros((128, NBLK), np.float32)
    for lb in range(NBLK):
        g0 = q * 4096 + (lb - 4) * NB
        bm[:, lb] = 1.0 if (0 <= g0 < 16384) else 0.0
    put("blkmask", bm)
    put("one", np.ones((128, 1), np.float32))
    boh = np.zeros((128, 2), np.float32)
    boh[:, b] = 1.0
    put("boh", boh)
    cv = np.zeros((128, 8, 4), np.float32)
    cv[:, :, 0] = _pc(inp["c"][0])
    cv[:, :, 1] = _pc(inp["c"][1])
    cv[:, :, 2] = _pc(inp["c_ctx"])
    put("cvec", cv.reshape(128, 32))
    return s


class G:
    pass


def build_program():
    nc = bass.Bass("TRN2", target_bir_lowering=False)
    P = Prog(nc)
    g = G()
    g.nc, g.P = nc, P

    def din(name, shape, dt=F32):
        return nc.dram_tensor(name, list(shape), dt, kind="ExternalInput").ap()

    def dscr(name, shape, dt=F32):
        kind = "ExternalOutput" if name in DEBUG_OUT else "Internal"
        return nc.dram_tensor(name, list(shape), dt, kind=kind).ap()

    g.x_loc = din("x_loc", [TL, D])
    g.ctx_in = din("ctx_in", [CTX, D])
    g.small_in = din("small", [128, NS])
    g.modw_in = din("modw", [DEPTH, D, 1536])
    g.win_in = din("w_in", [DEPTH * D, 6144])
    g.wa_in = din("w_a", [DEPTH * D, D])
    g.wb_in = din("w_b", [DEPTH * D, D])
    g.wo_in = din("w_o", [DEPTH * D, D])
    g.f1_in = din("ffn_w1", [D, DFF])
    g.f3_in = din("ffn_w3", [D, DFF])
    g.f2_in = din("ffn_w2", [DFF, D])
    if not NO_MOE:
        g.m1_in = din("moe_w1", [NE * D, DFF])
        g.m3_in = din("moe_w3", [NE * D, DFF])
        g.m2_in = din("moe_w2", [NE * DFF, D])
    g.gw_in = din("gatew", [DEPTH * 2 * 2 * 8 * 128, 128])
    g.rt_in = din("router", [D, NE])
    g.out = nc.dram_tensor("out", [8 * NB, D], F32, kind="ExternalOutput").ap()

    g.win = dscr("win_bf", [DEPTH * D, 6144], BF16)
    g.wa = dscr("wa_bf", [DEPTH * D, D], BF16)
    g.wb = dscr("wb_bf", [DEPTH * D, D], BF16)
    g.wo = dscr("wo_bf", [DEPTH * D, D], BF16)
    g.f1 = dscr("f1_bf", [D, DFF], BF16)
    g.f3 = dscr("f3_bf", [D, DFF], BF16)
    g.f2 = dscr("f2_bf", [DFF, D], BF16)
    g.m1 = dscr("m1_bf", [NE * D, DFF], BF16)
    g.m3 = dscr("m3_bf", [NE * D, DFF], BF16)
    g.m2 = dscr("m2_bf", [NE * DFF, D], BF16)
    g.gw = dscr("gw_bf", [DEPTH * 2 * 2 * 8 * 128, 128], BF16)

    def seg_arrays(pfx, T):
        a = {}
        for nm, dt in (("XT", F32), ("HX", BF16), ("U", F32), ("G", BF16), ("V", BF16),
                       ("SGA", BF16), ("SGB", BF16), ("S", F32), ("AF", BF16), ("AB", BF16),
                       ("CV", F32)):
            a[nm] = dscr(pfx + nm, [KT, 128, T], dt)
        return a
    g.lat = seg_arrays("L_", TL)
    g.ctxa = seg_arrays("C_", TC)
    g.comb = dscr("COMB", [NE, 128, TL], F32)
    g.sum_in = dscr("sum_in", [128, 256], F32)
    g.sum_all = dscr("sum_all", [4 * 128, 256], F32)
    g.mod_in = dscr("mod_in", [128, 96], F32)
    g.mod_all = dscr("mod_all", [4 * 128, 96], F32)
    g.htab = dscr("htab", [2, 128, KT, 48], F32)
    g.ex_in = dscr("ex_in", [8, 128, 2048], F32)
    g.ex_all = [dscr("ex_all%d" % u, [4, 128, 2048], F32) for u in range(8)]

    def sb(name, shape, dt=F32):
        return nc.alloc_sbuf_tensor("sb_" + name, list(shape), dt)
    g.small = sb("small", [128, NS])
    g.ident = sb("ident", [128, 128])
    g.ones_b = sb("ones_b", [128, 128], BF16)
    g.zeros = sb("zeros", [128, NB])
    g.modx = sb("modx", [128, DEPTH, 48])
    g.modc = sb("modc", [128, DEPTH, 48])
    g.nrm = sb("nrm", [128, DEPTH, 2, 4, 8])
    g.cl = sb("cl", [128, DEPTH, 2, 2, 8])
    g.sumt = sb("sumt", [128, KT, 8, 4])
    g.sctx = sb("sctx", [128, 2, KT])
    g.hin = sb("hin", [128, 2, KT, 12])
    g.hzero = sb("hzero", [128, 2, KT, 12])
    g.ps = [nc.alloc_psum_tensor("psb%d" % i, [128, NB], F32) for i in range(8)]
    return g


def S(g, name, w=None):
    o, ww = SOFF[name]
    return g.small[:, o:o + (ww if w is None else w)]


def Scol(g, name, i):
    o, _ = SOFF[name]
    return g.small[:, o + i:o + i + 1]


def dynval(g, e, name):
    if not hasattr(g, "_dyn"):
        pid = e.partition_id()
        g._dyn = {
            "rl": e.snap((pid + 3) % 4, min_val=0, max_val=3),
            "rr": e.snap((pid + 1) % 4, min_val=0, max_val=3),
            "b0": e.snap((pid % 4) * 8 + 6, min_val=6, max_val=30),
            "b1": e.snap((pid % 4) * 8 + 7, min_val=7, max_val=31),
        }
    return g._dyn[name]


class Stage:
    def __init__(self, g):
        self.g = g
        self.es = ExitStack()

    def __enter__(self):
        self.g.P.barrier()
        return self

    def __exit__(self, *a):
        self.g.P.barrier()
        self.es.close()
        return False

    def sb(self, base, shape, dt=F32):
        nm = self.g.P.name(base)
        t = self.es.enter_context(self.g.nc.sbuf_tensor(nm, list(shape), dt))
        return t, nm

    def ring(self, base, shape, dt, n):
        return [self.sb(base, shape, dt) for _ in range(n)]


def blk_view(arr, s, n):
    return arr[:, :, s:s + n].rearrange("c p t -> p c t")


def st_setup(g):
    P, nc = g.P, g.nc
    with Stage(g) as st:
        P.dma("sp", g.small[:], g.small_in, reads=[], writes=["small"])
        P.op("pool", lambda e: e.memset(g.ident[:], 0.0), writes=["ident"])
        P.op("pool", lambda e: e.affine_select(out=g.ident[:], in_=g.ident[:], pattern=[[-1, 128]],
                                               compare_op=ALU.not_equal, fill=1.0, base=0,
                                               channel_multiplier=1),
             reads=["ident"], writes=["ident"])
        P.op("pool", lambda e: e.memset(g.ones_b[:], 1.0), writes=["ones_b"])
        P.op("pool", lambda e: e.memset(g.zeros[:], 0.0), writes=["zeros"])
        P.op("pool", lambda e: e.memset(g.hzero[:], 0.0), writes=["hzero"])
        zt, zk = st.sb("zt", [128, KT, TC], F32)
        zb, zbk = st.sb("zb", [128, KT, TC], BF16)
        P.op("dve", lambda e: e.memset(zt[:], 0.0), writes=[zk])
        P.op("dve", lambda e: e.memset(zb[:], 0.0), writes=[zbk])
        P.dma("sp", blk_view(g.ctxa["U"], 0, TC), zt[:], reads=[zk], writes=["C_U"])
        P.dma("sp", blk_view(g.ctxa["V"], 0, TC), zb[:], reads=[zbk], writes=["C_V"])
        sc, sck = st.sb("sc", [128, 8, 4], F32)
        P.op("act", lambda e: e.activation(out=sc[:].rearrange("p a b -> p (a b)"), in_=S(g, "cvec"),
                                           func=AF.Silu), reads=["small"], writes=[sck])
        mw = st.ring("mw", [128, 8, 1536], F32, 1)
        mo, mok = st.sb("mo", [128, 96], F32)
        psm = g.ps[0]
        for l in range(DEPTH):
            t, tk = mw[0]
            P.dma("sp", t[:], g.modw_in[l].rearrange("(k p) n -> p k n", p=128), reads=[], writes=[tk])
            for c in range(12):
                for k in range(8):
                    P.op("pe", lambda e, t=t, c=c, k=k, l=l: e.matmul(
                        psm[:, (l * 12 + c) * 4:(l * 12 + c) * 4 + 4], lhsT=t[:, k, c * 128:(c + 1) * 128],
                        rhs=sc[:, k, :], start=(k == 0), stop=(k == 7)),
                        reads=[tk, sck], writes=["ps0"], sig=(k == 7))
        P.op("dve", lambda e: e.tensor_copy(out=mo[:], in_=psm[:, 0:96]), reads=["ps0"], writes=[mok])
        P.dma("sp", g.mod_in, mo[:], reads=[mok], writes=["mod_in"])
        P.collective("AllGather", ins=[g.mod_in], outs=[g.mod_all], groups=[[0, 1, 2, 3], [4, 5, 6, 7]],
                     reads=["mod_in"], writes=["mod_all"])
        ma, mak = st.sb("ma", [128, DEPTH, 4, 12, 4], F32)
        for l in range(DEPTH):
            P.dma("sp", ma[:, l], g.mod_all.rearrange("(r p) (l c j) -> p l r c j", p=128, l=DEPTH, c=12)[:, l],
                  reads=["mod_all"], writes=[mak])
        for l in range(DEPTH):
            v = ma[:, l].rearrange("p r c j -> p (r c) j")
            mb = S(g, "modb%d" % l)
            P.op("dve", lambda e, v=v, l=l: e.tensor_single_scalar(out=g.modx[:, l, :], in_=v[:, :, 0],
                                                            scalar=Scol(g, "boh", 0), op=ALU.mult),
                 reads=[mak, "small"], writes=["modx"])
            P.op("dve", lambda e, v=v, l=l: e.scalar_tensor_tensor(out=g.modx[:, l, :], in0=v[:, :, 1],
                                                                   scalar=Scol(g, "boh", 1), in1=g.modx[:, l, :],
                                                                   op0=ALU.mult, op1=ALU.add),
                 reads=[mak, "small", "modx"], writes=["modx"])
            P.op("dve", lambda e, l=l, mb=mb: e.tensor_tensor(out=g.modx[:, l, :], in0=g.modx[:, l, :], in1=mb, op=ALU.add),
                 reads=["modx", "small"], writes=["modx"])
            P.op("dve", lambda e, v=v, l=l, mb=mb: e.tensor_tensor(out=g.modc[:, l, :], in0=v[:, :, 2], in1=mb, op=ALU.add),
                 reads=[mak, "small"], writes=["modc"])
            for si, mv in enumerate((g.modx, g.modc)):
                P.op("dve", lambda e, l=l, si=si, mv=mv: e.scalar_tensor_tensor(
                    out=g.nrm[:, l, si, 0, :], in0=mv[:, l, 8:16], scalar=1.0, in1=S(g, "n1g%d" % l),
                    op0=ALU.add, op1=ALU.mult), reads=["modx", "modc", "small"], writes=["nrm"])
                P.op("dve", lambda e, l=l, si=si, mv=mv: e.scalar_tensor_tensor(
                    out=g.nrm[:, l, si, 1, :], in0=mv[:, l, 32:40], scalar=1.0, in1=S(g, "n2g%d" % l),
                    op0=ALU.add, op1=ALU.mult), reads=["modx", "modc", "small", "nrm"], writes=["nrm"])
            for d in range(2):
                tmp, tmk = st.sb("cltmp", [128, 8], F32)
                P.op("act", lambda e, l=l, d=d, tmp=tmp: e.activation(out=tmp[:], in_=S(g, "lam%d%d" % (l, d)),
                                                                      func=AF.Exp, scale=-1.0),
                     reads=["small"], writes=[tmk])
                P.op("act", lambda e, tmp=tmp: e.activation(out=tmp[:], in_=tmp[:], func=AF.Ln, bias=1.0),
                     reads=[tmk], writes=[tmk])
                P.op("dve", lambda e, l=l, d=d, tmp=tmp: e.tensor_single_scalar(out=g.cl[:, l, d, 0, :], in_=tmp[:],
                                                                         scalar=-8.0, op=ALU.mult),
                     reads=[tmk], writes=["cl"])
                P.op("dve", lambda e, l=l, d=d, tmp=tmp: e.tensor_single_scalar(out=g.cl[:, l, d, 1, :], in_=tmp[:],
                                                                         scalar=-16.0, op=ALU.mult),
                     reads=[tmk, "cl"], writes=["cl"])


def modvec(g, l, si, which):
    mv = g.modx if si == 0 else g.modc
    if which == "A1":
        return g.nrm[:, l, si, 0, :]
    if which == "A2":
        return g.nrm[:, l, si, 1, :]
    return {"B1": mv[:, l, 0:8], "G1": mv[:, l, 16:24], "B2": mv[:, l, 24:32], "G2": mv[:, l, 40:48]}[which]


def st_convert(g, which):
    P = g.P
    if which == "mix":
        pairs = [(g.win_in, g.win), (g.wa_in, g.wa), (g.wb_in, g.wb), (g.wo_in, g.wo), (g.gw_in, g.gw),
                 (g.f1_in, g.f1), (g.f3_in, g.f3), (g.f2_in, g.f2)]
    else:
        pairs = [(g.m1_in, g.m1), (g.m3_in, g.m3), (g.m2_in, g.m2)]
    wi_ = 0
    W = 4096
    with Stage(g) as st:
        src = st.ring("cvs", [128, W], F32, 3)
        dst = st.ring("cvd", [128, W], BF16, 3)
        i = 0
        for a, b in pairs:
            av = a.rearrange("(p r) n -> p (r n)", p=128)
            bv = b.rearrange("(p r) n -> p (r n)", p=128)
            Fd = av.shape[1]
            for o in range(0, Fd, W):
                w = min(W, Fd - o)
                s, sk = src[i % 3]
                d, dk = dst[i % 3]
                P.dma("sp", s[:, 0:w], av[:, o:o + w], reads=[], writes=[sk])
                eng = ("act", "dve", "act")[i % 3]
                if eng == "act":
                    P.op("act", lambda e, s=s, d=d, w=w: e.copy(out=d[:, 0:w], in_=s[:, 0:w]), reads=[sk], writes=[dk])
                else:
                    P.op(eng, lambda e, s=s, d=d, w=w: e.tensor_copy(out=d[:, 0:w], in_=s[:, 0:w]), reads=[sk], writes=[dk])
                P.dma("pool", bv[:, o:o + w], d[:, 0:w], reads=[dk], writes=[("wcv", id(b), o)])
                i += 1


def conv_pieces(g, which, W=4096):
    if which == "mix":
        pairs = [(g.win_in, g.win), (g.wa_in, g.wa), (g.wb_in, g.wb), (g.wo_in, g.wo), (g.gw_in, g.gw),
                 (g.f1_in, g.f1), (g.f3_in, g.f3), (g.f2_in, g.f2)]
    else:
        pairs = [(g.m1_in, g.m1), (g.m3_in, g.m3), (g.m2_in, g.m2)]
    out = []
    for a, b in pairs:
        av = a.rearrange("(p r) n -> p (r n)", p=128)
        bv = b.rearrange("(p r) n -> p (r n)", p=128)
        Fd = av.shape[1]
        for o in range(0, Fd, W):
            out.append((av, bv, o, min(W, Fd - o), id(b)))
    return out


def bg_convert(g, st, pieces, eng="dve", W=4096):
    P = g.P
    src = st.ring("bgs", [128, W], F32, 3)
    dst = st.ring("bgd", [128, W], BF16, 3)
    for i, (av, bv, o, w, bid) in enumerate(pieces):
        s_, sk = src[i % 3]
        d_, dk = dst[i % 3]
        P.dma("sp", s_[:, 0:w], av[:, o:o + w], reads=[], writes=[sk])
        if eng == "act":
            P.op("act", lambda e, s_=s_, d_=d_, w=w: e.copy(out=d_[:, 0:w], in_=s_[:, 0:w]), reads=[sk], writes=[dk])
        else:
            P.op(eng, lambda e, s_=s_, d_=d_, w=w: e.tensor_copy(out=d_[:, 0:w], in_=s_[:, 0:w]), reads=[sk], writes=[dk])
        P.dma("pool", bv[:, o:o + w], d_[:, 0:w], reads=[dk], writes=[("wcv", bid, o)])
        yield


def st_transpose_in(g):
    P = g.P
    with Stage(g) as st:
        xin = st.ring("xin", [128, 4, D], F32, 2)
        xtb = st.ring("xtb", [128, KT, NB], F32, 2)
        jobs = [("L", lb) for lb in range(2, 14)] + [("C", 0)]
        for ji, (kind, lb) in enumerate(jobs):
            xi, xik = xin[ji % 2]
            xo, xok = xtb[ji % 2]
            if kind == "L":
                na = 4
                P.dma("sp", xi[:], g.x_loc[lb * NB:(lb + 1) * NB, :].rearrange("(a p) d -> p a d", p=128), writes=[xik])
            else:
                na = 2
                P.dma("sp", xi[:, 0:2], g.ctx_in.rearrange("(a p) d -> p a d", p=128), writes=[xik])
            for c in range(KT):
                ps = g.ps[c]
                for a in range(na):
                    P.op("pe", lambda e, ps=ps, xi=xi, a=a, c=c: e.transpose(
                        out=ps[:, a * 128:(a + 1) * 128], in_=xi[:, a, c * 128:(c + 1) * 128], identity=g.ident[:]),
                        reads=[xik, "ident"], writes=["ps%d" % c])
                n = na * 128
                if c % 2 == 0:
                    P.op("act", lambda e, ps=ps, xo=xo, c=c, n=n: e.copy(out=xo[:, c, 0:n], in_=ps[:, 0:n]),
                         reads=["ps%d" % c], writes=[(xok, c)])
                else:
                    P.op("dve", lambda e, ps=ps, xo=xo, c=c, n=n: e.tensor_copy(out=xo[:, c, 0:n], in_=ps[:, 0:n]),
                         reads=["ps%d" % c], writes=[(xok, c)])
            rk = [(xok, c) for c in range(KT)]
            if kind == "L":
                P.dma("pool", blk_view(g.lat["XT"], lb * NB, NB), xo[:], reads=rk, writes=[("L_XT", lb)])
            else:
                P.dma("pool", blk_view(g.ctxa["XT"], CPAD, CTX), xo[:, :, 0:CTX], reads=rk, writes=[("C_XT", 0)])


def norm_core(g, st, xt, xtk, n, A, B, bufs, out_bf=None, out_f32=None, psi=7, sq_eng="act"):
    P = g.P
    sq, sqk = bufs["sq"]
    rs, rsk = bufs["rs"]
    ps = g.ps[psi]
    psk = "ps%d" % psi
    P.op("act", lambda e: e.activation(out=sq[:, :, 0:n], in_=xt[:, :, 0:n], func=AF.Square),
         reads=[xtk], writes=[sqk])
    for k in range(KT):
        P.op("pe", lambda e, k=k: e.matmul(ps[:, 0:n], lhsT=g.ones_b[:], rhs=sq[:, k, 0:n], start=(k == 0), stop=(k == 7)),
             reads=[sqk, "ones_b"], writes=[psk], sig=(k == 7))
    P.op("act", lambda e: e.activation(out=rs[:, 0:n], in_=ps[:, 0:n], func=AF.Sqrt, scale=1.0 / D, bias=EPS),
         reads=[psk], writes=[rsk])
    P.op("dve", lambda e: e.reciprocal(out=rs[:, 0:n], in_=rs[:, 0:n]), reads=[rsk], writes=[rsk])
    P.op("dve", lambda e: e.tensor_tensor(out=xt[:, :, 0:n], in0=xt[:, :, 0:n],
                                          in1=rs[:, 0:n].unsqueeze(1).to_broadcast([128, KT, n]), op=ALU.mult),
         reads=[xtk, rsk], writes=[xtk])
    for c in range(KT):
        eng = "act" if c % 2 == 0 else "dve"
        for (ot, wants) in ((out_bf, True), (out_f32, True)):
            if ot is None:
                continue
            o, ok = ot
            if B is None:
                if eng == "act":
                    P.op("act", lambda e, o=o, c=c: e.activation(out=o[:, c, 0:n], in_=xt[:, c, 0:n], func=AF.Identity,
                                                                 scale=A[:, c:c + 1]),
                         reads=[xtk, "nrm", "small"], writes=[(ok, c)])
                else:
                    P.op("dve", lambda e, o=o, c=c: e.tensor_single_scalar(out=o[:, c, 0:n], in_=xt[:, c, 0:n],
                                                                    scalar=A[:, c:c + 1], op=ALU.mult),
                         reads=[xtk, "nrm", "small"], writes=[(ok, c)])
            else:
                if eng == "act":
                    P.op("act", lambda e, o=o, c=c: e.activation(out=o[:, c, 0:n], in_=xt[:, c, 0:n], func=AF.Identity,
                                                                 scale=A[:, c:c + 1], bias=B[:, c:c + 1]),
                         reads=[xtk, "nrm", "modx", "modc"], writes=[(ok, c)])
                else:
                    P.op("dve", lambda e, o=o, c=c: e.tensor_scalar(out=o[:, c, 0:n], in0=xt[:, c, 0:n],
                                                                    scalar1=A[:, c:c + 1], scalar2=B[:, c:c + 1],
                                                                    op0=ALU.mult, op1=ALU.add),
                         reads=[xtk, "nrm", "modx", "modc"], writes=[(ok, c)])


def seg_blocks(kind, lbs):
    if kind == "L":
        return [("L", lb * NB, NB, lb) for lb in lbs]
    return [("C", CPAD, CTX, 0)]


def arrs(g, kind):
    return g.lat if kind == "L" else g.ctxa


def st_norm(g, l, which, jobs, router=False):
    P = g.P
    with Stage(g) as st:
        xts = st.ring("nxt", [128, KT, NB], F32, 2)
        hxs = st.ring("nhx", [128, KT, NB], BF16, 2)
        bufs = {"sq": st.sb("nsq", [128, KT, NB], BF16), "rs": st.sb("nrs", [128, NB], F32)}
        if router:
            yfs = st.sb("nyf", [128, KT, NB], F32)
            rt, rtk = st.sb("rt", [128, KT, NE], F32)
            P.dma("sp", rt[:], g.rt_in.rearrange("(k p) e -> p k e", p=128), writes=[rtk])
            lg, lgk = st.sb("lg", [128, 4, NE], F32)
            mx, mxk = st.sb("mx", [128, 4, 8], F32)
            ex, exk = st.sb("ex", [128, 4, NE], F32)
            mk, mkk = st.sb("mk", [128, 4, NE], F32)
            dn, dnk = st.sb("dn", [128, 4], F32)
            nm1, nm1k = st.sb("nm1", [128, 4], F32)
            cbb, cbbk = st.sb("cbb", [128, NE, 128], F32)
            cmo = st.ring("cmo", [128, NE, NB], F32, 2)
        def job(ji, kind, s, n, lb):
            A = arrs(g, kind)
            si = 0 if kind == "L" else 1
            xt, xtk = xts[ji % 2]
            hx, hxk = hxs[ji % 2]
            P.dma("sp", xt[:, :, 0:n], blk_view(A["XT"], s, n), reads=[(kind + "_XT", lb)], writes=[xtk])
            Av = modvec(g, l, si, "A%d" % which)
            Bv = modvec(g, l, si, "B%d" % which)
            if router:
                norm_core(g, st, xt, xtk, n, Av, Bv, bufs, out_f32=yfs)
                yf, yfk = yfs
                P.op("pool", lambda e, hx=hx, yf=yf: e.tensor_copy(out=hx[:, :, 0:n], in_=yf[:, :, 0:n]),
                     reads=[(yfk, c) for c in range(KT)], writes=[(hxk, c) for c in range(KT)])
                psl = g.ps[6]
                for a in range(4):
                    for k in range(KT):
                        P.op("pe", lambda e, a=a, k=k, yf=yf: e.matmul(psl[:, a * 8:(a + 1) * 8],
                                                                      lhsT=yf[:, k, a * 128:(a + 1) * 128], rhs=rt[:, k, :],
                                                                      start=(k == 0), stop=(k == 7)),
                             reads=[(yfk, k), rtk], writes=["ps6"], sig=(k == 7))
                P.op("dve", lambda e: e.tensor_copy(out=lg[:].rearrange("p a e -> p (a e)"), in_=psl[:, 0:32]),
                     reads=["ps6"], writes=[lgk])
                for a in range(4):
                    P.op("dve", lambda e, a=a: e.max(out=mx[:, a, :], in_=lg[:, a, :]), reads=[lgk], writes=[mxk])
                P.op("dve", lambda e: e.tensor_single_scalar(out=nm1[:], in_=mx[:, :, 0], scalar=-1.0, op=ALU.mult),
                     reads=[mxk], writes=[nm1k])
                for a in range(4):
                    P.op("act", lambda e, a=a: e.activation(out=ex[:, a, :], in_=lg[:, a, :], func=AF.Exp,
                                                            bias=nm1[:, a:a + 1], scale=1.0),
                         reads=[lgk, nm1k], writes=[exk])
                    P.op("dve", lambda e, a=a: e.tensor_single_scalar(out=mk[:, a, :], in_=lg[:, a, :], scalar=mx[:, a, 1:2], op=ALU.is_ge),
                         reads=[lgk, mxk], writes=[mkk])
                P.op("dve", lambda e: e.tensor_tensor(out=ex[:], in0=ex[:], in1=mk[:], op=ALU.mult),
                     reads=[exk, mkk], writes=[exk])
                P.op("dve", lambda e: e.tensor_reduce(out=dn[:], in_=ex[:], axis=mybir.AxisListType.X, op=ALU.add),
                     reads=[exk], writes=[dnk])
                P.op("dve", lambda e: e.reciprocal(out=dn[:], in_=dn[:]), reads=[dnk], writes=[dnk])
                P.op("dve", lambda e: e.tensor_tensor(out=ex[:], in0=ex[:], in1=dn[:].unsqueeze(2).to_broadcast([128, 4, NE]),
                                                      op=ALU.mult), reads=[exk, dnk], writes=[exk])
                co, cok = cmo[ji % 2]
                for a in range(4):
                    for ee in range(NE):
                        P.op("dve", lambda e, a=a, ee=ee: e.tensor_scalar(out=cbb[:, ee, :], in0=g.ident[:], scalar1=g.zeros[:, 0:1],
                                                                          scalar2=ex[:, a, ee:ee + 1], op0=ALU.mult, op1=ALU.add),
                             reads=[exk, "ident", (cbbk, ee)], writes=[(cbbk, ee)])
                    for ee in range(NE):
                        pse = g.ps[ee % 4]
                        P.op("pe", lambda e, a=a, ee=ee, pse=pse: e.matmul(pse[:, 0:128], lhsT=cbb[:, ee, :], rhs=g.ident[:],
                                                                          start=True, stop=True),
                             reads=[(cbbk, ee), "ident"], writes=["ps%d" % (ee % 4)])
                        P.op("act", lambda e, a=a, ee=ee, pse=pse, co=co: e.copy(out=co[:, ee, a * 128:(a + 1) * 128],
                                                                                in_=pse[:, 0:128]),
                             reads=["ps%d" % (ee % 4)], writes=[(cok, ee)])
                P.dma("pool", g.comb[:, :, s:s + n].rearrange("e p t -> p e t"), co[:],
                      reads=[(cok, ee) for ee in range(NE)], writes=[("COMB", lb)])
            else:
                norm_core(g, st, xt, xtk, n, Av, Bv, bufs, out_bf=(hx, hxk))
            P.dma("pool", blk_view(A["HX"], s, n), hx[:, :, 0:n], reads=[(hxk, c) for c in range(KT)],
                  writes=[(kind + "_HX", lb)])
        for ji, (kind, s, n, lb) in enumerate(jobs):
            job(ji, kind, s, n, lb)


def load_w(g, st, name, wdram, row0, col0, ncols, kt=KT):
    t, k = st.sb(name, [128, kt, ncols], BF16)
    g.P.dma("sp", t[:], wdram[row0:row0 + kt * 128, col0:col0 + ncols].rearrange("(k p) n -> p k n", p=128),
            reads=[], writes=[k])
    return t, k


def st_zu(g, l, jobs):
    P = g.P
    with Stage(g) as st:
        w, wk = load_w(g, st, "wu", g.win, l * D, 0, 1024)
        hxs = st.ring("zhx", [128, KT, NB], BF16, 2)
        ub = st.ring("zub", [128, KT, NB], F32, 2)
        def job(ji, kind, s, n, lb):
            A = arrs(g, kind)
            hx, hxk = hxs[ji % 2]
            u, uk = ub[ji % 2]
            P.dma("sp", hx[:, :, 0:n], blk_view(A["HX"], s, n), reads=[(kind + "_HX", lb)], writes=[hxk])
            msk = Scol(g, "blkmask", lb) if kind == "L" else Scol(g, "one", 0)
            for m in range(KT):
                ps = g.ps[m % 4]
                for k in range(KT):
                    P.op("pe", lambda e, ps=ps, m=m, k=k, hx=hx: e.matmul(ps[:, 0:n], lhsT=w[:, k, m * 128:(m + 1) * 128],
                                                                         rhs=hx[:, k, 0:n], start=(k == 0), stop=(k == 7)),
                         reads=[wk, hxk], writes=["ps%d" % (m % 4)], sig=(k == 7))
                P.op("dve", lambda e, ps=ps, m=m, u=u, msk=msk: e.tensor_single_scalar(out=u[:, m, 0:n], in_=ps[:, 0:n], scalar=msk, op=ALU.mult),
                     reads=["ps%d" % (m % 4), "small"], writes=[(uk, m)])
            P.dma("pool", blk_view(A["U"], s, n), u[:, :, 0:n], reads=[(uk, m) for m in range(KT)],
                  writes=[(kind + "_U", lb)])
        for ji, (kind, s, n, lb) in enumerate(jobs):
            job(ji, kind, s, n, lb)


def st_zgm(g, l, jobs, bg_pieces=None, bg_every=2):
    P = g.P
    with Stage(g) as st:
        wg, wgk = load_w(g, st, "wg", g.win, l * D, 1024, 1024)
        wm, wmk = load_w(g, st, "wm", g.win, l * D, 4096, 2048)
        hxs = st.ring("zhx", [128, KT, NB], BF16, 2)
        ob = st.ring("zgo", [128, 3 * KT, NB], BF16, 2)
        bg = bg_convert(g, st, bg_pieces) if bg_pieces else None
        bgc = 0
        pi = 0
        def job(ji, kind, s, n, lb):
            nonlocal pi, bgc
            A = arrs(g, kind)
            hx, hxk = hxs[ji % 2]
            o, ok = ob[ji % 2]
            P.dma("sp", hx[:, :, 0:n], blk_view(A["HX"], s, n), reads=[(kind + "_HX", lb)], writes=[hxk])
            for m in range(3 * KT):
                ps = g.ps[pi % 6]
                psk = "ps%d" % (pi % 6)
                pi += 1
                wt, wtk, mm = (wg, wgk, m) if m < KT else (wm, wmk, m - KT)
                for k in range(KT):
                    P.op("pe", lambda e, ps=ps, wt=wt, mm=mm, k=k, hx=hx: e.matmul(
                        ps[:, 0:n], lhsT=wt[:, k, mm * 128:(mm + 1) * 128], rhs=hx[:, k, 0:n], start=(k == 0), stop=(k == 7)),
                        reads=[wtk, hxk], writes=[psk], sig=(k == 7))
                fn = AF.Gelu_apprx_tanh if m < KT else AF.Sigmoid
                P.op("act", lambda e, ps=ps, o=o, m=m, fn=fn: e.activation(out=o[:, m, 0:n], in_=ps[:, 0:n], func=fn),
                     reads=[psk], writes=[(ok, m)])
                if bg is not None:
                    bgc += 1
                    if bgc % bg_every == 0:
                        next(bg, None)
            P.dma("pool", blk_view(A["G"], s, n), o[:, 0:KT, 0:n], reads=[(ok, m) for m in range(KT)],
                  writes=[(kind + "_G", lb)])
            P.dma("pool", blk_view(A["SGA"], s, n), o[:, KT:2 * KT, 0:n], reads=[(ok, m) for m in range(KT, 2 * KT)],
                  writes=[(kind + "_SGA", lb)])
            P.dma("pool", blk_view(A["SGB"], s, n), o[:, 2 * KT:3 * KT, 0:n], reads=[(ok, m) for m in range(2 * KT, 3 * KT)],
                  writes=[(kind + "_SGB", lb)])
        for ji, (kind, s, n, lb) in enumerate(jobs):
            job(ji, kind, s, n, lb)
        if bg is not None:
            for _ in bg:
                pass


def st_zv(g, l, jobs, bg_pieces=None, bg_every=2):
    P = g.P
    with Stage(g) as st:
        w, wk = load_w(g, st, "wv", g.win, l * D, 2048, 2048)
        hxs = st.ring("zhx", [128, KT, NB], BF16, 2)
        vb = st.ring("zvb", [128, KT, NB], BF16, 2)
        sgs = st.ring("zsg", [128, NB], F32, 3)
        bg = bg_convert(g, st, bg_pieces, eng="act") if bg_pieces else None
        bgc = 0
        pi = 0
        def job(ji, kind, s, n, lb):
            nonlocal pi, bgc
            A = arrs(g, kind)
            hx, hxk = hxs[ji % 2]
            v, vk = vb[ji % 2]
            P.dma("sp", hx[:, :, 0:n], blk_view(A["HX"], s, n), reads=[(kind + "_HX", lb)], writes=[hxk])
            msk = Scol(g, "blkmask", lb) if kind == "L" else Scol(g, "one", 0)
            for m in range(KT):
                pa, pb = g.ps[(2 * pi) % 6], g.ps[(2 * pi + 1) % 6]
                pak, pbk = "ps%d" % ((2 * pi) % 6), "ps%d" % ((2 * pi + 1) % 6)
                sg, sgk = sgs[pi % 3]
                pi += 1
                for (ps, psk, mm) in ((pa, pak, m), (pb, pbk, KT + m)):
                    for k in range(KT):
                        P.op("pe", lambda e, ps=ps, mm=mm, k=k, hx=hx: e.matmul(
                            ps[:, 0:n], lhsT=w[:, k, mm * 128:(mm + 1) * 128], rhs=hx[:, k, 0:n], start=(k == 0), stop=(k == 7)),
                            reads=[wk, hxk], writes=[psk], sig=(k == 7))
                P.op("act", lambda e, pb=pb, sg=sg: e.activation(out=sg[:, 0:n], in_=pb[:, 0:n], func=AF.Sigmoid),
                     reads=[pbk], writes=[sgk])
                P.op("dve", lambda e, pa=pa, sg=sg, v=v, m=m, msk=msk: e.scalar_tensor_tensor(
                    out=v[:, m, 0:n], in0=pa[:, 0:n], scalar=msk, in1=sg[:, 0:n], op0=ALU.mult, op1=ALU.mult),
                    reads=[pak, sgk, "small"], writes=[(vk, m)])
                if bg is not None:
                    bgc += 1
                    if bgc % bg_every == 0:
                        next(bg, None)
            P.dma("pool", blk_view(A["V"], s, n), v[:, :, 0:n], reads=[(vk, m) for m in range(KT)],
                  writes=[(kind + "_V", lb)])
        for ji, (kind, s, n, lb) in enumerate(jobs):
            job(ji, kind, s, n, lb)
        if bg is not None:
            for _ in bg:
                pass


def st_scan(g, l, jobs):
    P = g.P
    with Stage(g) as st:
        gw, gwk = st.sb("gw", [128, 2, 2, KT, 128], BF16)
        P.dma("sp", gw[:].rearrange("p d t c m -> p (d t c) m"),
              g.gw[l * 4096:(l + 1) * 4096, :].rearrange("(x p) m -> p x m", p=128), reads=["gw_bf"], writes=[gwk])
        uhs = st.ring("uh", [128, KT, NB + 3], F32, 2)
        ucs = st.ring("uc", [128, KT, NB], F32, 2)
        ucb, ucbk = st.sb("ucb", [128, KT, NB], BF16)
        so = st.ring("so", [128, KT, NB], F32, 2)
        afo = st.ring("afo", [128, KT, NB], BF16, 2)
        abo = st.ring("abo", [128, KT, NB], BF16, 2)
        R = lambda nm, k=4: st.ring(nm, [128, NB], F32, k)
        rr, gi, aa, a2, bb, hh = R("rr"), R("gi"), R("aa"), R("a2"), R("bb"), R("hh", 4)
        rsm = st.ring("rsm", [128, 2], F32, 4)
        cw = S(g, "cw4_%d" % l)
        cb = S(g, "cb4_%d" % l)
        it = 0

        def job(ji, kind, s, n, lb):
            nonlocal it
            A = arrs(g, kind)
            uh, uhk = uhs[ji % 2]
            uc, uck = ucs[ji % 2]
            sot, sok = so[ji % 2]
            aft, afk = afo[ji % 2]
            abt, abk = abo[ji % 2]
            rks = [(kind + "_U", b) for b in ((lb - 1, lb, lb + 1) if kind == "L" else (0,))]
            P.dma("sp", uh[:, :, 0:n + 3], blk_view(A["U"], s - 2, n + 3), reads=rks, writes=[uhk])
            for c in range(KT):
                P.op("dve", lambda e, c=c: e.tensor_scalar(out=uc[:, c, 0:n], in0=uh[:, c, 0:n],
                                                          scalar1=cw[:, c:c + 1], scalar2=cb[:, c:c + 1],
                                                          op0=ALU.mult, op1=ALU.add),
                     reads=[uhk, "small"], writes=[(uck, c)])
                for j in range(1, 4):
                    P.op("dve", lambda e, c=c, j=j: e.scalar_tensor_tensor(
                        out=uc[:, c, 0:n], in0=uh[:, c, j:j + n], scalar=cw[:, j * 8 + c:j * 8 + c + 1], in1=uc[:, c, 0:n],
                        op0=ALU.mult, op1=ALU.add), reads=[uhk, "small", (uck, c)], writes=[(uck, c)])
                P.op("pool", lambda e, c=c: e.tensor_copy(out=ucb[:, c, 0:n], in_=uc[:, c, 0:n]),
                     reads=[(uck, c)], writes=[(ucbk, c)])
            def stA(c):
                nonlocal it
                T = []
                for d in range(2):
                    T.append((rr[it % 4], gi[it % 4], aa[it % 4], a2[it % 4], bb[it % 4], hh[it % 4], rsm[it % 4],
                              g.ps[(2 * it) % 8], "ps%d" % ((2 * it) % 8), g.ps[(2 * it + 1) % 8], "ps%d" % ((2 * it + 1) % 8)))
                    it += 1
                for d in range(2):
                    pr, prk, pi_, pik = T[d][7], T[d][8], T[d][9], T[d][10]
                    P.op("pe", lambda e, pr=pr, d=d, c=c: e.matmul(pr[:, 0:n], lhsT=gw[:, d, 0, c, :], rhs=ucb[:, c, 0:n],
                                                                  start=True, stop=True),
                         reads=[gwk, (ucbk, c)], writes=[prk])
                    P.op("pe", lambda e, pi_=pi_, d=d, c=c: e.matmul(pi_[:, 0:n], lhsT=gw[:, d, 1, c, :], rhs=ucb[:, c, 0:n],
                                                                    start=True, stop=True),
                         reads=[gwk, (ucbk, c)], writes=[pik])
                for d in range(2):
                    (r_, rk_), (gi_, gik_), _, _, _, _, (rs_, rsk_), pr, prk, pi_, pik = T[d]
                    br = Scol(g, "br%d%d" % (l, d), c)
                    bi = Scol(g, "bi%d%d" % (l, d), c)
                    P.op("act", lambda e, pr=pr, r_=r_, br=br, rs_=rs_: e.activation(out=r_[:, 0:n], in_=pr[:, 0:n], func=AF.Sigmoid, bias=br,
                                                                                accum_out=rs_[:, 0:1]),
                         reads=[prk, "small"], writes=[rk_, rsk_])
                    P.op("act", lambda e, pi_=pi_, gi_=gi_, bi=bi: e.activation(out=gi_[:, 0:n], in_=pi_[:, 0:n], func=AF.Sigmoid, bias=bi),
                         reads=[pik, "small"], writes=[gik_])
                for d in range(2):
                    (r_, rk_), _, (a_, ak_), _, _, _, (rs_, rsk_) = T[d][0:7]
                    P.op("act", lambda e, r_=r_, a_=a_, d=d, c=c: e.activation(out=a_[:, 0:n], in_=r_[:, 0:n], func=AF.Exp,
                                                                             scale=g.cl[:, l, d, 0, c:c + 1]),
                         reads=[rk_, "cl"], writes=[ak_])
                    if kind == "L" and 4 <= lb < 12:
                        P.op("act", lambda e, rs_=rs_, c=c, d=d: e.activation(
                            out=g.sumt[:, c, lb - 4, 2 * d:2 * d + 1], in_=rs_[:, 0:1], func=AF.Exp, scale=g.cl[:, l, d, 0, c:c + 1]),
                            reads=[rsk_, "cl"], writes=["sumt"])
                for d in range(2):
                    _, _, (a_, ak_), (a2_, a2k_) = T[d][0:4]
                    P.op("dve", lambda e, a_=a_, a2_=a2_: e.tensor_tensor(out=a2_[:, 0:n], in0=a_[:, 0:n], in1=a_[:, 0:n], op=ALU.mult),
                         reads=[ak_], writes=[a2k_])
                return T

            def stB(c, T):
                for d in range(2):
                    (a2_, a2k_) = T[d][3]
                    P.op("act", lambda e, a2_=a2_: e.activation(out=a2_[:, 0:n], in_=a2_[:, 0:n], func=AF.Sqrt, scale=-1.0, bias=1.0),
                         reads=[a2k_], writes=[a2k_])
                for d in range(2):
                    _, (gi_, gik_), (a_, ak_), (a2_, a2k_), (b_, bk_), (h_, hk_) = T[d][0:6]
                    P.op("pool", lambda e, gi_=gi_, c=c, b_=b_: e.tensor_tensor(out=b_[:, 0:n], in0=gi_[:, 0:n], in1=uc[:, c, 0:n], op=ALU.mult),
                         reads=[gik_, (uck, c)], writes=[bk_])
                    P.op("pool", lambda e, a2_=a2_, b_=b_: e.tensor_tensor(out=b_[:, 0:n], in0=b_[:, 0:n], in1=a2_[:, 0:n], op=ALU.mult),
                         reads=[bk_, a2k_], writes=[bk_])
                    At, Atk = (aft, afk) if d == 0 else (abt, abk)
                    if d == 0:
                        P.op("dve", lambda e, a_=a_, b_=b_, h_=h_: e.tensor_tensor_scan(out=h_[:, 0:n], data0=a_[:, 0:n], data1=b_[:, 0:n],
                                                                                       initial=0.0, op0=ALU.mult, op1=ALU.add),
                             reads=[ak_, bk_], writes=[hk_])
                        P.op("dve", lambda e, a_=a_, At=At, c=c: e.tensor_tensor_scan(out=At[:, c, 0:n], data0=a_[:, 0:n], data1=g.zeros[:, 0:n],
                                                                                     initial=1.0, op0=ALU.mult, op1=ALU.add),
                             reads=[ak_, "zeros"], writes=[(Atk, c)])
                        hfwd = (h_, hk_)
                        e0, e1 = n - 1, n
                    else:
                        P.op("dve", lambda e, a_=a_, b_=b_, h_=h_: e.tensor_tensor_scan(out=h_[:, 0:n][:, ::-1],
                                                                                       data0=a_[:, 0:n][:, ::-1], data1=b_[:, 0:n][:, ::-1],
                                                                                       initial=0.0, op0=ALU.mult, op1=ALU.add),
                             reads=[ak_, bk_], writes=[hk_])
                        P.op("dve", lambda e, a_=a_, At=At, c=c: e.tensor_tensor_scan(out=At[:, c, 0:n][:, ::-1], data0=a_[:, 0:n][:, ::-1],
                                                                                     data1=g.zeros[:, 0:n], initial=1.0, op0=ALU.mult, op1=ALU.add),
                             reads=[ak_, "zeros"], writes=[(Atk, c)])
                        hf_, hfk_ = hfwd
                        P.op("dve", lambda e, h_=h_, hf_=hf_, c=c: e.tensor_tensor(out=sot[:, c, 0:n], in0=hf_[:, 0:n], in1=h_[:, 0:n], op=ALU.add),
                             reads=[hk_, hfk_], writes=[(sok, c)])
                        e0, e1 = 0, 1
                    if kind == "L" and 4 <= lb < 12:
                        P.op("pool", lambda e, h_=h_, c=c, d=d, e0=e0, e1=e1: e.tensor_copy(
                            out=g.sumt[:, c, lb - 4, 2 * d + 1:2 * d + 2], in_=h_[:, e0:e1]), reads=[hk_, "sumt"], writes=["sumt"])
                    if kind == "C":
                        P.op("pool", lambda e, h_=h_, c=c, d=d, e0=e0, e1=e1: e.tensor_copy(
                            out=g.sctx[:, d, c:c + 1], in_=h_[:, e0:e1]), reads=[hk_, "sctx"], writes=["sctx"])

            Tn = stA(0)
            for c in range(KT):
                Tc = Tn
                if c + 1 < KT:
                    Tn = stA(c + 1)
                stB(c, Tc)
            P.dma("pool", blk_view(A["S"], s, n), sot[:, :, 0:n], reads=[(sok, c) for c in range(KT)], writes=[(kind + "_S", lb)])
            P.dma("pool", blk_view(A["AF"], s, n), aft[:, :, 0:n], reads=[(afk, c) for c in range(KT)], writes=[(kind + "_AF", lb)])
            P.dma("pool", blk_view(A["AB"], s, n), abt[:, :, 0:n], reads=[(abk, c) for c in range(KT)], writes=[(kind + "_AB", lb)])
        for ji, (kind, s, n, lb) in enumerate(jobs):
            job(ji, kind, s, n, lb)


def st_carry(g, l):
    P = g.P
    with Stage(g) as st:
        P.dma("sp", g.sum_in, g.sumt[:].rearrange("p c b f -> p (c b f)"), reads=["sumt"], writes=["sum_in"])
        P.collective("AllGather", ins=[g.sum_in], outs=[g.sum_all], groups=[[0, 1, 2, 3], [4, 5, 6, 7]],
                     reads=["sum_in"], writes=["sum_all"])
        sa, sak = st.sb("sa", [128, 4, KT, 8, 4], F32)
        P.dma("sp", sa[:], g.sum_all.rearrange("(r p) (c b f) -> p r c b f", p=128, c=KT, b=8), reads=["sum_all"], writes=[sak])
        ht, htk = st.sb("ht", [128, 2, KT, 48], F32)
        P.op("dve", lambda e: e.memset(ht[:], 0.0), writes=[htk])
        tmp, tmk = st.sb("ctmp", [128, KT], F32)
        P.op("dve", lambda e: e.tensor_copy(out=ht[:, 0, :, 8], in_=g.sctx[:, 0, :]), reads=["sctx", htk], writes=[htk])
        for gb in range(32):
            r, b = gb // 8, gb % 8
            P.op("dve", lambda e, r=r, b=b, gb=gb: e.tensor_tensor(out=tmp[:], in0=sa[:, r, :, b, 0], in1=ht[:, 0, :, 8 + gb], op=ALU.mult),
                 reads=[sak, htk], writes=[tmk])
            P.op("dve", lambda e, r=r, b=b, gb=gb: e.tensor_tensor(out=ht[:, 0, :, 9 + gb], in0=tmp[:], in1=sa[:, r, :, b, 1], op=ALU.add),
                 reads=[sak, tmk, htk], writes=[htk])
        P.op("dve", lambda e: e.tensor_copy(out=ht[:, 1, :, 40], in_=g.sctx[:, 1, :]), reads=["sctx", htk], writes=[htk])
        for gb in range(31, -1, -1):
            r, b = gb // 8, gb % 8
            P.op("dve", lambda e, r=r, b=b, gb=gb: e.tensor_tensor(out=tmp[:], in0=sa[:, r, :, b, 2], in1=ht[:, 1, :, 9 + gb], op=ALU.mult),
                 reads=[sak, htk], writes=[tmk])
            P.op("dve", lambda e, r=r, b=b, gb=gb: e.tensor_tensor(out=ht[:, 1, :, 8 + gb], in0=tmp[:], in1=sa[:, r, :, b, 3], op=ALU.add),
                 reads=[sak, tmk, htk], writes=[htk])
        P.dma("sp", g.htab.rearrange("d p c x -> p d c x"), ht[:], reads=[htk], writes=["htab"])

        def dyn(e, d):
            base = dynval(g, e, "b%d" % d)
            return e.dma_start(out=g.hin[:, d, :, :],
                               in_=g.htab[d:d + 1, :, :, bass.ds(base, 12)].rearrange("o p c x -> p (o c) x"))
        P.dma("sp", None, None, reads=["htab"], writes=["hin"], fn=lambda e: dyn(e, 0))
        P.dma("sp", None, None, reads=["htab"], writes=["hin"], fn=lambda e: dyn(e, 1))


def st_conv(g, l, kinds, bg_pieces=None):
    P = g.P
    with Stage(g) as st:
        dg = st.ring("dg", [128, 31, 128], BF16, 2)
        vrl = st.ring("vrl", [128, TL], BF16, 2)
        cvo = st.ring("cvo", [128, NB], F32, 3)
        bg = bg_convert(g, st, bg_pieces, eng="act") if bg_pieces else None
        oi = 0
        for c in range(KT):
            dgt, dgk = dg[c % 2]
            for j in range(31):
                P.op("dve", lambda e, j=j, c=c, dgt=dgt: e.tensor_single_scalar(out=dgt[:, j, :], in_=g.ident[:],
                                                                        scalar=Scol(g, "cw31_%d" % l, j * 8 + c), op=ALU.mult),
                     reads=["ident", "small", (dgk, j)], writes=[(dgk, j)])
            for (kind, lbs) in kinds:
                A = arrs(g, kind)
                vr, vrk = vrl[oi % 2]
                if kind == "L":
                    lo, hi = (lbs[0] - 2) * NB, (lbs[-1] + 3) * NB
                    P.dma("sp", vr[:, lo:hi], A["V"][c, :, lo:hi], reads=[("L_V", b) for b in range(lbs[0] - 2, lbs[-1] + 3)],
                          writes=[vrk])
                    stride = 64
                    blocks = [(lb * NB, NB, lb) for lb in lbs]
                else:
                    P.dma("sp", vr[:, 0:TC], A["V"][c, :, :], reads=[("C_V", 0), "C_V"], writes=[vrk])
                    stride = 1
                    blocks = [(CPAD, CTX, 0)]
                for (s, n, lb) in blocks:
                    ps = g.ps[oi % 4]
                    psk = "ps%d" % (oi % 4)
                    o, ok = cvo[oi % 3]
                    oi += 1
                    for j in range(31):
                        off = s + (j - 15) * stride
                        P.op("pe", lambda e, ps=ps, j=j, off=off, vr=vr, dgt=dgt, n=n: e.matmul(
                            ps[:, 0:n], lhsT=dgt[:, j, :], rhs=vr[:, off:off + n], start=(j == 0), stop=(j == 30)),
                            reads=[(dgk, j), vrk], writes=[psk], sig=(j == 30))
                    P.op("act", lambda e, ps=ps, o=o, n=n, c=c: e.activation(out=o[:, 0:n], in_=ps[:, 0:n], func=AF.Identity,
                                                                           bias=Scol(g, "cb31_%d" % l, c), scale=1.0),
                         reads=[psk, "small"], writes=[ok])
                    P.dma("pool", A["CV"][c, :, s:s + n], o[:, 0:n], reads=[ok], writes=[(kind + "_CV", lb, c)])
                    if bg is not None:
                        next(bg, None)
        if bg is not None:
            for _ in bg:
                pass


def st_mix(g, l, jobs):
    P = g.P
    H = 256
    with Stage(g) as st:
        wa, wak = load_w(g, st, "wa", g.wa, l * D, 0, D)
        wb, wbk = load_w(g, st, "wb", g.wb, l * D, 0, D)
        wo, wok = load_w(g, st, "wo", g.wo, l * D, 0, D)
        cvs = st.ring("mcv", [128, KT, H], F32, 2)
        ss = st.ring("ms", [128, KT, H], F32, 2)
        afs = st.ring("maf", [128, KT, H], BF16, 2)
        abs_ = st.ring("mab", [128, KT, H], BF16, 2)
        gs = st.ring("mg", [128, KT, H], BF16, 2)
        sgas = st.ring("msga", [128, KT, H], BF16, 2)
        sgbs = st.ring("msgb", [128, KT, H], BF16, 2)
        xts = st.ring("mxt", [128, KT, H], F32, 2)
        cvb, cvbk = st.sb("cvb", [128, KT, H], BF16)
        sqb, sqbk = st.sb("sqb", [128, KT, H], BF16)
        lno, lnok = st.sb("lno", [128, KT, H], BF16)
        mbt, mbk = st.sb("mbt", [128, KT, H], F32)
        hst, hsk = st.sb("hst", [128, KT, H], F32)
        hsb, hsbk = st.sb("hsb", [128, KT, H], BF16)
        mt, mtk = st.sb("mt", [128, KT, H], BF16)
        mu, muk = st.sb("mu", [128, H], F32)
        var, vark = st.sb("var", [128, H], F32)
        tmp1, tmp1k = st.sb("tmp1", [128, H], F32)
        lng, lnb = S(g, "lng%d" % l), S(g, "lnb%d" % l)
        it = 0
        pi = 0

        def nps():
            nonlocal pi
            r = (g.ps[pi % 6], "ps%d" % (pi % 6))
            pi += 1
            return r
        halves_ = []
        for (kind, s0, n0, lb) in jobs:
            A = arrs(g, kind)
            si = 0 if kind == "L" else 1
            def half_body(kind, lb, s0, n0, s, A, si):
                nonlocal it
                n = min(H, s0 + n0 - s)
                cv, cvk = cvs[it % 2]
                sst, ssk = ss[it % 2]
                af, afk = afs[it % 2]
                ab, abk = abs_[it % 2]
                gt, gk = gs[it % 2]
                sga, sgak = sgas[it % 2]
                sgb, sgbk = sgbs[it % 2]
                xt, xtk = xts[it % 2]
                it += 1
                def prep():
                    P.dma("sp", cv[:, :, 0:n], blk_view(A["CV"], s, n), reads=[(kind + "_CV", lb, c) for c in range(KT)], writes=[cvk])
                    P.dma("sp", sst[:, :, 0:n], blk_view(A["S"], s, n), reads=[(kind + "_S", lb)], writes=[ssk])
                    P.dma("sp", af[:, :, 0:n], blk_view(A["AF"], s, n), reads=[(kind + "_AF", lb)], writes=[afk])
                    P.dma("sp", ab[:, :, 0:n], blk_view(A["AB"], s, n), reads=[(kind + "_AB", lb)], writes=[abk])
                    P.dma("sp", gt[:, :, 0:n], blk_view(A["G"], s, n), reads=[(kind + "_G", lb)], writes=[gk])
                    P.dma("sp", sga[:, :, 0:n], blk_view(A["SGA"], s, n), reads=[(kind + "_SGA", lb)], writes=[sgak])
                    P.dma("sp", sgb[:, :, 0:n], blk_view(A["SGB"], s, n), reads=[(kind + "_SGB", lb)], writes=[sgbk])
                    P.op("act", lambda e, cv=cv: e.copy(out=cvb[:, :, 0:n], in_=cv[:, :, 0:n]), reads=[cvk], writes=[cvbk])
                    P.op("act", lambda e, cv=cv: e.activation(out=sqb[:, :, 0:n], in_=cv[:, :, 0:n], func=AF.Square), reads=[cvk], writes=[sqbk])
                    p1, p1k = g.ps[6], "ps6"
                    p2, p2k = g.ps[7], "ps7"
                    for k in range(KT):
                        P.op("pe", lambda e, k=k: e.matmul(p1[:, 0:n], lhsT=g.ones_b[:], rhs=cvb[:, k, 0:n], start=(k == 0), stop=(k == 7)),
                             reads=[cvbk, "ones_b"], writes=[p1k], sig=(k == 7))
                    for k in range(KT):
                        P.op("pe", lambda e, k=k: e.matmul(p2[:, 0:n], lhsT=g.ones_b[:], rhs=sqb[:, k, 0:n], start=(k == 0), stop=(k == 7)),
                             reads=[sqbk, "ones_b"], writes=[p2k], sig=(k == 7))
                    P.op("dve", lambda e: e.tensor_single_scalar(out=mu[:, 0:n], in_=p1[:, 0:n], scalar=1.0 / D, op=ALU.mult),
                         reads=[p1k], writes=[muk])
                    P.op("dve", lambda e: e.tensor_tensor(out=tmp1[:, 0:n], in0=mu[:, 0:n], in1=mu[:, 0:n], op=ALU.mult),
                         reads=[muk], writes=[tmp1k])
                    P.op("dve", lambda e: e.scalar_tensor_tensor(out=var[:, 0:n], in0=p2[:, 0:n], scalar=1.0 / D, in1=tmp1[:, 0:n],
                                                                 op0=ALU.mult, op1=ALU.subtract),
                         reads=[p2k, tmp1k], writes=[vark])
                    P.op("dve", lambda e: e.tensor_single_scalar(out=var[:, 0:n], in_=var[:, 0:n], scalar=0.0, op=ALU.max),
                         reads=[vark], writes=[vark])
                    P.op("act", lambda e: e.activation(out=var[:, 0:n], in_=var[:, 0:n], func=AF.Sqrt, bias=EPS, scale=1.0),
                         reads=[vark], writes=[vark])
                    P.op("dve", lambda e: e.reciprocal(out=var[:, 0:n], in_=var[:, 0:n]), reads=[vark], writes=[vark])
                    P.op("dve", lambda e, cv=cv: e.tensor_tensor(out=cv[:, :, 0:n], in0=cv[:, :, 0:n],
                                                                in1=mu[:, 0:n].unsqueeze(1).to_broadcast([128, KT, n]), op=ALU.subtract),
                         reads=[cvk, muk], writes=[cvk])
                    P.op("dve", lambda e, cv=cv: e.tensor_tensor(out=cv[:, :, 0:n], in0=cv[:, :, 0:n],
                                                                in1=var[:, 0:n].unsqueeze(1).to_broadcast([128, KT, n]), op=ALU.mult),
                         reads=[cvk, vark], writes=[cvk])
                    for c in range(KT):
                        P.op("act", lambda e, c=c, cv=cv: e.activation(out=lno[:, c, 0:n], in_=cv[:, c, 0:n], func=AF.Silu,
                                                                      scale=lng[:, c:c + 1], bias=lnb[:, c:c + 1]),
                             reads=[cvk, "small"], writes=[(lnok, c)])
                def main1():
                    for m in range(KT):
                        ps, psk = nps()
                        for k in range(KT):
                            P.op("pe", lambda e, ps=ps, m=m, k=k: e.matmul(ps[:, 0:n], lhsT=wb[:, k, m * 128:(m + 1) * 128], rhs=lno[:, k, 0:n],
                                                                          start=(k == 0), stop=(k == 7)),
                                 reads=[wbk, (lnok, k)], writes=[psk], sig=(k == 7))
                        P.op("dve", lambda e, ps=ps, m=m, sgb=sgb: e.tensor_tensor(out=mbt[:, m, 0:n], in0=ps[:, 0:n], in1=sgb[:, m, 0:n], op=ALU.mult),
                             reads=[psk, sgbk], writes=[(mbk, m)])
                def main2():
                    P.dma("sp", xt[:, :, 0:n], blk_view(A["XT"], s, n), reads=[(kind + "_XT", lb)], writes=[xtk])
                    hin = g.hin if kind == "L" else g.hzero
                    bidx = (lb - 2) if kind == "L" else 0
                    for c in range(KT):
                        P.op("dve", lambda e, c=c, af=af, sst=sst: e.scalar_tensor_tensor(
                            out=hst[:, c, 0:n], in0=af[:, c, 0:n], scalar=hin[:, 0, c, bidx:bidx + 1], in1=sst[:, c, 0:n],
                            op0=ALU.mult, op1=ALU.add), reads=[afk, ssk, "hin", "hzero"], writes=[(hsk, c)])
                        P.op("dve", lambda e, c=c, ab=ab: e.scalar_tensor_tensor(
                            out=hst[:, c, 0:n], in0=ab[:, c, 0:n], scalar=hin[:, 1, c, bidx:bidx + 1], in1=hst[:, c, 0:n],
                            op0=ALU.mult, op1=ALU.add), reads=[abk, (hsk, c), "hin", "hzero"], writes=[(hsk, c)])
                        P.op("pool", lambda e, c=c, gt=gt: e.tensor_tensor(out=hsb[:, c, 0:n], in0=hst[:, c, 0:n], in1=gt[:, c, 0:n], op=ALU.mult),
                             reads=[(hsk, c), gk], writes=[(hsbk, c)])
                    for m in range(KT):
                        ps, psk = nps()
                        for k in range(KT):
                            P.op("pe", lambda e, ps=ps, m=m, k=k: e.matmul(ps[:, 0:n], lhsT=wa[:, k, m * 128:(m + 1) * 128], rhs=hsb[:, k, 0:n],
                                                                          start=(k == 0), stop=(k == 7)),
                                 reads=[wak, (hsbk, k)], writes=[psk], sig=(k == 7))
                        P.op("dve", lambda e, ps=ps, m=m, sga=sga: e.tensor_tensor(out=hst[:, m, 0:n], in0=ps[:, 0:n], in1=sga[:, m, 0:n], op=ALU.mult),
                             reads=[psk, sgak], writes=[(hsk, m)])
                        P.op("pool", lambda e, m=m: e.tensor_tensor(out=mt[:, m, 0:n], in0=hst[:, m, 0:n], in1=mbt[:, m, 0:n], op=ALU.add),
                             reads=[(hsk, m), (mbk, m)], writes=[(mtk, m)])
                    g1 = modvec(g, l, si, "G1")
                    for m in range(KT):
                        ps, psk = nps()
                        for k in range(KT):
                            P.op("pe", lambda e, ps=ps, m=m, k=k: e.matmul(ps[:, 0:n], lhsT=wo[:, k, m * 128:(m + 1) * 128], rhs=mt[:, k, 0:n],
                                                                          start=(k == 0), stop=(k == 7)),
                                 reads=[wok, (mtk, k)], writes=[psk], sig=(k == 7))
                        P.op("dve", lambda e, ps=ps, m=m, xt=xt: e.scalar_tensor_tensor(
                            out=xt[:, m, 0:n], in0=ps[:, 0:n], scalar=g1[:, m:m + 1], in1=xt[:, m, 0:n], op0=ALU.mult, op1=ALU.add),
                            reads=[psk, xtk, "modx", "modc"], writes=[xtk])
                    P.dma("pool", blk_view(A["XT"], s, n), xt[:, :, 0:n], reads=[xtk], writes=[(kind + "_XT", lb)])
                return prep, main1, main2
            for s in range(s0, s0 + n0, H):
                halves_.append(half_body(kind, lb, s0, n0, s, A, si))
        halves_[0][0]()
        for i_ in range(len(halves_)):
            halves_[i_][1]()
            if i_ + 1 < len(halves_):
                halves_[i_ + 1][0]()
            halves_[i_][2]()


def st_ffn(g, l, sblocks, moe, publish=False):
    P = g.P
    CH = 256
    NCH = DFF // CH
    TB = 1024
    with Stage(g) as st:
        hxs = st.ring("fhx", [128, KT, TB], BF16, 1)
        acc, acck = st.sb("facc", [128, KT, TB], F32)
        cmb = st.ring("fcmb", [128, TB], F32, 2)
        w1s = st.ring("fw1", [128, KT, 2 * CH], BF16, 3)
        w3s = st.ring("fw3", [128, KT, 2 * CH], BF16, 3)
        w2s = st.ring("fw2", [128, 4, D], BF16, 3)
        sil = st.ring("fsil", [128, NB], BF16, 3)
        gts = st.ring("fgt", [128, 4, NB], BF16, 2)
        xts = st.ring("fxt", [128, KT, NB], F32, 2)
        ne = NE if moe else 1
        wi = 0
        hi_ = 0
        gi_ = 0
        pi = 0
        groups = [(c0, min(2, NCH - c0)) for c0 in range(0, NCH, 2)]
        def sb_body(kind, s0, n0, lbs):
            nonlocal wi, hi_, gi_, pi
            A = arrs(g, kind)
            si = 0 if kind == "L" else 1
            hx, hxk = hxs[0]
            P.dma("sp", hx[:, :, 0:n0], blk_view(A["HX"], s0, n0), reads=[(kind + "_HX", lb) for lb in lbs], writes=[hxk])
            halves = [(o, min(NB, n0 - o)) for o in range(0, n0, NB)]
            steps = [(ex, gi2, c0, nc_, ho, hn) for ex in range(ne) for gi2, (c0, nc_) in enumerate(groups) for (ho, hn) in halves]
            loaded = {}
            cms = {}

            def ensure(ex, gi2, c0, nc_):
                nonlocal wi
                if moe and ex not in cms:
                    cm, cmk = cmb[ex % 2]
                    P.dma("sp", cm[:, 0:n0], g.comb[ex, :, s0:s0 + n0], reads=[("COMB", lb) for lb in lbs], writes=[cmk])
                    cms[ex] = (cm, cmk)
                if (ex, gi2) in loaded:
                    return
                if moe:
                    W1, W3, W2 = g.m1, g.m3, g.m2
                    r1, r2 = ex * D, ex * DFF
                else:
                    W1, W3, W2 = g.f1, g.f3, g.f2
                    r1, r2 = 0, 0
                w1, w1k = w1s[wi % 3]
                w3, w3k = w3s[wi % 3]
                w2, w2k = w2s[wi % 3]
                wi += 1
                cw_ = nc_ * CH
                nj = nc_ * 2
                P.dma("sp", w1[:, :, 0:cw_], W1[r1:r1 + D, c0 * CH:c0 * CH + cw_].rearrange("(k p) n -> p k n", p=128),
                      reads=[], writes=[w1k])
                P.dma("sp", w3[:, :, 0:cw_], W3[r1:r1 + D, c0 * CH:c0 * CH + cw_].rearrange("(k p) n -> p k n", p=128),
                      reads=[], writes=[w3k])
                P.dma("sp", w2[:, 0:nj, :], W2[r2 + c0 * CH:r2 + c0 * CH + cw_, :].rearrange("(k p) n -> p k n", p=128),
                      reads=[], writes=[w2k])
                loaded[(ex, gi2)] = (w1, w1k, w3, w3k, w2, w2k, nj)

            def emit_h(step):
                nonlocal hi_, gi_, pi
                ex, gi2, c0, nc_, ho, hn = step
                ensure(ex, gi2, c0, nc_)
                w1, w1k, w3, w3k, w2, w2k, nj = loaded[(ex, gi2)]
                gt, gtk = gts[gi_ % 2]
                gi_ += 1
                for j in range(nj):
                    p1, p1k = g.ps[pi % 4], "ps%d" % (pi % 4)
                    p3, p3k = g.ps[(pi + 1) % 4], "ps%d" % ((pi + 1) % 4)
                    pi += 2
                    for k in range(KT):
                        P.op("pe", lambda e, p1=p1, w1=w1, j=j, k=k, ho=ho, hn=hn: e.matmul(
                            p1[:, 0:hn], lhsT=w1[:, k, j * 128:(j + 1) * 128], rhs=hx[:, k, ho:ho + hn], start=(k == 0), stop=(k == 7)),
                            reads=[w1k, hxk], writes=[p1k], sig=(k == 7))
                    for k in range(KT):
                        P.op("pe", lambda e, p3=p3, w3=w3, j=j, k=k, ho=ho, hn=hn: e.matmul(
                            p3[:, 0:hn], lhsT=w3[:, k, j * 128:(j + 1) * 128], rhs=hx[:, k, ho:ho + hn], start=(k == 0), stop=(k == 7)),
                            reads=[w3k, hxk], writes=[p3k], sig=(k == 7))
                    sl, slk = sil[hi_ % 3]
                    hi_ += 1
                    P.op("act", lambda e, p1=p1, sl=sl, hn=hn: e.activation(out=sl[:, 0:hn], in_=p1[:, 0:hn], func=AF.Silu),
                         reads=[p1k], writes=[slk])
                    if moe:
                        cm, cmk = cms[ex]
                        P.op("pool", lambda e, sl=sl, cm=cm, ho=ho, hn=hn: e.tensor_tensor(out=sl[:, 0:hn], in0=sl[:, 0:hn],
                                                                                          in1=cm[:, ho:ho + hn], op=ALU.mult),
                             reads=[slk, cmk], writes=[slk])
                    P.op("dve", lambda e, p3=p3, sl=sl, gt=gt, j=j, hn=hn: e.tensor_tensor(out=gt[:, j, 0:hn], in0=p3[:, 0:hn],
                                                                                          in1=sl[:, 0:hn], op=ALU.mult),
                         reads=[p3k, slk], writes=[(gtk, j)])
                return (gt, gtk)

            def emit_w2(step, H, first):
                ex, gi2, c0, nc_, ho, hn = step
                w1, w1k, w3, w3k, w2, w2k, nj = loaded[(ex, gi2)]
                gt, gtk = H
                for m in range(KT):
                    po, pok = g.ps[4 + (m % 4)], "ps%d" % (4 + (m % 4))
                    for j in range(nj):
                        P.op("pe", lambda e, po=po, w2=w2, j=j, m=m, gt=gt, hn=hn, nj=nj: e.matmul(
                            po[:, 0:hn], lhsT=w2[:, j, m * 128:(m + 1) * 128], rhs=gt[:, j, 0:hn], start=(j == 0), stop=(j == nj - 1)),
                            reads=[w2k, (gtk, j)], writes=[pok], sig=(j == nj - 1))
                    if first:
                        P.op("act", lambda e, po=po, m=m, ho=ho, hn=hn: e.copy(out=acc[:, m, ho:ho + hn], in_=po[:, 0:hn]),
                             reads=[pok], writes=[(acck, m, ho)])
                    else:
                        P.op("dve", lambda e, po=po, m=m, ho=ho, hn=hn: e.tensor_tensor(out=acc[:, m, ho:ho + hn], in0=po[:, 0:hn],
                                                                                       in1=acc[:, m, ho:ho + hn], op=ALU.add),
                             reads=[pok, (acck, m, ho)], writes=[(acck, m, ho)])

            Hc = emit_h(steps[0])
            for i in range(len(steps)):
                Hn = emit_h(steps[i + 1]) if i + 1 < len(steps) else None
                emit_w2(steps[i], Hc, first=(i < len(halves)))
                Hc = Hn
            g2 = modvec(g, l, si, "G2")
            for hi2, (ho, hn) in enumerate(halves):
                xt, xtk = xts[hi2 % 2]
                lb = lbs[hi2] if kind == "L" else 0
                P.dma("sp", xt[:, :, 0:hn], blk_view(A["XT"], s0 + ho, hn), reads=[(kind + "_XT", lb)], writes=[xtk])
                for m in range(KT):
                    P.op("dve", lambda e, m=m, xt=xt, ho=ho, hn=hn: e.scalar_tensor_tensor(
                        out=xt[:, m, 0:hn], in0=acc[:, m, ho:ho + hn], scalar=g2[:, m:m + 1], in1=xt[:, m, 0:hn], op0=ALU.mult, op1=ALU.add),
                        reads=[(acck, m, ho), xtk, "modx", "modc"], writes=[xtk])
                P.dma("pool", blk_view(A["XT"], s0 + ho, hn), xt[:, :, 0:hn], reads=[xtk], writes=[(kind + "_XT", lb)])
                if publish and kind == "L" and lb in (4, 5, 10, 11):
                    eb = (4, 5, 10, 11).index(lb)
                    for cg in range(2):
                        P.dma("pool", g.ex_in[2 * eb + cg].rearrange("p (c t) -> p c t", c=4), xt[:, 4 * cg:4 * cg + 4, :],
                              reads=[xtk], writes=[("ex_in", 2 * eb + cg)])
                        P.collective("AllGather", ins=[g.ex_in[2 * eb + cg]], outs=[g.ex_all[2 * eb + cg].rearrange("r p f -> (r p) f")],
                                     groups=[[0, 1, 2, 3], [4, 5, 6, 7]], reads=[("ex_in", 2 * eb + cg)],
                                     writes=[("ex_all", 2 * eb + cg)])
        for (kind, s0, n0, lbs) in sblocks:
            sb_body(kind, s0, n0, lbs)


def st_exchange(g):
    P = g.P
    XT = g.lat["XT"]
    with Stage(g) as st:
        hb = st.ring("exh", [128, 4, NB], F32, 3)
        i = 0
        for (lb, eb, off) in ((2, 2, 3), (3, 3, 3), (12, 0, 1), (13, 1, 1)):
            for cg in range(2):
                u = 2 * eb + cg
                t, tk = hb[i % 3]
                i += 1

                for hh_ in range(2):
                    def dyn(e, t=t, u=u, off=off, hh_=hh_):
                        r = dynval(g, e, "rl" if off == 3 else "rr")
                        return e.dma_start(out=t[:, 2 * hh_:2 * hh_ + 2, :].rearrange("p c t -> p (c t)"),
                                           in_=g.ex_all[u][bass.ds(r, 1), :, 1024 * hh_:1024 * hh_ + 1024].rearrange("o p f -> p (o f)"))
                    P.dma("sp", None, None, reads=[("ex_all", u)], writes=[tk], fn=dyn)
                P.dma("sp", XT[4 * cg:4 * cg + 4, :, lb * NB:(lb + 1) * NB].rearrange("c p t -> p c t"), t[:],
                      reads=[tk], writes=[("L_XT", lb, cg)])


def st_final(g):
    P = g.P
    with Stage(g) as st:
        xts = st.ring("oxt", [128, KT, NB], F32, 2)
        ys = st.ring("oy", [128, KT, NB], F32, 2)
        outs = st.ring("oo", [128, 4, D], F32, 2)
        bufs = {"sq": st.sb("osq", [128, KT, NB], BF16), "rs": st.sb("ors", [128, NB], F32)}
        fg = S(g, "fing")
        for ji, lb in enumerate(range(4, 12)):
            xt, xtk = xts[ji % 2]
            y, yk = ys[ji % 2]
            oo, ook = outs[ji % 2]
            P.dma("sp", xt[:], blk_view(g.lat["XT"], lb * NB, NB), reads=[("L_XT", lb)], writes=[xtk])
            norm_core(g, st, xt, xtk, NB, fg, None, bufs, out_f32=(y, yk))
            for a in range(4):
                for half in range(2):
                    ps = g.ps[(a * 2 + half) % 6]
                    psk = "ps%d" % ((a * 2 + half) % 6)
                    for cc in range(4):
                        c = half * 4 + cc
                        P.op("pe", lambda e, ps=ps, y=y, a=a, c=c, cc=cc: e.transpose(
                            out=ps[:, cc * 128:(cc + 1) * 128], in_=y[:, c, a * 128:(a + 1) * 128], identity=g.ident[:]),
                            reads=[(yk, c), "ident"], writes=[psk])
                    if half == 0:
                        P.op("act", lambda e, ps=ps, oo=oo, a=a: e.copy(out=oo[:, a, 0:512], in_=ps[:, 0:512]),
                             reads=[psk], writes=[(ook, a, 0)])
                    else:
                        P.op("dve", lambda e, ps=ps, oo=oo, a=a: e.tensor_copy(out=oo[:, a, 512:1024], in_=ps[:, 0:512]),
                             reads=[psk], writes=[(ook, a, 1)])
            P.dma("pool", g.out[(lb - 4) * NB:(lb - 3) * NB, :].rearrange("(a p) d -> p a d", p=128), oo[:],
                  reads=[(ook, a, h) for a in range(4) for h in range(2)], writes=[("out", lb)])


def build():
    g = build_program()
    L = lambda lbs: seg_blocks("L", lbs)
    C = seg_blocks("C", None)

    def stop(name):
        return STOP_AFTER == name
    st_setup(g)
    if stop("setup"):
        g.P.emit(); return g
    st_convert(g, "mix")
    st_transpose_in(g)
    if stop("tin"):
        g.P.emit(); return g
    stopped = False
    for l in range(DEPTH):
        last = l == DEPTH - 1
        nblk = list(range(2, 14))
        ublk = list(range(3, 13))
        pblk = list(range(4, 12))
        if l == 1:
            st_exchange(g)
        st_norm(g, l, 1, L(nblk) + C)
        st_zu(g, l, L(ublk) + C)
        st_scan(g, l, C + L(pblk))
        if stop("scan%d" % l):
            stopped = True
            break
        st_carry(g, l)
        if stop("carry%d" % l):
            stopped = True
            break
        bgA = bgB = bgC = None
        if l == 0 and not NO_MOE:
            pcs = conv_pieces(g, "moe")
            n1_, n2_ = (len(pcs) * 4) // 10, (len(pcs) * 7) // 10
            bgA, bgB, bgC = pcs[:n1_], pcs[n1_:n2_], pcs[n2_:]
        st_zgm(g, l, L(pblk) + (C if not last else []), bg_pieces=bgA, bg_every=4)
        st_zv(g, l, L(nblk) + (C if not last else []), bg_pieces=bgB, bg_every=3)
        st_conv(g, l, [("L", pblk)] + ([("C", None)] if not last else []), bg_pieces=bgC)
        if stop("conv%d" % l):
            stopped = True
            break
        st_mix(g, l, L(pblk) + (C if not last else []))
        if stop("mix%d" % l):
            stopped = True
            break
        moe = (l % 2 == 1)
        st_norm(g, l, 2, L(pblk) + (C if not last else []), router=moe)
        sbl = [("L", pblk[i] * NB, 2 * NB, [pblk[i], pblk[i + 1]]) for i in range(0, len(pblk), 2)]
        if not last:
            sbl = [sbl[0], sbl[-1]] + sbl[1:-1]
            sbl.append(("C", CPAD, CTX, [0]))
        st_ffn(g, l, sbl, moe, publish=not last)
        if stop("ffn%d" % l):
            stopped = True
            break
    if not stopped:
        st_final(g)
    g.P.emit()
    return g


def make_in_maps(inp):
    x = np.asarray(inp["x"], np.float32)
    maps = []
    gw = np.zeros((DEPTH, 2, 2, 8, 128, 128), np.float32)
    for l in range(DEPTH):
        for d in range(2):
            for ti, nm in enumerate(("lru_wr", "lru_wi")):
                w = np.asarray(inp[nm][l][d], np.float32)
                for c in range(8):
                    gw[l, d, ti, c, 0:64, 0:64] = w[2 * c]
                    gw[l, d, ti, c, 64:128, 64:128] = w[2 * c + 1]
    gw = gw.reshape(-1, 128)
    shared = {} if NO_MOE else {
        "moe_w1": np.ascontiguousarray(np.asarray(inp["moe_w1"], np.float32)[0].reshape(NE * D, DFF)),
        "moe_w3": np.ascontiguousarray(np.asarray(inp["moe_w3"], np.float32)[0].reshape(NE * D, DFF)),
        "moe_w2": np.ascontiguousarray(np.asarray(inp["moe_w2"], np.float32)[0].reshape(NE * DFF, D)),
    }
    shared.update({
        "w_in": np.ascontiguousarray(np.asarray(inp["w_in"], np.float32).reshape(DEPTH * D, 6144)),
        "w_a": np.ascontiguousarray(np.asarray(inp["w_branch_a"], np.float32).reshape(DEPTH * D, D)),
        "w_b": np.ascontiguousarray(np.asarray(inp["w_branch_b"], np.float32).reshape(DEPTH * D, D)),
        "w_o": np.ascontiguousarray(np.asarray(inp["w_out"], np.float32).reshape(DEPTH * D, D)),
        "ffn_w1": np.ascontiguousarray(np.asarray(inp["ffn_w1"], np.float32)[0]),
        "ffn_w3": np.ascontiguousarray(np.asarray(inp["ffn_w3"], np.float32)[0]),
        "ffn_w2": np.ascontiguousarray(np.asarray(inp["ffn_w2"], np.float32)[0]),
        "gatew": gw,
        "router": np.ascontiguousarray(np.asarray(inp["moe_router"], np.float32)[0]),
    })
    modw = np.asarray(inp["mod_w"], np.float32)
    for core in range(8):
        b, q = core // 4, core % 4
        xl = np.zeros((TL, D), np.float32)
        g0 = q * 4096 - 4 * NB
        lo, hi = max(g0, 0), min(g0 + TL, 16384)
        xl[lo - g0:hi - g0] = x[b, lo:hi]
        m = dict(shared)
        m["x_loc"] = xl
        m["ctx_in"] = np.ascontiguousarray(np.asarray(inp["ctx"], np.float32)[b])
        m["small"] = _build_small(inp, core)
        m["modw"] = np.ascontiguousarray(modw[:, :, q * 1536:(q + 1) * 1536])
        maps.append(m)
    return maps


_CACHE = {}


def kernel(**inputs):
    if "g" not in _CACHE:
        _CACHE["g"] = build()
    g = _CACHE["g"]
    maps = make_in_maps(inputs)
    res = run_bass_kernel_spmd(g.nc, maps, core_ids=list(range(8)))
    out = np.zeros((2, 16384, D), np.float32)
    for core in range(8):
        b, q = core // 4, core % 4
        out[b, q * 4096:(q + 1) * 4096] = res.results[core]["out"]
    _CACHE["last"] = res
    return out
```
